# Optimizing a Trainium2 kernel written in Bass

```python
import math
import jax
import jax.numpy as jnp
from jax import lax
import numpy as np

D_MODEL = 1024
BATCH = 4
SEQ = 4096
DEPTH = 2

GRID_W = 64
CTX_LEN = 256
RMS_EPS = 1e-6
GN_EPS = 1e-5
N_BRANCH = 3
MIX_W = 512

RET_HEADS = 4
RET_DK = 128
RET_DV = MIX_W // RET_HEADS
RET_QK_W = RET_HEADS * RET_DK
RET_CHUNK = 128

S5_GROUP = 16
S5_GROUPS = MIX_W // S5_GROUP
S5_STATE = 64
S5_DT_MIN = 1e-3
S5_DT_MAX = 1e-1

HY_SHORT = 3
HY_EMB = 33
HY_BANDS = (HY_EMB - 1) // 2
HY_FHID = 64
HY_DECAY_TARGET = 1e-2
HY_FAST_PCT = 0.3
HY_SLOW_PCT = 1.5

MOE_GROUPS = 4
MOE_PER_GROUP = 8
MOE_EXPERTS = MOE_GROUPS * MOE_PER_GROUP
MOE_HIDDEN = 512
MOE_TOPK = 2
MOE_BLOCK = 128

COL_Q = 0
COL_K = COL_Q + RET_QK_W
COL_V = COL_K + RET_QK_W
COL_G = COL_V + MIX_W
COL_S5 = COL_G + MIX_W
COL_HY = COL_S5 + MIX_W
COL_GATE = COL_HY + 3 * MIX_W
IN_COLS = COL_GATE + N_BRANCH * D_MODEL

kernel_name = 'hybrid_retention_s5_hyena_hmoe_flow_block'


def rms_norm(x, g):
    xf = x.astype(jnp.float32)
    y = xf * lax.rsqrt(jnp.mean(xf * xf, axis=-1, keepdims=True) + RMS_EPS)
    return (y * g.astype(jnp.float32)).astype(x.dtype)


def grid_pos_embed(n_tokens, dtype):
    rows = n_tokens // GRID_W
    quarter = D_MODEL // 4
    omega = 1.0 / (10000.0 ** (jnp.arange(quarter, dtype=jnp.float32) / quarter))
    ang_r = jnp.arange(rows, dtype=jnp.float32)[:, None] * omega
    ang_c = jnp.arange(GRID_W, dtype=jnp.float32)[:, None] * omega
    emb_r = jnp.concatenate([jnp.sin(ang_r), jnp.cos(ang_r)], axis=-1)
    emb_c = jnp.concatenate([jnp.sin(ang_c), jnp.cos(ang_c)], axis=-1)
    half = D_MODEL // 2
    pe = jnp.concatenate([jnp.broadcast_to(emb_r[:, None, :], (rows, GRID_W, half)),
                          jnp.broadcast_to(emb_c[None, :, :], (rows, GRID_W, half))], axis=-1)
    return pe.reshape(rows * GRID_W, D_MODEL).astype(dtype)


def _heads(t):
    return t.astype(jnp.float32).reshape(t.shape[0], t.shape[1], RET_HEADS, -1)


def _ret_masks(log_gamma, include_diag):
    lg = log_gamma.astype(jnp.float32)
    idx = jnp.arange(RET_CHUNK, dtype=jnp.float32)
    diff = idx[:, None] - idx[None, :]
    keep = (diff >= 0) if include_diag else (diff > 0)
    inner = jnp.where(keep[None], jnp.exp(lg[:, None, None] * jnp.maximum(diff, 0.0)[None]), 0.0)
    q_dec = jnp.exp(lg[:, None] * (idx + 1.0)[None])
    k_dec = jnp.exp(lg[:, None] * (RET_CHUNK - 1.0 - idx)[None])
    c_dec = jnp.exp(lg * RET_CHUNK)
    return inner, q_dec, k_dec, c_dec


def retention_scan(q, k, v, log_gamma, s0, include_diag):
    bsz, n_tok = q.shape[0], q.shape[1]
    n_chunks = n_tok // RET_CHUNK

    def chunks(t):
        return t.reshape(bsz, n_chunks, RET_CHUNK, RET_HEADS, t.shape[-1]).transpose(1, 0, 3, 2, 4)

    inner, q_dec, k_dec, c_dec = _ret_masks(log_gamma, include_diag)

    def step(s, qkv):
        qi, ki, vi = qkv
        att = jnp.einsum('bhid,bhjd->bhij', qi, ki) * inner
        o = jnp.einsum('bhij,bhje->bhie', att, vi) + jnp.einsum('bhid,bhde->bhie', qi * q_dec[..., None], s)
        s = s * c_dec[:, None, None] + jnp.einsum('bhjd,bhje->bhde', ki * k_dec[..., None], vi)
        return s, o

    _, o = lax.scan(step, s0, (chunks(q), chunks(k), chunks(v)))
    return o.transpose(1, 0, 3, 2, 4).reshape(bsz, n_tok, RET_HEADS, RET_DV)


def retention_state(k, v, log_gamma):
    n_tok = k.shape[1]
    dist = (n_tok - 1 - jnp.arange(n_tok)).astype(jnp.float32)
    w = jnp.exp(dist[:, None] * log_gamma.astype(jnp.float32)[None])
    return jnp.einsum('blhd,lh,blhe->bhde', k, w, v)


def bi_retention(q, k, v, log_decay, s0_f, s0_b):
    flip = lambda t: jnp.flip(t, axis=1)
    o_f = retention_scan(q, k, v, log_decay[0], s0_f, True)
    o_b = retention_scan(flip(q), flip(k), flip(v), log_decay[1], s0_b, False)
    return o_f + flip(o_b)


def retention_readout(o, g, gn_g):
    mu = jnp.mean(o, axis=-1, keepdims=True)
    var = jnp.mean(jnp.square(o - mu), axis=-1, keepdims=True)
    on = ((o - mu) * lax.rsqrt(var + GN_EPS)).reshape(g.shape) * gn_g.astype(jnp.float32)
    return (on * jax.nn.silu(g.astype(jnp.float32))).astype(g.dtype)


def _s5_groups(t):
    return t.astype(jnp.float32).reshape(t.shape[0], t.shape[1], S5_GROUPS, S5_GROUP).astype(jnp.complex64)


def s5_discretize(lam_re, lam_im, log_dt, b_re, b_im):
    lam = lax.complex(lam_re.astype(jnp.float32), lam_im.astype(jnp.float32))
    lam_dt = lam * jnp.exp(log_dt.astype(jnp.float32))[:, None]
    a_bar = jnp.exp(lam_dt)
    bmat = lax.complex(b_re.astype(jnp.float32), b_im.astype(jnp.float32))
    b_bar = ((a_bar - 1.0) / lam)[..., None] * bmat
    return lam_dt, a_bar, b_bar


def s5_state(u, lam_dt, b_bar):
    n_tok = u.shape[1]
    dist = (n_tok - 1 - jnp.arange(n_tok)).astype(jnp.float32)
    pw = jnp.exp(dist[:, None, None] * lam_dt[None])
    return jnp.einsum('lgn,gnp,blgp->bgn', pw, b_bar, u)


def s5_scan(u, a_bar, b_bar, c, h0, reverse):
    bu = jnp.einsum('gnp,blgp->blgn', b_bar, u)
    if reverse:
        bu = jnp.flip(bu, axis=1)
    bu = bu.at[:, 0].add(a_bar[None] * h0)
    a = jnp.broadcast_to(a_bar, bu.shape)

    def combine(e1, e2):
        return e1[0] * e2[0], e2[0] * e1[1] + e2[1]

    _, h = lax.associative_scan(combine, (a, bu), axis=1)
    if reverse:
        h = jnp.flip(h, axis=1)
    return jnp.einsum('gpn,blgn->blgp', c, h).real


def s5_readout(y, u, d, glu_w1, glu_w2):
    yy = y.reshape(u.shape) + d.astype(jnp.float32) * u.astype(jnp.float32)
    g = jax.nn.gelu(yy)
    return ((g @ glu_w1.astype(jnp.float32)) * jax.nn.sigmoid(g @ glu_w2.astype(jnp.float32))).astype(u.dtype)


def s5_mixer(u, uc, lam_re, lam_im, log_dt, b_re, b_im, c_re, c_im, d, glu_w1, glu_w2, need_ctx):
    ul, ucl = _s5_groups(u), _s5_groups(uc)
    y = 0.0
    yc = 0.0
    for direction in range(2):
        rev = direction == 1
        lam_dt, a_bar, b_bar = s5_discretize(lam_re[direction], lam_im[direction], log_dt[direction],
                                             b_re[direction], b_im[direction])
        cmat = lax.complex(c_re[direction].astype(jnp.float32), c_im[direction].astype(jnp.float32))
        h0 = s5_state(jnp.flip(ucl, axis=1) if rev else ucl, lam_dt, b_bar)
        y = y + s5_scan(ul, a_bar, b_bar, cmat, h0, rev)
        if need_ctx:
            yc = yc + s5_scan(ucl, a_bar, b_bar, cmat, jnp.zeros_like(h0), rev)
    out = s5_readout(y, u, d, glu_w1, glu_w2)
    out_c = s5_readout(yc, uc, d, glu_w1, glu_w2) if need_ctx else None
    return out, out_c


def short_conv(z, w, b):
    zp = jnp.pad(z, ((0, 0), (1, 1), (0, 0)))
    return zp[:, :-2] * w[0] + zp[:, 1:-1] * w[1] + zp[:, 2:] * w[2] + b


def hyena_filter(n_tok, w1, b1, w2, b2, w3, freq):
    f32 = jnp.float32
    t = jnp.linspace(0.0, 1.0, n_tok, dtype=f32)[:, None]
    w = 2.0 * math.pi * jnp.arange(n_tok, dtype=f32)[:, None] / n_tok
    f = jnp.linspace(1e-4, HY_BANDS - 1, HY_BANDS, dtype=f32)[None]
    z = jnp.concatenate([t, jnp.cos(f * w), -jnp.sin(f * w)], axis=-1)
    freq = freq.astype(f32)
    h = jnp.sin(freq[0] * (z @ w1.astype(f32) + b1.astype(f32)))
    h = jnp.sin(freq[1] * (h @ w2.astype(f32) + b2.astype(f32)))
    h = h @ w3.astype(f32)
    offset = jnp.abs(jnp.arange(n_tok) - n_tok // 2).astype(f32) / n_tok
    deltas = jnp.abs(jnp.linspace(math.log(HY_DECAY_TARGET) / HY_SLOW_PCT,
                                  math.log(HY_DECAY_TARGET) / HY_FAST_PCT, MIX_W, dtype=f32))
    return h * jnp.exp(-offset[:, None] * deltas[None])


def long_conv_centred(u, h):
    n_tok = u.shape[1]
    n_fft = 2 * n_tok
    y = jnp.fft.irfft(jnp.fft.rfft(u, n=n_fft, axis=1) * jnp.fft.rfft(h, n=n_fft, axis=0)[None], n=n_fft, axis=1)
    return y[:, n_tok // 2: n_tok // 2 + n_tok]


def hyena_seq(z, short_w, short_b, filt, bias):
    zs = short_conv(z, short_w, short_b).astype(jnp.float32)
    x0, x1, v = jnp.split(zs, 3, axis=-1)
    y = x1 * v
    y = long_conv_centred(y, filt) + y * bias.astype(jnp.float32)
    return (x0 * y).astype(z.dtype)


def merge_branches(ys, gate_logits, w_branch, w_out):
    y = jnp.stack(ys, axis=2)
    branch = jnp.einsum('blnc,ncd->blnd', y, w_branch)
    gates = jax.nn.sigmoid(gate_logits.astype(jnp.float32)).astype(branch.dtype).reshape(branch.shape)
    return jnp.sum(gates * branch, axis=2) @ w_out


def token_mixer(h, hc, w_in, ret_log_decay, ret_gn_g,
                s5_lambda_re, s5_lambda_im, s5_log_dt, s5_b_re, s5_b_im, s5_c_re, s5_c_im, s5_d,
                s5_glu_w1, s5_glu_w2, hy_short_w, hy_short_b, hy_f_w1, hy_f_b1, hy_f_w2, hy_f_b2,
                hy_f_w3, hy_f_freq, hy_bias, w_branch, w_out, need_ctx):
    z = h @ w_in
    if need_ctx:
        zc = hc @ w_in
        kvc, uc = zc[..., COL_K:COL_G], zc[..., COL_S5:COL_HY]
    else:
        kvc = hc @ w_in[:, COL_K:COL_G]
        uc = hc @ w_in[:, COL_S5:COL_HY]
    kc, vc = _heads(kvc[..., :RET_QK_W]), _heads(kvc[..., RET_QK_W:])
    q_scale = RET_DK ** -0.5

    s_f = retention_state(kc, vc, ret_log_decay[0])
    s_b = retention_state(jnp.flip(kc, axis=1), jnp.flip(vc, axis=1), ret_log_decay[1])
    q = _heads(z[..., COL_Q:COL_K]) * q_scale
    o = bi_retention(q, _heads(z[..., COL_K:COL_V]), _heads(z[..., COL_V:COL_G]), ret_log_decay, s_f, s_b)
    y_ret = retention_readout(o, z[..., COL_G:COL_S5], ret_gn_g)

    y_s5, y_s5c = s5_mixer(z[..., COL_S5:COL_HY], uc, s5_lambda_re, s5_lambda_im, s5_log_dt,
                           s5_b_re, s5_b_im, s5_c_re, s5_c_im, s5_d, s5_glu_w1, s5_glu_w2, need_ctx)

    filt_p = (hy_f_w1, hy_f_b1, hy_f_w2, hy_f_b2, hy_f_w3, hy_f_freq)
    y_hy = hyena_seq(z[..., COL_HY:COL_GATE], hy_short_w, hy_short_b, hyena_filter(z.shape[1], *filt_p), hy_bias)

    out = merge_branches((y_ret, y_s5, y_hy), z[..., COL_GATE:], w_branch, w_out)
    if not need_ctx:
        return out, None

    zero_s = jnp.zeros_like(s_f)
    qc = _heads(zc[..., COL_Q:COL_K]) * q_scale
    y_retc = retention_readout(bi_retention(qc, kc, vc, ret_log_decay, zero_s, zero_s), zc[..., COL_G:COL_S5], ret_gn_g)
    y_hyc = hyena_seq(zc[..., COL_HY:COL_GATE], hy_short_w, hy_short_b, hyena_filter(zc.shape[1], *filt_p), hy_bias)
    out_c = merge_branches((y_retc, y_s5c, y_hyc), zc[..., COL_GATE:], w_branch, w_out)
    return out, out_c


def grouped_expert_ffn(t, expert_ids, weights, w1, w3, w2):
    n_tok, d = t.shape
    n_slots = expert_ids.shape[0]
    k = n_slots // n_tok
    token = jnp.arange(n_slots) // k
    order = jnp.argsort(expert_ids)
    e_sorted, tok_sorted, w_sorted = expert_ids[order], token[order], weights[order]
    counts = jnp.bincount(expert_ids, length=MOE_EXPERTS)
    padded = (counts + MOE_BLOCK - 1) // MOE_BLOCK * MOE_BLOCK
    starts = jnp.cumsum(counts) - counts
    pends = jnp.cumsum(padded)
    pstarts = pends - padded
    dest = pstarts[e_sorted] + jnp.arange(n_slots) - starts[e_sorted]
    n_blocks = -(-n_slots // MOE_BLOCK) + MOE_EXPERTS
    buf = jnp.zeros((n_blocks * MOE_BLOCK, d), t.dtype).at[dest].set(t[tok_sorted])
    block_expert = jnp.minimum(jnp.searchsorted(pends, jnp.arange(n_blocks) * MOE_BLOCK, side='right'),
                               MOE_EXPERTS - 1)

    def expert_block(args):
        xb, e = args
        return (jax.nn.silu(xb @ w1[e]) * (xb @ w3[e])) @ w2[e]

    ybuf = lax.map(expert_block, (buf.reshape(n_blocks, MOE_BLOCK, d), block_expert)).reshape(-1, d)
    out = jnp.zeros((n_tok, d), jnp.float32).at[tok_sorted].add(ybuf[dest].astype(jnp.float32) * w_sorted[:, None])
    return out.astype(t.dtype)


def hier_moe(t, w_group, b_group, w_expert, b_expert, w1, w3, w2):
    n_tok = t.shape[0]
    tf = t.astype(jnp.float32)
    p_group = jax.nn.softmax(tf @ w_group.astype(jnp.float32) + b_group.astype(jnp.float32), axis=-1)
    p_top, g_idx = lax.top_k(p_group, 1)
    logits = (tf @ w_expert.astype(jnp.float32) + b_expert.astype(jnp.float32)).reshape(n_tok, MOE_GROUPS, MOE_PER_GROUP)
    in_group = jnp.take_along_axis(logits, g_idx[:, :, None], axis=1)[:, 0]
    l_top, e_idx = lax.top_k(in_group, MOE_TOPK)
    weights = jax.nn.softmax(l_top, axis=-1) * p_top
    experts = g_idx * MOE_PER_GROUP + e_idx
    return grouped_expert_ffn(t, experts.reshape(-1), weights.reshape(-1), w1, w3, w2)


def setup_inputs(seed: int = 0) -> dict:
    key = jax.random.key(seed)
    keys = iter(jax.random.split(key, 64))
    f32 = jnp.float32

    def normal(shape, scale):
        return jax.random.normal(next(keys), shape, f32) * scale

    d = D_MODEL
    g, n, p = S5_GROUPS, S5_STATE, S5_GROUP
    ret_base = jnp.log1p(-(2.0 ** (-5.0 - jnp.arange(RET_HEADS, dtype=f32))))
    return {
        'x': normal((BATCH, SEQ, d), 1.0),
        'c': normal((BATCH, d), 1.0),
        'ctx': normal((BATCH, CTX_LEN, d), 1.0),
        'c_ctx': normal((d,), 1.0),
        'w_mod': normal((DEPTH, d, 6 * d), 0.5 * d ** -0.5),
        'b_mod': normal((DEPTH, 6 * d), 0.01),
        'norm1_g': 1.0 + normal((DEPTH, d), 0.01),
        'norm2_g': 1.0 + normal((DEPTH, d), 0.01),
        'w_in': normal((DEPTH, d, IN_COLS), d ** -0.5),
        'ret_log_decay': ret_base * (1.0 + normal((DEPTH, 2, RET_HEADS), 0.05)),
        'ret_gn_g': 1.0 + normal((DEPTH, MIX_W), 0.01),
        's5_lambda_re': -0.5 + normal((DEPTH, 2, g, n), 0.01),
        's5_lambda_im': math.pi * jnp.arange(n, dtype=f32) + normal((DEPTH, 2, g, n), 0.01),
        's5_log_dt': jax.random.uniform(next(keys), (DEPTH, 2, g), f32, math.log(S5_DT_MIN), math.log(S5_DT_MAX)),
        's5_b_re': normal((DEPTH, 2, g, n, p), (2 * p) ** -0.5),
        's5_b_im': normal((DEPTH, 2, g, n, p), (2 * p) ** -0.5),
        's5_c_re': normal((DEPTH, 2, g, p, n), n ** -0.5),
        's5_c_im': normal((DEPTH, 2, g, p, n), n ** -0.5),
        's5_d': normal((DEPTH, MIX_W), 1.0),
        's5_glu_w1': normal((DEPTH, MIX_W, MIX_W), MIX_W ** -0.5),
        's5_glu_w2': normal((DEPTH, MIX_W, MIX_W), MIX_W ** -0.5),
        'hy_short_w': normal((DEPTH, HY_SHORT, 3 * MIX_W), 0.5),
        'hy_short_b': normal((DEPTH, 3 * MIX_W), 0.01),
        'hy_f_w1': normal((DEPTH, HY_EMB, HY_FHID), HY_EMB ** -0.5),
        'hy_f_b1': normal((DEPTH, HY_FHID), 0.1),
        'hy_f_w2': normal((DEPTH, HY_FHID, HY_FHID), HY_FHID ** -0.5),
        'hy_f_b2': normal((DEPTH, HY_FHID), 0.1),
        'hy_f_w3': normal((DEPTH, HY_FHID, MIX_W), 0.05 * HY_FHID ** -0.5),
        'hy_f_freq': 1.0 + normal((DEPTH, 2, HY_FHID), 0.01),
        'hy_bias': normal((DEPTH, MIX_W), 0.5),
        'w_branch': normal((DEPTH, N_BRANCH, MIX_W, d), MIX_W ** -0.5),
        'w_out': normal((DEPTH, d, d), d ** -0.5),
        'moe_w_group': normal((DEPTH, d, MOE_GROUPS), d ** -0.5),
        'moe_b_group': normal((DEPTH, MOE_GROUPS), 0.01),
        'moe_w_expert': normal((DEPTH, d, MOE_EXPERTS), d ** -0.5),
        'moe_b_expert': normal((DEPTH, MOE_EXPERTS), 0.01),
        'moe_w1': normal((DEPTH, MOE_EXPERTS, d, MOE_HIDDEN), d ** -0.5),
        'moe_w3': normal((DEPTH, MOE_EXPERTS, d, MOE_HIDDEN), d ** -0.5),
        'moe_w2': normal((DEPTH, MOE_EXPERTS, MOE_HIDDEN, d), MOE_HIDDEN ** -0.5),
        'final_norm_g': 1.0 + normal((d,), 0.01),
    }


def reference(x, c, ctx, c_ctx, w_mod, b_mod, norm1_g, norm2_g, w_in, ret_log_decay, ret_gn_g,
              s5_lambda_re, s5_lambda_im, s5_log_dt, s5_b_re, s5_b_im, s5_c_re, s5_c_im, s5_d,
              s5_glu_w1, s5_glu_w2, hy_short_w, hy_short_b, hy_f_w1, hy_f_b1, hy_f_w2, hy_f_b2,
              hy_f_w3, hy_f_freq, hy_bias, w_branch, w_out, moe_w_group, moe_b_group,
              moe_w_expert, moe_b_expert, moe_w1, moe_w3, moe_w2, final_norm_g):
    bsz, n_lat, d = x.shape
    x = x + grid_pos_embed(n_lat, x.dtype)[None]
    xc = ctx
    silu_c = jax.nn.silu(c)
    silu_cc = jax.nn.silu(c_ctx)
    for l in range(DEPTH):
        last = l == DEPTH - 1
        mod = (silu_c @ w_mod[l] + b_mod[l])[:, None, :]
        sh1, sc1, g1, sh2, sc2, g2 = jnp.split(mod, 6, axis=-1)
        n_ctx_mod = 2 if last else 6
        modc = (silu_cc @ w_mod[l][:, :n_ctx_mod * d] + b_mod[l][:n_ctx_mod * d])[None, None, :]
        cm = jnp.split(modc, n_ctx_mod, axis=-1)
        h = rms_norm(x, norm1_g[l]) * (1 + sc1) + sh1
        hc = rms_norm(xc, norm1_g[l]) * (1 + cm[1]) + cm[0]
        mix, mix_c = token_mixer(h, hc, w_in[l], ret_log_decay[l], ret_gn_g[l],
                                 s5_lambda_re[l], s5_lambda_im[l], s5_log_dt[l], s5_b_re[l], s5_b_im[l],
                                 s5_c_re[l], s5_c_im[l], s5_d[l], s5_glu_w1[l], s5_glu_w2[l],
                                 hy_short_w[l], hy_short_b[l], hy_f_w1[l], hy_f_b1[l], hy_f_w2[l], hy_f_b2[l],
                                 hy_f_w3[l], hy_f_freq[l], hy_bias[l], w_branch[l], w_out[l],
                                 need_ctx=not last)
        x = x + g1 * mix
        h2 = rms_norm(x, norm2_g[l]) * (1 + sc2) + sh2
        moe_p = (moe_w_group[l], moe_b_group[l], moe_w_expert[l], moe_b_expert[l], moe_w1[l], moe_w3[l], moe_w2[l])
        if last:
            x = x + g2 * hier_moe(h2.reshape(-1, d), *moe_p).reshape(x.shape)
        else:
            xc = xc + cm[2] * mix_c
            h2c = rms_norm(xc, norm2_g[l]) * (1 + cm[4]) + cm[3]
            y = hier_moe(jnp.concatenate([h2.reshape(-1, d), h2c.reshape(-1, d)], axis=0), *moe_p)
            x = x + g2 * y[: bsz * n_lat].reshape(x.shape)
            xc = xc + cm[5] * y[bsz * n_lat:].reshape(xc.shape)
    return rms_norm(x, final_norm_g)
```

```python
import math
from contextlib import ExitStack
import numpy as np
import ml_dtypes
import concourse.bass as bass
import concourse.mybir as mybir
from concourse.bass_utils import run_bass_kernel_spmd

F32 = mybir.dt.float32
BF16 = mybir.dt.bfloat16
I32 = mybir.dt.int32
ALU = mybir.AluOpType
AF = mybir.ActivationFunctionType
AX = mybir.AxisListType

D = 1024
NT = 34
T = NT * 128
CTX = 256
L = 4096
DEPTH = 2
TG = [(0, 256)] + [(256 + 512 * i, 512) for i in range(8)]
TWO_PI = 2.0 * math.pi
NBLK = 100


class Buf:
    __slots__ = ("name", "lw", "rd")

    def __init__(self, name=""):
        self.name = name
        self.lw = None
        self.rd = {}


class FW:
    ENG = ("pe", "dve", "act", "pool", "sp")
    STREAMS = {"d0": 8, "d1": 8, "d2": 8, "d3": 4}
    ENGNAME = {"pe": "tensor", "dve": "vector", "act": "scalar", "pool": "gpsimd", "sp": "sync"}

    def __init__(self, nc, sems):
        self.nc = nc
        self.sems = sems
        self.ops = {e: [] for e in self.ENG}
        self.cnt = {s: 0 for s in self.sem_names()}
        self.seen = {e: {} for e in self.ENG}
        self.n_ops = 0
        self.ndma = {s: 0 for s in self.STREAMS}

    @classmethod
    def sem_names(cls):
        r = [e for e in cls.ENG if e != "sp"]
        for s, k in cls.STREAMS.items():
            r += [f"{s}_{i}" for i in range(k)]
        return r

    def _emit(self, eng, sem, inc, fn, reads, writes, nosync_same=False, extra_wait=None):
        waits = {}

        def need(sv):
            if sv is None:
                return
            s, v = sv
            if nosync_same and s == eng:
                return
            if waits.get(s, 0) < v:
                waits[s] = v
        if extra_wait is not None:
            need(extra_wait)
        for b in reads:
            need(b.lw)
        for b in writes:
            need(b.lw)
            for s, v in b.rd.items():
                need((s, v))
        wl = []
        for s, v in waits.items():
            if self.seen[eng].get(s, 0) < v:
                self.seen[eng][s] = v
                wl.append((s, v))
        self.cnt[sem] += inc
        v = self.cnt[sem]
        for b in reads:
            if b.rd.get(sem, 0) < v:
                b.rd[sem] = v
        for b in writes:
            b.lw = (sem, v)
            b.rd = {}
        self.ops[eng].append((wl, fn, sem, inc))
        self.n_ops += 1

    def op(self, eng, name, R, W, **kw):
        self._emit(eng, eng, 1, lambda e: getattr(e, name)(**kw), R, W, nosync_same=(eng == "pe"))

    def opf(self, eng, fn, R, W):
        self._emit(eng, eng, 1, fn, R, W, nosync_same=(eng == "pe"))

    def _dma_sem(self, stream):
        i = self.ndma[stream]
        self.ndma[stream] += 1
        sem = f"{stream}_{i % self.STREAMS[stream]}"
        return sem, (sem, self.cnt[sem])

    def dma(self, R, W, q="sp", stream="d0", **kw):
        sem, ew = self._dma_sem(stream)
        self._emit(q, sem, 16, lambda e: e.dma_start(**kw), R, W, extra_wait=ew)

    def dmaf(self, fn, R, W, q="pool", stream="d1"):
        sem, ew = self._dma_sem(stream)
        self._emit(q, sem, 16, fn, R, W, extra_wait=ew)

    def flush(self):
        nc = self.nc
        final = dict(self.cnt)
        sems = self.sems
        with nc.Block() as block:
            for e in self.ENG:
                ops = self.ops[e]

                def body(engine, ops=ops, e=e):
                    for wl, fn, sem, inc in ops:
                        for s, v in wl:
                            engine.wait_ge(sems[s], v)
                        ins = fn(engine)
                        ins.then_inc(sems[sem], inc)
                    for s, v in final.items():
                        if v > 0:
                            engine.wait_ge(sems[s], v)
                getattr(block, self.ENGNAME[e])(body)
        for e in self.ENG:
            self.ops[e] = []
            self.seen[e] = dict(final)


def _consts():
    c = {}
    c["ident_f"] = np.eye(128, dtype=np.float32)
    c["antiid_f"] = np.ascontiguousarray(np.eye(128, dtype=np.float32)[::-1])
    rows = L // 64
    quarter = D // 4
    omega = (1.0 / (np.float32(10000.0) ** (np.arange(quarter, dtype=np.float32) / np.float32(quarter)))).astype(np.float32)
    ang_r = np.arange(rows, dtype=np.float32)[:, None] * omega
    ang_c = np.arange(64, dtype=np.float32)[:, None] * omega
    emb_r = np.concatenate([np.sin(ang_r), np.cos(ang_r)], -1)
    emb_c = np.concatenate([np.sin(ang_c), np.cos(ang_c)], -1)
    pe = np.concatenate([np.broadcast_to(emb_r[:, None, :], (rows, 64, D // 2)),
                         np.broadcast_to(emb_c[None, :, :], (rows, 64, D // 2))], -1)
    c["pe"] = np.ascontiguousarray(pe.reshape(L, D).astype(np.float32))
    j = np.arange(128, dtype=np.float32)[:, None]
    i = np.arange(128, dtype=np.float32)[None, :]
    rt = np.zeros((128, 6, 128), np.float32)
    rt[:, 0] = np.maximum(i - j, 0)
    rt[:, 1] = np.maximum(j - i, 0)
    rt[:, 2] = (i >= j)
    rt[:, 3] = (j > i)
    rt[:, 4] = i + 1.0
    rt[:, 5] = 128.0 - i
    c["rtab"] = rt
    rc = np.zeros((128, 6), np.float32)
    rc[:, 0] = 127.0 - np.arange(128)
    rc[:, 1] = np.arange(128)
    rc[:, 2] = 128.0
    rc[:, 3] = np.arange(128)
    rc[:, 4] = np.arange(128) + 1.0
    rc[:, 5] = 128.0 - np.arange(128)
    c["rcol"] = rc
    for nm, n in (("zT", L), ("zTc", CTX)):
        t = np.linspace(0.0, 1.0, n, dtype=np.float32)[:, None]
        w = (2.0 * math.pi * np.arange(n, dtype=np.float32)[:, None] / n).astype(np.float32)
        f = np.linspace(1e-4, 15, 16, dtype=np.float32)[None]
        z = np.concatenate([t, np.cos(f * w), -np.sin(f * w)], -1).astype(np.float32)
        c[nm] = np.ascontiguousarray(z.T)
        off = (np.abs(np.arange(n) - n // 2).astype(np.float32) / n).astype(np.float32)
        c["off" + nm[2:]] = np.ascontiguousarray(np.broadcast_to(off[None], (128, n)))
    deltas = np.abs(np.linspace(math.log(1e-2) / 1.5, math.log(1e-2) / 0.3, 512, dtype=np.float32))
    c["ndelta"] = np.ascontiguousarray((-deltas).reshape(4, 128).T.astype(np.float32))
    c["iota512"] = np.ascontiguousarray(np.broadcast_to(np.arange(1, 513, dtype=np.float32)[None], (128, 512)))
    tri = (np.arange(128)[:, None] < np.arange(128)[None, :]).astype(np.float32)
    c["tris"] = tri
    c["iotab"] = np.ascontiguousarray(np.broadcast_to(np.arange(NBLK, dtype=np.float32)[None], (128, NBLK)))
    return c


class Builder:
    def __init__(self, debug=None):
        self.debug = debug or []
        self.nc = bass.Bass("TRN2", target_bir_lowering=False)
        self.din = {}
        self.dbg_out = []

    def inp(self, name, shape, dt=F32):
        t = self.nc.dram_tensor(name, list(shape), dt, kind="ExternalInput")
        self.din[name] = t
        return t

    def scratch(self, name, shape, dt):
        return self.nc.dram_tensor(name, list(shape), dt, kind="Internal")

    def build(self, in_shapes):
        nc = self.nc
        I = {k: self.inp(k, v.shape, {np.dtype("float32"): F32, np.dtype("int32"): I32}[v.dtype]) for k, v in in_shapes.items()}
        self.I = I
        self.out = nc.dram_tensor("out", [L, D], F32, kind="ExternalOutput")
        self.XR = self.scratch("XR", [T, D], F32)
        self.YT = self.scratch("YT", [1536, T], BF16)
        self.H2 = self.scratch("H2", [T, D], BF16)
        self.MB = self.scratch("MB", [NBLK * 128, D], BF16)
        self.MY = self.scratch("MY", [NBLK * 128, D], F32)
        self.GF = self.scratch("GF", [512, 4352], BF16)
        self.GFc = self.scratch("GFc", [512, 512], BF16)
        self.bXR = [Buf() for _ in range(NT)]
        self.bYT = [Buf() for _ in range(12)]
        self.bH2 = [Buf() for _ in range(NT)]
        self.bMB = Buf()
        self.bMY = Buf()
        self.bGF = [Buf() for _ in range(4)]
        self.bGFc = [Buf() for _ in range(4)]
        self.bout = Buf()
        with ExitStack() as es:
            self.sems = {s: es.enter_context(nc.semaphore(s)) for s in FW.sem_names()}
            self.fw = FW(nc, self.sems)
            with ExitStack() as gs:
                self.gs = gs
                self.alloc_globals()
                self.phase_init()
                for l in range(DEPTH):
                    self.layer(l)
                    if getattr(self, "stop", None):
                        break
                self.phase_final()
        return nc

    def sb(self, es, name, shape, dt):
        self._uid = getattr(self, "_uid", 0) + 1
        t = es.enter_context(self.nc.sbuf_tensor(f"s{self._uid}_{name}", list(shape), dt))
        assert self.nc.sbuf_bytes_remaining >= 32768 + 128, (name, self.nc.sbuf_bytes_remaining)
        return t, Buf(name)

    def alloc_globals(self):
        nc, gs, fw, I = self.nc, self.gs, self.fw, self.I
        self.PF = []
        for i in range(6):
            t = gs.enter_context(nc.psum_tensor(f"pf{i}", [128, 512], F32))
            self.PF.append((t, Buf(f"pf{i}")))
        self.PB = []
        for i in range(2):
            t = gs.enter_context(nc.psum_tensor(f"pb{i}", [128, 1024], BF16))
            self.PB.append((t, Buf(f"pb{i}")))
        self.pfi = 0
        self.pbi = 0
        self.identf, self.b_identf = self.sb(gs, "identf", [128, 128], F32)
        self.identb, self.b_identb = self.sb(gs, "identb", [128, 128], BF16)
        self.antib, self.b_antib = self.sb(gs, "antib", [128, 128], BF16)
        self.rcol, self.b_rcol = self.sb(gs, "rcol", [128, 6], F32)
        self.epsc, self.b_epsc = self.sb(gs, "epsc", [128, 4], F32)
        self.modT, self.b_modT = self.sb(gs, "modT", [128, 48, 2], F32)
        self.modP, self.b_modP = self.sb(gs, "modP", [128, 48, 2], F32)
        self.GBd = self.scratch("GBd", [128, 4, 1024], F32)
        self.b_GBd = Buf()
        self.scT, self.b_scT = self.sb(gs, "scT", [128, 8, 2], F32)
        self.LG, _ = self.sb(gs, "LG", [128, NT, 36], F32)
        self.b_LG = [Buf() for _ in range(NT)]
        fw.dma([], [self.b_identf], out=self.identf[:], in_=I["ident_f"].ap())
        fw.op("dve", "tensor_copy", [self.b_identf], [self.b_identb], out=self.identb[:], in_=self.identf[:])
        af, baf = self.sb(gs, "antif", [128, 128], F32)
        fw.dma([], [baf], out=af[:], in_=I["antiid_f"].ap())
        fw.op("dve", "tensor_copy", [baf], [self.b_antib], out=self.antib[:], in_=af[:])
        fw.dma([], [self.b_rcol], out=self.rcol[:], in_=I["rcol"].ap())
        fw.op("pool", "memset", [], [self.b_epsc], ap=self.epsc[:, 0:1], constant=1e-6)
        fw.op("pool", "memset", [self.b_epsc], [self.b_epsc], ap=self.epsc[:, 1:2], constant=1e-5)
        fw.op("pool", "memset", [self.b_epsc], [self.b_epsc], ap=self.epsc[:, 2:3], constant=math.pi / 2)
        fw.op("pool", "memset", [self.b_epsc], [self.b_epsc], ap=self.epsc[:, 3:4], constant=0.0)
        ct, bct = self.sb(gs, "cT", [128, 8, 2], F32)
        fw.dma([], [bct], out=ct[:], in_=I["cT"].ap())
        fw.op("act", "activation", [bct], [self.b_scT], out=self.scT[:], in_=ct[:], func=AF.Silu)

    def pf(self):
        r = self.PF[self.pfi % len(self.PF)]
        self.pfi += 1
        return r

    def pb(self):
        r = self.PB[self.pbi % len(self.PB)]
        self.pbi += 1
        return r

    def dump(self, name, ap, shape, R, dt=F32):
        if name not in self.debug:
            return
        t = self.nc.dram_tensor("dbg_" + name, list(shape), dt, kind="ExternalOutput")
        self.dbg_out.append("dbg_" + name)
        self.fw.dma(R, [Buf()], out=t.ap(), in_=ap, stream="d2")

    def phase_init(self):
        fw, I = self.fw, self.I
        with ExitStack() as es:
            xt = [self.sb(es, f"xi{i}", [128, D], F32) for i in range(3)]
            pt = [self.sb(es, f"pi{i}", [128, D], F32) for i in range(3)]
            for t in range(NT):
                x, bx = xt[t % 3]
                if t < 2:
                    fw.dma([], [bx], out=x[:], in_=I["ctx"][t * 128:(t + 1) * 128, :])
                else:
                    p, bp = pt[t % 3]
                    r0 = (t - 2) * 128
                    fw.dma([], [bx], out=x[:], in_=I["x"][r0:r0 + 128, :])
                    fw.dma([], [bp], out=p[:], in_=I["pe"][r0:r0 + 128, :])
                    fw.op("pool", "tensor_tensor", [bx, bp], [bx], out=x[:], in0=x[:], in1=p[:], op=ALU.add)
                fw.dma([bx], [self.bXR[t]], out=self.XR[t * 128:(t + 1) * 128, :], in_=x[:], stream="d2")
            fw.flush()

    def phase_mod(self, l):
        fw, I = self.fw, self.I
        with ExitStack() as es:
            wm = [self.sb(es, f"wm{i}", [128, 8, 512], F32) for i in range(2)]
            bT, bbT = self.sb(es, "bmodT", [128, 48], F32)
            brow, bbrow = self.sb(es, "bmrow", [2, 4, 512], F32)
            rows, brows = self.sb(es, "mrows", [2, 512], F32)
            sel, bsel = self.sb(es, "selw", [2, 2, 128], F32)
            gst_ = [self.sb(es, f"gst{i}", [128, 512], F32) for i in range(2)]
            fw.dma([], [bbT], out=bT[:], in_=I["bmodT"][l])
            for w in range(2):
                for gi, c0 in enumerate((2048, 2560, 5120, 5632)):
                    fw.dma([], [bbrow], out=brow[w:w + 1, gi, :], in_=I["b_mod"][l:l + 1, c0:c0 + 512])
            fw.dma([], [bsel], out=sel[:], in_=I["selw"].ap())
            for cb in range(12):
                w_, bw = wm[cb % 2]
                fw.dma([], [bw], out=w_[:], in_=I["w_mod"][l][:, cb * 512:(cb + 1) * 512].rearrange("(k p) c -> p k c", p=128))
                pt_, bp = self.pf()
                for s in range(4):
                    j = cb * 4 + s
                    for k in range(8):
                        fw.op("pe", "matmul", [bw, self.b_scT], [bp], out=pt_[:, s * 2:(s + 1) * 2], lhsT=w_[:, k, s * 128:(s + 1) * 128],
                              rhs=self.scT[:, k, :], start=(k == 0), stop=(k == 7))
                fw.op("dve", "tensor_tensor", [bp, bbT], [self.b_modT], out=self.modT[:, cb * 4:(cb + 1) * 4, :],
                      in0=pt_[:, 0:8].rearrange("p (s w) -> p s w", w=2),
                      in1=bT[:, cb * 4:(cb + 1) * 4].unsqueeze(2).to_broadcast([128, 4, 2]), op=ALU.add)
                if cb in (4, 5, 10, 11):
                    gi = {4: 0, 5: 1, 10: 2, 11: 3}[cb]
                    pr, bpr = self.pf()
                    for k in range(8):
                        fw.op("pe", "matmul", [bw, self.b_scT], [bpr], out=pr[0:2, :], lhsT=self.scT[:, k, :], rhs=w_[:, k, :],
                              start=(k == 0), stop=(k == 7))
                    fw.op("dve", "tensor_tensor", [bpr, bbrow], [brows], out=rows[:], in0=pr[0:2, :], in1=brow[:, gi, :], op=ALU.add)
                    for w in range(2):
                        pbc, bpbc = self.pf()
                        fw.op("pe", "matmul", [brows, bsel], [bpbc], out=pbc[:], lhsT=sel[:, w, :], rhs=rows[:], start=True, stop=True)
                        slot = (0 if gi < 2 else 2) + w
                        half = gi % 2
                        gst, bgst = gst_[(gi * 2 + w) % 2]
                        fw.op("act", "copy", [bpbc], [bgst], out=gst[:], in_=pbc[:])
                        fw.dma([bgst], [self.b_GBd], out=self.GBd[:, slot, half * 512:(half + 1) * 512], in_=gst[:], stream="d2")
            fw.op("dve", "tensor_scalar", [self.b_modT], [self.b_modP], out=self.modP[:], in0=self.modT[:], scalar1=1.0, scalar2=None, op0=ALU.add)
            self.dump(f"modT{l}", self.modT[:], [128, 48, 2], [self.b_modT])
            fw.flush()

    def rms_stats(self, x, bx, st, bst, mv, bmv, rstd, brstd, eps_col=0):
        fw = self.fw
        fw.op("dve", "bn_stats", [bx], [bst], out=st[:, 0, :], in_=x[:, 0:512])
        fw.op("dve", "bn_stats", [bx, bst], [bst], out=st[:, 1, :], in_=x[:, 512:1024])
        fw.op("dve", "bn_aggr", [bst], [bmv], out=mv[:, 0:2], in_=st[:])
        fw.op("dve", "scalar_tensor_tensor", [bmv], [bmv], out=mv[:, 2:3], in0=mv[:, 0:1], scalar=mv[:, 0:1], in1=mv[:, 1:2], op0=ALU.mult, op1=ALU.add)
        fw.op("act", "activation", [bmv, self.b_epsc], [bmv], out=mv[:, 3:4], in_=mv[:, 2:3], func=AF.Sqrt, bias=self.epsc[:, eps_col:eps_col + 1], scale=1.0)
        fw.op("dve", "reciprocal", [bmv], [brstd], out=rstd[:], in_=mv[:, 3:4])

    def phase_norm1(self, l, hT, bhT):
        fw, I = self.fw, self.I
        with ExitStack() as es:
            gB, bgB = self.sb(es, "n1g", [128, D], F32)
            fw.dma([], [bgB], out=gB[:], in_=I["norm1_g"][l:l + 1, :].to_broadcast([128, D]))
            xs_ = [self.sb(es, f"n1x{i}", [128, D], F32) for i in range(2)]
            xb_ = [self.sb(es, f"n1xb{i}", [128, D], BF16) for i in range(2)]
            st_ = [self.sb(es, f"n1st{i}", [128, 2, 6], F32) for i in range(2)]
            mv_ = [self.sb(es, f"n1mv{i}", [128, 4], F32) for i in range(2)]
            rs_ = [self.sb(es, f"n1rs{i}", [128, 1], F32) for i in range(2)]
            for t in range(NT):
                w = 1 if t < 2 else 0
                x, bx = xs_[t % 2]; xb, bxb = xb_[t % 2]; st, bst = st_[t % 2]; mv, bmv = mv_[t % 2]; rs, brs = rs_[t % 2]
                fw.dma([self.bXR[t]], [bx], out=x[:], in_=self.XR[t * 128:(t + 1) * 128, :])
                self.rms_stats(x, bx, st, bst, mv, bmv, rs, brs)
                fw.op("dve", "scalar_tensor_tensor", [bx, brs, bgB], [bxb], out=xb[:], in0=x[:], scalar=rs[:, 0:1], in1=gB[:], op0=ALU.mult, op1=ALU.mult)
                pb_, bpb = self.pb()
                for k in range(8):
                    fw.op("pe", "transpose", [bxb, self.b_identb], [bpb], out=pb_[:, k * 128:(k + 1) * 128], in_=xb[:, k * 128:(k + 1) * 128], identity=self.identb[:])
                for k in range(8):
                    fw.op("act", "activation", [bpb, self.b_modT, self.b_modP], [bhT[t]], out=hT[:, k, t * 128:(t + 1) * 128], in_=pb_[:, k * 128:(k + 1) * 128],
                          func=AF.Identity, scale=self.modP[:, 8 + k, w:w + 1], bias=self.modT[:, k, w:w + 1])
            fw.flush()

    def layer(self, l):
        fw = self.fw
        self.phase_mod(l)
        with ExitStack() as es:
            hT, _ = self.sb(es, "hT", [128, 8, T], BF16)
            bhT = [Buf(f"hT{t}") for t in range(NT)]
            self.phase_norm1(l, hT, bhT)
            self.dump(f"hT{l}", hT[:], [128, 8, T], bhT, dt=BF16)
            if self.debug:
                fw.flush()
            if self.stop == "norm1":
                return
            self.phase_ret(l, hT, bhT)
            if self.stop == "ret":
                return
            self.phase_s5(l, hT, bhT)
            if self.stop == "s5":
                return
            self.phase_hy(l, hT, bhT)
            if self.stop == "hy":
                return
            self.phase_merge(l, hT, bhT)
        if self.stop == "merge":
            return
        self.phase_moe(l)

    def load_w_cols(self, dst_ap, bdst, l, c0, ncols):
        self.fw.dma([], [bdst], q="pool", stream="d1", out=dst_ap,
                    in_=self.I["w_in"][l][:, c0:c0 + ncols].rearrange("(k p) c -> p k c", p=128))

    def tiles_of(self, t0, n):
        return range(t0 // 128, (t0 + n) // 128)

    def phase_ret(self, l, hT, bhT):
        fw, I = self.fw, self.I
        QS = 128.0 ** -0.5
        with ExitStack() as es:
            rtab, brtab = self.sb(es, "rtab", [128, 6, 128], F32)
            lgB, blgB = self.sb(es, "lgB", [128, 8], F32)
            gnB, bgnB = self.sb(es, "gnB", [128, 512], F32)
            fw.dma([], [brtab], out=rtab[:], in_=I["rtab"].ap())
            fw.dma([], [blgB], out=lgB[:], in_=I["ret_log_decay"][l:l + 1, :].to_broadcast([128, 8]))
            fw.dma([], [bgnB], out=gnB[:], in_=I["ret_gn_g"][l:l + 1, :].to_broadcast([128, 512]))
            MT, bMT = self.sb(es, "MT", [128, 128], F32)
            tm1, btm1 = self.sb(es, "tm1", [128, 128], F32)
            tm2, btm2 = self.sb(es, "tm2", [128, 128], F32)
            dcol, bdcol = self.sb(es, "dcol", [128, 6], F32)
            wqk, bwqk = self.sb(es, "wqk", [128, 8, 256], BF16)
            wkvg, bwkvg = self.sb(es, "wkvg", [128, 8, 384], BF16)
            qT, bqT = self.sb(es, "qT", [128, T], BF16)
            kT, bkT = self.sb(es, "kT", [128, T], BF16)
            ktok, bktok = self.sb(es, "ktok", [128, NT, 128], BF16)
            vtok, bvtok = self.sb(es, "vtok", [128, NT, 128], BF16)
            vdf, bvdf = self.sb(es, "vdf", [128, NT, 128], BF16)
            vdb, bvdb = self.sb(es, "vdb", [128, NT, 128], BF16)
            gs, bgs = self.sb(es, "gs", [128, NT, 128], BF16)
            SfB, bSfB = self.sb(es, "SfB", [128, NT, 128], BF16)
            SbB, bSbB = self.sb(es, "SbB", [128, NT, 128], BF16)
            Sf, bSf = self.sb(es, "Sf", [128, 128], F32)
            Sb, bSb = self.sb(es, "Sb", [128, 128], F32)
            yTt_ = [self.sb(es, f"yTt{i}", [128, 128], BF16) for i in range(2)]
            attm_ = [self.sb(es, f"attm{i}", [128, 128], BF16) for i in range(2)]
            on_ = [self.sb(es, f"on{i}", [128, 128], F32) for i in range(2)]
            yk_ = [self.sb(es, f"yk{i}", [128, 128], BF16) for i in range(2)]
            st_ = [self.sb(es, f"rst{i}", [128, 6], F32) for i in range(2)]
            mv_ = [self.sb(es, f"rmv{i}", [128, 4], F32) for i in range(2)]
            bq_t = [Buf() for _ in range(NT)]
            bk_t = [Buf() for _ in range(NT)]
            btok = [Buf() for _ in range(NT)]
            bSf_t = [Buf() for _ in range(NT)]
            bSb_t = [Buf() for _ in range(NT)]
            for hd in range(4):
                lgf = lgB[:, hd:hd + 1]
                lgb = lgB[:, 4 + hd:5 + hd]
                fw.op("act", "activation", [brtab, blgB], [btm1], out=tm1[:], in_=rtab[:, 0, :], func=AF.Exp, scale=lgf)
                fw.op("act", "activation", [brtab, blgB], [btm2], out=tm2[:], in_=rtab[:, 1, :], func=AF.Exp, scale=lgb)
                fw.op("dve", "tensor_tensor", [btm1, brtab], [btm1], out=tm1[:], in0=tm1[:], in1=rtab[:, 2, :], op=ALU.mult)
                fw.op("dve", "tensor_tensor", [btm2, brtab], [btm2], out=tm2[:], in0=tm2[:], in1=rtab[:, 3, :], op=ALU.mult)
                fw.op("dve", "tensor_tensor", [btm1, btm2], [bMT], out=MT[:], in0=tm1[:], in1=tm2[:], op=ALU.add)
                fw.op("act", "activation", [self.b_rcol, blgB], [bdcol], out=dcol[:, 0:1], in_=self.rcol[:, 0:1], func=AF.Exp, scale=lgf)
                fw.op("act", "activation", [self.b_rcol, blgB, bdcol], [bdcol], out=dcol[:, 1:2], in_=self.rcol[:, 1:2], func=AF.Exp, scale=lgb)
                fw.op("act", "activation", [self.b_rcol, blgB, bdcol], [bdcol], out=dcol[:, 2:3], in_=self.rcol[:, 2:3], func=AF.Exp, scale=lgf)
                fw.op("act", "activation", [self.b_rcol, blgB, bdcol], [bdcol], out=dcol[:, 3:4], in_=self.rcol[:, 2:3], func=AF.Exp, scale=lgb)
                fw.op("act", "activation", [self.b_rcol, blgB, bdcol], [bdcol], out=dcol[:, 4:5], in_=self.rcol[:, 4:5], func=AF.Exp, scale=lgf)
                fw.op("act", "activation", [self.b_rcol, blgB, bdcol], [bdcol], out=dcol[:, 5:6], in_=self.rcol[:, 5:6], func=AF.Exp, scale=lgb)
                self.load_w_cols(wqk[:, :, 0:128], bwqk, l, hd * 128, 128)
                self.load_w_cols(wqk[:, :, 128:256], bwqk, l, 512 + hd * 128, 128)
                self.load_w_cols(wkvg[:, :, 0:128], bwkvg, l, 512 + hd * 128, 128)
                self.load_w_cols(wkvg[:, :, 128:256], bwkvg, l, 1024 + hd * 128, 128)
                self.load_w_cols(wkvg[:, :, 256:384], bwkvg, l, 1536 + hd * 128, 128)
                for (t0, n) in TG:
                    tl = list(self.tiles_of(t0, n))
                    hb = [bhT[t] for t in tl]
                    nb = n // 128
                    pq, bpq = self.pf()
                    for k in range(8):
                        fw.op("pe", "matmul", hb + [bwqk], [bpq], out=pq[:, 0:n], lhsT=wqk[:, k, 0:128], rhs=hT[:, k, t0:t0 + n], start=(k == 0), stop=(k == 7))
                    wq_ = [bq_t[t] for t in tl]
                    fw.op("act", "mul", [bpq], wq_, out=qT[:, t0:t0 + n], in_=pq[:, 0:n], mul=QS)
                    pk, bpk = self.pf()
                    for k in range(8):
                        fw.op("pe", "matmul", hb + [bwqk], [bpk], out=pk[:, 0:n], lhsT=wqk[:, k, 128:256], rhs=hT[:, k, t0:t0 + n], start=(k == 0), stop=(k == 7))
                    fw.op("act", "copy", [bpk], [bk_t[t] for t in tl], out=kT[:, t0:t0 + n], in_=pk[:, 0:n])
                for t in range(NT):
                    pt_, bp = self.pf()
                    for k in range(8):
                        fw.op("pe", "matmul", [bhT[t], bwkvg], [bp], out=pt_[:, 0:384], lhsT=hT[:, k, t * 128:(t + 1) * 128], rhs=wkvg[:, k, :], start=(k == 0), stop=(k == 7))
                    fw.op("act", "copy", [bp], [btok[t]], out=ktok[:, t, :], in_=pt_[:, 0:128])
                    fw.op("act", "copy", [bp], [btok[t]], out=vtok[:, t, :], in_=pt_[:, 128:256])
                    fw.op("dve", "tensor_scalar", [bp, bdcol], [btok[t]], out=vdf[:, t, :], in0=pt_[:, 128:256], scalar1=dcol[:, 0:1], scalar2=None, op0=ALU.mult)
                    fw.op("dve", "tensor_scalar", [bp, bdcol], [btok[t]], out=vdb[:, t, :], in0=pt_[:, 128:256], scalar1=dcol[:, 1:2], scalar2=None, op0=ALU.mult)
                    fw.op("act", "activation", [bp], [btok[t]], out=gs[:, t, :], in_=pt_[:, 256:384], func=AF.Silu)
                for (S, bS, SB, bSB_t, vd, ci, order) in ((Sf, bSf, SfB, bSf_t, vdf, 2, list(range(NT))),
                                                          (Sb, bSb, SbB, bSb_t, vdb, 3, [1, 0] + list(range(NT - 1, 1, -1)))):
                    fw.op("pool", "memset", [], [bS], ap=S[:], constant=0.0)
                    for t in order:
                        fw.op("act", "copy", [bS], [bSB_t[t]], out=SB[:, t, :], in_=S[:])
                        pkv, bpkv = self.pf()
                        fw.op("pe", "matmul", [btok[t]], [bpkv], out=pkv[:, 0:128], lhsT=ktok[:, t, :], rhs=vd[:, t, :], start=True, stop=True)
                        fw.op("dve", "scalar_tensor_tensor", [bS, bpkv, bdcol], [bS], out=S[:], in0=S[:], scalar=dcol[:, ci:ci + 1], in1=pkv[:, 0:128], op0=ALU.mult, op1=ALU.add)
                for t in range(NT):
                    cs = slice(t * 128, (t + 1) * 128)
                    attm, battm = attm_[t % 2]; on, bon = on_[t % 2]; yk, byk = yk_[t % 2]; st, bst = st_[t % 2]; mv, bmv = mv_[t % 2]
                    pa, bpa = self.pf()
                    fw.op("pe", "matmul", [bk_t[t], bq_t[t]], [bpa], out=pa[:, 0:128], lhsT=kT[:, cs], rhs=qT[:, cs], start=True, stop=True)
                    fw.op("dve", "tensor_tensor", [bpa, bMT], [battm], out=attm[:], in0=pa[:, 0:128], in1=MT[:], op=ALU.mult)
                    po, bpo = self.pf()
                    fw.op("pe", "matmul", [battm, btok[t]], [bpo], out=po[:, 0:128], lhsT=attm[:], rhs=vtok[:, t, :], start=True, stop=True)
                    fw.op("pe", "matmul", [bq_t[t], bSf_t[t]], [bpo], out=po[:, 128:256], lhsT=qT[:, cs], rhs=SfB[:, t, :], start=True, stop=True)
                    fw.op("pe", "matmul", [bq_t[t], bSb_t[t]], [bpo], out=po[:, 256:384], lhsT=qT[:, cs], rhs=SbB[:, t, :], start=True, stop=True)
                    fw.op("act", "copy", [bpo], [bon], out=on[:], in_=po[:, 0:128])
                    fw.op("dve", "scalar_tensor_tensor", [bpo, bdcol, bon], [bon], out=on[:], in0=po[:, 128:256], scalar=dcol[:, 4:5], in1=on[:], op0=ALU.mult, op1=ALU.add)
                    fw.op("dve", "scalar_tensor_tensor", [bpo, bdcol, bon], [bon], out=on[:], in0=po[:, 256:384], scalar=dcol[:, 5:6], in1=on[:], op0=ALU.mult, op1=ALU.add)
                    fw.op("dve", "bn_stats", [bon], [bst], out=st[:], in_=on[:])
                    fw.op("dve", "bn_aggr", [bst], [bmv], out=mv[:, 0:2], in_=st[:])
                    fw.op("act", "activation", [bmv, self.b_epsc], [bmv], out=mv[:, 2:3], in_=mv[:, 1:2], func=AF.Sqrt, bias=self.epsc[:, 1:2], scale=1.0)
                    fw.op("dve", "reciprocal", [bmv], [bmv], out=mv[:, 3:4], in_=mv[:, 2:3])
                    fw.op("dve", "tensor_scalar", [bon, bmv], [bon], out=on[:], in0=on[:], scalar1=mv[:, 0:1], scalar2=mv[:, 3:4], op0=ALU.subtract, op1=ALU.mult)
                    fw.op("dve", "tensor_tensor", [bon, bgnB], [bon], out=on[:], in0=on[:], in1=gnB[:, hd * 128:(hd + 1) * 128], op=ALU.mult)
                    fw.op("dve", "tensor_tensor", [bon, btok[t]], [byk], out=yk[:], in0=on[:], in1=gs[:, t, :], op=ALU.mult)
                    pb_, bpb = self.pb()
                    fw.op("pe", "transpose", [byk, self.b_identb], [bpb], out=pb_[:, 0:128], in_=yk[:], identity=self.identb[:])
                    yTt, byTt = yTt_[t % 2]
                    fw.op("act", "copy", [bpb], [byTt], out=yTt[:], in_=pb_[:, 0:128])
                    fw.dma([byTt], [self.bYT[hd]], out=self.YT[hd * 128:(hd + 1) * 128, cs], in_=yTt[:], stream="d2")
            if self.debug:
                self.dump_yt("yret%d" % l, 0, 4)
            fw.flush()

    def dump_yt(self, name, c0, nchunks):
        if name not in self.debug:
            return
        t = self.nc.dram_tensor("dbg_" + name, [nchunks * 128, T], BF16, kind="ExternalOutput")
        self.dbg_out.append("dbg_" + name)
        self.fw.dma([self.bYT[c0 + i] for i in range(nchunks)], [Buf()], out=t.ap(), in_=self.YT[c0 * 128:(c0 + nchunks) * 128, :], stream="d2")

    def sincos(self, src, bsrc, shape, sin_out, cos_out, bout, ki, bki, kf, bkf, eng="dve"):
        fw = self.fw
        PI_ = 3.1415925
        fw.op("dve", "tensor_scalar", [bsrc], [bki], out=ki, in0=src, scalar1=1.0 / TWO_PI, scalar2=None, op0=ALU.mult)
        fw.op("dve", "tensor_copy", [bki], [bkf], out=kf, in_=ki)
        fw.op("dve", "scalar_tensor_tensor", [bkf, bsrc], [bkf], out=kf, in0=kf, scalar=-TWO_PI, in1=src, op0=ALU.mult, op1=ALU.add)
        fw.op("dve", "tensor_scalar", [bkf], [bkf], out=kf, in0=kf, scalar1=-PI_, scalar2=PI_, op0=ALU.max, op1=ALU.min)
        if sin_out is not None:
            fw.op("act", "activation", [bkf], [bout], out=sin_out, in_=kf, func=AF.Sin)
        if cos_out is not None:
            fw.op("dve", "scalar_tensor_tensor", [bkf], [bkf], out=kf, in0=kf, scalar=-1.0, in1=kf, op0=ALU.mult, op1=ALU.max)
            fw.op("act", "activation", [bkf, self.b_epsc], [bout], out=cos_out, in_=kf, func=AF.Sin, scale=-1.0, bias=self.epsc[:, 2:3])

    def phase_s5(self, l, hT, bhT):
        fw, I = self.fw, self.I
        CH = [(0, 256)] + [(256 + 512 * i, 512) for i in range(8)]
        with ExitStack() as es:
            prm, bprm = self.sb(es, "s5prm", [128, 3, 32], F32)
            for dr in range(2):
                fw.dma([], [bprm], out=prm[:, 0, dr * 16:(dr + 1) * 16], in_=I["s5_lre"][l, dr])
                fw.dma([], [bprm], out=prm[:, 1, dr * 16:(dr + 1) * 16], in_=I["s5_lim"][l, dr])
                fw.dma([], [bprm], out=prm[:, 2, dr * 16:(dr + 1) * 16], in_=I["s5_ldt"][l, dr])
            dsc, bdsc = self.sb(es, "s5dsc", [128, 12, 32], F32)
            ki32, bki32 = self.sb(es, "s5ki", [128, 32], I32)
            kf32, bkf32 = self.sb(es, "s5kf", [128, 32], F32)
            DT, LR, TH, R_, SN, CS, AR, AI, DEN, CR, CI, NCR = [dsc[:, i, :] for i in range(12)]
            lre, lim = prm[:, 0, :], prm[:, 1, :]
            B2 = [bprm, bdsc]
            fw.op("act", "activation", [bprm], [bdsc], out=DT, in_=prm[:, 2, :], func=AF.Exp)
            fw.op("dve", "tensor_tensor", B2, [bdsc], out=LR, in0=lre, in1=DT, op=ALU.mult)
            fw.op("dve", "tensor_tensor", B2, [bdsc], out=TH, in0=lim, in1=DT, op=ALU.mult)
            fw.op("act", "activation", [bdsc], [bdsc], out=R_, in_=LR, func=AF.Exp)
            self.sincos(TH, bdsc, None, SN, CS, bdsc, ki32[:], bki32, kf32[:], bkf32)
            fw.op("dve", "tensor_tensor", B2, [bdsc], out=AR, in0=R_, in1=CS, op=ALU.mult)
            fw.op("dve", "tensor_tensor", B2, [bdsc], out=AI, in0=R_, in1=SN, op=ALU.mult)
            fw.op("dve", "tensor_scalar", B2, [bdsc], out=AR, in0=AR, scalar1=-1.0, scalar2=None, op0=ALU.add)
            fw.op("dve", "tensor_tensor", B2, [bdsc], out=DEN, in0=lre, in1=lre, op=ALU.mult)
            fw.op("dve", "tensor_tensor", B2, [bdsc], out=LR, in0=lim, in1=lim, op=ALU.mult)
            fw.op("dve", "tensor_tensor", B2, [bdsc], out=DEN, in0=DEN, in1=LR, op=ALU.add)
            fw.op("dve", "reciprocal", B2, [bdsc], out=DEN, in_=DEN)
            fw.op("dve", "tensor_tensor", B2, [bdsc], out=CR, in0=AR, in1=lre, op=ALU.mult)
            fw.op("dve", "tensor_tensor", B2, [bdsc], out=LR, in0=AI, in1=lim, op=ALU.mult)
            fw.op("dve", "tensor_tensor", B2, [bdsc], out=CR, in0=CR, in1=LR, op=ALU.add)
            fw.op("dve", "tensor_tensor", B2, [bdsc], out=CR, in0=CR, in1=DEN, op=ALU.mult)
            fw.op("dve", "tensor_tensor", B2, [bdsc], out=CI, in0=AI, in1=lre, op=ALU.mult)
            fw.op("dve", "tensor_tensor", B2, [bdsc], out=LR, in0=AR, in1=lim, op=ALU.mult)
            fw.op("dve", "tensor_tensor", B2, [bdsc], out=CI, in0=CI, in1=LR, op=ALU.subtract)
            fw.op("dve", "tensor_tensor", B2, [bdsc], out=CI, in0=CI, in1=DEN, op=ALU.mult)
            fw.op("dve", "tensor_scalar", B2, [bdsc], out=NCR, in0=CR, scalar1=-1.0, scalar2=None, op0=ALU.mult)
            cut = getattr(self, 's5_cut', 0)
            if cut == 1:
                self.dump('dsc', dsc[:], [128, 12, 32], [bdsc])
                fw.flush()
                return
            Bb, bBb = self.sb(es, "s5Bb", [128, 2, 2, 4, 2, 128], BF16)
            Cb, bCb = self.sb(es, "s5Cb", [128, 2, 2, 4, 64], BF16)
            for dr in range(2):
                for ri in range(2):
                    fw.dma([], [bBb], q="pool", stream="d1", out=Bb[:, dr, ri], in_=I["s5_B"][l, dr, ri])
            dT_, bdT = self.sb(es, "s5dT", [128, 4], F32)
            fw.dma([], [bdT], out=dT_[:], in_=I["s5_dT"][l])
            iota, biota = self.sb(es, "s5iota", [128, 512], F32)
            fw.dma([], [biota], out=iota[:], in_=I["iota512"].ap())
            wu, bwu = self.sb(es, "s5wu", [128, 8, 128], BF16)
            uf, buf_ = self.sb(es, "s5uf", [128, T], F32)
            ub, bub = self.sb(es, "s5ub", [128, T], BF16)
            yacc, byacc = self.sb(es, "s5yacc", [128, T], F32)
            gb_ = [self.sb(es, f"s5gb{i}", [128, 512], BF16) for i in range(2)]
            tb = {}
            for nm in ("cos", "sin", "dre", "dim", "rf"):
                tb[nm] = self.sb(es, "s5t_" + nm, [128, 512], F32)
            wk = {}
            for nm in ("pre", "pim", "t1", "t2", "t3", "t4", "gre", "gim", "hre", "nhim"):
                wk[nm] = self.sb(es, "s5w_" + nm, [128, 512], F32)
            hreb, bhreb = self.sb(es, "s5hreb", [128, 512], BF16)
            nhimb, bnhimb = self.sb(es, "s5nhimb", [128, 512], BF16)
            carry, bcarry = self.sb(es, "s5carry", [128, 2], F32)
            self.GS5 = getattr(self, "GS5", None) or self.scratch("GS5", [512, T], BF16)
            bGS5 = [Buf() for _ in range(4)]
            ychunk = [Buf() for _ in CH]
            if cut == 2:
                fw.flush()
                return
            for o in range(4):
                for dr in range(2):
                    for ri in range(2):
                        fw.dma([], [bCb], q="pool", stream="d1", out=Cb[:, dr, ri], in_=I["s5_C"][l, dr, ri][:, o * 4:(o + 1) * 4, :])
                    fw.op("act", "mul", [bCb], [bCb], out=Cb[:, dr, 1], in_=Cb[:, dr, 1], mul=-1.0)
                self.load_w_cols(wu[:], bwu, l, 2048 + o * 128, 128)
                for (t0, n) in TG:
                    hb = [bhT[t] for t in self.tiles_of(t0, n)]
                    pu, bpu = self.pf()
                    for k in range(8):
                        fw.op("pe", "matmul", hb + [bwu], [bpu], out=pu[:, 0:n], lhsT=wu[:, k, :], rhs=hT[:, k, t0:t0 + n], start=(k == 0), stop=(k == 7))
                    fw.op("act", "copy", [bpu], [buf_], out=uf[:, t0:t0 + n], in_=pu[:, 0:n])
                    fw.op("dve", "tensor_copy", [buf_], [bub], out=ub[:, t0:t0 + n], in_=uf[:, t0:t0 + n])
                if cut == 5:
                    fw.flush()
                    return
                fw.op("pool", "memset", [], ychunk, ap=yacc[:], constant=0.0)
                if cut == 3:
                    fw.flush()
                    return
                lim = getattr(self, "s5_lim", None)
                for dr in range(2):
                    if lim is not None and dr >= lim[0]:
                        break
                    for q4 in range(4):
                        pt = o * 4 + q4
                        c = dr * 16 + pt
                        ps = slice(64 * (q4 // 2), 64 * (q4 // 2) + 64)
                        jj = q4 % 2
                        (cosT, bcos), (sinT, bsin) = tb["cos"], tb["sin"]
                        kiT, bkiT = wk["t4"][0][:].bitcast(I32), wk["t4"][1]
                        (dreT, bdre), (dimT, bdim), (rfT, brf) = tb["dre"], tb["dim"], tb["rf"]
                        t1, bt1 = wk["t1"]
                        fw.op("dve", "tensor_scalar", [biota, bdsc], [bt1], out=t1[:], in0=iota[:], scalar1=TH[:, c:c + 1], scalar2=None, op0=ALU.mult)
                        fw.op("dve", "tensor_scalar", [bt1], [bkiT], out=kiT, in0=t1[:], scalar1=1.0 / TWO_PI, scalar2=None, op0=ALU.mult)
                        t2, bt2 = wk["t2"]
                        fw.op("dve", "tensor_copy", [bkiT], [bt2], out=t2[:], in_=kiT)
                        fw.op("dve", "scalar_tensor_tensor", [bt2, bt1], [bt2], out=t2[:], in0=t2[:], scalar=-TWO_PI, in1=t1[:], op0=ALU.mult, op1=ALU.add)
                        fw.op("dve", "tensor_scalar", [bt2], [bt2], out=t2[:], in0=t2[:], scalar1=-3.1415925, scalar2=3.1415925, op0=ALU.max, op1=ALU.min)
                        fw.op("act", "activation", [bt2], [bsin], out=sinT[:], in_=t2[:], func=AF.Sin)
                        fw.op("dve", "scalar_tensor_tensor", [bt2], [bt2], out=t2[:], in0=t2[:], scalar=-1.0, in1=t2[:], op0=ALU.mult, op1=ALU.max)
                        fw.op("act", "activation", [bt2, self.b_epsc], [bcos], out=cosT[:], in_=t2[:], func=AF.Sin, scale=-1.0, bias=self.epsc[:, 2:3])
                        fw.op("dve", "tensor_scalar", [bcos, bdsc], [bdre], out=dreT[:], in0=cosT[:], scalar1=CR[:, c:c + 1], scalar2=None, op0=ALU.mult)
                        fw.op("dve", "scalar_tensor_tensor", [bsin, bdsc, bdre], [bdre], out=dreT[:], in0=sinT[:], scalar=CI[:, c:c + 1], in1=dreT[:], op0=ALU.mult, op1=ALU.add)
                        fw.op("dve", "tensor_scalar", [bcos, bdsc], [bdim], out=dimT[:], in0=cosT[:], scalar1=CI[:, c:c + 1], scalar2=None, op0=ALU.mult)
                        fw.op("dve", "scalar_tensor_tensor", [bsin, bdsc, bdim], [bdim], out=dimT[:], in0=sinT[:], scalar=NCR[:, c:c + 1], in1=dimT[:], op0=ALU.mult, op1=ALU.add)
                        fw.op("dve", "tensor_scalar", [biota], [brf], out=rfT[:], in0=iota[:], scalar1=0.0, scalar2=None, op0=ALU.mult)
                        fw.op("dve", "tensor_scalar", [brf, bdsc], [brf], out=rfT[:], in0=rfT[:], scalar1=R_[:, c:c + 1], scalar2=None, op0=ALU.add)
                        fw.op("pool", "memset", [], [bcarry], ap=carry[:], constant=0.0)
                        order = list(range(9)) if dr == 0 else [0] + list(range(8, 0, -1))
                        if lim is not None:
                            order = order[:lim[1]]
                        for ci_ in order:
                            t0, n = CH[ci_]
                            rv = (lambda ap: ap) if dr == 0 else (lambda ap: ap[:, ::-1])
                            pP, bpP = self.pf()
                            pQ, bpQ = self.pf()
                            fw.op("pe", "matmul", [bBb, bub], [bpP], out=pP[:, 0:n], lhsT=Bb[ps, dr, 0, o, jj, :], rhs=ub[ps, t0:t0 + n], start=True, stop=True)
                            fw.op("pe", "matmul", [bBb, bub], [bpQ], out=pQ[:, 0:n], lhsT=Bb[ps, dr, 1, o, jj, :], rhs=ub[ps, t0:t0 + n], start=True, stop=True)
                            pre, bpre = wk["pre"]; pim, bpim = wk["pim"]
                            fw.op("act", "copy", [bpP], [bpre], out=rv(pre[:, 0:n]), in_=pP[:, 0:n])
                            fw.op("act", "copy", [bpQ], [bpim], out=rv(pim[:, 0:n]), in_=pQ[:, 0:n])
                            t2, bt2 = wk["t2"]; t3, bt3 = wk["t3"]; t4, bt4 = wk["t4"]
                            gre, bgre = wk["gre"]; gim, bgim = wk["gim"]; hre, bhre = wk["hre"]; nhim, bnhim = wk["nhim"]
                            N = slice(0, n)
                            fw.op("dve", "tensor_tensor", [bpre, bdre], [bt1], out=t1[:, N], in0=pre[:, N], in1=dreT[:, N], op=ALU.mult)
                            fw.op("dve", "tensor_tensor", [bpim, bdim], [bt2], out=t2[:, N], in0=pim[:, N], in1=dimT[:, N], op=ALU.mult)
                            fw.op("dve", "tensor_tensor", [bt1, bt2], [bt1], out=t1[:, N], in0=t1[:, N], in1=t2[:, N], op=ALU.subtract)
                            fw.op("pool", "tensor_tensor", [bpre, bdim], [bt3], out=t3[:, N], in0=pre[:, N], in1=dimT[:, N], op=ALU.mult)
                            fw.op("pool", "tensor_tensor", [bpim, bdre], [bt4], out=t4[:, N], in0=pim[:, N], in1=dreT[:, N], op=ALU.mult)
                            fw.op("pool", "tensor_tensor", [bt3, bt4], [bt3], out=t3[:, N], in0=t3[:, N], in1=t4[:, N], op=ALU.add)
                            fw.op("dve", "tensor_tensor_scan", [brf, bt1, bcarry], [bgre], out=gre[:, N], data0=rfT[:, N], data1=t1[:, N], initial=carry[:, 0:1], op0=ALU.mult, op1=ALU.add)
                            fw.op("dve", "tensor_tensor_scan", [brf, bt3, bcarry], [bgim], out=gim[:, N], data0=rfT[:, N], data1=t3[:, N], initial=carry[:, 1:2], op0=ALU.mult, op1=ALU.add)
                            fw.op("dve", "tensor_tensor", [bcos, bgre], [bhre], out=hre[:, N], in0=cosT[:, N], in1=gre[:, N], op=ALU.mult)
                            fw.op("pool", "tensor_tensor", [bsin, bgim], [bt4], out=t4[:, N], in0=sinT[:, N], in1=gim[:, N], op=ALU.mult)
                            fw.op("dve", "tensor_tensor", [bhre, bt4], [bhre], out=hre[:, N], in0=hre[:, N], in1=t4[:, N], op=ALU.subtract)
                            fw.op("pool", "tensor_tensor", [bsin, bgre], [bnhim], out=nhim[:, N], in0=sinT[:, N], in1=gre[:, N], op=ALU.mult)
                            fw.op("pool", "tensor_tensor", [bcos, bgim], [bt2], out=t2[:, N], in0=cosT[:, N], in1=gim[:, N], op=ALU.mult)
                            fw.op("pool", "tensor_tensor", [bnhim, bt2], [bnhim], out=nhim[:, N], in0=nhim[:, N], in1=t2[:, N], op=ALU.add)
                            fw.op("act", "copy", [bhre], [bhreb], out=rv(hreb[:, N]), in_=hre[:, N])
                            fw.op("act", "copy", [bnhim], [bnhimb], out=rv(nhimb[:, N]), in_=nhim[:, N])
                            fw.op("act", "copy", [bhre], [bcarry], out=carry[:, 0:1], in_=hre[:, n - 1:n])
                            fw.op("act", "copy", [bnhim, bcarry], [bcarry], out=carry[:, 1:2], in_=nhim[:, n - 1:n])
                            py, bpy = self.pf()
                            fw.op("pe", "matmul", [bCb, bhreb], [bpy], out=py[ps, 0:n], lhsT=Cb[:, dr, 0, q4, :], rhs=hreb[:, N], start=True, stop=False)
                            fw.op("pe", "matmul", [bCb, bnhimb], [bpy], out=py[ps, 0:n], lhsT=Cb[:, dr, 1, q4, :], rhs=nhimb[:, N], start=False, stop=True)
                            fw.op("dve", "tensor_tensor", [bpy, ychunk[ci_]], [ychunk[ci_]], out=yacc[ps, t0:t0 + n], in0=py[ps, 0:n], in1=yacc[ps, t0:t0 + n], op=ALU.add)
                if cut == 4:
                    fw.flush()
                    return
                fw.op("dve", "scalar_tensor_tensor", [buf_, bdT] + ychunk, ychunk, out=yacc[:], in0=uf[:], scalar=dT_[:, o:o + 1], in1=yacc[:], op0=ALU.mult, op1=ALU.add)
                for gi_, (t0, n) in enumerate(TG):
                    gb, bgb = gb_[gi_ % 2]
                    fw.op("act", "activation", ychunk, [bgb], out=gb[:, 0:n], in_=yacc[:, t0:t0 + n], func=AF.Gelu_apprx_tanh)
                    fw.dma([bgb], [bGS5[o]], out=self.GS5[o * 128:(o + 1) * 128, t0:t0 + n], in_=gb[:, 0:n], stream="d2")
            fw.flush()
        with ExitStack() as es:
            W1b, bW1b = self.sb(es, "s5W1b", [128, 4, 512], BF16)
            W2b, bW2b = self.sb(es, "s5W2b", [128, 4, 512], BF16)
            fw.dma([], [bW1b], q="pool", stream="d1", out=W1b[:], in_=I["s5_glu_w1"][l].rearrange("(k p) c -> p k c", p=128))
            fw.dma([], [bW2b], q="pool", stream="d1", out=W2b[:], in_=I["s5_glu_w2"][l].rearrange("(k p) c -> p k c", p=128))
            g_ = [self.sb(es, f"s5g{i}", [128, 4, 512], BF16) for i in range(2)]
            sg_ = [self.sb(es, f"s5sg{i}", [128, 512], F32) for i in range(2)]
            yo_ = [self.sb(es, f"s5yo{i}", [128, 512], BF16) for i in range(2)]
            for gi, (t0, n) in enumerate(TG):
                g, bg = g_[gi % 2]
                fw.dma(bGS5, [bg], out=g[:, :, 0:n], in_=self.GS5[:, t0:t0 + n].rearrange("(k p) t -> p k t", p=128))
                for c_ in range(4):
                    sg, bsg = sg_[c_ % 2]; yo, byo = yo_[c_ % 2]
                    pA, bpA = self.pf()
                    pB, bpB = self.pf()
                    for k in range(4):
                        fw.op("pe", "matmul", [bW1b, bg], [bpA], out=pA[:, 0:n], lhsT=W1b[:, k, c_ * 128:(c_ + 1) * 128], rhs=g[:, k, 0:n], start=(k == 0), stop=(k == 3))
                    for k in range(4):
                        fw.op("pe", "matmul", [bW2b, bg], [bpB], out=pB[:, 0:n], lhsT=W2b[:, k, c_ * 128:(c_ + 1) * 128], rhs=g[:, k, 0:n], start=(k == 0), stop=(k == 3))
                    fw.op("act", "activation", [bpB], [bsg], out=sg[:, 0:n], in_=pB[:, 0:n], func=AF.Sigmoid)
                    fw.op("dve", "tensor_tensor", [bpA, bsg], [byo], out=yo[:, 0:n], in0=pA[:, 0:n], in1=sg[:, 0:n], op=ALU.mult)
                    fw.dma([byo], [self.bYT[4 + c_]], out=self.YT[512 + c_ * 128:512 + (c_ + 1) * 128, t0:t0 + n], in_=yo[:, 0:n], stream="d2")
            if self.debug:
                self.dump_yt("ys5%d" % l, 4, 4)
            fw.flush()

    def phase_hy(self, l, hT, bhT):
        fw, I = self.fw, self.I
        do_ctx = (l == 0)
        with ExitStack() as es:
            w1, bw1 = self.sb(es, "hfw1", [33, 64], F32)
            w2, bw2 = self.sb(es, "hfw2", [64, 64], F32)
            w3, bw3 = self.sb(es, "hfw3", [64, 512], F32)
            fb, bfb = self.sb(es, "hffb", [64, 6], F32)
            nd_, bnd = self.sb(es, "hfnd", [128, 4], F32)
            fw.dma([], [bw1], out=w1[:], in_=I["hy_f_w1"][l])
            fw.dma([], [bw2], out=w2[:], in_=I["hy_f_w2"][l])
            fw.dma([], [bw3], out=w3[:], in_=I["hy_f_w3"][l])
            fw.dma([], [bfb], out=fb[:, 0:1], in_=I["hy_f_b1"][l])
            fw.dma([], [bfb], out=fb[:, 1:2], in_=I["hy_f_b2"][l])
            fw.dma([], [bfb], out=fb[:, 2:4], in_=I["hy_f_fr"][l])
            fw.dma([], [bnd], out=nd_[:], in_=I["ndelta"].ap())
            fw.op("dve", "tensor_tensor", [bfb], [bfb], out=fb[:, 4:6], in0=fb[:, 0:2], in1=fb[:, 2:4], op=ALU.mult)
            zt_ = [self.sb(es, f"hfz{i}", [33, 512], F32) for i in range(2)]
            of_ = [self.sb(es, f"hfo{i}", [128, 512], F32) for i in range(2)]
            a_, ba_ = self.sb(es, "hfa", [64, 512], F32)
            hh_, bhh = self.sb(es, "hfh", [64, 512], F32)
            ki, bki = self.sb(es, "hfki", [64, 512], I32)
            kf, bkf = self.sb(es, "hfkf", [64, 512], F32)
            win, bwin = self.sb(es, "hfwin", [128, 512], F32)
            frev = [self.sb(es, f"hffr{i}", [128, L], BF16) for i in range(4)]
            zpad, bzpad = self.sb(es, "hfzp", [128, 128], BF16)
            fw.op("pool", "memset", [], [bzpad], ap=zpad[:], constant=0.0)
            jobs = [("zT", "off", L, self.GF, self.bGF, 4352)]
            if do_ctx:
                jobs.append(("zTc", "offc", CTX, self.GFc, self.bGFc, 512))
            for (zn, on, n, GFt, bGFt, LEN) in jobs:
                cn = min(512, n)
                for ch in range(n // cn):
                    c0 = ch * cn
                    zt, bzt = zt_[ch % 2]
                    of, bof = of_[ch % 2]
                    fw.dma([], [bzt], out=zt[:, 0:cn], in_=I[zn][:, c0:c0 + cn])
                    fw.dma([], [bof], out=of[:, 0:cn], in_=I[on][:, c0:c0 + cn])
                    p1, bp1 = self.pf()
                    fw.op("pe", "matmul", [bw1, bzt], [bp1], out=p1[0:64, 0:cn], lhsT=w1[:], rhs=zt[:, 0:cn], start=True, stop=True)
                    fw.op("act", "activation", [bp1, bfb], [ba_], out=a_[:, 0:cn], in_=p1[0:64, 0:cn], func=AF.Identity, scale=fb[:, 2:3], bias=fb[:, 4:5])
                    self.sincos(a_[:, 0:cn], ba_, None, hh_[:, 0:cn], None, bhh, ki[:, 0:cn], bki, kf[:, 0:cn], bkf)
                    p2, bp2 = self.pf()
                    fw.op("pe", "matmul", [bw2, bhh], [bp2], out=p2[0:64, 0:cn], lhsT=w2[:], rhs=hh_[:, 0:cn], start=True, stop=True)
                    fw.op("act", "activation", [bp2, bfb], [ba_], out=a_[:, 0:cn], in_=p2[0:64, 0:cn], func=AF.Identity, scale=fb[:, 3:4], bias=fb[:, 5:6])
                    self.sincos(a_[:, 0:cn], ba_, None, hh_[:, 0:cn], None, bhh, ki[:, 0:cn], bki, kf[:, 0:cn], bkf)
                    for ct in range(4):
                        fr, bfr = frev[ct]
                        p3, bp3 = self.pf()
                        fw.op("pe", "matmul", [bw3, bhh], [bp3], out=p3[:, 0:cn], lhsT=w3[:, ct * 128:(ct + 1) * 128], rhs=hh_[:, 0:cn], start=True, stop=True)
                        fw.op("act", "activation", [bof, bnd], [bwin], out=win[:, 0:cn], in_=of[:, 0:cn], func=AF.Exp, scale=nd_[:, ct:ct + 1])
                        fw.op("dve", "tensor_tensor", [bp3, bwin], [bfr], out=fr[:, n - c0 - cn:n - c0][:, ::-1], in0=p3[:, 0:cn], in1=win[:, 0:cn], op=ALU.mult)
                for ct in range(4):
                    fr, bfr = frev[ct]
                    rs = slice(ct * 128, (ct + 1) * 128)
                    fw.dma([bfr], [bGFt[ct]], out=GFt[rs, 128:128 + n], in_=fr[:, 0:n], stream="d2")
                    fw.dma([bzpad], [bGFt[ct]], out=GFt[rs, 0:128], in_=zpad[:], stream="d2")
                    fw.dma([bzpad], [bGFt[ct]], out=GFt[rs, 128 + n:LEN], in_=zpad[:, 0:LEN - 128 - n], stream="d2")
            fw.flush()
        with ExitStack() as es:
            wx, bwx = self.sb(es, "hywx", [128, 8, 128], BF16)
            zraw, bzraw = self.sb(es, "hyzraw", [128, T], F32)
            A, bA = self.sb(es, "hyA", [128, T], F32)
            B, bB = self.sb(es, "hyB", [128, T], F32)
            yvb, byvb = self.sb(es, "hyyvb", [128, T], BF16)
            yout_ = [self.sb(es, f"hyyout{i}", [128, 512], BF16) for i in range(2)]
            Upad, bUpad = self.sb(es, "hyUpad", [128, 32, 128], BF16)
            Upc, bUpc = self.sb(es, "hyUpc", [128, 2, 128], BF16)
            TT_ = [self.sb(es, f"hyTT{i}", [128, 33 * 128], BF16) for i in range(2)]
            yrev, byrev = self.sb(es, "hyyrev", [128, 32, 128], BF16)
            tmp, btmp = self.sb(es, "hytmp", [128, 512], F32)
            swT, bswT = self.sb(es, "hyswT", [128, 12, 3], F32)
            sbT, bsbT = self.sb(es, "hysbT", [128, 12], F32)
            biasT, bbiasT = self.sb(es, "hybiasT", [128, 4], F32)
            fw.dma([], [bswT], out=swT[:], in_=I["hy_swT"][l])
            fw.dma([], [bsbT], out=sbT[:], in_=I["hy_sbT"][l])
            fw.dma([], [bbiasT], out=biasT[:], in_=I["hy_biasT"][l])
            segs = [(0, CTX), (CTX, T)]
            tti = 0
            for ct in range(4):
                for j, dst, bdst in ((1, A, bA), (2, B, bB), (0, B, bB)):
                    if j == 0:
                        fw.op("dve", "tensor_tensor", [bA, bB], [bA], out=A[:], in0=A[:], in1=B[:], op=ALU.mult)
                        fw.op("act", "copy", [bA], [byvb], out=yvb[:], in_=A[:])
                    cidx = j * 4 + ct
                    self.load_w_cols(wx[:], bwx, l, 2560 + j * 512 + ct * 128, 128)
                    for (t0, n) in TG:
                        hb = [bhT[t] for t in self.tiles_of(t0, n)]
                        pz, bpz = self.pf()
                        for k in range(8):
                            fw.op("pe", "matmul", hb + [bwx], [bpz], out=pz[:, 0:n], lhsT=wx[:, k, :], rhs=hT[:, k, t0:t0 + n], start=(k == 0), stop=(k == 7))
                        fw.op("act", "copy", [bpz], [bzraw], out=zraw[:, t0:t0 + n], in_=pz[:, 0:n])
                    fw.op("act", "activation", [bzraw, bswT, bsbT], [bdst], out=dst[:], in_=zraw[:], func=AF.Identity, scale=swT[:, cidx, 1:2], bias=sbT[:, cidx:cidx + 1])
                    for (s, e) in segs:
                        fw.op("dve", "scalar_tensor_tensor", [bzraw, bswT, bdst], [bdst], out=dst[:, s + 1:e], in0=zraw[:, s:e - 1], scalar=swT[:, cidx, 0:1], in1=dst[:, s + 1:e], op0=ALU.mult, op1=ALU.add)
                        fw.op("dve", "scalar_tensor_tensor", [bzraw, bswT, bdst], [bdst], out=dst[:, s:e - 1], in0=zraw[:, s + 1:e], scalar=swT[:, cidx, 2:3], in1=dst[:, s:e - 1], op0=ALU.mult, op1=ALU.add)
                convs = [(CTX, 32, 16, self.GF, self.bGF, 4352, Upad, bUpad)]
                if do_ctx:
                    convs.append((0, 2, 1, self.GFc, self.bGFc, 512, Upc, bUpc))
                for (tok0, nblk, nd, GFt, bGFt, LEN, Up, bUp) in convs:
                    nm = 2 * nd + 1
                    for g0 in range(0, nblk, 8):
                        gn = min(8, nblk - g0)
                        pb_, bpb = self.pb()
                        for b in range(gn):
                            c0 = tok0 + (g0 + b) * 128
                            fw.op("pe", "transpose", [byvb, self.b_identb], [bpb], out=pb_[:, b * 128:(b + 1) * 128], in_=yvb[:, c0:c0 + 128], identity=self.identb[:])
                        fw.op("act", "copy", [bpb], [bUp], out=Up[:, g0:g0 + gn, :], in_=pb_[:, 0:gn * 128].rearrange("p (b c) -> p b c", c=128))
                    for cg in range(8):
                        pc, bpc = self.pf()
                        for c16 in range(16):
                            cc = cg * 16 + c16
                            TT, bTT = TT_[tti % 2]
                            tti += 1
                            row = ct * 128 + cc
                            fw.dma([bGFt[ct]], [bTT], out=TT[:, 0:nm * 128], in_=bass.AP(GFt, row * LEN, [[1, 128], [1, nm * 128]]))
                            morder = [nd] + [m for m in range(nm) if m != nd]
                            for mi, m in enumerate(morder):
                                bo0 = max(0, nd - m)
                                bo1 = min(nblk, nblk + nd - m)
                                fw.op("pe", "matmul", [bTT, bUp], [bpc], out=pc[:, c16 * nblk + bo0:c16 * nblk + bo1], lhsT=TT[:, m * 128:(m + 1) * 128],
                                      rhs=Up[:, m + bo0 - nd:m + bo1 - nd, cc], start=(mi == 0), stop=(mi == nm - 1))
                        fw.op("act", "copy", [bpc], [byrev], out=yrev[:, 0:nblk, cg * 16:(cg + 1) * 16].rearrange("p b c -> p c b"),
                              in_=pc[:, 0:16 * nblk].rearrange("p (c b) -> p c b", b=nblk))
                    for g0 in range(0, nblk, 4):
                        gn = min(4, nblk - g0)
                        pt_, bpt = self.pf()
                        for b in range(gn):
                            fw.op("pe", "matmul", [byrev, self.b_antib], [bpt], out=pt_[:, b * 128:(b + 1) * 128], lhsT=yrev[:, g0 + b, :], rhs=self.antib[:], start=True, stop=True)
                        cs = slice(tok0 + g0 * 128, tok0 + (g0 + gn) * 128)
                        W_ = gn * 128
                        fw.op("dve", "scalar_tensor_tensor", [bA, bbiasT, bpt], [btmp], out=tmp[:, 0:W_], in0=A[:, cs], scalar=biasT[:, ct:ct + 1], in1=pt_[:, 0:W_], op0=ALU.mult, op1=ALU.add)
                        yout, byout = yout_[(g0 // 4) % 2]
                        fw.op("pool", "tensor_tensor", [btmp, bB], [byout], out=yout[:, 0:W_], in0=tmp[:, 0:W_], in1=B[:, cs], op=ALU.mult)
                        fw.dma([byout], [self.bYT[8 + ct]], out=self.YT[1024 + ct * 128:1024 + (ct + 1) * 128, cs], in_=yout[:, 0:W_], stream="d2")
                if not do_ctx:
                    yout, byout = yout_[0]
                    fw.op("pool", "memset", [], [byout], ap=yout[:, 0:CTX], constant=0.0)
                    fw.dma([byout], [self.bYT[8 + ct]], out=self.YT[1024 + ct * 128:1024 + (ct + 1) * 128, 0:CTX], in_=yout[:, 0:CTX], stream="d2")
            if self.debug:
                self.dump_yt("yhy%d" % l, 8, 4)
            fw.flush()

    def phase_merge(self, l, hT, bhT):
        fw, I = self.fw, self.I
        last = (l == DEPTH - 1)
        self.MTs = getattr(self, "MTs", None) or self.scratch("MTs", [D, T], BF16)
        bMTs = [Buf() for _ in TG]
        groups = [(gi, t0, n) for gi, (t0, n) in enumerate(TG) if not (last and gi == 0)]
        groups1 = [(gi, t0 + h_ * 256, 256) for (gi, t0, n) in groups for h_ in range(n // 256)]
        with ExitStack() as es:
            wbr, bwbr = self.sb(es, "mgwbr", [128, 12, D], BF16)
            wg, bwg = self.sb(es, "mgwg", [128, 8, 3072], BF16)
            fw.dma([], [bwbr], q="pool", stream="d1", out=wbr[:], in_=I["w_branch"][l].rearrange("(k p) d -> p k d", p=128))
            for nb in range(3):
                self.load_w_cols(wg[:, :, nb * 1024:(nb + 1) * 1024], bwg, l, 4096 + nb * 1024, 1024)
            yt_ = [self.sb(es, f"mgyt{i}", [128, 12, 256], BF16) for i in range(1)]
            macc, bmacc = self.sb(es, "mgmacc", [128, 8, 256], F32)
            mT, bmT = self.sb(es, "mgmT", [128, 8, 256], BF16)
            sg_ = [self.sb(es, f"mgsg{i}", [128, 256], F32) for i in range(2)]
            tp_ = [self.sb(es, f"mgtp{i}", [128, 256], F32) for i in range(2)]
            it = 0
            for (gi, t0, n) in groups1:
                hb = [bhT[t] for t in self.tiles_of(t0, n)]
                yt, byt = yt_[0]
                fw.dma(self.bYT, [byt], out=yt[:, :, 0:n], in_=self.YT[:, t0:t0 + n].rearrange("(k p) t -> p k t", p=128))
                for nb in range(3):
                    for dc in range(8):
                        sg, bsg = sg_[it % 2]; tp, btp = tp_[it % 2]
                        it += 1
                        pg, bpg = self.pf()
                        for k in range(8):
                            fw.op("pe", "matmul", hb + [bwg], [bpg], out=pg[:, 0:n], lhsT=wg[:, k, nb * 1024 + dc * 128:nb * 1024 + (dc + 1) * 128], rhs=hT[:, k, t0:t0 + n], start=(k == 0), stop=(k == 7))
                        pbn, bpbn = self.pf()
                        for k in range(4):
                            fw.op("pe", "matmul", [byt, bwbr], [bpbn], out=pbn[:, 0:n], lhsT=wbr[:, nb * 4 + k, dc * 128:(dc + 1) * 128], rhs=yt[:, nb * 4 + k, 0:n], start=(k == 0), stop=(k == 3))
                        fw.op("act", "activation", [bpg], [bsg], out=sg[:, 0:n], in_=pg[:, 0:n], func=AF.Sigmoid)
                        if nb == 0:
                            fw.op("dve", "tensor_tensor", [bpbn, bsg], [bmacc], out=macc[:, dc, 0:n], in0=pbn[:, 0:n], in1=sg[:, 0:n], op=ALU.mult)
                        else:
                            fw.op("dve", "tensor_tensor", [bpbn, bsg], [btp], out=tp[:, 0:n], in0=pbn[:, 0:n], in1=sg[:, 0:n], op=ALU.mult)
                            fw.op("pool", "tensor_tensor", [btp, bmacc], [bmacc], out=macc[:, dc, 0:n], in0=tp[:, 0:n], in1=macc[:, dc, 0:n], op=ALU.add)
                fw.op("act", "copy", [bmacc], [bmT], out=mT[:, :, 0:n], in_=macc[:, :, 0:n])
                fw.dma([bmT], [bMTs[gi]], out=self.MTs[:, t0:t0 + n].rearrange("(k p) t -> p k t", p=128), in_=mT[:, :, 0:n], stream="d2")
            fw.flush()
        with ExitStack() as es:
            wout, bwout = self.sb(es, "mgwout", [128, 8, D], BF16)
            fw.dma([], [bwout], q="pool", stream="d1", out=wout[:], in_=I["w_out"][l].rearrange("(k p) d -> p k d", p=128))
            wr, bwr = self.sb(es, "mgwr", [128, 8, 36], F32)
            fw.dma([], [bwr], out=wr[:], in_=I["moe_wr"][l].rearrange("(k p) c -> p k c", p=128))
            brB, bbrB = self.sb(es, "mgbrB", [128, 36], F32)
            fw.dma([], [bbrB], out=brB[:], in_=I["moe_br"][l:l + 1, :].to_broadcast([128, 36]))
            n2g, bn2g = self.sb(es, "mgn2g", [128, D], F32)
            G1t, bG1t = self.sb(es, "mgG1t", [128, 2, D], F32)
            fw.dma([self.b_GBd], [bG1t], out=G1t[:], in_=self.GBd[:, 0:2, :])
            fw.dma([], [bn2g], out=n2g[:], in_=I["norm2_g"][l:l + 1, :].to_broadcast([128, D]))
            mt_ = [self.sb(es, f"mgmt{i}", [128, 8, 512], BF16) for i in range(2)]
            x_ = [self.sb(es, f"mgx{i}", [128, D], F32) for i in range(2)]
            xn_ = [self.sb(es, f"mgxn{i}", [128, D], F32) for i in range(2)]
            xs2, bxs2 = self.sb(es, "mgxs2", [128, D], F32)
            tmpx, btmpx = self.sb(es, "mgtmpx", [128, D], F32)
            st_ = [self.sb(es, f"mgst{i}", [128, 2, 6], F32) for i in range(2)]
            mv_ = [self.sb(es, f"mgmv{i}", [128, 4], F32) for i in range(2)]
            rs_ = [self.sb(es, f"mgrs{i}", [128, 1], F32) for i in range(2)]
            h2f, bh2f = self.sb(es, "mgh2f", [128, 8, 128], F32)
            h2tok_ = [self.sb(es, f"mgh2t{i}", [128, D], BF16) for i in range(2)]
            for ii, (gi, t0, n) in enumerate(groups):
                w = 1 if gi == 0 else 0
                mt, bmt = mt_[ii % 2]
                fw.dma([bMTs[gi]], [bmt], out=mt[:, :, 0:n], in_=self.MTs[:, t0:t0 + n].rearrange("(k p) t -> p k t", p=128))
                for t in self.tiles_of(t0, n):
                    tt = t - t0 // 128
                    x, bx = x_[t % 2]; xn, bxn = xn_[t % 2]; st, bst = st_[t % 2]; mv, bmv = mv_[t % 2]; rs, brs = rs_[t % 2]
                    h2tok, bh2tok = h2tok_[t % 2]
                    fw.dma([self.bXR[t]], [bx], out=x[:], in_=self.XR[t * 128:(t + 1) * 128, :])
                    for half in range(2):
                        hs = slice(half * 512, (half + 1) * 512)
                        pm, bpm = self.pf()
                        for k in range(8):
                            fw.op("pe", "matmul", [bmt, bwout], [bpm], out=pm[:], lhsT=mt[:, k, tt * 128:(tt + 1) * 128], rhs=wout[:, k, hs], start=(k == 0), stop=(k == 7))
                        fw.op("dve", "tensor_tensor", [bpm, bG1t], [btmpx], out=tmpx[:, hs], in0=pm[:], in1=G1t[:, w, hs], op=ALU.mult)
                        fw.op("pool", "tensor_tensor", [btmpx, bx], [bxn], out=xn[:, hs], in0=tmpx[:, hs], in1=x[:, hs], op=ALU.add)
                    fw.dma([bxn], [self.bXR[t]], out=self.XR[t * 128:(t + 1) * 128, :], in_=xn[:], stream="d2")
                    self.rms_stats(xn, bxn, st, bst, mv, bmv, rs, brs)
                    fw.op("dve", "scalar_tensor_tensor", [bxn, brs, bn2g], [bxs2], out=xs2[:], in0=xn[:], scalar=rs[:, 0:1], in1=n2g[:], op0=ALU.mult, op1=ALU.mult)
                    for hf in range(2):
                        pt_, bpt = self.pf()
                        for kk in range(4):
                            k = hf * 4 + kk
                            fw.op("pe", "transpose", [bxs2, self.b_identf], [bpt], out=pt_[:, kk * 128:(kk + 1) * 128], in_=xs2[:, k * 128:(k + 1) * 128], identity=self.identf[:])
                        for kk in range(4):
                            k = hf * 4 + kk
                            fw.op("act", "activation", [bpt, self.b_modT, self.b_modP], [bh2f], out=h2f[:, k, :], in_=pt_[:, kk * 128:(kk + 1) * 128],
                                  func=AF.Identity, scale=self.modP[:, 32 + k, w:w + 1], bias=self.modT[:, 24 + k, w:w + 1])
                    fw.op("dve", "tensor_copy", [bh2f], [bhT[t]], out=hT[:, :, t * 128:(t + 1) * 128], in_=h2f[:])
                    pl, bpl = self.pf()
                    for k in range(8):
                        fw.op("pe", "matmul", [bh2f, bwr], [bpl], out=pl[:, 0:36], lhsT=h2f[:, k, :], rhs=wr[:, k, :], start=(k == 0), stop=(k == 7))
                    fw.op("dve", "tensor_tensor", [bpl, bbrB], [self.b_LG[t]], out=self.LG[:, t, :], in0=pl[:, 0:36], in1=brB[:], op=ALU.add)
                    pb_, bpb = self.pb()
                    for k in range(8):
                        fw.op("pe", "transpose", [bhT[t], self.b_identb], [bpb], out=pb_[:, k * 128:(k + 1) * 128], in_=hT[:, k, t * 128:(t + 1) * 128], identity=self.identb[:])
                    fw.op("act", "copy", [bpb], [bh2tok], out=h2tok[:], in_=pb_[:])
                    fw.dma([bh2tok], [self.bH2[t]], out=self.H2[t * 128:(t + 1) * 128, :], in_=h2tok[:], stream="d2")
            self.dump(f"LG{l}", self.LG[:], [128, NT, 36], self.b_LG)
            fw.flush()

    def phase_moe(self, l):
        fw, I = self.fw, self.I
        last = (l == DEPTH - 1)
        tiles = list(range(2, NT)) if last else list(range(NT))
        NB = 2 * len(tiles) + 32
        IOA = bass.IndirectOffsetOnAxis
        with ExitStack() as es:
            M1, bM1 = self.sb(es, "moM1", [128, NT, 32], F32)
            M2, bM2 = self.sb(es, "moM2", [128, NT, 32], F32)
            MS, bMS = self.sb(es, "moMS", [128, NT, 32], BF16)
            W12, bW12 = self.sb(es, "moW12", [128, NT, 2], F32)
            RK, bRK = self.sb(es, "moRK", [128, NT, 32], F32)
            DST, bDST = self.sb(es, "moDST", [128, NT, 2], F32)
            DSTi, bDSTi = self.sb(es, "moDSTi", [128, NT, 2], I32)
            sc, bsc = self.sb(es, "mosc", [128, 64], F32)
            ig, big = self.sb(es, "moig", [128, 8], F32)
            mx8, bmx8 = self.sb(es, "momx8", [128, 8], F32)
            E12, bE12 = self.sb(es, "moE12", [128, 2, 8], F32)
            trisb, btrisb = self.sb(es, "motris", [128, 128], BF16)
            onesb, bonesb = self.sb(es, "moones", [128, 128], BF16)
            trf, btrf = self.sb(es, "motrf", [128, 128], F32)
            fw.dma([], [btrf], out=trf[:], in_=I["tris"].ap())
            fw.op("dve", "tensor_copy", [btrf], [btrisb], out=trisb[:], in_=trf[:])
            fw.op("pool", "memset", [], [bonesb], ap=onesb[:], constant=1.0)
            run, brun = self.sb(es, "morun", [128, 32], F32)
            fw.op("pool", "memset", [], [brun], ap=run[:], constant=0.0)
            G = sc[:, 0:4]; EG = sc[:, 4:8]
            gmax = sc[:, 8:9]; ngmax = sc[:, 9:10]; sgm = sc[:, 10:11]; ptop = sc[:, 11:12]
            nm1 = sc[:, 12:13]; e21 = sc[:, 13:14]; w1c = sc[:, 14:15]; w2c = sc[:, 15:16]
            S = [bsc]
            for t in tiles:
                Lg = self.LG[:, t, 0:4]
                Le = self.LG[:, t, 4:36]
                bl = self.b_LG[t]
                fw.op("dve", "tensor_reduce", [bl], S, out=gmax, in_=Lg, axis=AX.X, op=ALU.max)
                fw.op("dve", "tensor_scalar", [bl] + S, S, out=G, in0=Lg, scalar1=gmax, scalar2=None, op0=ALU.is_equal)
                fw.op("dve", "tensor_scalar", S, S, out=ngmax, in0=gmax, scalar1=-1.0, scalar2=None, op0=ALU.mult)
                fw.op("act", "activation", [bl] + S, S, out=EG, in_=Lg, func=AF.Exp, bias=ngmax, scale=1.0)
                fw.op("dve", "tensor_reduce", S, S, out=sgm, in_=EG, axis=AX.X, op=ALU.add)
                fw.op("dve", "reciprocal", S, S, out=ptop, in_=sgm)
                fw.op("dve", "tensor_scalar", [bl] + S, [big], out=ig[:], in0=Le[:, 0:8], scalar1=sc[:, 0:1], scalar2=None, op0=ALU.mult)
                for g in range(1, 4):
                    fw.op("dve", "scalar_tensor_tensor", [bl, big] + S, [big], out=ig[:], in0=Le[:, 8 * g:8 * g + 8], scalar=sc[:, g:g + 1], in1=ig[:], op0=ALU.mult, op1=ALU.add)
                fw.op("dve", "max", [big], [bmx8], out=mx8[:], in_=ig[:])
                fw.op("dve", "tensor_scalar", [big, bmx8], [bE12], out=E12[:, 0, :], in0=ig[:], scalar1=mx8[:, 0:1], scalar2=None, op0=ALU.is_equal)
                fw.op("dve", "tensor_scalar", [big, bmx8], [bE12], out=E12[:, 1, :], in0=ig[:], scalar1=mx8[:, 1:2], scalar2=None, op0=ALU.is_equal)
                fw.op("dve", "tensor_scalar", [bmx8] + S, S, out=nm1, in0=mx8[:, 0:1], scalar1=-1.0, scalar2=None, op0=ALU.mult)
                fw.op("act", "activation", [bmx8] + S, S, out=e21, in_=mx8[:, 1:2], func=AF.Exp, bias=nm1, scale=1.0)
                fw.op("dve", "tensor_scalar", S, S, out=w1c, in0=e21, scalar1=1.0, scalar2=None, op0=ALU.add)
                fw.op("dve", "reciprocal", S, S, out=w1c, in_=w1c)
                fw.op("dve", "tensor_tensor", S, S, out=w2c, in0=e21, in1=w1c, op=ALU.mult)
                fw.op("dve", "tensor_scalar", S, [bW12], out=W12[:, t, :], in0=sc[:, 14:16], scalar1=ptop, scalar2=None, op0=ALU.mult)
                for g in range(4):
                    fw.op("dve", "tensor_scalar", [bE12] + S, [bM1], out=M1[:, t, 8 * g:8 * g + 8], in0=E12[:, 0, :], scalar1=sc[:, g:g + 1], scalar2=None, op0=ALU.mult)
                    fw.op("dve", "tensor_scalar", [bE12] + S, [bM2], out=M2[:, t, 8 * g:8 * g + 8], in0=E12[:, 1, :], scalar1=sc[:, g:g + 1], scalar2=None, op0=ALU.mult)
                fw.op("dve", "tensor_tensor", [bM1, bM2], [bMS], out=MS[:, t, :], in0=M1[:, t, :], in1=M2[:, t, :], op=ALU.add)
                pr, bpr = self.pf()
                fw.op("pe", "matmul", [btrisb, bMS], [bpr], out=pr[:, 0:32], lhsT=trisb[:], rhs=MS[:, t, :], start=True, stop=True)
                fw.op("pe", "matmul", [bonesb, bMS], [bpr], out=pr[:, 32:64], lhsT=onesb[:], rhs=MS[:, t, :], start=True, stop=True)
                fw.op("dve", "tensor_tensor", [bpr, brun], [bRK], out=RK[:, t, :], in0=pr[:, 0:32], in1=run[:], op=ALU.add)
                fw.op("dve", "tensor_tensor", [bpr, brun], [brun], out=run[:], in0=pr[:, 32:64], in1=run[:], op=ALU.add)
            nbi, bnbi = self.sb(es, "monbi", [128, 32], I32)
            nbf, bnbf = self.sb(es, "monbf", [128, 32], F32)
            one32, bone32 = self.sb(es, "moone32", [128, 32], F32)
            pend, bpend = self.sb(es, "mopend", [128, 32], F32)
            pst, bpst = self.sb(es, "mopst", [128, 32], F32)
            fw.op("pool", "memset", [], [bone32], ap=one32[:], constant=1.0)
            fw.op("dve", "tensor_scalar", [brun], [bnbi], out=nbi[:], in0=run[:], scalar1=1.0 / 128.0, scalar2=0.49609375, op0=ALU.mult, op1=ALU.add)
            fw.op("dve", "tensor_copy", [bnbi], [bnbf], out=nbf[:], in_=nbi[:])
            fw.op("dve", "tensor_tensor_scan", [bone32, bnbf], [bpend], out=pend[:], data0=one32[:], data1=nbf[:], initial=0.0, op0=ALU.mult, op1=ALU.add)
            fw.op("dve", "tensor_tensor", [bpend, bnbf], [bpst], out=pst[:], in0=pend[:], in1=nbf[:], op=ALU.subtract)
            fw.op("dve", "tensor_scalar", [bpst], [bpst], out=pst[:], in0=pst[:], scalar1=128.0, scalar2=None, op0=ALU.mult)
            tmp32, btmp32 = self.sb(es, "motmp32", [128, 32], F32)
            for t in tiles:
                fw.op("dve", "tensor_tensor", [bRK, bpst], [btmp32], out=tmp32[:], in0=RK[:, t, :], in1=pst[:], op=ALU.add)
                fw.op("dve", "tensor_tensor", [btmp32, bM1], [bRK], out=RK[:, t, :], in0=tmp32[:], in1=M1[:, t, :], op=ALU.mult)
                fw.op("dve", "tensor_reduce", [bRK], [bDST], out=DST[:, t, 0:1], in_=RK[:, t, :], axis=AX.X, op=ALU.add)
                fw.op("dve", "tensor_tensor", [btmp32, bM2], [bRK], out=RK[:, t, :], in0=tmp32[:], in1=M2[:, t, :], op=ALU.mult)
                fw.op("dve", "tensor_reduce", [bRK], [bDST], out=DST[:, t, 1:2], in_=RK[:, t, :], axis=AX.X, op=ALU.add)
            fw.op("dve", "tensor_copy", [bDST], [bDSTi], out=DSTi[:, tiles[0]:NT, :], in_=DST[:, tiles[0]:NT, :])
            iob, biob = self.sb(es, "moiob", [128, NBLK], F32)
            fw.dma([], [biob], out=iob[:], in_=I["iotab"].ap())
            cmp_, bcmp = self.sb(es, "mocmp", [128, NBLK, 32], F32)
            be, bbe = self.sb(es, "mobe", [128, NBLK], F32)
            fw.op("dve", "tensor_tensor", [biob, bpend], [bcmp], out=cmp_[:, 0:NB, :], in0=iob[:, 0:NB].unsqueeze(2).to_broadcast([128, NB, 32]),
                  in1=pend[:].unsqueeze(1).to_broadcast([128, NB, 32]), op=ALU.is_ge)
            fw.op("dve", "tensor_reduce", [bcmp], [bbe], out=be[:, 0:NB], in_=cmp_[:, 0:NB, :], axis=AX.X, op=ALU.add)
            fw.op("dve", "tensor_scalar", [bbe], [bbe], out=be[:, 0:NB], in0=be[:, 0:NB], scalar1=31.0, scalar2=None, op0=ALU.min)
            b1f, bb1f = self.sb(es, "mob1f", [128, NBLK], F32)
            b2f, bb2f = self.sb(es, "mob2f", [128, NBLK], F32)
            wi1, bwi1 = self.sb(es, "mowi1", [128, NBLK, 8], I32)
            wi2, bwi2 = self.sb(es, "mowi2", [128, NBLK, 4], I32)
            fw.op("dve", "tensor_scalar", [bbe], [bb1f], out=b1f[:, 0:NB], in0=be[:, 0:NB], scalar1=1024.0, scalar2=None, op0=ALU.mult)
            fw.op("dve", "tensor_scalar", [bb1f, self.b_rcol], [bb1f], out=b1f[:, 0:NB], in0=b1f[:, 0:NB], scalar1=self.rcol[:, 3:4], scalar2=None, op0=ALU.add)
            fw.op("dve", "tensor_scalar", [bbe], [bb2f], out=b2f[:, 0:NB], in0=be[:, 0:NB], scalar1=512.0, scalar2=None, op0=ALU.mult)
            fw.op("dve", "tensor_scalar", [bb2f, self.b_rcol], [bb2f], out=b2f[:, 0:NB], in0=b2f[:, 0:NB], scalar1=self.rcol[:, 3:4], scalar2=None, op0=ALU.add)
            for k in range(8):
                fw.op("dve", "tensor_scalar", [bb1f], [bwi1], out=wi1[:, 0:NB, k], in0=b1f[:, 0:NB], scalar1=128.0 * k + l * 32768.0, scalar2=None, op0=ALU.add)
            for k in range(4):
                fw.op("dve", "tensor_scalar", [bb2f], [bwi2], out=wi2[:, 0:NB, k], in0=b2f[:, 0:NB], scalar1=128.0 * k + l * 16384.0, scalar2=None, op0=ALU.add)
            self.dump(f"DST{l}", DST[:], [128, NT, 2], [bDST])
            self.dump(f"W12{l}", W12[:], [128, NT, 2], [bW12])
            self.dump(f"BE{l}", be[:], [128, NBLK], [bbe])
            hx_ = [self.sb(es, f"mohx{i}", [128, D], BF16) for i in range(2)]
            zt_, bzt_ = self.sb(es, "mozero", [128, 4, D], BF16)
            fw.op("pool", "memset", [], [bzt_], ap=zt_[:], constant=0.0)
            for b4 in range(0, NB, 4):
                nb4 = min(4, NB - b4)
                fw.dma([bzt_], [self.bMB], out=self.MB[b4 * 128:(b4 + nb4) * 128, :].rearrange("(b p) d -> p b d", p=128), in_=zt_[:, 0:nb4, :], stream="d2")
            for t in tiles:
                hx, bhx = hx_[t % 2]
                fw.dma([self.bH2[t]], [bhx], out=hx[:], in_=self.H2[t * 128:(t + 1) * 128, :])
                for k in range(2):
                    fw.dmaf(lambda e, hx=hx, t=t, k=k: e.indirect_dma_start(out=self.MB[:, :], out_offset=IOA(ap=DSTi[:, t, k:k + 1], axis=0), in_=hx[:, :], in_offset=None),
                            [bhx, bDSTi], [self.bMB])
            X_ = [self.sb(es, f"moX{i}", [128, D], BF16) for i in range(2)]
            XT_ = [self.sb(es, f"moXT{i}", [128, 8, 128], BF16) for i in range(2)]
            w1g_ = [self.sb(es, f"mow1g{i}", [128, 8, 512], BF16) for i in range(2)]
            w3g_ = [self.sb(es, f"mow3g{i}", [128, 8, 512], BF16) for i in range(2)]
            w2g_ = [self.sb(es, f"mow2g{i}", [128, 4, D], BF16) for i in range(2)]
            sl_ = [self.sb(es, f"mosl{i}", [128, 512], F32) for i in range(2)]
            gT_ = [self.sb(es, f"mogT{i}", [128, 512], BF16) for i in range(2)]
            yb_ = [self.sb(es, f"moyb{i}", [128, D], F32) for i in range(2)]
            w1d, w3d, w2d = I["moe_w1"], I["moe_w3"], I["moe_w2"]
            for blk in range(NB):
                X, bX = X_[blk % 2]; XT, bXT = XT_[blk % 2]
                w1g, bw1g = w1g_[blk % 2]; w3g, bw3g = w3g_[blk % 2]; w2g, bw2g = w2g_[blk % 2]
                sl, bsl = sl_[blk % 2]; gT, bgT = gT_[blk % 2]; yb, byb = yb_[blk % 2]
                fw.dma([self.bMB], [bX], out=X[:], in_=self.MB[blk * 128:(blk + 1) * 128, :])
                for k in range(8):
                    fw.dmaf(lambda e, w1g=w1g, k=k, blk=blk: e.indirect_dma_start(out=w1g[:, k, :], out_offset=None, in_=w1d[:, :], in_offset=IOA(ap=wi1[:, blk, k:k + 1], axis=0)),
                            [bwi1], [bw1g])
                    fw.dmaf(lambda e, w3g=w3g, k=k, blk=blk: e.indirect_dma_start(out=w3g[:, k, :], out_offset=None, in_=w3d[:, :], in_offset=IOA(ap=wi1[:, blk, k:k + 1], axis=0)),
                            [bwi1], [bw3g])
                for k in range(4):
                    fw.dmaf(lambda e, w2g=w2g, k=k, blk=blk: e.indirect_dma_start(out=w2g[:, k, :], out_offset=None, in_=w2d[:, :], in_offset=IOA(ap=wi2[:, blk, k:k + 1], axis=0)),
                            [bwi2], [bw2g])
                pb_, bpb = self.pb()
                for k in range(8):
                    fw.op("pe", "transpose", [bX, self.b_identb], [bpb], out=pb_[:, k * 128:(k + 1) * 128], in_=X[:, k * 128:(k + 1) * 128], identity=self.identb[:])
                fw.op("act", "copy", [bpb], [bXT], out=XT[:].rearrange("p k t -> p (k t)"), in_=pb_[:])
                p1, bp1 = self.pf()
                p3, bp3 = self.pf()
                for hc in range(4):
                    for k in range(8):
                        fw.op("pe", "matmul", [bw1g, bXT], [bp1], out=p1[:, hc * 128:(hc + 1) * 128], lhsT=w1g[:, k, hc * 128:(hc + 1) * 128], rhs=XT[:, k, :], start=(k == 0), stop=(k == 7))
                for hc in range(4):
                    for k in range(8):
                        fw.op("pe", "matmul", [bw3g, bXT], [bp3], out=p3[:, hc * 128:(hc + 1) * 128], lhsT=w3g[:, k, hc * 128:(hc + 1) * 128], rhs=XT[:, k, :], start=(k == 0), stop=(k == 7))
                fw.op("act", "activation", [bp1], [bsl], out=sl[:], in_=p1[:], func=AF.Silu)
                fw.op("dve", "tensor_tensor", [bp3, bsl], [bgT], out=gT[:], in0=p3[:], in1=sl[:], op=ALU.mult)
                for half in range(2):
                    py, bpy = self.pf()
                    for hc in range(4):
                        fw.op("pe", "matmul", [bgT, bw2g], [bpy], out=py[:], lhsT=gT[:, hc * 128:(hc + 1) * 128], rhs=w2g[:, hc, half * 512:(half + 1) * 512], start=(hc == 0), stop=(hc == 3))
                    fw.op("act", "copy", [bpy], [byb], out=yb[:, half * 512:(half + 1) * 512], in_=py[:])
                fw.dma([byb], [self.bMY], out=self.MY[blk * 128:(blk + 1) * 128, :], in_=yb[:], stream="d2")
            y1_ = [self.sb(es, f"moy1{i}", [128, D], F32) for i in range(2)]
            G2t, bG2t = self.sb(es, "moG2t", [128, 2, D], F32)
            fw.dma([self.b_GBd], [bG2t], out=G2t[:], in_=self.GBd[:, 2:4, :])
            y2_ = [self.sb(es, f"moy2{i}", [128, D], F32) for i in range(2)]
            xx_ = [self.sb(es, f"moxx{i}", [128, D], F32) for i in range(2)]
            st_ = [self.sb(es, f"most{i}", [128, 2, 6], F32) for i in range(2)]
            mv_ = [self.sb(es, f"momv{i}", [128, 4], F32) for i in range(2)]
            rs_ = [self.sb(es, f"mors{i}", [128, 1], F32) for i in range(2)]
            if last:
                fng, bfng = self.sb(es, "mofng", [128, D], F32)
                fw.dma([], [bfng], out=fng[:], in_=I["final_norm_g"][0:1, :].to_broadcast([128, D]))
            for t in tiles:
                w = 1 if t < 2 else 0
                y1, by1 = y1_[t % 2]; y2, by2 = y2_[t % 2]; xx, bxx = xx_[t % 2]
                fw.dmaf(lambda e, y1=y1, t=t: e.indirect_dma_start(out=y1[:, :], out_offset=None, in_=self.MY[:, :], in_offset=IOA(ap=DSTi[:, t, 0:1], axis=0)), [self.bMY, bDSTi], [by1])
                fw.dmaf(lambda e, y2=y2, t=t: e.indirect_dma_start(out=y2[:, :], out_offset=None, in_=self.MY[:, :], in_offset=IOA(ap=DSTi[:, t, 1:2], axis=0)), [self.bMY, bDSTi], [by2])
                fw.dma([self.bXR[t]], [bxx], out=xx[:], in_=self.XR[t * 128:(t + 1) * 128, :])
                fw.op("dve", "tensor_scalar", [by1, bW12], [by1], out=y1[:], in0=y1[:], scalar1=W12[:, t, 0:1], scalar2=None, op0=ALU.mult)
                fw.op("dve", "scalar_tensor_tensor", [by2, bW12, by1], [by1], out=y1[:], in0=y2[:], scalar=W12[:, t, 1:2], in1=y1[:], op0=ALU.mult, op1=ALU.add)
                fw.op("pool", "tensor_tensor", [by1, bG2t], [by1], out=y1[:], in0=y1[:], in1=G2t[:, w, :], op=ALU.mult)
                fw.op("pool", "tensor_tensor", [by1, bxx], [bxx], out=xx[:], in0=y1[:], in1=xx[:], op=ALU.add)
                if not last:
                    fw.dma([bxx], [self.bXR[t]], out=self.XR[t * 128:(t + 1) * 128, :], in_=xx[:], stream="d2")
                else:
                    st, bst = st_[t % 2]; mv, bmv = mv_[t % 2]; rs, brs = rs_[t % 2]
                    self.rms_stats(xx, bxx, st, bst, mv, bmv, rs, brs)
                    fw.op("dve", "scalar_tensor_tensor", [bxx, brs, bfng], [by2], out=y2[:], in0=xx[:], scalar=rs[:, 0:1], in1=fng[:], op0=ALU.mult, op1=ALU.mult)
                    fw.dma([by2], [self.bout], out=self.out[(t - 2) * 128:(t - 1) * 128, :], in_=y2[:], stream="d2")
            fw.flush()

    def phase_final(self):
        pass


def prep_inputs(inp, b, keys=None):
    f = np.float32
    d = {}
    d.update(_consts())
    d["x"] = np.ascontiguousarray(inp["x"][b])
    d["ctx"] = np.ascontiguousarray(inp["ctx"][b])
    cc = np.stack([inp["c"][b], inp["c_ctx"]], -1)
    d["cT"] = np.ascontiguousarray(cc.reshape(8, 128, 2).transpose(1, 0, 2))
    d["w_mod"] = inp["w_mod"]
    d["b_mod"] = inp["b_mod"]
    d["bmodT"] = np.ascontiguousarray(inp["b_mod"].reshape(DEPTH, 48, 128).transpose(0, 2, 1))
    selw = np.zeros((2, 2, 128), f)
    selw[0, 0] = 1.0
    selw[1, 1] = 1.0
    d["selw"] = selw
    d["norm1_g"] = inp["norm1_g"]
    d["norm2_g"] = inp["norm2_g"]
    d["final_norm_g"] = inp["final_norm_g"].reshape(1, D)
    d["w_in"] = inp["w_in"]
    d["ret_log_decay"] = inp["ret_log_decay"].reshape(DEPTH, 8)
    d["ret_gn_g"] = inp["ret_gn_g"]
    def pairT(a):
        return np.ascontiguousarray(a.reshape(DEPTH, 2, 16, 128).transpose(0, 1, 3, 2))
    d["s5_lre"] = pairT(inp["s5_lambda_re"])
    d["s5_lim"] = pairT(inp["s5_lambda_im"])
    d["s5_ldt"] = pairT(np.repeat(inp["s5_log_dt"][..., None], 64, -1))
    sB = np.zeros((DEPTH, 2, 2, 128, 4, 2, 128), f)
    sC = np.zeros((DEPTH, 2, 2, 128, 16, 64), f)
    for ri, (bk, ck) in enumerate((("s5_b_re", "s5_c_re"), ("s5_b_im", "s5_c_im"))):
        Bm = inp[bk]
        Cm = inp[ck]
        for pt in range(16):
            o, q4 = pt // 4, pt % 4
            for g2 in range(2):
                g = 2 * pt + g2
                sB[:, :, ri, q4 * 32 + g2 * 16:q4 * 32 + g2 * 16 + 16, o, q4 % 2, g2 * 64:(g2 + 1) * 64] = Bm[:, :, g].transpose(0, 1, 3, 2)
                sC[:, :, ri, g2 * 64:(g2 + 1) * 64, pt, (q4 % 2) * 32 + g2 * 16:(q4 % 2) * 32 + g2 * 16 + 16] = Cm[:, :, g].transpose(0, 1, 3, 2)
    d["s5_B"] = sB
    d["s5_C"] = sC
    d["s5_dT"] = np.ascontiguousarray(inp["s5_d"].reshape(DEPTH, 4, 128).transpose(0, 2, 1))
    d["s5_glu_w1"] = inp["s5_glu_w1"]
    d["s5_glu_w2"] = inp["s5_glu_w2"]
    d["hy_swT"] = np.ascontiguousarray(inp["hy_short_w"].transpose(0, 2, 1).reshape(DEPTH, 12, 128, 3).transpose(0, 2, 1, 3))
    d["hy_sbT"] = np.ascontiguousarray(inp["hy_short_b"].reshape(DEPTH, 12, 128).transpose(0, 2, 1))
    d["hy_biasT"] = np.ascontiguousarray(inp["hy_bias"].reshape(DEPTH, 4, 128).transpose(0, 2, 1))
    d["hy_f_w1"] = inp["hy_f_w1"]
    d["hy_f_w2"] = inp["hy_f_w2"]
    d["hy_f_w3"] = inp["hy_f_w3"]
    d["hy_f_b1"] = inp["hy_f_b1"].reshape(DEPTH, 64, 1)
    d["hy_f_b2"] = inp["hy_f_b2"].reshape(DEPTH, 64, 1)
    d["hy_f_fr"] = np.ascontiguousarray(inp["hy_f_freq"].transpose(0, 2, 1))
    d["w_branch"] = inp["w_branch"].reshape(DEPTH, 1536, D)
    d["w_out"] = inp["w_out"]
    d["moe_wr"] = np.ascontiguousarray(np.concatenate([inp["moe_w_group"], inp["moe_w_expert"]], -1))
    d["moe_br"] = np.ascontiguousarray(np.concatenate([inp["moe_b_group"], inp["moe_b_expert"]], -1))
    d["moe_w1"] = inp["moe_w1"].reshape(DEPTH * 32 * 1024, 512)
    d["moe_w3"] = inp["moe_w3"].reshape(DEPTH * 32 * 1024, 512)
    d["moe_w2"] = inp["moe_w2"].reshape(DEPTH * 32 * 512, 1024)
    if keys is not None:
        d = {k: v for k, v in d.items() if k in keys}
    return d


_NC_CACHE = {}


def run_cores(inputs, n_cores=8, debug=None, stop=None, keys=None):
    per_core = [prep_inputs(inputs, c % 4, keys) for c in range(n_cores)]
    bd = Builder(debug=debug)
    bd.stop = stop
    import os
    bd.s5_cut = int(os.environ.get("S5_CUT", "0"))
    if os.environ.get("S5_LIM"):
        bd.s5_lim = tuple(int(v) for v in os.environ["S5_LIM"].split(","))
    nc = bd.build(per_core[0])
    res = run_bass_kernel_spmd(nc, per_core, core_ids=list(range(n_cores)))
    return res, bd


def kernel(**inputs):
    inputs = {k: np.asarray(v) for k, v in inputs.items()}
    res, bd = run_cores(inputs, n_cores=4)
    out = np.stack([res.results[b]["out"] for b in range(4)], 0)
    return out.astype(np.float32)
```

```python
import math
from contextlib import ExitStack
import numpy as np
import ml_dtypes
import concourse.bass as bass
import concourse.mybir as mybir
from concourse.bass_utils import run_bass_kernel_spmd

F32 = mybir.dt.float32
BF16 = mybir.dt.bfloat16
I32 = mybir.dt.int32
ALU = mybir.AluOpType
AF = mybir.ActivationFunctionType
AX = mybir.AxisListType

D = 1024
NT = 34
T = NT * 128
CTX = 256
L = 4096
DEPTH = 2
TG = [(0, 256)] + [(256 + 512 * i, 512) for i in range(8)]
TWO_PI = 2.0 * math.pi
NBLK = 100


class Buf:
    __slots__ = ("name", "lw", "rd")

    def __init__(self, name=""):
        self.name = name
        self.lw = None
        self.rd = {}


class FW:
    ENG = ("pe", "dve", "act", "pool", "sp")
    STREAMS = {"d0": 12, "d1": 16, "d2": 8, "d3": 2}
    ENGNAME = {"pe": "tensor", "dve": "vector", "act": "scalar", "pool": "gpsimd", "sp": "sync"}

    def __init__(self, nc, sems):
        self.nc = nc
        self.sems = sems
        self.ops = {e: [] for e in self.ENG}
        self.cnt = {s: 0 for s in self.sem_names()}
        self.seen = {e: {} for e in self.ENG}
        self.n_ops = 0
        self.ndma = {s: 0 for s in self.STREAMS}

    @classmethod
    def sem_names(cls):
        r = [e for e in cls.ENG if e != "sp"]
        for s, k in cls.STREAMS.items():
            r += [f"{s}_{i}" for i in range(k)]
        return r

    def _emit(self, eng, sem, inc, fn, reads, writes, nosync_same=False, extra_wait=None):
        waits = {}

        def need(sv):
            if sv is None:
                return
            s, v = sv
            if nosync_same and s == eng:
                return
            if waits.get(s, 0) < v:
                waits[s] = v
        if extra_wait is not None:
            need(extra_wait)
        for b in reads:
            need(b.lw)
        for b in writes:
            need(b.lw)
            for s, v in b.rd.items():
                need((s, v))
        wl = []
        for s, v in waits.items():
            if self.seen[eng].get(s, 0) < v:
                self.seen[eng][s] = v
                wl.append((s, v))
        self.cnt[sem] += inc
        v = self.cnt[sem]
        for b in reads:
            if b.rd.get(sem, 0) < v:
                b.rd[sem] = v
        for b in writes:
            b.lw = (sem, v)
            b.rd = {}
        self.ops[eng].append((wl, fn, sem, inc))
        self.n_ops += 1

    def op(self, eng, name, R, W, **kw):
        self._emit(eng, eng, 1, lambda e: getattr(e, name)(**kw), R, W, nosync_same=(eng == "pe"))

    def opf(self, eng, fn, R, W):
        self._emit(eng, eng, 1, fn, R, W, nosync_same=(eng == "pe"))

    def _dma_sem(self, stream):
        i = self.ndma[stream]
        self.ndma[stream] += 1
        sem = f"{stream}_{i % self.STREAMS[stream]}"
        return sem, (sem, self.cnt[sem])

    def dma(self, R, W, q="sp", stream="d0", **kw):
        sem, ew = self._dma_sem(stream)
        self._emit(q, sem, 16, lambda e: e.dma_start(**kw), R, W, extra_wait=ew)

    def dmaf(self, fn, R, W, q="pool", stream="d1"):
        sem, ew = self._dma_sem(stream)
        self._emit(q, sem, 16, fn, R, W, extra_wait=ew)

    def flush(self):
        nc = self.nc
        final = dict(self.cnt)
        sems = self.sems
        with nc.Block() as block:
            for e in self.ENG:
                ops = self.ops[e]

                def body(engine, ops=ops, e=e):
                    for wl, fn, sem, inc in ops:
                        for s, v in wl:
                            engine.wait_ge(sems[s], v)
                        ins = fn(engine)
                        ins.then_inc(sems[sem], inc)
                    for s, v in final.items():
                        if v > 0:
                            engine.wait_ge(sems[s], v)
                getattr(block, self.ENGNAME[e])(body)
        for e in self.ENG:
            self.ops[e] = []
            self.seen[e] = dict(final)


def _consts():
    c = {}
    c["ident_f"] = np.eye(128, dtype=np.float32)
    c["antiid_f"] = np.ascontiguousarray(np.eye(128, dtype=np.float32)[::-1])
    rows = L // 64
    quarter = D // 4
    omega = (1.0 / (np.float32(10000.0) ** (np.arange(quarter, dtype=np.float32) / np.float32(quarter)))).astype(np.float32)
    ang_r = np.arange(rows, dtype=np.float32)[:, None] * omega
    ang_c = np.arange(64, dtype=np.float32)[:, None] * omega
    emb_r = np.concatenate([np.sin(ang_r), np.cos(ang_r)], -1)
    emb_c = np.concatenate([np.sin(ang_c), np.cos(ang_c)], -1)
    pe = np.concatenate([np.broadcast_to(emb_r[:, None, :], (rows, 64, D // 2)),
                         np.broadcast_to(emb_c[None, :, :], (rows, 64, D // 2))], -1)
    c["pe"] = np.ascontiguousarray(pe.reshape(L, D).astype(np.float32))
    j = np.arange(128, dtype=np.float32)[:, None]
    i = np.arange(128, dtype=np.float32)[None, :]
    rt = np.zeros((128, 6, 128), np.float32)
    rt[:, 0] = np.maximum(i - j, 0)
    rt[:, 1] = np.maximum(j - i, 0)
    rt[:, 2] = (i >= j)
    rt[:, 3] = (j > i)
    rt[:, 4] = i + 1.0
    rt[:, 5] = 128.0 - i
    c["rtab"] = rt
    rc = np.zeros((128, 6), np.float32)
    rc[:, 0] = 127.0 - np.arange(128)
    rc[:, 1] = np.arange(128)
    rc[:, 2] = 128.0
    rc[:, 3] = np.arange(128)
    rc[:, 4] = np.arange(128) + 1.0
    rc[:, 5] = 128.0 - np.arange(128)
    c["rcol"] = rc
    for nm, n in (("zT", L), ("zTc", CTX)):
        t = np.linspace(0.0, 1.0, n, dtype=np.float32)[:, None]
        w = (2.0 * math.pi * np.arange(n, dtype=np.float32)[:, None] / n).astype(np.float32)
        f = np.linspace(1e-4, 15, 16, dtype=np.float32)[None]
        z = np.concatenate([t, np.cos(f * w), -np.sin(f * w)], -1).astype(np.float32)
        c[nm] = np.ascontiguousarray(z.T)
        off = (np.abs(np.arange(n) - n // 2).astype(np.float32) / n).astype(np.float32)
        c["off" + nm[2:]] = np.ascontiguousarray(np.broadcast_to(off[None], (128, n)))
    deltas = np.abs(np.linspace(math.log(1e-2) / 1.5, math.log(1e-2) / 0.3, 512, dtype=np.float32))
    c["ndelta"] = np.ascontiguousarray((-deltas).reshape(4, 128).T.astype(np.float32))
    c["iota512"] = np.ascontiguousarray(np.broadcast_to(np.arange(1, 513, dtype=np.float32)[None], (128, 512)))
    tri = (np.arange(128)[:, None] < np.arange(128)[None, :]).astype(np.float32)
    c["tris"] = tri
    c["iotab"] = np.ascontiguousarray(np.broadcast_to(np.arange(NBLK, dtype=np.float32)[None], (128, NBLK)))
    return c


class Builder:
    def __init__(self, debug=None):
        self.debug = debug or []
        self.nc = bass.Bass("TRN2", target_bir_lowering=False)
        self.din = {}
        self.dbg_out = []

    def inp(self, name, shape, dt=F32):
        t = self.nc.dram_tensor(name, list(shape), dt, kind="ExternalInput")
        self.din[name] = t
        return t

    def scratch(self, name, shape, dt):
        return self.nc.dram_tensor(name, list(shape), dt, kind="Internal")

    def build(self, in_shapes):
        nc = self.nc
        I = {k: self.inp(k, v.shape, {np.dtype("float32"): F32, np.dtype("int32"): I32}[v.dtype]) for k, v in in_shapes.items()}
        self.I = I
        self.out = nc.dram_tensor("out", [L, D], F32, kind="ExternalOutput")
        self.XR = self.scratch("XR", [T, D], F32)
        self.YT = self.scratch("YT", [1536, T], BF16)
        self.H2 = self.scratch("H2", [T, D], BF16)
        self.MB = self.scratch("MB", [NBLK * 128, D], BF16)
        self.MY = self.scratch("MY", [NBLK * 128, D], F32)
        self.GF = self.scratch("GF", [512, 4352], BF16)
        self.GFc = self.scratch("GFc", [512, 512], BF16)
        self.bXR = [Buf() for _ in range(NT)]
        self.bYT = [Buf() for _ in range(12)]
        self.bH2 = [Buf() for _ in range(NT)]
        self.bMB = Buf()
        self.bMY = Buf()
        self.bGF = [Buf() for _ in range(4)]
        self.bGFc = [Buf() for _ in range(4)]
        self.bout = Buf()
        with ExitStack() as es:
            self.sems = {s: es.enter_context(nc.semaphore(s)) for s in FW.sem_names()}
            self.fw = FW(nc, self.sems)
            with ExitStack() as gs:
                self.gs = gs
                self.alloc_globals()
                self.phase_init()
                for l in range(DEPTH):
                    self.layer(l)
                    if getattr(self, "stop", None):
                        break
                self.phase_final()
        return nc

    def sb(self, es, name, shape, dt):
        self._uid = getattr(self, "_uid", 0) + 1
        t = es.enter_context(self.nc.sbuf_tensor(f"s{self._uid}_{name}", list(shape), dt))
        assert self.nc.sbuf_bytes_remaining >= 32768 + 128, (name, self.nc.sbuf_bytes_remaining)
        return t, Buf(name)

    def alloc_globals(self):
        nc, gs, fw, I = self.nc, self.gs, self.fw, self.I
        self.PF = []
        for i in range(6):
            t = gs.enter_context(nc.psum_tensor(f"pf{i}", [128, 512], F32))
            self.PF.append((t, Buf(f"pf{i}")))
        self.PB = []
        for i in range(2):
            t = gs.enter_context(nc.psum_tensor(f"pb{i}", [128, 1024], BF16))
            self.PB.append((t, Buf(f"pb{i}")))
        self.pfi = 0
        self.pbi = 0
        self.identf, self.b_identf = self.sb(gs, "identf", [128, 128], F32)
        self.identb, self.b_identb = self.sb(gs, "identb", [128, 128], BF16)
        self.antib, self.b_antib = self.sb(gs, "antib", [128, 128], BF16)
        self.rcol, self.b_rcol = self.sb(gs, "rcol", [128, 6], F32)
        self.epsc, self.b_epsc = self.sb(gs, "epsc", [128, 4], F32)
        self.modT, self.b_modT = self.sb(gs, "modT", [128, 48, 2], F32)
        self.modP, self.b_modP = self.sb(gs, "modP", [128, 48, 2], F32)
        self.GBd = self.scratch("GBd", [128, 4, 1024], F32)
        self.b_GBd = Buf()
        self.scT, self.b_scT = self.sb(gs, "scT", [128, 8, 2], F32)
        self.LG, _ = self.sb(gs, "LG", [128, NT, 36], F32)
        self.b_LG = [Buf() for _ in range(NT)]
        fw.dma([], [self.b_identf], out=self.identf[:], in_=I["ident_f"].ap())
        fw.op("dve", "tensor_copy", [self.b_identf], [self.b_identb], out=self.identb[:], in_=self.identf[:])
        af, baf = self.sb(gs, "antif", [128, 128], F32)
        fw.dma([], [baf], out=af[:], in_=I["antiid_f"].ap())
        fw.op("dve", "tensor_copy", [baf], [self.b_antib], out=self.antib[:], in_=af[:])
        fw.dma([], [self.b_rcol], out=self.rcol[:], in_=I["rcol"].ap())
        fw.op("pool", "memset", [], [self.b_epsc], ap=self.epsc[:, 0:1], constant=1e-6)
        fw.op("pool", "memset", [self.b_epsc], [self.b_epsc], ap=self.epsc[:, 1:2], constant=1e-5)
        fw.op("pool", "memset", [self.b_epsc], [self.b_epsc], ap=self.epsc[:, 2:3], constant=math.pi / 2)
        fw.op("pool", "memset", [self.b_epsc], [self.b_epsc], ap=self.epsc[:, 3:4], constant=0.0)
        ct, bct = self.sb(gs, "cT", [128, 8, 2], F32)
        fw.dma([], [bct], out=ct[:], in_=I["cT"].ap())
        fw.op("act", "activation", [bct], [self.b_scT], out=self.scT[:], in_=ct[:], func=AF.Silu)

    def pf(self):
        r = self.PF[self.pfi % len(self.PF)]
        self.pfi += 1
        return r

    def pb(self):
        r = self.PB[self.pbi % len(self.PB)]
        self.pbi += 1
        return r

    def dump(self, name, ap, shape, R, dt=F32):
        if name not in self.debug:
            return
        t = self.nc.dram_tensor("dbg_" + name, list(shape), dt, kind="ExternalOutput")
        self.dbg_out.append("dbg_" + name)
        self.fw.dma(R, [Buf()], out=t.ap(), in_=ap, stream="d2")

    def phase_init(self):
        fw, I = self.fw, self.I
        with ExitStack() as es:
            xt = [self.sb(es, f"xi{i}", [128, D], F32) for i in range(3)]
            pt = [self.sb(es, f"pi{i}", [128, D], F32) for i in range(3)]
            for t in range(NT):
                x, bx = xt[t % 3]
                if t < 2:
                    fw.dma([], [bx], out=x[:], in_=I["ctx"][t * 128:(t + 1) * 128, :])
                else:
                    p, bp = pt[t % 3]
                    r0 = (t - 2) * 128
                    fw.dma([], [bx], out=x[:], in_=I["x"][r0:r0 + 128, :])
                    fw.dma([], [bp], out=p[:], in_=I["pe"][r0:r0 + 128, :])
                    fw.op("pool", "tensor_tensor", [bx, bp], [bx], out=x[:], in0=x[:], in1=p[:], op=ALU.add)
                fw.dma([bx], [self.bXR[t]], out=self.XR[t * 128:(t + 1) * 128, :], in_=x[:], stream="d2")
            fw.flush()

    def phase_mod(self, l):
        fw, I = self.fw, self.I
        with ExitStack() as es:
            wm = [self.sb(es, f"wm{i}", [128, 8, 512], F32) for i in range(2)]
            bT, bbT = self.sb(es, "bmodT", [128, 48], F32)
            brow, bbrow = self.sb(es, "bmrow", [2, 4, 512], F32)
            rows, brows = self.sb(es, "mrows", [2, 512], F32)
            sel, bsel = self.sb(es, "selw", [2, 2, 128], F32)
            gst_ = [self.sb(es, f"gst{i}", [128, 512], F32) for i in range(2)]
            fw.dma([], [bbT], out=bT[:], in_=I["bmodT"][l])
            for w in range(2):
                for gi, c0 in enumerate((2048, 2560, 5120, 5632)):
                    fw.dma([], [bbrow], out=brow[w:w + 1, gi, :], in_=I["b_mod"][l:l + 1, c0:c0 + 512])
            fw.dma([], [bsel], out=sel[:], in_=I["selw"].ap())
            for cb in range(12):
                w_, bw = wm[cb % 2]
                fw.dma([], [bw], out=w_[:], in_=I["w_mod"][l][:, cb * 512:(cb + 1) * 512].rearrange("(k p) c -> p k c", p=128))
                pt_, bp = self.pf()
                for s in range(4):
                    j = cb * 4 + s
                    for k in range(8):
                        fw.op("pe", "matmul", [bw, self.b_scT], [bp], out=pt_[:, s * 2:(s + 1) * 2], lhsT=w_[:, k, s * 128:(s + 1) * 128],
                              rhs=self.scT[:, k, :], start=(k == 0), stop=(k == 7))
                fw.op("dve", "tensor_tensor", [bp, bbT], [self.b_modT], out=self.modT[:, cb * 4:(cb + 1) * 4, :],
                      in0=pt_[:, 0:8].rearrange("p (s w) -> p s w", w=2),
                      in1=bT[:, cb * 4:(cb + 1) * 4].unsqueeze(2).to_broadcast([128, 4, 2]), op=ALU.add)
                if cb in (4, 5, 10, 11):
                    gi = {4: 0, 5: 1, 10: 2, 11: 3}[cb]
                    pr, bpr = self.pf()
                    for k in range(8):
                        fw.op("pe", "matmul", [bw, self.b_scT], [bpr], out=pr[0:2, :], lhsT=self.scT[:, k, :], rhs=w_[:, k, :],
                              start=(k == 0), stop=(k == 7))
                    fw.op("dve", "tensor_tensor", [bpr, bbrow], [brows], out=rows[:], in0=pr[0:2, :], in1=brow[:, gi, :], op=ALU.add)
                    for w in range(2):
                        pbc, bpbc = self.pf()
                        fw.op("pe", "matmul", [brows, bsel], [bpbc], out=pbc[:], lhsT=sel[:, w, :], rhs=rows[:], start=True, stop=True)
                        slot = (0 if gi < 2 else 2) + w
                        half = gi % 2
                        gst, bgst = gst_[(gi * 2 + w) % 2]
                        fw.op("act", "copy", [bpbc], [bgst], out=gst[:], in_=pbc[:])
                        fw.dma([bgst], [self.b_GBd], out=self.GBd[:, slot, half * 512:(half + 1) * 512], in_=gst[:], stream="d2")
            fw.op("dve", "tensor_scalar", [self.b_modT], [self.b_modP], out=self.modP[:], in0=self.modT[:], scalar1=1.0, scalar2=None, op0=ALU.add)
            self.dump(f"modT{l}", self.modT[:], [128, 48, 2], [self.b_modT])
            fw.flush()

    def rms_stats(self, x, bx, st, bst, mv, bmv, rstd, brstd, eps_col=0):
        fw = self.fw
        fw.op("dve", "bn_stats", [bx], [bst], out=st[:, 0, :], in_=x[:, 0:512])
        fw.op("dve", "bn_stats", [bx, bst], [bst], out=st[:, 1, :], in_=x[:, 512:1024])
        fw.op("dve", "bn_aggr", [bst], [bmv], out=mv[:, 0:2], in_=st[:])
        fw.op("dve", "scalar_tensor_tensor", [bmv], [bmv], out=mv[:, 2:3], in0=mv[:, 0:1], scalar=mv[:, 0:1], in1=mv[:, 1:2], op0=ALU.mult, op1=ALU.add)
        fw.op("act", "activation", [bmv, self.b_epsc], [bmv], out=mv[:, 3:4], in_=mv[:, 2:3], func=AF.Sqrt, bias=self.epsc[:, eps_col:eps_col + 1], scale=1.0)
        fw.op("dve", "reciprocal", [bmv], [brstd], out=rstd[:], in_=mv[:, 3:4])

    def phase_norm1(self, l, hT, bhT):
        fw, I = self.fw, self.I
        with ExitStack() as es:
            gB, bgB = self.sb(es, "n1g", [128, D], F32)
            fw.dma([], [bgB], out=gB[:], in_=I["norm1_g"][l:l + 1, :].to_broadcast([128, D]))
            xs_ = [self.sb(es, f"n1x{i}", [128, D], F32) for i in range(2)]
            xb_ = [self.sb(es, f"n1xb{i}", [128, D], BF16) for i in range(2)]
            st_ = [self.sb(es, f"n1st{i}", [128, 2, 6], F32) for i in range(2)]
            mv_ = [self.sb(es, f"n1mv{i}", [128, 4], F32) for i in range(2)]
            rs_ = [self.sb(es, f"n1rs{i}", [128, 1], F32) for i in range(2)]
            for t in range(NT):
                w = 1 if t < 2 else 0
                x, bx = xs_[t % 2]; xb, bxb = xb_[t % 2]; st, bst = st_[t % 2]; mv, bmv = mv_[t % 2]; rs, brs = rs_[t % 2]
                fw.dma([self.bXR[t]], [bx], out=x[:], in_=self.XR[t * 128:(t + 1) * 128, :])
                self.rms_stats(x, bx, st, bst, mv, bmv, rs, brs)
                fw.op("dve", "scalar_tensor_tensor", [bx, brs, bgB], [bxb], out=xb[:], in0=x[:], scalar=rs[:, 0:1], in1=gB[:], op0=ALU.mult, op1=ALU.mult)
                pb_, bpb = self.pb()
                for k in range(8):
                    fw.op("pe", "transpose", [bxb, self.b_identb], [bpb], out=pb_[:, k * 128:(k + 1) * 128], in_=xb[:, k * 128:(k + 1) * 128], identity=self.identb[:])
                for k in range(8):
                    fw.op("act", "activation", [bpb, self.b_modT, self.b_modP], [bhT[t]], out=hT[:, k, t * 128:(t + 1) * 128], in_=pb_[:, k * 128:(k + 1) * 128],
                          func=AF.Identity, scale=self.modP[:, 8 + k, w:w + 1], bias=self.modT[:, k, w:w + 1])
            fw.flush()

    def layer(self, l):
        fw = self.fw
        self.phase_mod(l)
        with ExitStack() as es:
            hT, _ = self.sb(es, "hT", [128, 8, T], BF16)
            bhT = [Buf(f"hT{t}") for t in range(NT)]
            self.phase_norm1(l, hT, bhT)
            self.dump(f"hT{l}", hT[:], [128, 8, T], bhT, dt=BF16)
            if self.debug:
                fw.flush()
            if self.stop == "norm1":
                return
            self.phase_ret(l, hT, bhT)
            if self.stop == "ret":
                return
            self.phase_s5(l, hT, bhT)
            if self.stop == "s5":
                return
            self.phase_hy(l, hT, bhT)
            if self.stop == "hy":
                return
            self.phase_merge(l, hT, bhT)
        if self.stop == "merge":
            return
        self.phase_moe(l)

    def load_w_cols(self, dst_ap, bdst, l, c0, ncols):
        self.fw.dma([], [bdst], q="pool", stream="d1", out=dst_ap,
                    in_=self.I["w_in"][l][:, c0:c0 + ncols].rearrange("(k p) c -> p k c", p=128))

    def tiles_of(self, t0, n):
        return range(t0 // 128, (t0 + n) // 128)

    def phase_ret(self, l, hT, bhT):
        fw, I = self.fw, self.I
        QS = 128.0 ** -0.5
        with ExitStack() as es:
            rtab, brtab = self.sb(es, "rtab", [128, 6, 128], F32)
            lgB, blgB = self.sb(es, "lgB", [128, 8], F32)
            gnB, bgnB = self.sb(es, "gnB", [128, 512], F32)
            fw.dma([], [brtab], out=rtab[:], in_=I["rtab"].ap())
            fw.dma([], [blgB], out=lgB[:], in_=I["ret_log_decay"][l:l + 1, :].to_broadcast([128, 8]))
            fw.dma([], [bgnB], out=gnB[:], in_=I["ret_gn_g"][l:l + 1, :].to_broadcast([128, 512]))
            MT, bMT = self.sb(es, "MT", [128, 128], F32)
            tm1, btm1 = self.sb(es, "tm1", [128, 128], F32)
            tm2, btm2 = self.sb(es, "tm2", [128, 128], F32)
            dcol, bdcol = self.sb(es, "dcol", [128, 6], F32)
            wqk, bwqk = self.sb(es, "wqk", [128, 8, 256], BF16)
            wkvg, bwkvg = self.sb(es, "wkvg", [128, 8, 384], BF16)
            qT, bqT = self.sb(es, "qT", [128, T], BF16)
            kT, bkT = self.sb(es, "kT", [128, T], BF16)
            ktok, bktok = self.sb(es, "ktok", [128, NT, 128], BF16)
            vtok, bvtok = self.sb(es, "vtok", [128, NT, 128], BF16)
            vdf, bvdf = self.sb(es, "vdf", [128, NT, 128], BF16)
            vdb, bvdb = self.sb(es, "vdb", [128, NT, 128], BF16)
            gs, bgs = self.sb(es, "gs", [128, NT, 128], BF16)
            SfB, bSfB = self.sb(es, "SfB", [128, NT, 128], BF16)
            SbB, bSbB = self.sb(es, "SbB", [128, NT, 128], BF16)
            Sf, bSf = self.sb(es, "Sf", [128, 128], F32)
            Sb, bSb = self.sb(es, "Sb", [128, 128], F32)
            yTt_ = [self.sb(es, f"yTt{i}", [128, 128], BF16) for i in range(2)]
            attm_ = [self.sb(es, f"attm{i}", [128, 128], BF16) for i in range(2)]
            on_ = [self.sb(es, f"on{i}", [128, 128], F32) for i in range(2)]
            yk_ = [self.sb(es, f"yk{i}", [128, 128], BF16) for i in range(2)]
            st_ = [self.sb(es, f"rst{i}", [128, 6], F32) for i in range(2)]
            mv_ = [self.sb(es, f"rmv{i}", [128, 4], F32) for i in range(2)]
            bq_t = [Buf() for _ in range(NT)]
            bk_t = [Buf() for _ in range(NT)]
            btok = [Buf() for _ in range(NT)]
            bSf_t = [Buf() for _ in range(NT)]
            bSb_t = [Buf() for _ in range(NT)]
            for hd in range(4):
                lgf = lgB[:, hd:hd + 1]
                lgb = lgB[:, 4 + hd:5 + hd]
                fw.op("act", "activation", [brtab, blgB], [btm1], out=tm1[:], in_=rtab[:, 0, :], func=AF.Exp, scale=lgf)
                fw.op("act", "activation", [brtab, blgB], [btm2], out=tm2[:], in_=rtab[:, 1, :], func=AF.Exp, scale=lgb)
                fw.op("dve", "tensor_tensor", [btm1, brtab], [btm1], out=tm1[:], in0=tm1[:], in1=rtab[:, 2, :], op=ALU.mult)
                fw.op("dve", "tensor_tensor", [btm2, brtab], [btm2], out=tm2[:], in0=tm2[:], in1=rtab[:, 3, :], op=ALU.mult)
                fw.op("dve", "tensor_tensor", [btm1, btm2], [bMT], out=MT[:], in0=tm1[:], in1=tm2[:], op=ALU.add)
                fw.op("act", "activation", [self.b_rcol, blgB], [bdcol], out=dcol[:, 0:1], in_=self.rcol[:, 0:1], func=AF.Exp, scale=lgf)
                fw.op("act", "activation", [self.b_rcol, blgB, bdcol], [bdcol], out=dcol[:, 1:2], in_=self.rcol[:, 1:2], func=AF.Exp, scale=lgb)
                fw.op("act", "activation", [self.b_rcol, blgB, bdcol], [bdcol], out=dcol[:, 2:3], in_=self.rcol[:, 2:3], func=AF.Exp, scale=lgf)
                fw.op("act", "activation", [self.b_rcol, blgB, bdcol], [bdcol], out=dcol[:, 3:4], in_=self.rcol[:, 2:3], func=AF.Exp, scale=lgb)
                fw.op("act", "activation", [self.b_rcol, blgB, bdcol], [bdcol], out=dcol[:, 4:5], in_=self.rcol[:, 4:5], func=AF.Exp, scale=lgf)
                fw.op("act", "activation", [self.b_rcol, blgB, bdcol], [bdcol], out=dcol[:, 5:6], in_=self.rcol[:, 5:6], func=AF.Exp, scale=lgb)
                self.load_w_cols(wqk[:, :, 0:128], bwqk, l, hd * 128, 128)
                self.load_w_cols(wqk[:, :, 128:256], bwqk, l, 512 + hd * 128, 128)
                self.load_w_cols(wkvg[:, :, 0:128], bwkvg, l, 512 + hd * 128, 128)
                self.load_w_cols(wkvg[:, :, 128:256], bwkvg, l, 1024 + hd * 128, 128)
                self.load_w_cols(wkvg[:, :, 256:384], bwkvg, l, 1536 + hd * 128, 128)
                for (t0, n) in TG:
                    tl = list(self.tiles_of(t0, n))
                    hb = [bhT[t] for t in tl]
                    nb = n // 128
                    pq, bpq = self.pf()
                    for k in range(8):
                        fw.op("pe", "matmul", hb + [bwqk], [bpq], out=pq[:, 0:n], lhsT=wqk[:, k, 0:128], rhs=hT[:, k, t0:t0 + n], start=(k == 0), stop=(k == 7))
                    wq_ = [bq_t[t] for t in tl]
                    fw.op("act", "mul", [bpq], wq_, out=qT[:, t0:t0 + n], in_=pq[:, 0:n], mul=QS)
                    pk, bpk = self.pf()
                    for k in range(8):
                        fw.op("pe", "matmul", hb + [bwqk], [bpk], out=pk[:, 0:n], lhsT=wqk[:, k, 128:256], rhs=hT[:, k, t0:t0 + n], start=(k == 0), stop=(k == 7))
                    fw.op("act", "copy", [bpk], [bk_t[t] for t in tl], out=kT[:, t0:t0 + n], in_=pk[:, 0:n])
                for t in range(NT):
                    pt_, bp = self.pf()
                    for k in range(8):
                        fw.op("pe", "matmul", [bhT[t], bwkvg], [bp], out=pt_[:, 0:384], lhsT=hT[:, k, t * 128:(t + 1) * 128], rhs=wkvg[:, k, :], start=(k == 0), stop=(k == 7))
                    fw.op("act", "copy", [bp], [btok[t]], out=ktok[:, t, :], in_=pt_[:, 0:128])
                    fw.op("act", "copy", [bp], [btok[t]], out=vtok[:, t, :], in_=pt_[:, 128:256])
                    fw.op("dve", "tensor_scalar", [bp, bdcol], [btok[t]], out=vdf[:, t, :], in0=pt_[:, 128:256], scalar1=dcol[:, 0:1], scalar2=None, op0=ALU.mult)
                    fw.op("dve", "tensor_scalar", [bp, bdcol], [btok[t]], out=vdb[:, t, :], in0=pt_[:, 128:256], scalar1=dcol[:, 1:2], scalar2=None, op0=ALU.mult)
                    fw.op("act", "activation", [bp], [btok[t]], out=gs[:, t, :], in_=pt_[:, 256:384], func=AF.Silu)
                for (S, bS, SB, bSB_t, vd, ci, order) in ((Sf, bSf, SfB, bSf_t, vdf, 2, list(range(NT))),
                                                          (Sb, bSb, SbB, bSb_t, vdb, 3, [1, 0] + list(range(NT - 1, 1, -1)))):
                    fw.op("pool", "memset", [], [bS], ap=S[:], constant=0.0)
                    for t in order:
                        fw.op("act", "copy", [bS], [bSB_t[t]], out=SB[:, t, :], in_=S[:])
                        pkv, bpkv = self.pf()
                        fw.op("pe", "matmul", [btok[t]], [bpkv], out=pkv[:, 0:128], lhsT=ktok[:, t, :], rhs=vd[:, t, :], start=True, stop=True)
                        fw.op("dve", "scalar_tensor_tensor", [bS, bpkv, bdcol], [bS], out=S[:], in0=S[:], scalar=dcol[:, ci:ci + 1], in1=pkv[:, 0:128], op0=ALU.mult, op1=ALU.add)
                for t in range(NT):
                    cs = slice(t * 128, (t + 1) * 128)
                    attm, battm = attm_[t % 2]; on, bon = on_[t % 2]; yk, byk = yk_[t % 2]; st, bst = st_[t % 2]; mv, bmv = mv_[t % 2]
                    pa, bpa = self.pf()
                    fw.op("pe", "matmul", [bk_t[t], bq_t[t]], [bpa], out=pa[:, 0:128], lhsT=kT[:, cs], rhs=qT[:, cs], start=True, stop=True)
                    fw.op("dve", "tensor_tensor", [bpa, bMT], [battm], out=attm[:], in0=pa[:, 0:128], in1=MT[:], op=ALU.mult)
                    po, bpo = self.pf()
                    fw.op("pe", "matmul", [battm, btok[t]], [bpo], out=po[:, 0:128], lhsT=attm[:], rhs=vtok[:, t, :], start=True, stop=True)
                    fw.op("pe", "matmul", [bq_t[t], bSf_t[t]], [bpo], out=po[:, 128:256], lhsT=qT[:, cs], rhs=SfB[:, t, :], start=True, stop=True)
                    fw.op("pe", "matmul", [bq_t[t], bSb_t[t]], [bpo], out=po[:, 256:384], lhsT=qT[:, cs], rhs=SbB[:, t, :], start=True, stop=True)
                    fw.op("act", "copy", [bpo], [bon], out=on[:], in_=po[:, 0:128])
                    fw.op("dve", "scalar_tensor_tensor", [bpo, bdcol, bon], [bon], out=on[:], in0=po[:, 128:256], scalar=dcol[:, 4:5], in1=on[:], op0=ALU.mult, op1=ALU.add)
                    fw.op("dve", "scalar_tensor_tensor", [bpo, bdcol, bon], [bon], out=on[:], in0=po[:, 256:384], scalar=dcol[:, 5:6], in1=on[:], op0=ALU.mult, op1=ALU.add)
                    fw.op("dve", "bn_stats", [bon], [bst], out=st[:], in_=on[:])
                    fw.op("dve", "bn_aggr", [bst], [bmv], out=mv[:, 0:2], in_=st[:])
                    fw.op("act", "activation", [bmv, self.b_epsc], [bmv], out=mv[:, 2:3], in_=mv[:, 1:2], func=AF.Sqrt, bias=self.epsc[:, 1:2], scale=1.0)
                    fw.op("dve", "reciprocal", [bmv], [bmv], out=mv[:, 3:4], in_=mv[:, 2:3])
                    fw.op("dve", "tensor_scalar", [bon, bmv], [bon], out=on[:], in0=on[:], scalar1=mv[:, 0:1], scalar2=mv[:, 3:4], op0=ALU.subtract, op1=ALU.mult)
                    fw.op("dve", "tensor_tensor", [bon, bgnB], [bon], out=on[:], in0=on[:], in1=gnB[:, hd * 128:(hd + 1) * 128], op=ALU.mult)
                    fw.op("dve", "tensor_tensor", [bon, btok[t]], [byk], out=yk[:], in0=on[:], in1=gs[:, t, :], op=ALU.mult)
                    pb_, bpb = self.pb()
                    fw.op("pe", "transpose", [byk, self.b_identb], [bpb], out=pb_[:, 0:128], in_=yk[:], identity=self.identb[:])
                    yTt, byTt = yTt_[t % 2]
                    fw.op("act", "copy", [bpb], [byTt], out=yTt[:], in_=pb_[:, 0:128])
                    fw.dma([byTt], [self.bYT[hd]], out=self.YT[hd * 128:(hd + 1) * 128, cs], in_=yTt[:], stream="d2")
            if self.debug:
                self.dump_yt("yret%d" % l, 0, 4)
            fw.flush()

    def dump_yt(self, name, c0, nchunks):
        if name not in self.debug:
            return
        t = self.nc.dram_tensor("dbg_" + name, [nchunks * 128, T], BF16, kind="ExternalOutput")
        self.dbg_out.append("dbg_" + name)
        self.fw.dma([self.bYT[c0 + i] for i in range(nchunks)], [Buf()], out=t.ap(), in_=self.YT[c0 * 128:(c0 + nchunks) * 128, :], stream="d2")

    def sincos(self, src, bsrc, shape, sin_out, cos_out, bout, ki, bki, kf, bkf, eng="dve"):
        fw = self.fw
        PI_ = 3.1415925
        fw.op("dve", "tensor_scalar", [bsrc], [bki], out=ki, in0=src, scalar1=1.0 / TWO_PI, scalar2=None, op0=ALU.mult)
        fw.op("dve", "tensor_copy", [bki], [bkf], out=kf, in_=ki)
        fw.op("dve", "scalar_tensor_tensor", [bkf, bsrc], [bkf], out=kf, in0=kf, scalar=-TWO_PI, in1=src, op0=ALU.mult, op1=ALU.add)
        fw.op("dve", "tensor_scalar", [bkf], [bkf], out=kf, in0=kf, scalar1=-PI_, scalar2=PI_, op0=ALU.max, op1=ALU.min)
        if sin_out is not None:
            fw.op("act", "activation", [bkf], [bout], out=sin_out, in_=kf, func=AF.Sin)
        if cos_out is not None:
            fw.op("dve", "scalar_tensor_tensor", [bkf], [bkf], out=kf, in0=kf, scalar=-1.0, in1=kf, op0=ALU.mult, op1=ALU.max)
            fw.op("act", "activation", [bkf, self.b_epsc], [bout], out=cos_out, in_=kf, func=AF.Sin, scale=-1.0, bias=self.epsc[:, 2:3])

    def phase_s5(self, l, hT, bhT):
        fw, I = self.fw, self.I
        CH = [(0, 256)] + [(256 + 512 * i, 512) for i in range(8)]
        with ExitStack() as es:
            prm, bprm = self.sb(es, "s5prm", [128, 3, 32], F32)
            for dr in range(2):
                fw.dma([], [bprm], out=prm[:, 0, dr * 16:(dr + 1) * 16], in_=I["s5_lre"][l, dr])
                fw.dma([], [bprm], out=prm[:, 1, dr * 16:(dr + 1) * 16], in_=I["s5_lim"][l, dr])
                fw.dma([], [bprm], out=prm[:, 2, dr * 16:(dr + 1) * 16], in_=I["s5_ldt"][l, dr])
            dsc, bdsc = self.sb(es, "s5dsc", [128, 12, 32], F32)
            ki32, bki32 = self.sb(es, "s5ki", [128, 32], I32)
            kf32, bkf32 = self.sb(es, "s5kf", [128, 32], F32)
            DT, LR, TH, R_, SN, CS, AR, AI, DEN, CR, CI, NCR = [dsc[:, i, :] for i in range(12)]
            lre, lim = prm[:, 0, :], prm[:, 1, :]
            B2 = [bprm, bdsc]
            fw.op("act", "activation", [bprm], [bdsc], out=DT, in_=prm[:, 2, :], func=AF.Exp)
            fw.op("dve", "tensor_tensor", B2, [bdsc], out=LR, in0=lre, in1=DT, op=ALU.mult)
            fw.op("dve", "tensor_tensor", B2, [bdsc], out=TH, in0=lim, in1=DT, op=ALU.mult)
            fw.op("act", "activation", [bdsc], [bdsc], out=R_, in_=LR, func=AF.Exp)
            self.sincos(TH, bdsc, None, SN, CS, bdsc, ki32[:], bki32, kf32[:], bkf32)
            fw.op("dve", "tensor_tensor", B2, [bdsc], out=AR, in0=R_, in1=CS, op=ALU.mult)
            fw.op("dve", "tensor_tensor", B2, [bdsc], out=AI, in0=R_, in1=SN, op=ALU.mult)
            fw.op("dve", "tensor_scalar", B2, [bdsc], out=AR, in0=AR, scalar1=-1.0, scalar2=None, op0=ALU.add)
            fw.op("dve", "tensor_tensor", B2, [bdsc], out=DEN, in0=lre, in1=lre, op=ALU.mult)
            fw.op("dve", "tensor_tensor", B2, [bdsc], out=LR, in0=lim, in1=lim, op=ALU.mult)
            fw.op("dve", "tensor_tensor", B2, [bdsc], out=DEN, in0=DEN, in1=LR, op=ALU.add)
            fw.op("dve", "reciprocal", B2, [bdsc], out=DEN, in_=DEN)
            fw.op("dve", "tensor_tensor", B2, [bdsc], out=CR, in0=AR, in1=lre, op=ALU.mult)
            fw.op("dve", "tensor_tensor", B2, [bdsc], out=LR, in0=AI, in1=lim, op=ALU.mult)
            fw.op("dve", "tensor_tensor", B2, [bdsc], out=CR, in0=CR, in1=LR, op=ALU.add)
            fw.op("dve", "tensor_tensor", B2, [bdsc], out=CR, in0=CR, in1=DEN, op=ALU.mult)
            fw.op("dve", "tensor_tensor", B2, [bdsc], out=CI, in0=AI, in1=lre, op=ALU.mult)
            fw.op("dve", "tensor_tensor", B2, [bdsc], out=LR, in0=AR, in1=lim, op=ALU.mult)
            fw.op("dve", "tensor_tensor", B2, [bdsc], out=CI, in0=CI, in1=LR, op=ALU.subtract)
            fw.op("dve", "tensor_tensor", B2, [bdsc], out=CI, in0=CI, in1=DEN, op=ALU.mult)
            fw.op("dve", "tensor_scalar", B2, [bdsc], out=NCR, in0=CR, scalar1=-1.0, scalar2=None, op0=ALU.mult)
            cut = getattr(self, 's5_cut', 0)
            if cut == 1:
                self.dump('dsc', dsc[:], [128, 12, 32], [bdsc])
                fw.flush()
                return
            Bb, bBb = self.sb(es, "s5Bb", [128, 2, 2, 4, 2, 128], BF16)
            Cb, bCb = self.sb(es, "s5Cb", [128, 2, 2, 4, 64], BF16)
            for dr in range(2):
                for ri in range(2):
                    fw.dma([], [bBb], q="pool", stream="d1", out=Bb[:, dr, ri], in_=I["s5_B"][l, dr, ri])
            dT_, bdT = self.sb(es, "s5dT", [128, 4], F32)
            fw.dma([], [bdT], out=dT_[:], in_=I["s5_dT"][l])
            iota, biota = self.sb(es, "s5iota", [128, 512], F32)
            fw.dma([], [biota], out=iota[:], in_=I["iota512"].ap())
            wu, bwu = self.sb(es, "s5wu", [128, 8, 128], BF16)
            uf, buf_ = self.sb(es, "s5uf", [128, T], F32)
            ub, bub = self.sb(es, "s5ub", [128, T], BF16)
            yacc, byacc = self.sb(es, "s5yacc", [128, T], F32)
            gb_ = [self.sb(es, f"s5gb{i}", [128, 512], BF16) for i in range(2)]
            tb = {}
            for nm in ("cos", "sin", "dre", "dim", "rf"):
                tb[nm] = self.sb(es, "s5t_" + nm, [128, 512], F32)
            wk = {}
            for nm in ("pre", "pim", "t1", "t2", "t3", "t4", "gre", "gim", "hre", "nhim"):
                wk[nm] = self.sb(es, "s5w_" + nm, [128, 512], F32)
            hreb, bhreb = self.sb(es, "s5hreb", [128, 512], BF16)
            nhimb, bnhimb = self.sb(es, "s5nhimb", [128, 512], BF16)
            carry, bcarry = self.sb(es, "s5carry", [128, 2], F32)
            self.GS5 = getattr(self, "GS5", None) or self.scratch("GS5", [512, T], BF16)
            bGS5 = [Buf() for _ in range(4)]
            ychunk = [Buf() for _ in CH]
            if cut == 2:
                fw.flush()
                return
            for o in range(4):
                for dr in range(2):
                    for ri in range(2):
                        fw.dma([], [bCb], q="pool", stream="d1", out=Cb[:, dr, ri], in_=I["s5_C"][l, dr, ri][:, o * 4:(o + 1) * 4, :])
                    fw.op("act", "mul", [bCb], [bCb], out=Cb[:, dr, 1], in_=Cb[:, dr, 1], mul=-1.0)
                self.load_w_cols(wu[:], bwu, l, 2048 + o * 128, 128)
                for (t0, n) in TG:
                    hb = [bhT[t] for t in self.tiles_of(t0, n)]
                    pu, bpu = self.pf()
                    for k in range(8):
                        fw.op("pe", "matmul", hb + [bwu], [bpu], out=pu[:, 0:n], lhsT=wu[:, k, :], rhs=hT[:, k, t0:t0 + n], start=(k == 0), stop=(k == 7))
                    fw.op("act", "copy", [bpu], [buf_], out=uf[:, t0:t0 + n], in_=pu[:, 0:n])
                    fw.op("dve", "tensor_copy", [buf_], [bub], out=ub[:, t0:t0 + n], in_=uf[:, t0:t0 + n])
                if cut == 5:
                    fw.flush()
                    return
                fw.op("pool", "memset", [], ychunk, ap=yacc[:], constant=0.0)
                if cut == 3:
                    fw.flush()
                    return
                lim = getattr(self, "s5_lim", None)
                for dr in range(2):
                    if lim is not None and dr >= lim[0]:
                        break
                    for q4 in range(4):
                        pt = o * 4 + q4
                        c = dr * 16 + pt
                        ps = slice(64 * (q4 // 2), 64 * (q4 // 2) + 64)
                        jj = q4 % 2
                        (cosT, bcos), (sinT, bsin) = tb["cos"], tb["sin"]
                        kiT, bkiT = wk["t4"][0][:].bitcast(I32), wk["t4"][1]
                        (dreT, bdre), (dimT, bdim), (rfT, brf) = tb["dre"], tb["dim"], tb["rf"]
                        t1, bt1 = wk["t1"]
                        fw.op("dve", "tensor_scalar", [biota, bdsc], [bt1], out=t1[:], in0=iota[:], scalar1=TH[:, c:c + 1], scalar2=None, op0=ALU.mult)
                        fw.op("dve", "tensor_scalar", [bt1], [bkiT], out=kiT, in0=t1[:], scalar1=1.0 / TWO_PI, scalar2=None, op0=ALU.mult)
                        t2, bt2 = wk["t2"]
                        fw.op("dve", "tensor_copy", [bkiT], [bt2], out=t2[:], in_=kiT)
                        fw.op("dve", "scalar_tensor_tensor", [bt2, bt1], [bt2], out=t2[:], in0=t2[:], scalar=-TWO_PI, in1=t1[:], op0=ALU.mult, op1=ALU.add)
                        fw.op("dve", "tensor_scalar", [bt2], [bt2], out=t2[:], in0=t2[:], scalar1=-3.1415925, scalar2=3.1415925, op0=ALU.max, op1=ALU.min)
                        fw.op("act", "activation", [bt2], [bsin], out=sinT[:], in_=t2[:], func=AF.Sin)
                        fw.op("dve", "scalar_tensor_tensor", [bt2], [bt2], out=t2[:], in0=t2[:], scalar=-1.0, in1=t2[:], op0=ALU.mult, op1=ALU.max)
                        fw.op("act", "activation", [bt2, self.b_epsc], [bcos], out=cosT[:], in_=t2[:], func=AF.Sin, scale=-1.0, bias=self.epsc[:, 2:3])
                        fw.op("dve", "tensor_scalar", [bcos, bdsc], [bdre], out=dreT[:], in0=cosT[:], scalar1=CR[:, c:c + 1], scalar2=None, op0=ALU.mult)
                        fw.op("dve", "scalar_tensor_tensor", [bsin, bdsc, bdre], [bdre], out=dreT[:], in0=sinT[:], scalar=CI[:, c:c + 1], in1=dreT[:], op0=ALU.mult, op1=ALU.add)
                        fw.op("dve", "tensor_scalar", [bcos, bdsc], [bdim], out=dimT[:], in0=cosT[:], scalar1=CI[:, c:c + 1], scalar2=None, op0=ALU.mult)
                        fw.op("dve", "scalar_tensor_tensor", [bsin, bdsc, bdim], [bdim], out=dimT[:], in0=sinT[:], scalar=NCR[:, c:c + 1], in1=dimT[:], op0=ALU.mult, op1=ALU.add)
                        fw.op("dve", "tensor_scalar", [biota], [brf], out=rfT[:], in0=iota[:], scalar1=0.0, scalar2=None, op0=ALU.mult)
                        fw.op("dve", "tensor_scalar", [brf, bdsc], [brf], out=rfT[:], in0=rfT[:], scalar1=R_[:, c:c + 1], scalar2=None, op0=ALU.add)
                        fw.op("pool", "memset", [], [bcarry], ap=carry[:], constant=0.0)
                        order = list(range(9)) if dr == 0 else [0] + list(range(8, 0, -1))
                        if lim is not None:
                            order = order[:lim[1]]
                        for ci_ in order:
                            t0, n = CH[ci_]
                            rv = (lambda ap: ap) if dr == 0 else (lambda ap: ap[:, ::-1])
                            pP, bpP = self.pf()
                            pQ, bpQ = self.pf()
                            fw.op("pe", "matmul", [bBb, bub], [bpP], out=pP[:, 0:n], lhsT=Bb[ps, dr, 0, o, jj, :], rhs=ub[ps, t0:t0 + n], start=True, stop=True)
                            fw.op("pe", "matmul", [bBb, bub], [bpQ], out=pQ[:, 0:n], lhsT=Bb[ps, dr, 1, o, jj, :], rhs=ub[ps, t0:t0 + n], start=True, stop=True)
                            pre, bpre = wk["pre"]; pim, bpim = wk["pim"]
                            fw.op("act", "copy", [bpP], [bpre], out=rv(pre[:, 0:n]), in_=pP[:, 0:n])
                            fw.op("act", "copy", [bpQ], [bpim], out=rv(pim[:, 0:n]), in_=pQ[:, 0:n])
                            t2, bt2 = wk["t2"]; t3, bt3 = wk["t3"]; t4, bt4 = wk["t4"]
                            gre, bgre = wk["gre"]; gim, bgim = wk["gim"]; hre, bhre = wk["hre"]; nhim, bnhim = wk["nhim"]
                            N = slice(0, n)
                            fw.op("dve", "tensor_tensor", [bpre, bdre], [bt1], out=t1[:, N], in0=pre[:, N], in1=dreT[:, N], op=ALU.mult)
                            fw.op("dve", "tensor_tensor", [bpim, bdim], [bt2], out=t2[:, N], in0=pim[:, N], in1=dimT[:, N], op=ALU.mult)
                            fw.op("dve", "tensor_tensor", [bt1, bt2], [bt1], out=t1[:, N], in0=t1[:, N], in1=t2[:, N], op=ALU.subtract)
                            fw.op("pool", "tensor_tensor", [bpre, bdim], [bt3], out=t3[:, N], in0=pre[:, N], in1=dimT[:, N], op=ALU.mult)
                            fw.op("pool", "tensor_tensor", [bpim, bdre], [bt4], out=t4[:, N], in0=pim[:, N], in1=dreT[:, N], op=ALU.mult)
                            fw.op("pool", "tensor_tensor", [bt3, bt4], [bt3], out=t3[:, N], in0=t3[:, N], in1=t4[:, N], op=ALU.add)
                            fw.op("dve", "tensor_tensor_scan", [brf, bt1, bcarry], [bgre], out=gre[:, N], data0=rfT[:, N], data1=t1[:, N], initial=carry[:, 0:1], op0=ALU.mult, op1=ALU.add)
                            fw.op("dve", "tensor_tensor_scan", [brf, bt3, bcarry], [bgim], out=gim[:, N], data0=rfT[:, N], data1=t3[:, N], initial=carry[:, 1:2], op0=ALU.mult, op1=ALU.add)
                            fw.op("dve", "tensor_tensor", [bcos, bgre], [bhre], out=hre[:, N], in0=cosT[:, N], in1=gre[:, N], op=ALU.mult)
                            fw.op("pool", "tensor_tensor", [bsin, bgim], [bt4], out=t4[:, N], in0=sinT[:, N], in1=gim[:, N], op=ALU.mult)
                            fw.op("dve", "tensor_tensor", [bhre, bt4], [bhre], out=hre[:, N], in0=hre[:, N], in1=t4[:, N], op=ALU.subtract)
                            fw.op("pool", "tensor_tensor", [bsin, bgre], [bnhim], out=nhim[:, N], in0=sinT[:, N], in1=gre[:, N], op=ALU.mult)
                            fw.op("pool", "tensor_tensor", [bcos, bgim], [bt2], out=t2[:, N], in0=cosT[:, N], in1=gim[:, N], op=ALU.mult)
                            fw.op("pool", "tensor_tensor", [bnhim, bt2], [bnhim], out=nhim[:, N], in0=nhim[:, N], in1=t2[:, N], op=ALU.add)
                            fw.op("act", "copy", [bhre], [bhreb], out=rv(hreb[:, N]), in_=hre[:, N])
                            fw.op("act", "copy", [bnhim], [bnhimb], out=rv(nhimb[:, N]), in_=nhim[:, N])
                            fw.op("act", "copy", [bhre], [bcarry], out=carry[:, 0:1], in_=hre[:, n - 1:n])
                            fw.op("act", "copy", [bnhim, bcarry], [bcarry], out=carry[:, 1:2], in_=nhim[:, n - 1:n])
                            py, bpy = self.pf()
                            fw.op("pe", "matmul", [bCb, bhreb], [bpy], out=py[ps, 0:n], lhsT=Cb[:, dr, 0, q4, :], rhs=hreb[:, N], start=True, stop=False)
                            fw.op("pe", "matmul", [bCb, bnhimb], [bpy], out=py[ps, 0:n], lhsT=Cb[:, dr, 1, q4, :], rhs=nhimb[:, N], start=False, stop=True)
                            fw.op("dve", "tensor_tensor", [bpy, ychunk[ci_]], [ychunk[ci_]], out=yacc[ps, t0:t0 + n], in0=py[ps, 0:n], in1=yacc[ps, t0:t0 + n], op=ALU.add)
                if cut == 4:
                    fw.flush()
                    return
                fw.op("dve", "scalar_tensor_tensor", [buf_, bdT] + ychunk, ychunk, out=yacc[:], in0=uf[:], scalar=dT_[:, o:o + 1], in1=yacc[:], op0=ALU.mult, op1=ALU.add)
                for gi_, (t0, n) in enumerate(TG):
                    gb, bgb = gb_[gi_ % 2]
                    fw.op("act", "activation", ychunk, [bgb], out=gb[:, 0:n], in_=yacc[:, t0:t0 + n], func=AF.Gelu_apprx_tanh)
                    fw.dma([bgb], [bGS5[o]], out=self.GS5[o * 128:(o + 1) * 128, t0:t0 + n], in_=gb[:, 0:n], stream="d2")
            fw.flush()
        with ExitStack() as es:
            W1b, bW1b = self.sb(es, "s5W1b", [128, 4, 512], BF16)
            W2b, bW2b = self.sb(es, "s5W2b", [128, 4, 512], BF16)
            fw.dma([], [bW1b], q="pool", stream="d1", out=W1b[:], in_=I["s5_glu_w1"][l].rearrange("(k p) c -> p k c", p=128))
            fw.dma([], [bW2b], q="pool", stream="d1", out=W2b[:], in_=I["s5_glu_w2"][l].rearrange("(k p) c -> p k c", p=128))
            g_ = [self.sb(es, f"s5g{i}", [128, 4, 512], BF16) for i in range(2)]
            sg_ = [self.sb(es, f"s5sg{i}", [128, 512], F32) for i in range(2)]
            yo_ = [self.sb(es, f"s5yo{i}", [128, 512], BF16) for i in range(2)]
            for gi, (t0, n) in enumerate(TG):
                g, bg = g_[gi % 2]
                fw.dma(bGS5, [bg], out=g[:, :, 0:n], in_=self.GS5[:, t0:t0 + n].rearrange("(k p) t -> p k t", p=128))
                for c_ in range(4):
                    sg, bsg = sg_[c_ % 2]; yo, byo = yo_[c_ % 2]
                    pA, bpA = self.pf()
                    pB, bpB = self.pf()
                    for k in range(4):
                        fw.op("pe", "matmul", [bW1b, bg], [bpA], out=pA[:, 0:n], lhsT=W1b[:, k, c_ * 128:(c_ + 1) * 128], rhs=g[:, k, 0:n], start=(k == 0), stop=(k == 3))
                    for k in range(4):
                        fw.op("pe", "matmul", [bW2b, bg], [bpB], out=pB[:, 0:n], lhsT=W2b[:, k, c_ * 128:(c_ + 1) * 128], rhs=g[:, k, 0:n], start=(k == 0), stop=(k == 3))
                    fw.op("act", "activation", [bpB], [bsg], out=sg[:, 0:n], in_=pB[:, 0:n], func=AF.Sigmoid)
                    fw.op("dve", "tensor_tensor", [bpA, bsg], [byo], out=yo[:, 0:n], in0=pA[:, 0:n], in1=sg[:, 0:n], op=ALU.mult)
                    fw.dma([byo], [self.bYT[4 + c_]], out=self.YT[512 + c_ * 128:512 + (c_ + 1) * 128, t0:t0 + n], in_=yo[:, 0:n], stream="d2")
            if self.debug:
                self.dump_yt("ys5%d" % l, 4, 4)
            fw.flush()

    def phase_hy(self, l, hT, bhT):
        fw, I = self.fw, self.I
        do_ctx = (l == 0)
        with ExitStack() as es:
            w1, bw1 = self.sb(es, "hfw1", [33, 64], F32)
            w2, bw2 = self.sb(es, "hfw2", [64, 64], F32)
            w3, bw3 = self.sb(es, "hfw3", [64, 512], F32)
            fb, bfb = self.sb(es, "hffb", [64, 6], F32)
            nd_, bnd = self.sb(es, "hfnd", [128, 4], F32)
            fw.dma([], [bw1], out=w1[:], in_=I["hy_f_w1"][l])
            fw.dma([], [bw2], out=w2[:], in_=I["hy_f_w2"][l])
            fw.dma([], [bw3], out=w3[:], in_=I["hy_f_w3"][l])
            fw.dma([], [bfb], out=fb[:, 0:1], in_=I["hy_f_b1"][l])
            fw.dma([], [bfb], out=fb[:, 1:2], in_=I["hy_f_b2"][l])
            fw.dma([], [bfb], out=fb[:, 2:4], in_=I["hy_f_fr"][l])
            fw.dma([], [bnd], out=nd_[:], in_=I["ndelta"].ap())
            fw.op("dve", "tensor_tensor", [bfb], [bfb], out=fb[:, 4:6], in0=fb[:, 0:2], in1=fb[:, 2:4], op=ALU.mult)
            zt_ = [self.sb(es, f"hfz{i}", [33, 512], F32) for i in range(2)]
            of_ = [self.sb(es, f"hfo{i}", [128, 512], F32) for i in range(2)]
            a_, ba_ = self.sb(es, "hfa", [64, 512], F32)
            hh_, bhh = self.sb(es, "hfh", [64, 512], F32)
            ki, bki = self.sb(es, "hfki", [64, 512], I32)
            kf, bkf = self.sb(es, "hfkf", [64, 512], F32)
            win, bwin = self.sb(es, "hfwin", [128, 512], F32)
            frev = [self.sb(es, f"hffr{i}", [128, L], BF16) for i in range(4)]
            zpad, bzpad = self.sb(es, "hfzp", [128, 128], BF16)
            fw.op("pool", "memset", [], [bzpad], ap=zpad[:], constant=0.0)
            jobs = [("zT", "off", L, self.GF, self.bGF, 4352)]
            if do_ctx:
                jobs.append(("zTc", "offc", CTX, self.GFc, self.bGFc, 512))
            for (zn, on, n, GFt, bGFt, LEN) in jobs:
                cn = min(512, n)
                for ch in range(n // cn):
                    c0 = ch * cn
                    zt, bzt = zt_[ch % 2]
                    of, bof = of_[ch % 2]
                    fw.dma([], [bzt], out=zt[:, 0:cn], in_=I[zn][:, c0:c0 + cn])
                    fw.dma([], [bof], out=of[:, 0:cn], in_=I[on][:, c0:c0 + cn])
                    p1, bp1 = self.pf()
                    fw.op("pe", "matmul", [bw1, bzt], [bp1], out=p1[0:64, 0:cn], lhsT=w1[:], rhs=zt[:, 0:cn], start=True, stop=True)
                    fw.op("act", "activation", [bp1, bfb], [ba_], out=a_[:, 0:cn], in_=p1[0:64, 0:cn], func=AF.Identity, scale=fb[:, 2:3], bias=fb[:, 4:5])
                    self.sincos(a_[:, 0:cn], ba_, None, hh_[:, 0:cn], None, bhh, ki[:, 0:cn], bki, kf[:, 0:cn], bkf)
                    p2, bp2 = self.pf()
                    fw.op("pe", "matmul", [bw2, bhh], [bp2], out=p2[0:64, 0:cn], lhsT=w2[:], rhs=hh_[:, 0:cn], start=True, stop=True)
                    fw.op("act", "activation", [bp2, bfb], [ba_], out=a_[:, 0:cn], in_=p2[0:64, 0:cn], func=AF.Identity, scale=fb[:, 3:4], bias=fb[:, 5:6])
                    self.sincos(a_[:, 0:cn], ba_, None, hh_[:, 0:cn], None, bhh, ki[:, 0:cn], bki, kf[:, 0:cn], bkf)
                    for ct in range(4):
                        fr, bfr = frev[ct]
                        p3, bp3 = self.pf()
                        fw.op("pe", "matmul", [bw3, bhh], [bp3], out=p3[:, 0:cn], lhsT=w3[:, ct * 128:(ct + 1) * 128], rhs=hh_[:, 0:cn], start=True, stop=True)
                        fw.op("act", "activation", [bof, bnd], [bwin], out=win[:, 0:cn], in_=of[:, 0:cn], func=AF.Exp, scale=nd_[:, ct:ct + 1])
                        fw.op("dve", "tensor_tensor", [bp3, bwin], [bfr], out=fr[:, n - c0 - cn:n - c0][:, ::-1], in0=p3[:, 0:cn], in1=win[:, 0:cn], op=ALU.mult)
                for ct in range(4):
                    fr, bfr = frev[ct]
                    rs = slice(ct * 128, (ct + 1) * 128)
                    fw.dma([bfr], [bGFt[ct]], out=GFt[rs, 128:128 + n], in_=fr[:, 0:n], stream="d2")
                    fw.dma([bzpad], [bGFt[ct]], out=GFt[rs, 0:128], in_=zpad[:], stream="d2")
                    fw.dma([bzpad], [bGFt[ct]], out=GFt[rs, 128 + n:LEN], in_=zpad[:, 0:LEN - 128 - n], stream="d2")
            fw.flush()
        with ExitStack() as es:
            wx, bwx = self.sb(es, "hywx", [128, 8, 128], BF16)
            zraw, bzraw = self.sb(es, "hyzraw", [128, T], F32)
            A, bA = self.sb(es, "hyA", [128, T], F32)
            B, bB = self.sb(es, "hyB", [128, T], F32)
            yvb, byvb = self.sb(es, "hyyvb", [128, T], BF16)
            yout_ = [self.sb(es, f"hyyout{i}", [128, 512], BF16) for i in range(2)]
            Upad, bUpad = self.sb(es, "hyUpad", [128, 32, 128], BF16)
            Upc, bUpc = self.sb(es, "hyUpc", [128, 2, 128], BF16)
            TT_ = [self.sb(es, f"hyTT{i}", [128, 33 * 128], BF16) for i in range(2)]
            yrev, byrev = self.sb(es, "hyyrev", [128, 32, 128], BF16)
            tmp, btmp = self.sb(es, "hytmp", [128, 512], F32)
            swT, bswT = self.sb(es, "hyswT", [128, 12, 3], F32)
            sbT, bsbT = self.sb(es, "hysbT", [128, 12], F32)
            biasT, bbiasT = self.sb(es, "hybiasT", [128, 4], F32)
            fw.dma([], [bswT], out=swT[:], in_=I["hy_swT"][l])
            fw.dma([], [bsbT], out=sbT[:], in_=I["hy_sbT"][l])
            fw.dma([], [bbiasT], out=biasT[:], in_=I["hy_biasT"][l])
            segs = [(0, CTX), (CTX, T)]
            tti = 0
            for ct in range(4):
                for j, dst, bdst in ((1, A, bA), (2, B, bB), (0, B, bB)):
                    if j == 0:
                        fw.op("dve", "tensor_tensor", [bA, bB], [bA], out=A[:], in0=A[:], in1=B[:], op=ALU.mult)
                        fw.op("act", "copy", [bA], [byvb], out=yvb[:], in_=A[:])
                    cidx = j * 4 + ct
                    self.load_w_cols(wx[:], bwx, l, 2560 + j * 512 + ct * 128, 128)
                    for (t0, n) in TG:
                        hb = [bhT[t] for t in self.tiles_of(t0, n)]
                        pz, bpz = self.pf()
                        for k in range(8):
                            fw.op("pe", "matmul", hb + [bwx], [bpz], out=pz[:, 0:n], lhsT=wx[:, k, :], rhs=hT[:, k, t0:t0 + n], start=(k == 0), stop=(k == 7))
                        fw.op("act", "copy", [bpz], [bzraw], out=zraw[:, t0:t0 + n], in_=pz[:, 0:n])
                    fw.op("act", "activation", [bzraw, bswT, bsbT], [bdst], out=dst[:], in_=zraw[:], func=AF.Identity, scale=swT[:, cidx, 1:2], bias=sbT[:, cidx:cidx + 1])
                    for (s, e) in segs:
                        fw.op("dve", "scalar_tensor_tensor", [bzraw, bswT, bdst], [bdst], out=dst[:, s + 1:e], in0=zraw[:, s:e - 1], scalar=swT[:, cidx, 0:1], in1=dst[:, s + 1:e], op0=ALU.mult, op1=ALU.add)
                        fw.op("dve", "scalar_tensor_tensor", [bzraw, bswT, bdst], [bdst], out=dst[:, s:e - 1], in0=zraw[:, s + 1:e], scalar=swT[:, cidx, 2:3], in1=dst[:, s:e - 1], op0=ALU.mult, op1=ALU.add)
                convs = [(CTX, 32, 16, self.GF, self.bGF, 4352, Upad, bUpad)]
                if do_ctx:
                    convs.append((0, 2, 1, self.GFc, self.bGFc, 512, Upc, bUpc))
                for (tok0, nblk, nd, GFt, bGFt, LEN, Up, bUp) in convs:
                    nm = 2 * nd + 1
                    for g0 in range(0, nblk, 8):
                        gn = min(8, nblk - g0)
                        pb_, bpb = self.pb()
                        for b in range(gn):
                            c0 = tok0 + (g0 + b) * 128
                            fw.op("pe", "transpose", [byvb, self.b_identb], [bpb], out=pb_[:, b * 128:(b + 1) * 128], in_=yvb[:, c0:c0 + 128], identity=self.identb[:])
                        fw.op("act", "copy", [bpb], [bUp], out=Up[:, g0:g0 + gn, :], in_=pb_[:, 0:gn * 128].rearrange("p (b c) -> p b c", c=128))
                    for cg in range(8):
                        pc, bpc = self.pf()
                        for c16 in range(16):
                            cc = cg * 16 + c16
                            TT, bTT = TT_[tti % 2]
                            tti += 1
                            row = ct * 128 + cc
                            fw.dma([bGFt[ct]], [bTT], out=TT[:, 0:nm * 128], in_=bass.AP(GFt, row * LEN, [[1, 128], [1, nm * 128]]))
                            morder = [nd] + [m for m in range(nm) if m != nd]
                            for mi, m in enumerate(morder):
                                bo0 = max(0, nd - m)
                                bo1 = min(nblk, nblk + nd - m)
                                fw.op("pe", "matmul", [bTT, bUp], [bpc], out=pc[:, c16 * nblk + bo0:c16 * nblk + bo1], lhsT=TT[:, m * 128:(m + 1) * 128],
                                      rhs=Up[:, m + bo0 - nd:m + bo1 - nd, cc], start=(mi == 0), stop=(mi == nm - 1))
                        fw.op("act", "copy", [bpc], [byrev], out=yrev[:, 0:nblk, cg * 16:(cg + 1) * 16].rearrange("p b c -> p c b"),
                              in_=pc[:, 0:16 * nblk].rearrange("p (c b) -> p c b", b=nblk))
                    for g0 in range(0, nblk, 4):
                        gn = min(4, nblk - g0)
                        pt_, bpt = self.pf()
                        for b in range(gn):
                            fw.op("pe", "matmul", [byrev, self.b_antib], [bpt], out=pt_[:, b * 128:(b + 1) * 128], lhsT=yrev[:, g0 + b, :], rhs=self.antib[:], start=True, stop=True)
                        cs = slice(tok0 + g0 * 128, tok0 + (g0 + gn) * 128)
                        W_ = gn * 128
                        fw.op("dve", "scalar_tensor_tensor", [bA, bbiasT, bpt], [btmp], out=tmp[:, 0:W_], in0=A[:, cs], scalar=biasT[:, ct:ct + 1], in1=pt_[:, 0:W_], op0=ALU.mult, op1=ALU.add)
                        yout, byout = yout_[(g0 // 4) % 2]
                        fw.op("pool", "tensor_tensor", [btmp, bB], [byout], out=yout[:, 0:W_], in0=tmp[:, 0:W_], in1=B[:, cs], op=ALU.mult)
                        fw.dma([byout], [self.bYT[8 + ct]], out=self.YT[1024 + ct * 128:1024 + (ct + 1) * 128, cs], in_=yout[:, 0:W_], stream="d2")
                if not do_ctx:
                    yout, byout = yout_[0]
                    fw.op("pool", "memset", [], [byout], ap=yout[:, 0:CTX], constant=0.0)
                    fw.dma([byout], [self.bYT[8 + ct]], out=self.YT[1024 + ct * 128:1024 + (ct + 1) * 128, 0:CTX], in_=yout[:, 0:CTX], stream="d2")
            if self.debug:
                self.dump_yt("yhy%d" % l, 8, 4)
            fw.flush()

    def phase_merge(self, l, hT, bhT):
        fw, I = self.fw, self.I
        last = (l == DEPTH - 1)
        self.MTs = getattr(self, "MTs", None) or self.scratch("MTs", [D, T], BF16)
        bMTs = [Buf() for _ in TG]
        groups = [(gi, t0, n) for gi, (t0, n) in enumerate(TG) if not (last and gi == 0)]
        groups1 = [(gi, t0 + h_ * 256, 256) for (gi, t0, n) in groups for h_ in range(n // 256)]
        with ExitStack() as es:
            wbr, bwbr = self.sb(es, "mgwbr", [128, 12, D], BF16)
            wg, bwg = self.sb(es, "mgwg", [128, 8, 3072], BF16)
            fw.dma([], [bwbr], q="pool", stream="d1", out=wbr[:], in_=I["w_branch"][l].rearrange("(k p) d -> p k d", p=128))
            for nb in range(3):
                self.load_w_cols(wg[:, :, nb * 1024:(nb + 1) * 1024], bwg, l, 4096 + nb * 1024, 1024)
            yt_ = [self.sb(es, f"mgyt{i}", [128, 12, 256], BF16) for i in range(1)]
            macc, bmacc = self.sb(es, "mgmacc", [128, 8, 256], F32)
            mT, bmT = self.sb(es, "mgmT", [128, 8, 256], BF16)
            sg_ = [self.sb(es, f"mgsg{i}", [128, 256], F32) for i in range(2)]
            tp_ = [self.sb(es, f"mgtp{i}", [128, 256], F32) for i in range(2)]
            it = 0
            for (gi, t0, n) in groups1:
                hb = [bhT[t] for t in self.tiles_of(t0, n)]
                yt, byt = yt_[0]
                fw.dma(self.bYT, [byt], out=yt[:, :, 0:n], in_=self.YT[:, t0:t0 + n].rearrange("(k p) t -> p k t", p=128))
                for nb in range(3):
                    for dc in range(8):
                        sg, bsg = sg_[it % 2]; tp, btp = tp_[it % 2]
                        it += 1
                        pg, bpg = self.pf()
                        for k in range(8):
                            fw.op("pe", "matmul", hb + [bwg], [bpg], out=pg[:, 0:n], lhsT=wg[:, k, nb * 1024 + dc * 128:nb * 1024 + (dc + 1) * 128], rhs=hT[:, k, t0:t0 + n], start=(k == 0), stop=(k == 7))
                        pbn, bpbn = self.pf()
                        for k in range(4):
                            fw.op("pe", "matmul", [byt, bwbr], [bpbn], out=pbn[:, 0:n], lhsT=wbr[:, nb * 4 + k, dc * 128:(dc + 1) * 128], rhs=yt[:, nb * 4 + k, 0:n], start=(k == 0), stop=(k == 3))
                        fw.op("act", "activation", [bpg], [bsg], out=sg[:, 0:n], in_=pg[:, 0:n], func=AF.Sigmoid)
                        if nb == 0:
                            fw.op("dve", "tensor_tensor", [bpbn, bsg], [bmacc], out=macc[:, dc, 0:n], in0=pbn[:, 0:n], in1=sg[:, 0:n], op=ALU.mult)
                        else:
                            fw.op("dve", "tensor_tensor", [bpbn, bsg], [btp], out=tp[:, 0:n], in0=pbn[:, 0:n], in1=sg[:, 0:n], op=ALU.mult)
                            fw.op("pool", "tensor_tensor", [btp, bmacc], [bmacc], out=macc[:, dc, 0:n], in0=tp[:, 0:n], in1=macc[:, dc, 0:n], op=ALU.add)
                fw.op("act", "copy", [bmacc], [bmT], out=mT[:, :, 0:n], in_=macc[:, :, 0:n])
                fw.dma([bmT], [bMTs[gi]], out=self.MTs[:, t0:t0 + n].rearrange("(k p) t -> p k t", p=128), in_=mT[:, :, 0:n], stream="d2")
            fw.flush()
        with ExitStack() as es:
            wout, bwout = self.sb(es, "mgwout", [128, 8, D], BF16)
            fw.dma([], [bwout], q="pool", stream="d1", out=wout[:], in_=I["w_out"][l].rearrange("(k p) d -> p k d", p=128))
            wr, bwr = self.sb(es, "mgwr", [128, 8, 36], F32)
            fw.dma([], [bwr], out=wr[:], in_=I["moe_wr"][l].rearrange("(k p) c -> p k c", p=128))
            brB, bbrB = self.sb(es, "mgbrB", [128, 36], F32)
            fw.dma([], [bbrB], out=brB[:], in_=I["moe_br"][l:l + 1, :].to_broadcast([128, 36]))
            n2g, bn2g = self.sb(es, "mgn2g", [128, D], F32)
            G1t, bG1t = self.sb(es, "mgG1t", [128, 2, D], F32)
            fw.dma([self.b_GBd], [bG1t], out=G1t[:], in_=self.GBd[:, 0:2, :])
            fw.dma([], [bn2g], out=n2g[:], in_=I["norm2_g"][l:l + 1, :].to_broadcast([128, D]))
            mt_ = [self.sb(es, f"mgmt{i}", [128, 8, 512], BF16) for i in range(2)]
            x_ = [self.sb(es, f"mgx{i}", [128, D], F32) for i in range(2)]
            xn_ = [self.sb(es, f"mgxn{i}", [128, D], F32) for i in range(2)]
            xs2, bxs2 = self.sb(es, "mgxs2", [128, D], F32)
            tmpx, btmpx = self.sb(es, "mgtmpx", [128, D], F32)
            st_ = [self.sb(es, f"mgst{i}", [128, 2, 6], F32) for i in range(2)]
            mv_ = [self.sb(es, f"mgmv{i}", [128, 4], F32) for i in range(2)]
            rs_ = [self.sb(es, f"mgrs{i}", [128, 1], F32) for i in range(2)]
            h2f, bh2f = self.sb(es, "mgh2f", [128, 8, 128], F32)
            h2tok_ = [self.sb(es, f"mgh2t{i}", [128, D], BF16) for i in range(2)]
            for ii, (gi, t0, n) in enumerate(groups):
                w = 1 if gi == 0 else 0
                mt, bmt = mt_[ii % 2]
                fw.dma([bMTs[gi]], [bmt], out=mt[:, :, 0:n], in_=self.MTs[:, t0:t0 + n].rearrange("(k p) t -> p k t", p=128))
                for t in self.tiles_of(t0, n):
                    tt = t - t0 // 128
                    x, bx = x_[t % 2]; xn, bxn = xn_[t % 2]; st, bst = st_[t % 2]; mv, bmv = mv_[t % 2]; rs, brs = rs_[t % 2]
                    h2tok, bh2tok = h2tok_[t % 2]
                    fw.dma([self.bXR[t]], [bx], out=x[:], in_=self.XR[t * 128:(t + 1) * 128, :])
                    for half in range(2):
                        hs = slice(half * 512, (half + 1) * 512)
                        pm, bpm = self.pf()
                        for k in range(8):
                            fw.op("pe", "matmul", [bmt, bwout], [bpm], out=pm[:], lhsT=mt[:, k, tt * 128:(tt + 1) * 128], rhs=wout[:, k, hs], start=(k == 0), stop=(k == 7))
                        fw.op("dve", "tensor_tensor", [bpm, bG1t], [btmpx], out=tmpx[:, hs], in0=pm[:], in1=G1t[:, w, hs], op=ALU.mult)
                        fw.op("pool", "tensor_tensor", [btmpx, bx], [bxn], out=xn[:, hs], in0=tmpx[:, hs], in1=x[:, hs], op=ALU.add)
                    fw.dma([bxn], [self.bXR[t]], out=self.XR[t * 128:(t + 1) * 128, :], in_=xn[:], stream="d2")
                    self.rms_stats(xn, bxn, st, bst, mv, bmv, rs, brs)
                    fw.op("dve", "scalar_tensor_tensor", [bxn, brs, bn2g], [bxs2], out=xs2[:], in0=xn[:], scalar=rs[:, 0:1], in1=n2g[:], op0=ALU.mult, op1=ALU.mult)
                    for hf in range(2):
                        pt_, bpt = self.pf()
                        for kk in range(4):
                            k = hf * 4 + kk
                            fw.op("pe", "transpose", [bxs2, self.b_identf], [bpt], out=pt_[:, kk * 128:(kk + 1) * 128], in_=xs2[:, k * 128:(k + 1) * 128], identity=self.identf[:])
                        for kk in range(4):
                            k = hf * 4 + kk
                            fw.op("act", "activation", [bpt, self.b_modT, self.b_modP], [bh2f], out=h2f[:, k, :], in_=pt_[:, kk * 128:(kk + 1) * 128],
                                  func=AF.Identity, scale=self.modP[:, 32 + k, w:w + 1], bias=self.modT[:, 24 + k, w:w + 1])
                    fw.op("dve", "tensor_copy", [bh2f], [bhT[t]], out=hT[:, :, t * 128:(t + 1) * 128], in_=h2f[:])
                    pl, bpl = self.pf()
                    for k in range(8):
                        fw.op("pe", "matmul", [bh2f, bwr], [bpl], out=pl[:, 0:36], lhsT=h2f[:, k, :], rhs=wr[:, k, :], start=(k == 0), stop=(k == 7))
                    fw.op("dve", "tensor_tensor", [bpl, bbrB], [self.b_LG[t]], out=self.LG[:, t, :], in0=pl[:, 0:36], in1=brB[:], op=ALU.add)
                    pb_, bpb = self.pb()
                    for k in range(8):
                        fw.op("pe", "transpose", [bhT[t], self.b_identb], [bpb], out=pb_[:, k * 128:(k + 1) * 128], in_=hT[:, k, t * 128:(t + 1) * 128], identity=self.identb[:])
                    fw.op("act", "copy", [bpb], [bh2tok], out=h2tok[:], in_=pb_[:])
                    fw.dma([bh2tok], [self.bH2[t]], out=self.H2[t * 128:(t + 1) * 128, :], in_=h2tok[:], stream="d2")
            self.dump(f"LG{l}", self.LG[:], [128, NT, 36], self.b_LG)
            fw.flush()

    def phase_moe(self, l):
        fw, I = self.fw, self.I
        last = (l == DEPTH - 1)
        tiles = list(range(2, NT)) if last else list(range(NT))
        NB = 2 * len(tiles) + 32
        IOA = bass.IndirectOffsetOnAxis
        with ExitStack() as es:
            M1, bM1 = self.sb(es, "moM1", [128, NT, 32], F32)
            M2, bM2 = self.sb(es, "moM2", [128, NT, 32], F32)
            MS, bMS = self.sb(es, "moMS", [128, NT, 32], BF16)
            W12, bW12 = self.sb(es, "moW12", [128, NT, 2], F32)
            RK, bRK = self.sb(es, "moRK", [128, NT, 32], F32)
            DST, bDST = self.sb(es, "moDST", [128, NT, 2], F32)
            DSTi, bDSTi = self.sb(es, "moDSTi", [128, NT, 2], I32)
            sc, bsc = self.sb(es, "mosc", [128, 64], F32)
            ig, big = self.sb(es, "moig", [128, 8], F32)
            mx8, bmx8 = self.sb(es, "momx8", [128, 8], F32)
            E12, bE12 = self.sb(es, "moE12", [128, 2, 8], F32)
            trisb, btrisb = self.sb(es, "motris", [128, 128], BF16)
            onesb, bonesb = self.sb(es, "moones", [128, 128], BF16)
            trf, btrf = self.sb(es, "motrf", [128, 128], F32)
            fw.dma([], [btrf], out=trf[:], in_=I["tris"].ap())
            fw.op("dve", "tensor_copy", [btrf], [btrisb], out=trisb[:], in_=trf[:])
            fw.op("pool", "memset", [], [bonesb], ap=onesb[:], constant=1.0)
            run, brun = self.sb(es, "morun", [128, 32], F32)
            fw.op("pool", "memset", [], [brun], ap=run[:], constant=0.0)
            G = sc[:, 0:4]; EG = sc[:, 4:8]
            gmax = sc[:, 8:9]; ngmax = sc[:, 9:10]; sgm = sc[:, 10:11]; ptop = sc[:, 11:12]
            nm1 = sc[:, 12:13]; e21 = sc[:, 13:14]; w1c = sc[:, 14:15]; w2c = sc[:, 15:16]
            S = [bsc]
            for t in tiles:
                Lg = self.LG[:, t, 0:4]
                Le = self.LG[:, t, 4:36]
                bl = self.b_LG[t]
                fw.op("dve", "tensor_reduce", [bl], S, out=gmax, in_=Lg, axis=AX.X, op=ALU.max)
                fw.op("dve", "tensor_scalar", [bl] + S, S, out=G, in0=Lg, scalar1=gmax, scalar2=None, op0=ALU.is_equal)
                fw.op("dve", "tensor_scalar", S, S, out=ngmax, in0=gmax, scalar1=-1.0, scalar2=None, op0=ALU.mult)
                fw.op("act", "activation", [bl] + S, S, out=EG, in_=Lg, func=AF.Exp, bias=ngmax, scale=1.0)
                fw.op("dve", "tensor_reduce", S, S, out=sgm, in_=EG, axis=AX.X, op=ALU.add)
                fw.op("dve", "reciprocal", S, S, out=ptop, in_=sgm)
                fw.op("dve", "tensor_scalar", [bl] + S, [big], out=ig[:], in0=Le[:, 0:8], scalar1=sc[:, 0:1], scalar2=None, op0=ALU.mult)
                for g in range(1, 4):
                    fw.op("dve", "scalar_tensor_tensor", [bl, big] + S, [big], out=ig[:], in0=Le[:, 8 * g:8 * g + 8], scalar=sc[:, g:g + 1], in1=ig[:], op0=ALU.mult, op1=ALU.add)
                fw.op("dve", "max", [big], [bmx8], out=mx8[:], in_=ig[:])
                fw.op("dve", "tensor_scalar", [big, bmx8], [bE12], out=E12[:, 0, :], in0=ig[:], scalar1=mx8[:, 0:1], scalar2=None, op0=ALU.is_equal)
                fw.op("dve", "tensor_scalar", [big, bmx8], [bE12], out=E12[:, 1, :], in0=ig[:], scalar1=mx8[:, 1:2], scalar2=None, op0=ALU.is_equal)
                fw.op("dve", "tensor_scalar", [bmx8] + S, S, out=nm1, in0=mx8[:, 0:1], scalar1=-1.0, scalar2=None, op0=ALU.mult)
                fw.op("act", "activation", [bmx8] + S, S, out=e21, in_=mx8[:, 1:2], func=AF.Exp, bias=nm1, scale=1.0)
                fw.op("dve", "tensor_scalar", S, S, out=w1c, in0=e21, scalar1=1.0, scalar2=None, op0=ALU.add)
                fw.op("dve", "reciprocal", S, S, out=w1c, in_=w1c)
                fw.op("dve", "tensor_tensor", S, S, out=w2c, in0=e21, in1=w1c, op=ALU.mult)
                fw.op("dve", "tensor_scalar", S, [bW12], out=W12[:, t, :], in0=sc[:, 14:16], scalar1=ptop, scalar2=None, op0=ALU.mult)
                for g in range(4):
                    fw.op("dve", "tensor_scalar", [bE12] + S, [bM1], out=M1[:, t, 8 * g:8 * g + 8], in0=E12[:, 0, :], scalar1=sc[:, g:g + 1], scalar2=None, op0=ALU.mult)
                    fw.op("dve", "tensor_scalar", [bE12] + S, [bM2], out=M2[:, t, 8 * g:8 * g + 8], in0=E12[:, 1, :], scalar1=sc[:, g:g + 1], scalar2=None, op0=ALU.mult)
                fw.op("dve", "tensor_tensor", [bM1, bM2], [bMS], out=MS[:, t, :], in0=M1[:, t, :], in1=M2[:, t, :], op=ALU.add)
                pr, bpr = self.pf()
                fw.op("pe", "matmul", [btrisb, bMS], [bpr], out=pr[:, 0:32], lhsT=trisb[:], rhs=MS[:, t, :], start=True, stop=True)
                fw.op("pe", "matmul", [bonesb, bMS], [bpr], out=pr[:, 32:64], lhsT=onesb[:], rhs=MS[:, t, :], start=True, stop=True)
                fw.op("dve", "tensor_tensor", [bpr, brun], [bRK], out=RK[:, t, :], in0=pr[:, 0:32], in1=run[:], op=ALU.add)
                fw.op("dve", "tensor_tensor", [bpr, brun], [brun], out=run[:], in0=pr[:, 32:64], in1=run[:], op=ALU.add)
            nbi, bnbi = self.sb(es, "monbi", [128, 32], I32)
            nbf, bnbf = self.sb(es, "monbf", [128, 32], F32)
            one32, bone32 = self.sb(es, "moone32", [128, 32], F32)
            pend, bpend = self.sb(es, "mopend", [128, 32], F32)
            pst, bpst = self.sb(es, "mopst", [128, 32], F32)
            fw.op("pool", "memset", [], [bone32], ap=one32[:], constant=1.0)
            fw.op("dve", "tensor_scalar", [brun], [bnbi], out=nbi[:], in0=run[:], scalar1=1.0 / 128.0, scalar2=0.49609375, op0=ALU.mult, op1=ALU.add)
            fw.op("dve", "tensor_copy", [bnbi], [bnbf], out=nbf[:], in_=nbi[:])
            fw.op("dve", "tensor_tensor_scan", [bone32, bnbf], [bpend], out=pend[:], data0=one32[:], data1=nbf[:], initial=0.0, op0=ALU.mult, op1=ALU.add)
            fw.op("dve", "tensor_tensor", [bpend, bnbf], [bpst], out=pst[:], in0=pend[:], in1=nbf[:], op=ALU.subtract)
            fw.op("dve", "tensor_scalar", [bpst], [bpst], out=pst[:], in0=pst[:], scalar1=128.0, scalar2=None, op0=ALU.mult)
            tmp32, btmp32 = self.sb(es, "motmp32", [128, 32], F32)
            for t in tiles:
                fw.op("dve", "tensor_tensor", [bRK, bpst], [btmp32], out=tmp32[:], in0=RK[:, t, :], in1=pst[:], op=ALU.add)
                fw.op("dve", "tensor_tensor", [btmp32, bM1], [bRK], out=RK[:, t, :], in0=tmp32[:], in1=M1[:, t, :], op=ALU.mult)
                fw.op("dve", "tensor_reduce", [bRK], [bDST], out=DST[:, t, 0:1], in_=RK[:, t, :], axis=AX.X, op=ALU.add)
                fw.op("dve", "tensor_tensor", [btmp32, bM2], [bRK], out=RK[:, t, :], in0=tmp32[:], in1=M2[:, t, :], op=ALU.mult)
                fw.op("dve", "tensor_reduce", [bRK], [bDST], out=DST[:, t, 1:2], in_=RK[:, t, :], axis=AX.X, op=ALU.add)
            fw.op("dve", "tensor_copy", [bDST], [bDSTi], out=DSTi[:, tiles[0]:NT, :], in_=DST[:, tiles[0]:NT, :])
            iob, biob = self.sb(es, "moiob", [128, NBLK], F32)
            fw.dma([], [biob], out=iob[:], in_=I["iotab"].ap())
            cmp_, bcmp = self.sb(es, "mocmp", [128, NBLK, 32], F32)
            be, bbe = self.sb(es, "mobe", [128, NBLK], F32)
            fw.op("dve", "tensor_tensor", [biob, bpend], [bcmp], out=cmp_[:, 0:NB, :], in0=iob[:, 0:NB].unsqueeze(2).to_broadcast([128, NB, 32]),
                  in1=pend[:].unsqueeze(1).to_broadcast([128, NB, 32]), op=ALU.is_ge)
            fw.op("dve", "tensor_reduce", [bcmp], [bbe], out=be[:, 0:NB], in_=cmp_[:, 0:NB, :], axis=AX.X, op=ALU.add)
            fw.op("dve", "tensor_scalar", [bbe], [bbe], out=be[:, 0:NB], in0=be[:, 0:NB], scalar1=31.0, scalar2=None, op0=ALU.min)
            b1f, bb1f = self.sb(es, "mob1f", [128, NBLK], F32)
            b2f, bb2f = self.sb(es, "mob2f", [128, NBLK], F32)
            wi1, bwi1 = self.sb(es, "mowi1", [128, NBLK, 8], I32)
            wi2, bwi2 = self.sb(es, "mowi2", [128, NBLK, 4], I32)
            fw.op("dve", "tensor_scalar", [bbe], [bb1f], out=b1f[:, 0:NB], in0=be[:, 0:NB], scalar1=1024.0, scalar2=None, op0=ALU.mult)
            fw.op("dve", "tensor_scalar", [bb1f, self.b_rcol], [bb1f], out=b1f[:, 0:NB], in0=b1f[:, 0:NB], scalar1=self.rcol[:, 3:4], scalar2=None, op0=ALU.add)
            fw.op("dve", "tensor_scalar", [bbe], [bb2f], out=b2f[:, 0:NB], in0=be[:, 0:NB], scalar1=512.0, scalar2=None, op0=ALU.mult)
            fw.op("dve", "tensor_scalar", [bb2f, self.b_rcol], [bb2f], out=b2f[:, 0:NB], in0=b2f[:, 0:NB], scalar1=self.rcol[:, 3:4], scalar2=None, op0=ALU.add)
            for k in range(8):
                fw.op("dve", "tensor_scalar", [bb1f], [bwi1], out=wi1[:, 0:NB, k], in0=b1f[:, 0:NB], scalar1=128.0 * k + l * 32768.0, scalar2=None, op0=ALU.add)
            for k in range(4):
                fw.op("dve", "tensor_scalar", [bb2f], [bwi2], out=wi2[:, 0:NB, k], in0=b2f[:, 0:NB], scalar1=128.0 * k + l * 16384.0, scalar2=None, op0=ALU.add)
            self.dump(f"DST{l}", DST[:], [128, NT, 2], [bDST])
            self.dump(f"W12{l}", W12[:], [128, NT, 2], [bW12])
            self.dump(f"BE{l}", be[:], [128, NBLK], [bbe])
            hx_ = [self.sb(es, f"mohx{i}", [128, D], BF16) for i in range(2)]
            zt_, bzt_ = self.sb(es, "mozero", [128, 4, D], BF16)
            fw.op("pool", "memset", [], [bzt_], ap=zt_[:], constant=0.0)
            for b4 in range(0, NB, 4):
                nb4 = min(4, NB - b4)
                fw.dma([bzt_], [self.bMB], out=self.MB[b4 * 128:(b4 + nb4) * 128, :].rearrange("(b p) d -> p b d", p=128), in_=zt_[:, 0:nb4, :], stream="d2")
            for t in tiles:
                hx, bhx = hx_[t % 2]
                fw.dma([self.bH2[t]], [bhx], out=hx[:], in_=self.H2[t * 128:(t + 1) * 128, :])
                for k in range(2):
                    fw.dmaf(lambda e, hx=hx, t=t, k=k: e.indirect_dma_start(out=self.MB[:, :], out_offset=IOA(ap=DSTi[:, t, k:k + 1], axis=0), in_=hx[:, :], in_offset=None),
                            [bhx, bDSTi], [self.bMB])
            X_ = [self.sb(es, f"moX{i}", [128, D], BF16) for i in range(2)]
            XT_ = [self.sb(es, f"moXT{i}", [128, 8, 128], BF16) for i in range(2)]
            w1g_ = [self.sb(es, f"mow1g{i}", [128, 8, 512], BF16) for i in range(2)]
            w3g_ = [self.sb(es, f"mow3g{i}", [128, 8, 512], BF16) for i in range(2)]
            w2g_ = [self.sb(es, f"mow2g{i}", [128, 4, D], BF16) for i in range(2)]
            sl_ = [self.sb(es, f"mosl{i}", [128, 512], F32) for i in range(2)]
            gT_ = [self.sb(es, f"mogT{i}", [128, 512], BF16) for i in range(2)]
            yb_ = [self.sb(es, f"moyb{i}", [128, D], F32) for i in range(2)]
            w1d, w3d, w2d = I["moe_w1"], I["moe_w3"], I["moe_w2"]
            for blk in range(NB):
                X, bX = X_[blk % 2]; XT, bXT = XT_[blk % 2]
                w1g, bw1g = w1g_[blk % 2]; w3g, bw3g = w3g_[blk % 2]; w2g, bw2g = w2g_[blk % 2]
                sl, bsl = sl_[blk % 2]; gT, bgT = gT_[blk % 2]; yb, byb = yb_[blk % 2]
                fw.dma([self.bMB], [bX], out=X[:], in_=self.MB[blk * 128:(blk + 1) * 128, :])
                for k in range(8):
                    fw.dmaf(lambda e, w1g=w1g, k=k, blk=blk: e.indirect_dma_start(out=w1g[:, k, :], out_offset=None, in_=w1d[:, :], in_offset=IOA(ap=wi1[:, blk, k:k + 1], axis=0)),
                            [bwi1], [bw1g])
                    fw.dmaf(lambda e, w3g=w3g, k=k, blk=blk: e.indirect_dma_start(out=w3g[:, k, :], out_offset=None, in_=w3d[:, :], in_offset=IOA(ap=wi1[:, blk, k:k + 1], axis=0)),
                            [bwi1], [bw3g])
                for k in range(4):
                    fw.dmaf(lambda e, w2g=w2g, k=k, blk=blk: e.indirect_dma_start(out=w2g[:, k, :], out_offset=None, in_=w2d[:, :], in_offset=IOA(ap=wi2[:, blk, k:k + 1], axis=0)),
                            [bwi2], [bw2g])
                pb_, bpb = self.pb()
                for k in range(8):
                    fw.op("pe", "transpose", [bX, self.b_identb], [bpb], out=pb_[:, k * 128:(k + 1) * 128], in_=X[:, k * 128:(k + 1) * 128], identity=self.identb[:])
                fw.op("act", "copy", [bpb], [bXT], out=XT[:].rearrange("p k t -> p (k t)"), in_=pb_[:])
                p1, bp1 = self.pf()
                p3, bp3 = self.pf()
                for hc in range(4):
                    for k in range(8):
                        fw.op("pe", "matmul", [bw1g, bXT], [bp1], out=p1[:, hc * 128:(hc + 1) * 128], lhsT=w1g[:, k, hc * 128:(hc + 1) * 128], rhs=XT[:, k, :], start=(k == 0), stop=(k == 7))
                for hc in range(4):
                    for k in range(8):
                        fw.op("pe", "matmul", [bw3g, bXT], [bp3], out=p3[:, hc * 128:(hc + 1) * 128], lhsT=w3g[:, k, hc * 128:(hc + 1) * 128], rhs=XT[:, k, :], start=(k == 0), stop=(k == 7))
                fw.op("act", "activation", [bp1], [bsl], out=sl[:], in_=p1[:], func=AF.Silu)
                fw.op("dve", "tensor_tensor", [bp3, bsl], [bgT], out=gT[:], in0=p3[:], in1=sl[:], op=ALU.mult)
                for half in range(2):
                    py, bpy = self.pf()
                    for hc in range(4):
                        fw.op("pe", "matmul", [bgT, bw2g], [bpy], out=py[:], lhsT=gT[:, hc * 128:(hc + 1) * 128], rhs=w2g[:, hc, half * 512:(half + 1) * 512], start=(hc == 0), stop=(hc == 3))
                    fw.op("act", "copy", [bpy], [byb], out=yb[:, half * 512:(half + 1) * 512], in_=py[:])
                fw.dma([byb], [self.bMY], out=self.MY[blk * 128:(blk + 1) * 128, :], in_=yb[:], stream="d2")
            y1_ = [self.sb(es, f"moy1{i}", [128, D], F32) for i in range(2)]
            G2t, bG2t = self.sb(es, "moG2t", [128, 2, D], F32)
            fw.dma([self.b_GBd], [bG2t], out=G2t[:], in_=self.GBd[:, 2:4, :])
            y2_ = [self.sb(es, f"moy2{i}", [128, D], F32) for i in range(2)]
            xx_ = [self.sb(es, f"moxx{i}", [128, D], F32) for i in range(2)]
            st_ = [self.sb(es, f"most{i}", [128, 2, 6], F32) for i in range(2)]
            mv_ = [self.sb(es, f"momv{i}", [128, 4], F32) for i in range(2)]
            rs_ = [self.sb(es, f"mors{i}", [128, 1], F32) for i in range(2)]
            if last:
                fng, bfng = self.sb(es, "mofng", [128, D], F32)
                fw.dma([], [bfng], out=fng[:], in_=I["final_norm_g"][0:1, :].to_broadcast([128, D]))
            for t in tiles:
                w = 1 if t < 2 else 0
                y1, by1 = y1_[t % 2]; y2, by2 = y2_[t % 2]; xx, bxx = xx_[t % 2]
                fw.dmaf(lambda e, y1=y1, t=t: e.indirect_dma_start(out=y1[:, :], out_offset=None, in_=self.MY[:, :], in_offset=IOA(ap=DSTi[:, t, 0:1], axis=0)), [self.bMY, bDSTi], [by1])
                fw.dmaf(lambda e, y2=y2, t=t: e.indirect_dma_start(out=y2[:, :], out_offset=None, in_=self.MY[:, :], in_offset=IOA(ap=DSTi[:, t, 1:2], axis=0)), [self.bMY, bDSTi], [by2])
                fw.dma([self.bXR[t]], [bxx], out=xx[:], in_=self.XR[t * 128:(t + 1) * 128, :])
                fw.op("dve", "tensor_scalar", [by1, bW12], [by1], out=y1[:], in0=y1[:], scalar1=W12[:, t, 0:1], scalar2=None, op0=ALU.mult)
                fw.op("dve", "scalar_tensor_tensor", [by2, bW12, by1], [by1], out=y1[:], in0=y2[:], scalar=W12[:, t, 1:2], in1=y1[:], op0=ALU.mult, op1=ALU.add)
                fw.op("pool", "tensor_tensor", [by1, bG2t], [by1], out=y1[:], in0=y1[:], in1=G2t[:, w, :], op=ALU.mult)
                fw.op("pool", "tensor_tensor", [by1, bxx], [bxx], out=xx[:], in0=y1[:], in1=xx[:], op=ALU.add)
                if not last:
                    fw.dma([bxx], [self.bXR[t]], out=self.XR[t * 128:(t + 1) * 128, :], in_=xx[:], stream="d2")
                else:
                    st, bst = st_[t % 2]; mv, bmv = mv_[t % 2]; rs, brs = rs_[t % 2]
                    self.rms_stats(xx, bxx, st, bst, mv, bmv, rs, brs)
                    fw.op("dve", "scalar_tensor_tensor", [bxx, brs, bfng], [by2], out=y2[:], in0=xx[:], scalar=rs[:, 0:1], in1=fng[:], op0=ALU.mult, op1=ALU.mult)
                    fw.dma([by2], [self.bout], out=self.out[(t - 2) * 128:(t - 1) * 128, :], in_=y2[:], stream="d2")
            fw.flush()

    def phase_final(self):
        pass


def prep_inputs(inp, b, keys=None):
    f = np.float32
    d = {}
    d.update(_consts())
    d["x"] = np.ascontiguousarray(inp["x"][b])
    d["ctx"] = np.ascontiguousarray(inp["ctx"][b])
    cc = np.stack([inp["c"][b], inp["c_ctx"]], -1)
    d["cT"] = np.ascontiguousarray(cc.reshape(8, 128, 2).transpose(1, 0, 2))
    d["w_mod"] = inp["w_mod"]
    d["b_mod"] = inp["b_mod"]
    d["bmodT"] = np.ascontiguousarray(inp["b_mod"].reshape(DEPTH, 48, 128).transpose(0, 2, 1))
    selw = np.zeros((2, 2, 128), f)
    selw[0, 0] = 1.0
    selw[1, 1] = 1.0
    d["selw"] = selw
    d["norm1_g"] = inp["norm1_g"]
    d["norm2_g"] = inp["norm2_g"]
    d["final_norm_g"] = inp["final_norm_g"].reshape(1, D)
    d["w_in"] = inp["w_in"]
    d["ret_log_decay"] = inp["ret_log_decay"].reshape(DEPTH, 8)
    d["ret_gn_g"] = inp["ret_gn_g"]
    def pairT(a):
        return np.ascontiguousarray(a.reshape(DEPTH, 2, 16, 128).transpose(0, 1, 3, 2))
    d["s5_lre"] = pairT(inp["s5_lambda_re"])
    d["s5_lim"] = pairT(inp["s5_lambda_im"])
    d["s5_ldt"] = pairT(np.repeat(inp["s5_log_dt"][..., None], 64, -1))
    sB = np.zeros((DEPTH, 2, 2, 128, 4, 2, 128), f)
    sC = np.zeros((DEPTH, 2, 2, 128, 16, 64), f)
    for ri, (bk, ck) in enumerate((("s5_b_re", "s5_c_re"), ("s5_b_im", "s5_c_im"))):
        Bm = inp[bk]
        Cm = inp[ck]
        for pt in range(16):
            o, q4 = pt // 4, pt % 4
            for g2 in range(2):
                g = 2 * pt + g2
                sB[:, :, ri, q4 * 32 + g2 * 16:q4 * 32 + g2 * 16 + 16, o, q4 % 2, g2 * 64:(g2 + 1) * 64] = Bm[:, :, g].transpose(0, 1, 3, 2)
                sC[:, :, ri, g2 * 64:(g2 + 1) * 64, pt, (q4 % 2) * 32 + g2 * 16:(q4 % 2) * 32 + g2 * 16 + 16] = Cm[:, :, g].transpose(0, 1, 3, 2)
    d["s5_B"] = sB
    d["s5_C"] = sC
    d["s5_dT"] = np.ascontiguousarray(inp["s5_d"].reshape(DEPTH, 4, 128).transpose(0, 2, 1))
    d["s5_glu_w1"] = inp["s5_glu_w1"]
    d["s5_glu_w2"] = inp["s5_glu_w2"]
    d["hy_swT"] = np.ascontiguousarray(inp["hy_short_w"].transpose(0, 2, 1).reshape(DEPTH, 12, 128, 3).transpose(0, 2, 1, 3))
    d["hy_sbT"] = np.ascontiguousarray(inp["hy_short_b"].reshape(DEPTH, 12, 128).transpose(0, 2, 1))
    d["hy_biasT"] = np.ascontiguousarray(inp["hy_bias"].reshape(DEPTH, 4, 128).transpose(0, 2, 1))
    d["hy_f_w1"] = inp["hy_f_w1"]
    d["hy_f_w2"] = inp["hy_f_w2"]
    d["hy_f_w3"] = inp["hy_f_w3"]
    d["hy_f_b1"] = inp["hy_f_b1"].reshape(DEPTH, 64, 1)
    d["hy_f_b2"] = inp["hy_f_b2"].reshape(DEPTH, 64, 1)
    d["hy_f_fr"] = np.ascontiguousarray(inp["hy_f_freq"].transpose(0, 2, 1))
    d["w_branch"] = inp["w_branch"].reshape(DEPTH, 1536, D)
    d["w_out"] = inp["w_out"]
    d["moe_wr"] = np.ascontiguousarray(np.concatenate([inp["moe_w_group"], inp["moe_w_expert"]], -1))
    d["moe_br"] = np.ascontiguousarray(np.concatenate([inp["moe_b_group"], inp["moe_b_expert"]], -1))
    d["moe_w1"] = inp["moe_w1"].reshape(DEPTH * 32 * 1024, 512)
    d["moe_w3"] = inp["moe_w3"].reshape(DEPTH * 32 * 1024, 512)
    d["moe_w2"] = inp["moe_w2"].reshape(DEPTH * 32 * 512, 1024)
    if keys is not None:
        d = {k: v for k, v in d.items() if k in keys}
    return d


_NC_CACHE = {}


def run_cores(inputs, n_cores=8, debug=None, stop=None, keys=None):
    per_core = [prep_inputs(inputs, c % 4, keys) for c in range(n_cores)]
    bd = Builder(debug=debug)
    bd.stop = stop
    import os
    bd.s5_cut = int(os.environ.get("S5_CUT", "0"))
    if os.environ.get("S5_LIM"):
        bd.s5_lim = tuple(int(v) for v in os.environ["S5_LIM"].split(","))
    nc = bd.build(per_core[0])
    res = run_bass_kernel_spmd(nc, per_core, core_ids=list(range(n_cores)))
    return res, bd


def kernel(**inputs):
    inputs = {k: np.asarray(v) for k, v in inputs.items()}
    res, bd = run_cores(inputs, n_cores=4)
    out = np.stack([res.results[b]["out"] for b in range(4)], 0)
    return out.astype(np.float32)
```

```python
import math
from contextlib import ExitStack
import numpy as np
import ml_dtypes
import concourse.bass as bass
import concourse.mybir as mybir
from concourse.bass_utils import run_bass_kernel_spmd

F32 = mybir.dt.float32
BF16 = mybir.dt.bfloat16
I32 = mybir.dt.int32
ALU = mybir.AluOpType
AF = mybir.ActivationFunctionType
AX = mybir.AxisListType

D = 1024
NT = 34
T = NT * 128
CTX = 256
L = 4096
DEPTH = 2
TG = [(0, 256)] + [(256 + 512 * i, 512) for i in range(8)]
TWO_PI = 2.0 * math.pi
NBLK = 100


class Buf:
    __slots__ = ("name", "lw", "rd")

    def __init__(self, name=""):
        self.name = name
        self.lw = None
        self.rd = {}


class FW:
    ENG = ("pe", "dve", "act", "pool", "sp")
    STREAMS = {"d0": 12, "d1": 16, "d2": 8, "d3": 2}
    ENGNAME = {"pe": "tensor", "dve": "vector", "act": "scalar", "pool": "gpsimd", "sp": "sync"}

    def __init__(self, nc, sems):
        self.nc = nc
        self.sems = sems
        self.ops = {e: [] for e in self.ENG}
        self.cnt = {s: 0 for s in self.sem_names()}
        self.seen = {e: {} for e in self.ENG}
        self.n_ops = 0
        self.ndma = {s: 0 for s in self.STREAMS}

    @classmethod
    def sem_names(cls):
        r = [e for e in cls.ENG if e != "sp"]
        for s, k in cls.STREAMS.items():
            r += [f"{s}_{i}" for i in range(k)]
        return r

    def _emit(self, eng, sem, inc, fn, reads, writes, nosync_same=False, extra_wait=None):
        waits = {}

        def need(sv):
            if sv is None:
                return
            s, v = sv
            if nosync_same and s == eng:
                return
            if waits.get(s, 0) < v:
                waits[s] = v
        if extra_wait is not None:
            need(extra_wait)
        for b in reads:
            need(b.lw)
        for b in writes:
            need(b.lw)
            for s, v in b.rd.items():
                need((s, v))
        wl = []
        for s, v in waits.items():
            if self.seen[eng].get(s, 0) < v:
                self.seen[eng][s] = v
                wl.append((s, v))
        self.cnt[sem] += inc
        v = self.cnt[sem]
        for b in reads:
            if b.rd.get(sem, 0) < v:
                b.rd[sem] = v
        for b in writes:
            b.lw = (sem, v)
            b.rd = {}
        self.ops[eng].append((wl, fn, sem, inc))
        self.n_ops += 1

    def op(self, eng, name, R, W, **kw):
        self._emit(eng, eng, 1, lambda e: getattr(e, name)(**kw), R, W, nosync_same=(eng == "pe"))

    def opf(self, eng, fn, R, W):
        self._emit(eng, eng, 1, fn, R, W, nosync_same=(eng == "pe"))

    def _dma_sem(self, stream):
        i = self.ndma[stream]
        self.ndma[stream] += 1
        sem = f"{stream}_{i % self.STREAMS[stream]}"
        return sem, (sem, self.cnt[sem])

    def dma(self, R, W, q="sp", stream="d0", **kw):
        sem, ew = self._dma_sem(stream)
        self._emit(q, sem, 16, lambda e: e.dma_start(**kw), R, W, extra_wait=ew)

    def dmaf(self, fn, R, W, q="pool", stream="d1"):
        sem, ew = self._dma_sem(stream)
        self._emit(q, sem, 16, fn, R, W, extra_wait=ew)

    def flush(self):
        nc = self.nc
        final = dict(self.cnt)
        sems = self.sems
        comp = ["pe"]
        if not hasattr(self, "newcnt"):
            self.newcnt = {s: 0 for s in comp}
            self.oldbase = {s: 0 for s in comp}
            self.lastmap = {s: {0: 0} for s in comp}
        needed = {s: set() for s in comp}
        for e in self.ENG:
            for wl, fn, sem, inc in self.ops[e]:
                for s_, v in wl:
                    if s_ in needed:
                        needed[s_].add(v)
        vmap = {s: dict(self.lastmap[s]) for s in comp}
        plan = {}
        for e in comp:
            old = self.oldbase[e]
            ops = self.ops[e]
            marks = []
            for idx, (wl, fn, sem, inc) in enumerate(ops):
                if sem != e:
                    marks.append(False)
                    continue
                old += 1
                ck = (old in needed[e]) or (old == final[e])
                if ck:
                    self.newcnt[e] += 1
                    vmap[e][old] = self.newcnt[e]
                marks.append(ck)
            plan[e] = marks
            vmap[e][final[e]] = self.newcnt[e]
            self.oldbase[e] = final[e]
            self.lastmap[e] = {final[e]: self.newcnt[e]}

        def tr(s_, v):
            if s_ in vmap:
                return vmap[s_][v]
            return v
        with nc.Block() as block:
            for e in self.ENG:
                ops = self.ops[e]
                marks = plan.get(e)

                def body(engine, ops=ops, e=e, marks=marks):
                    for idx, (wl, fn, sem, inc) in enumerate(ops):
                        for s_, v in wl:
                            engine.wait_ge(sems[s_], tr(s_, v))
                        ins = fn(engine)
                        if sem in vmap:
                            if marks[idx]:
                                ins.then_inc(sems[sem], 1)
                        else:
                            ins.then_inc(sems[sem], inc)
                    for s_, v in final.items():
                        if v > 0:
                            engine.wait_ge(sems[s_], tr(s_, v))
                getattr(block, self.ENGNAME[e])(body)
        for e in self.ENG:
            self.ops[e] = []
            self.seen[e] = dict(final)


def _consts():
    c = {}
    c["ident_f"] = np.eye(128, dtype=np.float32)
    c["antiid_f"] = np.ascontiguousarray(np.eye(128, dtype=np.float32)[::-1])
    rows = L // 64
    quarter = D // 4
    omega = (1.0 / (np.float32(10000.0) ** (np.arange(quarter, dtype=np.float32) / np.float32(quarter)))).astype(np.float32)
    ang_r = np.arange(rows, dtype=np.float32)[:, None] * omega
    ang_c = np.arange(64, dtype=np.float32)[:, None] * omega
    emb_r = np.concatenate([np.sin(ang_r), np.cos(ang_r)], -1)
    emb_c = np.concatenate([np.sin(ang_c), np.cos(ang_c)], -1)
    pe = np.concatenate([np.broadcast_to(emb_r[:, None, :], (rows, 64, D // 2)),
                         np.broadcast_to(emb_c[None, :, :], (rows, 64, D // 2))], -1)
    c["pe"] = np.ascontiguousarray(pe.reshape(L, D).astype(np.float32))
    j = np.arange(128, dtype=np.float32)[:, None]
    i = np.arange(128, dtype=np.float32)[None, :]
    rt = np.zeros((128, 6, 128), np.float32)
    rt[:, 0] = np.maximum(i - j, 0)
    rt[:, 1] = np.maximum(j - i, 0)
    rt[:, 2] = (i >= j)
    rt[:, 3] = (j > i)
    rt[:, 4] = i + 1.0
    rt[:, 5] = 128.0 - i
    c["rtab"] = rt
    rc = np.zeros((128, 6), np.float32)
    rc[:, 0] = 127.0 - np.arange(128)
    rc[:, 1] = np.arange(128)
    rc[:, 2] = 128.0
    rc[:, 3] = np.arange(128)
    rc[:, 4] = np.arange(128) + 1.0
    rc[:, 5] = 128.0 - np.arange(128)
    c["rcol"] = rc
    for nm, n in (("zT", L), ("zTc", CTX)):
        t = np.linspace(0.0, 1.0, n, dtype=np.float32)[:, None]
        w = (2.0 * math.pi * np.arange(n, dtype=np.float32)[:, None] / n).astype(np.float32)
        f = np.linspace(1e-4, 15, 16, dtype=np.float32)[None]
        z = np.concatenate([t, np.cos(f * w), -np.sin(f * w)], -1).astype(np.float32)
        c[nm] = np.ascontiguousarray(z.T)
        off = (np.abs(np.arange(n) - n // 2).astype(np.float32) / n).astype(np.float32)
        c["off" + nm[2:]] = np.ascontiguousarray(np.broadcast_to(off[None], (128, n)))
    deltas = np.abs(np.linspace(math.log(1e-2) / 1.5, math.log(1e-2) / 0.3, 512, dtype=np.float32))
    c["ndelta"] = np.ascontiguousarray((-deltas).reshape(4, 128).T.astype(np.float32))
    c["iota512"] = np.ascontiguousarray(np.broadcast_to(np.arange(1, 513, dtype=np.float32)[None], (128, 512)))
    tri = (np.arange(128)[:, None] < np.arange(128)[None, :]).astype(np.float32)
    c["tris"] = tri
    c["iotab"] = np.ascontiguousarray(np.broadcast_to(np.arange(NBLK, dtype=np.float32)[None], (128, NBLK)))
    return c


class Builder:
    def __init__(self, debug=None):
        self.debug = debug or []
        self.nc = bass.Bass("TRN2", target_bir_lowering=False)
        self.din = {}
        self.dbg_out = []

    def inp(self, name, shape, dt=F32):
        t = self.nc.dram_tensor(name, list(shape), dt, kind="ExternalInput")
        self.din[name] = t
        return t

    def scratch(self, name, shape, dt):
        return self.nc.dram_tensor(name, list(shape), dt, kind="Internal")

    def build(self, in_shapes):
        nc = self.nc
        I = {k: self.inp(k, v.shape, {np.dtype("float32"): F32, np.dtype("int32"): I32}[v.dtype]) for k, v in in_shapes.items()}
        self.I = I
        self.out = nc.dram_tensor("out", [L, D], F32, kind="ExternalOutput")
        self.XR = self.scratch("XR", [T, D], F32)
        self.YT = self.scratch("YT", [1536, T], BF16)
        self.H2 = self.scratch("H2", [T, D], BF16)
        self.MB = self.scratch("MB", [NBLK * 128, D], BF16)
        self.MY = self.scratch("MY", [NBLK * 128, D], F32)
        self.GF = self.scratch("GF", [512, 4352], BF16)
        self.GFc = self.scratch("GFc", [512, 512], BF16)
        self.bXR = [Buf() for _ in range(NT)]
        self.bYT = [Buf() for _ in range(12)]
        self.bH2 = [Buf() for _ in range(NT)]
        self.bMB = Buf()
        self.bMY = Buf()
        self.bGF = [Buf() for _ in range(4)]
        self.bGFc = [Buf() for _ in range(4)]
        self.bout = Buf()
        with ExitStack() as es:
            self.sems = {s: es.enter_context(nc.semaphore(s)) for s in FW.sem_names()}
            self.fw = FW(nc, self.sems)
            with ExitStack() as gs:
                self.gs = gs
                self.alloc_globals()
                self.phase_init()
                for l in range(DEPTH):
                    self.layer(l)
                    if getattr(self, "stop", None):
                        break
                self.phase_final()
        return nc

    def sb(self, es, name, shape, dt):
        self._uid = getattr(self, "_uid", 0) + 1
        t = es.enter_context(self.nc.sbuf_tensor(f"s{self._uid}_{name}", list(shape), dt))
        assert self.nc.sbuf_bytes_remaining >= 32768 + 128, (name, self.nc.sbuf_bytes_remaining)
        return t, Buf(name)

    def alloc_globals(self):
        nc, gs, fw, I = self.nc, self.gs, self.fw, self.I
        self.PF = []
        for i in range(6):
            t = gs.enter_context(nc.psum_tensor(f"pf{i}", [128, 512], F32))
            self.PF.append((t, Buf(f"pf{i}")))
        self.PB = []
        for i in range(2):
            t = gs.enter_context(nc.psum_tensor(f"pb{i}", [128, 1024], BF16))
            self.PB.append((t, Buf(f"pb{i}")))
        self.pfi = 0
        self.pbi = 0
        self.identf, self.b_identf = self.sb(gs, "identf", [128, 128], F32)
        self.identb, self.b_identb = self.sb(gs, "identb", [128, 128], BF16)
        self.antib, self.b_antib = self.sb(gs, "antib", [128, 128], BF16)
        self.rcol, self.b_rcol = self.sb(gs, "rcol", [128, 6], F32)
        self.epsc, self.b_epsc = self.sb(gs, "epsc", [128, 4], F32)
        self.modT, self.b_modT = self.sb(gs, "modT", [128, 48, 2], F32)
        self.modP, self.b_modP = self.sb(gs, "modP", [128, 48, 2], F32)
        self.GBd = self.scratch("GBd", [128, 4, 1024], F32)
        self.b_GBd = Buf()
        self.scT, self.b_scT = self.sb(gs, "scT", [128, 8, 2], F32)
        self.LG, _ = self.sb(gs, "LG", [128, NT, 36], F32)
        self.b_LG = [Buf() for _ in range(NT)]
        fw.dma([], [self.b_identf], out=self.identf[:], in_=I["ident_f"].ap())
        fw.op("dve", "tensor_copy", [self.b_identf], [self.b_identb], out=self.identb[:], in_=self.identf[:])
        af, baf = self.sb(gs, "antif", [128, 128], F32)
        fw.dma([], [baf], out=af[:], in_=I["antiid_f"].ap())
        fw.op("dve", "tensor_copy", [baf], [self.b_antib], out=self.antib[:], in_=af[:])
        fw.dma([], [self.b_rcol], out=self.rcol[:], in_=I["rcol"].ap())
        fw.op("pool", "memset", [], [self.b_epsc], ap=self.epsc[:, 0:1], constant=1e-6)
        fw.op("pool", "memset", [self.b_epsc], [self.b_epsc], ap=self.epsc[:, 1:2], constant=1e-5)
        fw.op("pool", "memset", [self.b_epsc], [self.b_epsc], ap=self.epsc[:, 2:3], constant=math.pi / 2)
        fw.op("pool", "memset", [self.b_epsc], [self.b_epsc], ap=self.epsc[:, 3:4], constant=0.0)
        ct, bct = self.sb(gs, "cT", [128, 8, 2], F32)
        fw.dma([], [bct], out=ct[:], in_=I["cT"].ap())
        fw.op("act", "activation", [bct], [self.b_scT], out=self.scT[:], in_=ct[:], func=AF.Silu)

    def pf(self):
        r = self.PF[self.pfi % len(self.PF)]
        self.pfi += 1
        return r

    def pb(self):
        r = self.PB[self.pbi % len(self.PB)]
        self.pbi += 1
        return r

    def dump(self, name, ap, shape, R, dt=F32):
        if name not in self.debug:
            return
        t = self.nc.dram_tensor("dbg_" + name, list(shape), dt, kind="ExternalOutput")
        self.dbg_out.append("dbg_" + name)
        self.fw.dma(R, [Buf()], out=t.ap(), in_=ap, stream="d2")

    def phase_init(self):
        fw, I = self.fw, self.I
        with ExitStack() as es:
            xt = [self.sb(es, f"xi{i}", [128, D], F32) for i in range(3)]
            pt = [self.sb(es, f"pi{i}", [128, D], F32) for i in range(3)]
            for t in range(NT):
                x, bx = xt[t % 3]
                if t < 2:
                    fw.dma([], [bx], out=x[:], in_=I["ctx"][t * 128:(t + 1) * 128, :])
                else:
                    p, bp = pt[t % 3]
                    r0 = (t - 2) * 128
                    fw.dma([], [bx], out=x[:], in_=I["x"][r0:r0 + 128, :])
                    fw.dma([], [bp], out=p[:], in_=I["pe"][r0:r0 + 128, :])
                    fw.op("pool", "tensor_tensor", [bx, bp], [bx], out=x[:], in0=x[:], in1=p[:], op=ALU.add)
                fw.dma([bx], [self.bXR[t]], out=self.XR[t * 128:(t + 1) * 128, :], in_=x[:], stream="d2")
            fw.flush()

    def phase_mod(self, l):
        fw, I = self.fw, self.I
        with ExitStack() as es:
            wm = [self.sb(es, f"wm{i}", [128, 8, 512], F32) for i in range(2)]
            bT, bbT = self.sb(es, "bmodT", [128, 48], F32)
            brow, bbrow = self.sb(es, "bmrow", [2, 4, 512], F32)
            rows, brows = self.sb(es, "mrows", [2, 512], F32)
            sel, bsel = self.sb(es, "selw", [2, 2, 128], F32)
            gst_ = [self.sb(es, f"gst{i}", [128, 512], F32) for i in range(2)]
            fw.dma([], [bbT], out=bT[:], in_=I["bmodT"][l])
            for w in range(2):
                for gi, c0 in enumerate((2048, 2560, 5120, 5632)):
                    fw.dma([], [bbrow], out=brow[w:w + 1, gi, :], in_=I["b_mod"][l:l + 1, c0:c0 + 512])
            fw.dma([], [bsel], out=sel[:], in_=I["selw"].ap())
            for cb in range(12):
                w_, bw = wm[cb % 2]
                fw.dma([], [bw], out=w_[:], in_=I["w_mod"][l][:, cb * 512:(cb + 1) * 512].rearrange("(k p) c -> p k c", p=128))
                pt_, bp = self.pf()
                for s in range(4):
                    j = cb * 4 + s
                    for k in range(8):
                        fw.op("pe", "matmul", [bw, self.b_scT], [bp], out=pt_[:, s * 2:(s + 1) * 2], lhsT=w_[:, k, s * 128:(s + 1) * 128],
                              rhs=self.scT[:, k, :], start=(k == 0), stop=(k == 7))
                fw.op("dve", "tensor_tensor", [bp, bbT], [self.b_modT], out=self.modT[:, cb * 4:(cb + 1) * 4, :],
                      in0=pt_[:, 0:8].rearrange("p (s w) -> p s w", w=2),
                      in1=bT[:, cb * 4:(cb + 1) * 4].unsqueeze(2).to_broadcast([128, 4, 2]), op=ALU.add)
                if cb in (4, 5, 10, 11):
                    gi = {4: 0, 5: 1, 10: 2, 11: 3}[cb]
                    pr, bpr = self.pf()
                    for k in range(8):
                        fw.op("pe", "matmul", [bw, self.b_scT], [bpr], out=pr[0:2, :], lhsT=self.scT[:, k, :], rhs=w_[:, k, :],
                              start=(k == 0), stop=(k == 7))
                    fw.op("dve", "tensor_tensor", [bpr, bbrow], [brows], out=rows[:], in0=pr[0:2, :], in1=brow[:, gi, :], op=ALU.add)
                    for w in range(2):
                        pbc, bpbc = self.pf()
                        fw.op("pe", "matmul", [brows, bsel], [bpbc], out=pbc[:], lhsT=sel[:, w, :], rhs=rows[:], start=True, stop=True)
                        slot = (0 if gi < 2 else 2) + w
                        half = gi % 2
                        gst, bgst = gst_[(gi * 2 + w) % 2]
                        fw.op("act", "copy", [bpbc], [bgst], out=gst[:], in_=pbc[:])
                        fw.dma([bgst], [self.b_GBd], out=self.GBd[:, slot, half * 512:(half + 1) * 512], in_=gst[:], stream="d2")
            fw.op("dve", "tensor_scalar", [self.b_modT], [self.b_modP], out=self.modP[:], in0=self.modT[:], scalar1=1.0, scalar2=None, op0=ALU.add)
            self.dump(f"modT{l}", self.modT[:], [128, 48, 2], [self.b_modT])
            fw.flush()

    def rms_stats(self, x, bx, st, bst, mv, bmv, rstd, brstd, eps_col=0):
        fw = self.fw
        fw.op("dve", "bn_stats", [bx], [bst], out=st[:, 0, :], in_=x[:, 0:512])
        fw.op("dve", "bn_stats", [bx, bst], [bst], out=st[:, 1, :], in_=x[:, 512:1024])
        fw.op("dve", "bn_aggr", [bst], [bmv], out=mv[:, 0:2], in_=st[:])
        fw.op("dve", "scalar_tensor_tensor", [bmv], [bmv], out=mv[:, 2:3], in0=mv[:, 0:1], scalar=mv[:, 0:1], in1=mv[:, 1:2], op0=ALU.mult, op1=ALU.add)
        fw.op("act", "activation", [bmv, self.b_epsc], [bmv], out=mv[:, 3:4], in_=mv[:, 2:3], func=AF.Sqrt, bias=self.epsc[:, eps_col:eps_col + 1], scale=1.0)
        fw.op("dve", "reciprocal", [bmv], [brstd], out=rstd[:], in_=mv[:, 3:4])

    def phase_norm1(self, l, hT, bhT):
        fw, I = self.fw, self.I
        with ExitStack() as es:
            gB, bgB = self.sb(es, "n1g", [128, D], F32)
            fw.dma([], [bgB], out=gB[:], in_=I["norm1_g"][l:l + 1, :].to_broadcast([128, D]))
            xs_ = [self.sb(es, f"n1x{i}", [128, D], F32) for i in range(2)]
            xb_ = [self.sb(es, f"n1xb{i}", [128, D], BF16) for i in range(2)]
            st_ = [self.sb(es, f"n1st{i}", [128, 2, 6], F32) for i in range(2)]
            mv_ = [self.sb(es, f"n1mv{i}", [128, 4], F32) for i in range(2)]
            rs_ = [self.sb(es, f"n1rs{i}", [128, 1], F32) for i in range(2)]
            for t in range(NT):
                w = 1 if t < 2 else 0
                x, bx = xs_[t % 2]; xb, bxb = xb_[t % 2]; st, bst = st_[t % 2]; mv, bmv = mv_[t % 2]; rs, brs = rs_[t % 2]
                fw.dma([self.bXR[t]], [bx], out=x[:], in_=self.XR[t * 128:(t + 1) * 128, :])
                self.rms_stats(x, bx, st, bst, mv, bmv, rs, brs)
                fw.op("dve", "scalar_tensor_tensor", [bx, brs, bgB], [bxb], out=xb[:], in0=x[:], scalar=rs[:, 0:1], in1=gB[:], op0=ALU.mult, op1=ALU.mult)
                pb_, bpb = self.pb()
                for k in range(8):
                    fw.op("pe", "transpose", [bxb, self.b_identb], [bpb], out=pb_[:, k * 128:(k + 1) * 128], in_=xb[:, k * 128:(k + 1) * 128], identity=self.identb[:])
                for k in range(8):
                    fw.op("act", "activation", [bpb, self.b_modT, self.b_modP], [bhT[t]], out=hT[:, k, t * 128:(t + 1) * 128], in_=pb_[:, k * 128:(k + 1) * 128],
                          func=AF.Identity, scale=self.modP[:, 8 + k, w:w + 1], bias=self.modT[:, k, w:w + 1])
            fw.flush()

    def layer(self, l):
        fw = self.fw
        self.phase_mod(l)
        with ExitStack() as es:
            hT, _ = self.sb(es, "hT", [128, 8, T], BF16)
            bhT = [Buf(f"hT{t}") for t in range(NT)]
            self.phase_norm1(l, hT, bhT)
            self.dump(f"hT{l}", hT[:], [128, 8, T], bhT, dt=BF16)
            if self.debug:
                fw.flush()
            if self.stop == "norm1":
                return
            self.phase_ret(l, hT, bhT)
            if self.stop == "ret":
                return
            self.phase_s5(l, hT, bhT)
            if self.stop == "s5":
                return
            self.phase_hy(l, hT, bhT)
            if self.stop == "hy":
                return
            self.phase_merge(l, hT, bhT)
        if self.stop == "merge":
            return
        self.phase_moe(l)

    def load_w_cols(self, dst_ap, bdst, l, c0, ncols):
        self.fw.dma([], [bdst], q="pool", stream="d1", out=dst_ap,
                    in_=self.I["w_in"][l][:, c0:c0 + ncols].rearrange("(k p) c -> p k c", p=128))

    def tiles_of(self, t0, n):
        return range(t0 // 128, (t0 + n) // 128)

    def phase_ret(self, l, hT, bhT):
        fw, I = self.fw, self.I
        QS = 128.0 ** -0.5
        with ExitStack() as es:
            rtab, brtab = self.sb(es, "rtab", [128, 6, 128], F32)
            lgB, blgB = self.sb(es, "lgB", [128, 8], F32)
            gnB, bgnB = self.sb(es, "gnB", [128, 512], F32)
            fw.dma([], [brtab], out=rtab[:], in_=I["rtab"].ap())
            fw.dma([], [blgB], out=lgB[:], in_=I["ret_log_decay"][l:l + 1, :].to_broadcast([128, 8]))
            fw.dma([], [bgnB], out=gnB[:], in_=I["ret_gn_g"][l:l + 1, :].to_broadcast([128, 512]))
            MT, bMT = self.sb(es, "MT", [128, 128], F32)
            tm1, btm1 = self.sb(es, "tm1", [128, 128], F32)
            tm2, btm2 = self.sb(es, "tm2", [128, 128], F32)
            dcol, bdcol = self.sb(es, "dcol", [128, 6], F32)
            wqk, bwqk = self.sb(es, "wqk", [128, 8, 256], BF16)
            wkvg, bwkvg = self.sb(es, "wkvg", [128, 8, 384], BF16)
            qT, bqT = self.sb(es, "qT", [128, T], BF16)
            kT, bkT = self.sb(es, "kT", [128, T], BF16)
            ktok, bktok = self.sb(es, "ktok", [128, NT, 128], BF16)
            vtok, bvtok = self.sb(es, "vtok", [128, NT, 128], BF16)
            vdf, bvdf = self.sb(es, "vdf", [128, NT, 128], BF16)
            vdb, bvdb = self.sb(es, "vdb", [128, NT, 128], BF16)
            gs, bgs = self.sb(es, "gs", [128, NT, 128], BF16)
            SfB, bSfB = self.sb(es, "SfB", [128, NT, 128], BF16)
            SbB, bSbB = self.sb(es, "SbB", [128, NT, 128], BF16)
            Sf, bSf = self.sb(es, "Sf", [128, 128], F32)
            Sb, bSb = self.sb(es, "Sb", [128, 128], F32)
            yTt_ = [self.sb(es, f"yTt{i}", [128, 128], BF16) for i in range(2)]
            attm_ = [self.sb(es, f"attm{i}", [128, 128], BF16) for i in range(2)]
            on_ = [self.sb(es, f"on{i}", [128, 128], F32) for i in range(2)]
            yk_ = [self.sb(es, f"yk{i}", [128, 128], BF16) for i in range(2)]
            st_ = [self.sb(es, f"rst{i}", [128, 6], F32) for i in range(2)]
            mv_ = [self.sb(es, f"rmv{i}", [128, 4], F32) for i in range(2)]
            bq_t = [Buf() for _ in range(NT)]
            bk_t = [Buf() for _ in range(NT)]
            btok = [Buf() for _ in range(NT)]
            bSf_t = [Buf() for _ in range(NT)]
            bSb_t = [Buf() for _ in range(NT)]
            for hd in range(4):
                lgf = lgB[:, hd:hd + 1]
                lgb = lgB[:, 4 + hd:5 + hd]
                fw.op("act", "activation", [brtab, blgB], [btm1], out=tm1[:], in_=rtab[:, 0, :], func=AF.Exp, scale=lgf)
                fw.op("act", "activation", [brtab, blgB], [btm2], out=tm2[:], in_=rtab[:, 1, :], func=AF.Exp, scale=lgb)
                fw.op("dve", "tensor_tensor", [btm1, brtab], [btm1], out=tm1[:], in0=tm1[:], in1=rtab[:, 2, :], op=ALU.mult)
                fw.op("dve", "tensor_tensor", [btm2, brtab], [btm2], out=tm2[:], in0=tm2[:], in1=rtab[:, 3, :], op=ALU.mult)
                fw.op("dve", "tensor_tensor", [btm1, btm2], [bMT], out=MT[:], in0=tm1[:], in1=tm2[:], op=ALU.add)
                fw.op("act", "activation", [self.b_rcol, blgB], [bdcol], out=dcol[:, 0:1], in_=self.rcol[:, 0:1], func=AF.Exp, scale=lgf)
                fw.op("act", "activation", [self.b_rcol, blgB, bdcol], [bdcol], out=dcol[:, 1:2], in_=self.rcol[:, 1:2], func=AF.Exp, scale=lgb)
                fw.op("act", "activation", [self.b_rcol, blgB, bdcol], [bdcol], out=dcol[:, 2:3], in_=self.rcol[:, 2:3], func=AF.Exp, scale=lgf)
                fw.op("act", "activation", [self.b_rcol, blgB, bdcol], [bdcol], out=dcol[:, 3:4], in_=self.rcol[:, 2:3], func=AF.Exp, scale=lgb)
                fw.op("act", "activation", [self.b_rcol, blgB, bdcol], [bdcol], out=dcol[:, 4:5], in_=self.rcol[:, 4:5], func=AF.Exp, scale=lgf)
                fw.op("act", "activation", [self.b_rcol, blgB, bdcol], [bdcol], out=dcol[:, 5:6], in_=self.rcol[:, 5:6], func=AF.Exp, scale=lgb)
                self.load_w_cols(wqk[:, :, 0:128], bwqk, l, hd * 128, 128)
                self.load_w_cols(wqk[:, :, 128:256], bwqk, l, 512 + hd * 128, 128)
                self.load_w_cols(wkvg[:, :, 0:128], bwkvg, l, 512 + hd * 128, 128)
                self.load_w_cols(wkvg[:, :, 128:256], bwkvg, l, 1024 + hd * 128, 128)
                self.load_w_cols(wkvg[:, :, 256:384], bwkvg, l, 1536 + hd * 128, 128)
                for (t0, n) in TG:
                    tl = list(self.tiles_of(t0, n))
                    hb = [bhT[t] for t in tl]
                    nb = n // 128
                    pq, bpq = self.pf()
                    for k in range(8):
                        fw.op("pe", "matmul", hb + [bwqk], [bpq], out=pq[:, 0:n], lhsT=wqk[:, k, 0:128], rhs=hT[:, k, t0:t0 + n], start=(k == 0), stop=(k == 7))
                    wq_ = [bq_t[t] for t in tl]
                    fw.op("act", "mul", [bpq], wq_, out=qT[:, t0:t0 + n], in_=pq[:, 0:n], mul=QS)
                    pk, bpk = self.pf()
                    for k in range(8):
                        fw.op("pe", "matmul", hb + [bwqk], [bpk], out=pk[:, 0:n], lhsT=wqk[:, k, 128:256], rhs=hT[:, k, t0:t0 + n], start=(k == 0), stop=(k == 7))
                    fw.op("act", "copy", [bpk], [bk_t[t] for t in tl], out=kT[:, t0:t0 + n], in_=pk[:, 0:n])
                for t in range(NT):
                    pt_, bp = self.pf()
                    for k in range(8):
                        fw.op("pe", "matmul", [bhT[t], bwkvg], [bp], out=pt_[:, 0:384], lhsT=hT[:, k, t * 128:(t + 1) * 128], rhs=wkvg[:, k, :], start=(k == 0), stop=(k == 7))
                    fw.op("act", "copy", [bp], [btok[t]], out=ktok[:, t, :], in_=pt_[:, 0:128])
                    fw.op("act", "copy", [bp], [btok[t]], out=vtok[:, t, :], in_=pt_[:, 128:256])
                    fw.op("dve", "tensor_scalar", [bp, bdcol], [btok[t]], out=vdf[:, t, :], in0=pt_[:, 128:256], scalar1=dcol[:, 0:1], scalar2=None, op0=ALU.mult)
                    fw.op("dve", "tensor_scalar", [bp, bdcol], [btok[t]], out=vdb[:, t, :], in0=pt_[:, 128:256], scalar1=dcol[:, 1:2], scalar2=None, op0=ALU.mult)
                    fw.op("act", "activation", [bp], [btok[t]], out=gs[:, t, :], in_=pt_[:, 256:384], func=AF.Silu)
                for (S, bS, SB, bSB_t, vd, ci, order) in ((Sf, bSf, SfB, bSf_t, vdf, 2, list(range(NT))),
                                                          (Sb, bSb, SbB, bSb_t, vdb, 3, [1, 0] + list(range(NT - 1, 1, -1)))):
                    fw.op("pool", "memset", [], [bS], ap=S[:], constant=0.0)
                    for t in order:
                        fw.op("act", "copy", [bS], [bSB_t[t]], out=SB[:, t, :], in_=S[:])
                        pkv, bpkv = self.pf()
                        fw.op("pe", "matmul", [btok[t]], [bpkv], out=pkv[:, 0:128], lhsT=ktok[:, t, :], rhs=vd[:, t, :], start=True, stop=True)
                        fw.op("dve", "scalar_tensor_tensor", [bS, bpkv, bdcol], [bS], out=S[:], in0=S[:], scalar=dcol[:, ci:ci + 1], in1=pkv[:, 0:128], op0=ALU.mult, op1=ALU.add)
                for t in range(NT):
                    cs = slice(t * 128, (t + 1) * 128)
                    attm, battm = attm_[t % 2]; on, bon = on_[t % 2]; yk, byk = yk_[t % 2]; st, bst = st_[t % 2]; mv, bmv = mv_[t % 2]
                    pa, bpa = self.pf()
                    fw.op("pe", "matmul", [bk_t[t], bq_t[t]], [bpa], out=pa[:, 0:128], lhsT=kT[:, cs], rhs=qT[:, cs], start=True, stop=True)
                    fw.op("dve", "tensor_tensor", [bpa, bMT], [battm], out=attm[:], in0=pa[:, 0:128], in1=MT[:], op=ALU.mult)
                    po, bpo = self.pf()
                    fw.op("pe", "matmul", [battm, btok[t]], [bpo], out=po[:, 0:128], lhsT=attm[:], rhs=vtok[:, t, :], start=True, stop=True)
                    fw.op("pe", "matmul", [bq_t[t], bSf_t[t]], [bpo], out=po[:, 128:256], lhsT=qT[:, cs], rhs=SfB[:, t, :], start=True, stop=True)
                    fw.op("pe", "matmul", [bq_t[t], bSb_t[t]], [bpo], out=po[:, 256:384], lhsT=qT[:, cs], rhs=SbB[:, t, :], start=True, stop=True)
                    fw.op("act", "copy", [bpo], [bon], out=on[:], in_=po[:, 0:128])
                    fw.op("dve", "scalar_tensor_tensor", [bpo, bdcol, bon], [bon], out=on[:], in0=po[:, 128:256], scalar=dcol[:, 4:5], in1=on[:], op0=ALU.mult, op1=ALU.add)
                    fw.op("dve", "scalar_tensor_tensor", [bpo, bdcol, bon], [bon], out=on[:], in0=po[:, 256:384], scalar=dcol[:, 5:6], in1=on[:], op0=ALU.mult, op1=ALU.add)
                    fw.op("dve", "bn_stats", [bon], [bst], out=st[:], in_=on[:])
                    fw.op("dve", "bn_aggr", [bst], [bmv], out=mv[:, 0:2], in_=st[:])
                    fw.op("act", "activation", [bmv, self.b_epsc], [bmv], out=mv[:, 2:3], in_=mv[:, 1:2], func=AF.Sqrt, bias=self.epsc[:, 1:2], scale=1.0)
                    fw.op("dve", "reciprocal", [bmv], [bmv], out=mv[:, 3:4], in_=mv[:, 2:3])
                    fw.op("dve", "tensor_scalar", [bon, bmv], [bon], out=on[:], in0=on[:], scalar1=mv[:, 0:1], scalar2=mv[:, 3:4], op0=ALU.subtract, op1=ALU.mult)
                    fw.op("dve", "tensor_tensor", [bon, bgnB], [bon], out=on[:], in0=on[:], in1=gnB[:, hd * 128:(hd + 1) * 128], op=ALU.mult)
                    fw.op("dve", "tensor_tensor", [bon, btok[t]], [byk], out=yk[:], in0=on[:], in1=gs[:, t, :], op=ALU.mult)
                    pb_, bpb = self.pb()
                    fw.op("pe", "transpose", [byk, self.b_identb], [bpb], out=pb_[:, 0:128], in_=yk[:], identity=self.identb[:])
                    yTt, byTt = yTt_[t % 2]
                    fw.op("act", "copy", [bpb], [byTt], out=yTt[:], in_=pb_[:, 0:128])
                    fw.dma([byTt], [self.bYT[hd]], out=self.YT[hd * 128:(hd + 1) * 128, cs], in_=yTt[:], stream="d2")
            if self.debug:
                self.dump_yt("yret%d" % l, 0, 4)
            fw.flush()

    def dump_yt(self, name, c0, nchunks):
        if name not in self.debug:
            return
        t = self.nc.dram_tensor("dbg_" + name, [nchunks * 128, T], BF16, kind="ExternalOutput")
        self.dbg_out.append("dbg_" + name)
        self.fw.dma([self.bYT[c0 + i] for i in range(nchunks)], [Buf()], out=t.ap(), in_=self.YT[c0 * 128:(c0 + nchunks) * 128, :], stream="d2")

    def sincos(self, src, bsrc, shape, sin_out, cos_out, bout, ki, bki, kf, bkf, eng="dve"):
        fw = self.fw
        PI_ = 3.1415925
        fw.op("dve", "tensor_scalar", [bsrc], [bki], out=ki, in0=src, scalar1=1.0 / TWO_PI, scalar2=None, op0=ALU.mult)
        fw.op("dve", "tensor_copy", [bki], [bkf], out=kf, in_=ki)
        fw.op("dve", "scalar_tensor_tensor", [bkf, bsrc], [bkf], out=kf, in0=kf, scalar=-TWO_PI, in1=src, op0=ALU.mult, op1=ALU.add)
        fw.op("dve", "tensor_scalar", [bkf], [bkf], out=kf, in0=kf, scalar1=-PI_, scalar2=PI_, op0=ALU.max, op1=ALU.min)
        if sin_out is not None:
            fw.op("act", "activation", [bkf], [bout], out=sin_out, in_=kf, func=AF.Sin)
        if cos_out is not None:
            fw.op("dve", "scalar_tensor_tensor", [bkf], [bkf], out=kf, in0=kf, scalar=-1.0, in1=kf, op0=ALU.mult, op1=ALU.max)
            fw.op("act", "activation", [bkf, self.b_epsc], [bout], out=cos_out, in_=kf, func=AF.Sin, scale=-1.0, bias=self.epsc[:, 2:3])

    def phase_s5(self, l, hT, bhT):
        fw, I = self.fw, self.I
        CH = [(0, 256)] + [(256 + 512 * i, 512) for i in range(8)]
        with ExitStack() as es:
            prm, bprm = self.sb(es, "s5prm", [128, 3, 32], F32)
            for dr in range(2):
                fw.dma([], [bprm], out=prm[:, 0, dr * 16:(dr + 1) * 16], in_=I["s5_lre"][l, dr])
                fw.dma([], [bprm], out=prm[:, 1, dr * 16:(dr + 1) * 16], in_=I["s5_lim"][l, dr])
                fw.dma([], [bprm], out=prm[:, 2, dr * 16:(dr + 1) * 16], in_=I["s5_ldt"][l, dr])
            dsc, bdsc = self.sb(es, "s5dsc", [128, 12, 32], F32)
            ki32, bki32 = self.sb(es, "s5ki", [128, 32], I32)
            kf32, bkf32 = self.sb(es, "s5kf", [128, 32], F32)
            DT, LR, TH, R_, SN, CS, AR, AI, DEN, CR, CI, NCR = [dsc[:, i, :] for i in range(12)]
            lre, lim = prm[:, 0, :], prm[:, 1, :]
            B2 = [bprm, bdsc]
            fw.op("act", "activation", [bprm], [bdsc], out=DT, in_=prm[:, 2, :], func=AF.Exp)
            fw.op("dve", "tensor_tensor", B2, [bdsc], out=LR, in0=lre, in1=DT, op=ALU.mult)
            fw.op("dve", "tensor_tensor", B2, [bdsc], out=TH, in0=lim, in1=DT, op=ALU.mult)
            fw.op("act", "activation", [bdsc], [bdsc], out=R_, in_=LR, func=AF.Exp)
            self.sincos(TH, bdsc, None, SN, CS, bdsc, ki32[:], bki32, kf32[:], bkf32)
            fw.op("dve", "tensor_tensor", B2, [bdsc], out=AR, in0=R_, in1=CS, op=ALU.mult)
            fw.op("dve", "tensor_tensor", B2, [bdsc], out=AI, in0=R_, in1=SN, op=ALU.mult)
            fw.op("dve", "tensor_scalar", B2, [bdsc], out=AR, in0=AR, scalar1=-1.0, scalar2=None, op0=ALU.add)
            fw.op("dve", "tensor_tensor", B2, [bdsc], out=DEN, in0=lre, in1=lre, op=ALU.mult)
            fw.op("dve", "tensor_tensor", B2, [bdsc], out=LR, in0=lim, in1=lim, op=ALU.mult)
            fw.op("dve", "tensor_tensor", B2, [bdsc], out=DEN, in0=DEN, in1=LR, op=ALU.add)
            fw.op("dve", "reciprocal", B2, [bdsc], out=DEN, in_=DEN)
            fw.op("dve", "tensor_tensor", B2, [bdsc], out=CR, in0=AR, in1=lre, op=ALU.mult)
            fw.op("dve", "tensor_tensor", B2, [bdsc], out=LR, in0=AI, in1=lim, op=ALU.mult)
            fw.op("dve", "tensor_tensor", B2, [bdsc], out=CR, in0=CR, in1=LR, op=ALU.add)
            fw.op("dve", "tensor_tensor", B2, [bdsc], out=CR, in0=CR, in1=DEN, op=ALU.mult)
            fw.op("dve", "tensor_tensor", B2, [bdsc], out=CI, in0=AI, in1=lre, op=ALU.mult)
            fw.op("dve", "tensor_tensor", B2, [bdsc], out=LR, in0=AR, in1=lim, op=ALU.mult)
            fw.op("dve", "tensor_tensor", B2, [bdsc], out=CI, in0=CI, in1=LR, op=ALU.subtract)
            fw.op("dve", "tensor_tensor", B2, [bdsc], out=CI, in0=CI, in1=DEN, op=ALU.mult)
            fw.op("dve", "tensor_scalar", B2, [bdsc], out=NCR, in0=CR, scalar1=-1.0, scalar2=None, op0=ALU.mult)
            cut = getattr(self, 's5_cut', 0)
            if cut == 1:
                self.dump('dsc', dsc[:], [128, 12, 32], [bdsc])
                fw.flush()
                return
            Bb, bBb = self.sb(es, "s5Bb", [128, 2, 2, 4, 2, 128], BF16)
            Cb, bCb = self.sb(es, "s5Cb", [128, 2, 2, 4, 64], BF16)
            for dr in range(2):
                for ri in range(2):
                    fw.dma([], [bBb], q="pool", stream="d1", out=Bb[:, dr, ri], in_=I["s5_B"][l, dr, ri])
            dT_, bdT = self.sb(es, "s5dT", [128, 4], F32)
            fw.dma([], [bdT], out=dT_[:], in_=I["s5_dT"][l])
            iota, biota = self.sb(es, "s5iota", [128, 512], F32)
            fw.dma([], [biota], out=iota[:], in_=I["iota512"].ap())
            wu, bwu = self.sb(es, "s5wu", [128, 8, 128], BF16)
            uf, buf_ = self.sb(es, "s5uf", [128, T], F32)
            ub, bub = self.sb(es, "s5ub", [128, T], BF16)
            yacc, byacc = self.sb(es, "s5yacc", [128, T], F32)
            gb_ = [self.sb(es, f"s5gb{i}", [128, 512], BF16) for i in range(2)]
            tb = {}
            for nm in ("cos", "sin", "dre", "dim", "rf"):
                tb[nm] = self.sb(es, "s5t_" + nm, [128, 512], F32)
            wk = {}
            for nm in ("pre", "pim", "t1", "t2", "t3", "t4", "gre", "gim", "hre", "nhim"):
                wk[nm] = self.sb(es, "s5w_" + nm, [128, 512], F32)
            hreb, bhreb = self.sb(es, "s5hreb", [128, 512], BF16)
            nhimb, bnhimb = self.sb(es, "s5nhimb", [128, 512], BF16)
            carry, bcarry = self.sb(es, "s5carry", [128, 2], F32)
            self.GS5 = getattr(self, "GS5", None) or self.scratch("GS5", [512, T], BF16)
            bGS5 = [Buf() for _ in range(4)]
            ychunk = [Buf() for _ in CH]
            if cut == 2:
                fw.flush()
                return
            for o in range(4):
                for dr in range(2):
                    for ri in range(2):
                        fw.dma([], [bCb], q="pool", stream="d1", out=Cb[:, dr, ri], in_=I["s5_C"][l, dr, ri][:, o * 4:(o + 1) * 4, :])
                    fw.op("act", "mul", [bCb], [bCb], out=Cb[:, dr, 1], in_=Cb[:, dr, 1], mul=-1.0)
                self.load_w_cols(wu[:], bwu, l, 2048 + o * 128, 128)
                for (t0, n) in TG:
                    hb = [bhT[t] for t in self.tiles_of(t0, n)]
                    pu, bpu = self.pf()
                    for k in range(8):
                        fw.op("pe", "matmul", hb + [bwu], [bpu], out=pu[:, 0:n], lhsT=wu[:, k, :], rhs=hT[:, k, t0:t0 + n], start=(k == 0), stop=(k == 7))
                    fw.op("act", "copy", [bpu], [buf_], out=uf[:, t0:t0 + n], in_=pu[:, 0:n])
                    fw.op("dve", "tensor_copy", [buf_], [bub], out=ub[:, t0:t0 + n], in_=uf[:, t0:t0 + n])
                if cut == 5:
                    fw.flush()
                    return
                fw.op("pool", "memset", [], ychunk, ap=yacc[:], constant=0.0)
                if cut == 3:
                    fw.flush()
                    return
                lim = getattr(self, "s5_lim", None)
                for dr in range(2):
                    if lim is not None and dr >= lim[0]:
                        break
                    for q4 in range(4):
                        pt = o * 4 + q4
                        c = dr * 16 + pt
                        ps = slice(64 * (q4 // 2), 64 * (q4 // 2) + 64)
                        jj = q4 % 2
                        (cosT, bcos), (sinT, bsin) = tb["cos"], tb["sin"]
                        kiT, bkiT = wk["t4"][0][:].bitcast(I32), wk["t4"][1]
                        (dreT, bdre), (dimT, bdim), (rfT, brf) = tb["dre"], tb["dim"], tb["rf"]
                        t1, bt1 = wk["t1"]
                        fw.op("dve", "tensor_scalar", [biota, bdsc], [bt1], out=t1[:], in0=iota[:], scalar1=TH[:, c:c + 1], scalar2=None, op0=ALU.mult)
                        fw.op("dve", "tensor_scalar", [bt1], [bkiT], out=kiT, in0=t1[:], scalar1=1.0 / TWO_PI, scalar2=None, op0=ALU.mult)
                        t2, bt2 = wk["t2"]
                        fw.op("dve", "tensor_copy", [bkiT], [bt2], out=t2[:], in_=kiT)
                        fw.op("dve", "scalar_tensor_tensor", [bt2, bt1], [bt2], out=t2[:], in0=t2[:], scalar=-TWO_PI, in1=t1[:], op0=ALU.mult, op1=ALU.add)
                        fw.op("dve", "tensor_scalar", [bt2], [bt2], out=t2[:], in0=t2[:], scalar1=-3.1415925, scalar2=3.1415925, op0=ALU.max, op1=ALU.min)
                        fw.op("act", "activation", [bt2], [bsin], out=sinT[:], in_=t2[:], func=AF.Sin)
                        fw.op("dve", "scalar_tensor_tensor", [bt2], [bt2], out=t2[:], in0=t2[:], scalar=-1.0, in1=t2[:], op0=ALU.mult, op1=ALU.max)
                        fw.op("act", "activation", [bt2, self.b_epsc], [bcos], out=cosT[:], in_=t2[:], func=AF.Sin, scale=-1.0, bias=self.epsc[:, 2:3])
                        fw.op("dve", "tensor_scalar", [bcos, bdsc], [bdre], out=dreT[:], in0=cosT[:], scalar1=CR[:, c:c + 1], scalar2=None, op0=ALU.mult)
                        fw.op("dve", "scalar_tensor_tensor", [bsin, bdsc, bdre], [bdre], out=dreT[:], in0=sinT[:], scalar=CI[:, c:c + 1], in1=dreT[:], op0=ALU.mult, op1=ALU.add)
                        fw.op("dve", "tensor_scalar", [bcos, bdsc], [bdim], out=dimT[:], in0=cosT[:], scalar1=CI[:, c:c + 1], scalar2=None, op0=ALU.mult)
                        fw.op("dve", "scalar_tensor_tensor", [bsin, bdsc, bdim], [bdim], out=dimT[:], in0=sinT[:], scalar=NCR[:, c:c + 1], in1=dimT[:], op0=ALU.mult, op1=ALU.add)
                        fw.op("dve", "tensor_scalar", [biota], [brf], out=rfT[:], in0=iota[:], scalar1=0.0, scalar2=None, op0=ALU.mult)
                        fw.op("dve", "tensor_scalar", [brf, bdsc], [brf], out=rfT[:], in0=rfT[:], scalar1=R_[:, c:c + 1], scalar2=None, op0=ALU.add)
                        fw.op("pool", "memset", [], [bcarry], ap=carry[:], constant=0.0)
                        order = list(range(9)) if dr == 0 else [0] + list(range(8, 0, -1))
                        if lim is not None:
                            order = order[:lim[1]]
                        for ci_ in order:
                            t0, n = CH[ci_]
                            rv = (lambda ap: ap) if dr == 0 else (lambda ap: ap[:, ::-1])
                            pP, bpP = self.pf()
                            pQ, bpQ = self.pf()
                            fw.op("pe", "matmul", [bBb, bub], [bpP], out=pP[:, 0:n], lhsT=Bb[ps, dr, 0, o, jj, :], rhs=ub[ps, t0:t0 + n], start=True, stop=True)
                            fw.op("pe", "matmul", [bBb, bub], [bpQ], out=pQ[:, 0:n], lhsT=Bb[ps, dr, 1, o, jj, :], rhs=ub[ps, t0:t0 + n], start=True, stop=True)
                            pre, bpre = wk["pre"]; pim, bpim = wk["pim"]
                            fw.op("act", "copy", [bpP], [bpre], out=rv(pre[:, 0:n]), in_=pP[:, 0:n])
                            fw.op("act", "copy", [bpQ], [bpim], out=rv(pim[:, 0:n]), in_=pQ[:, 0:n])
                            t2, bt2 = wk["t2"]; t3, bt3 = wk["t3"]; t4, bt4 = wk["t4"]
                            gre, bgre = wk["gre"]; gim, bgim = wk["gim"]; hre, bhre = wk["hre"]; nhim, bnhim = wk["nhim"]
                            N = slice(0, n)
                            fw.op("dve", "tensor_tensor", [bpre, bdre], [bt1], out=t1[:, N], in0=pre[:, N], in1=dreT[:, N], op=ALU.mult)
                            fw.op("dve", "tensor_tensor", [bpim, bdim], [bt2], out=t2[:, N], in0=pim[:, N], in1=dimT[:, N], op=ALU.mult)
                            fw.op("dve", "tensor_tensor", [bt1, bt2], [bt1], out=t1[:, N], in0=t1[:, N], in1=t2[:, N], op=ALU.subtract)
                            fw.op("pool", "tensor_tensor", [bpre, bdim], [bt3], out=t3[:, N], in0=pre[:, N], in1=dimT[:, N], op=ALU.mult)
                            fw.op("pool", "tensor_tensor", [bpim, bdre], [bt4], out=t4[:, N], in0=pim[:, N], in1=dreT[:, N], op=ALU.mult)
                            fw.op("pool", "tensor_tensor", [bt3, bt4], [bt3], out=t3[:, N], in0=t3[:, N], in1=t4[:, N], op=ALU.add)
                            fw.op("dve", "tensor_tensor_scan", [brf, bt1, bcarry], [bgre], out=gre[:, N], data0=rfT[:, N], data1=t1[:, N], initial=carry[:, 0:1], op0=ALU.mult, op1=ALU.add)
                            fw.op("dve", "tensor_tensor_scan", [brf, bt3, bcarry], [bgim], out=gim[:, N], data0=rfT[:, N], data1=t3[:, N], initial=carry[:, 1:2], op0=ALU.mult, op1=ALU.add)
                            fw.op("dve", "tensor_tensor", [bcos, bgre], [bhre], out=hre[:, N], in0=cosT[:, N], in1=gre[:, N], op=ALU.mult)
                            fw.op("pool", "tensor_tensor", [bsin, bgim], [bt4], out=t4[:, N], in0=sinT[:, N], in1=gim[:, N], op=ALU.mult)
                            fw.op("dve", "tensor_tensor", [bhre, bt4], [bhre], out=hre[:, N], in0=hre[:, N], in1=t4[:, N], op=ALU.subtract)
                            fw.op("pool", "tensor_tensor", [bsin, bgre], [bnhim], out=nhim[:, N], in0=sinT[:, N], in1=gre[:, N], op=ALU.mult)
                            fw.op("pool", "tensor_tensor", [bcos, bgim], [bt2], out=t2[:, N], in0=cosT[:, N], in1=gim[:, N], op=ALU.mult)
                            fw.op("pool", "tensor_tensor", [bnhim, bt2], [bnhim], out=nhim[:, N], in0=nhim[:, N], in1=t2[:, N], op=ALU.add)
                            fw.op("act", "copy", [bhre], [bhreb], out=rv(hreb[:, N]), in_=hre[:, N])
                            fw.op("act", "copy", [bnhim], [bnhimb], out=rv(nhimb[:, N]), in_=nhim[:, N])
                            fw.op("act", "copy", [bhre], [bcarry], out=carry[:, 0:1], in_=hre[:, n - 1:n])
                            fw.op("act", "copy", [bnhim, bcarry], [bcarry], out=carry[:, 1:2], in_=nhim[:, n - 1:n])
                            py, bpy = self.pf()
                            fw.op("pe", "matmul", [bCb, bhreb], [bpy], out=py[ps, 0:n], lhsT=Cb[:, dr, 0, q4, :], rhs=hreb[:, N], start=True, stop=False)
                            fw.op("pe", "matmul", [bCb, bnhimb], [bpy], out=py[ps, 0:n], lhsT=Cb[:, dr, 1, q4, :], rhs=nhimb[:, N], start=False, stop=True)
                            fw.op("dve", "tensor_tensor", [bpy, ychunk[ci_]], [ychunk[ci_]], out=yacc[ps, t0:t0 + n], in0=py[ps, 0:n], in1=yacc[ps, t0:t0 + n], op=ALU.add)
                if cut == 4:
                    fw.flush()
                    return
                fw.op("dve", "scalar_tensor_tensor", [buf_, bdT] + ychunk, ychunk, out=yacc[:], in0=uf[:], scalar=dT_[:, o:o + 1], in1=yacc[:], op0=ALU.mult, op1=ALU.add)
                for gi_, (t0, n) in enumerate(TG):
                    gb, bgb = gb_[gi_ % 2]
                    fw.op("act", "activation", ychunk, [bgb], out=gb[:, 0:n], in_=yacc[:, t0:t0 + n], func=AF.Gelu_apprx_tanh)
                    fw.dma([bgb], [bGS5[o]], out=self.GS5[o * 128:(o + 1) * 128, t0:t0 + n], in_=gb[:, 0:n], stream="d2")
            fw.flush()
        with ExitStack() as es:
            W1b, bW1b = self.sb(es, "s5W1b", [128, 4, 512], BF16)
            W2b, bW2b = self.sb(es, "s5W2b", [128, 4, 512], BF16)
            fw.dma([], [bW1b], q="pool", stream="d1", out=W1b[:], in_=I["s5_glu_w1"][l].rearrange("(k p) c -> p k c", p=128))
            fw.dma([], [bW2b], q="pool", stream="d1", out=W2b[:], in_=I["s5_glu_w2"][l].rearrange("(k p) c -> p k c", p=128))
            g_ = [self.sb(es, f"s5g{i}", [128, 4, 512], BF16) for i in range(2)]
            sg_ = [self.sb(es, f"s5sg{i}", [128, 512], F32) for i in range(2)]
            yo_ = [self.sb(es, f"s5yo{i}", [128, 512], BF16) for i in range(2)]
            for gi, (t0, n) in enumerate(TG):
                g, bg = g_[gi % 2]
                fw.dma(bGS5, [bg], out=g[:, :, 0:n], in_=self.GS5[:, t0:t0 + n].rearrange("(k p) t -> p k t", p=128))
                for c_ in range(4):
                    sg, bsg = sg_[c_ % 2]; yo, byo = yo_[c_ % 2]
                    pA, bpA = self.pf()
                    pB, bpB = self.pf()
                    for k in range(4):
                        fw.op("pe", "matmul", [bW1b, bg], [bpA], out=pA[:, 0:n], lhsT=W1b[:, k, c_ * 128:(c_ + 1) * 128], rhs=g[:, k, 0:n], start=(k == 0), stop=(k == 3))
                    for k in range(4):
                        fw.op("pe", "matmul", [bW2b, bg], [bpB], out=pB[:, 0:n], lhsT=W2b[:, k, c_ * 128:(c_ + 1) * 128], rhs=g[:, k, 0:n], start=(k == 0), stop=(k == 3))
                    fw.op("act", "activation", [bpB], [bsg], out=sg[:, 0:n], in_=pB[:, 0:n], func=AF.Sigmoid)
                    fw.op("dve", "tensor_tensor", [bpA, bsg], [byo], out=yo[:, 0:n], in0=pA[:, 0:n], in1=sg[:, 0:n], op=ALU.mult)
                    fw.dma([byo], [self.bYT[4 + c_]], out=self.YT[512 + c_ * 128:512 + (c_ + 1) * 128, t0:t0 + n], in_=yo[:, 0:n], stream="d2")
            if self.debug:
                self.dump_yt("ys5%d" % l, 4, 4)
            fw.flush()

    def phase_hy(self, l, hT, bhT):
        fw, I = self.fw, self.I
        do_ctx = (l == 0)
        with ExitStack() as es:
            w1, bw1 = self.sb(es, "hfw1", [33, 64], F32)
            w2, bw2 = self.sb(es, "hfw2", [64, 64], F32)
            w3, bw3 = self.sb(es, "hfw3", [64, 512], F32)
            fb, bfb = self.sb(es, "hffb", [64, 6], F32)
            nd_, bnd = self.sb(es, "hfnd", [128, 4], F32)
            fw.dma([], [bw1], out=w1[:], in_=I["hy_f_w1"][l])
            fw.dma([], [bw2], out=w2[:], in_=I["hy_f_w2"][l])
            fw.dma([], [bw3], out=w3[:], in_=I["hy_f_w3"][l])
            fw.dma([], [bfb], out=fb[:, 0:1], in_=I["hy_f_b1"][l])
            fw.dma([], [bfb], out=fb[:, 1:2], in_=I["hy_f_b2"][l])
            fw.dma([], [bfb], out=fb[:, 2:4], in_=I["hy_f_fr"][l])
            fw.dma([], [bnd], out=nd_[:], in_=I["ndelta"].ap())
            fw.op("dve", "tensor_tensor", [bfb], [bfb], out=fb[:, 4:6], in0=fb[:, 0:2], in1=fb[:, 2:4], op=ALU.mult)
            zt_ = [self.sb(es, f"hfz{i}", [33, 512], F32) for i in range(2)]
            of_ = [self.sb(es, f"hfo{i}", [128, 512], F32) for i in range(2)]
            a_, ba_ = self.sb(es, "hfa", [64, 512], F32)
            hh_, bhh = self.sb(es, "hfh", [64, 512], F32)
            ki, bki = self.sb(es, "hfki", [64, 512], I32)
            kf, bkf = self.sb(es, "hfkf", [64, 512], F32)
            win, bwin = self.sb(es, "hfwin", [128, 512], F32)
            frev = [self.sb(es, f"hffr{i}", [128, L], BF16) for i in range(4)]
            zpad, bzpad = self.sb(es, "hfzp", [128, 128], BF16)
            fw.op("pool", "memset", [], [bzpad], ap=zpad[:], constant=0.0)
            jobs = [("zT", "off", L, self.GF, self.bGF, 4352)]
            if do_ctx:
                jobs.append(("zTc", "offc", CTX, self.GFc, self.bGFc, 512))
            for (zn, on, n, GFt, bGFt, LEN) in jobs:
                cn = min(512, n)
                for ch in range(n // cn):
                    c0 = ch * cn
                    zt, bzt = zt_[ch % 2]
                    of, bof = of_[ch % 2]
                    fw.dma([], [bzt], out=zt[:, 0:cn], in_=I[zn][:, c0:c0 + cn])
                    fw.dma([], [bof], out=of[:, 0:cn], in_=I[on][:, c0:c0 + cn])
                    p1, bp1 = self.pf()
                    fw.op("pe", "matmul", [bw1, bzt], [bp1], out=p1[0:64, 0:cn], lhsT=w1[:], rhs=zt[:, 0:cn], start=True, stop=True)
                    fw.op("act", "activation", [bp1, bfb], [ba_], out=a_[:, 0:cn], in_=p1[0:64, 0:cn], func=AF.Identity, scale=fb[:, 2:3], bias=fb[:, 4:5])
                    self.sincos(a_[:, 0:cn], ba_, None, hh_[:, 0:cn], None, bhh, ki[:, 0:cn], bki, kf[:, 0:cn], bkf)
                    p2, bp2 = self.pf()
                    fw.op("pe", "matmul", [bw2, bhh], [bp2], out=p2[0:64, 0:cn], lhsT=w2[:], rhs=hh_[:, 0:cn], start=True, stop=True)
                    fw.op("act", "activation", [bp2, bfb], [ba_], out=a_[:, 0:cn], in_=p2[0:64, 0:cn], func=AF.Identity, scale=fb[:, 3:4], bias=fb[:, 5:6])
                    self.sincos(a_[:, 0:cn], ba_, None, hh_[:, 0:cn], None, bhh, ki[:, 0:cn], bki, kf[:, 0:cn], bkf)
                    for ct in range(4):
                        fr, bfr = frev[ct]
                        p3, bp3 = self.pf()
                        fw.op("pe", "matmul", [bw3, bhh], [bp3], out=p3[:, 0:cn], lhsT=w3[:, ct * 128:(ct + 1) * 128], rhs=hh_[:, 0:cn], start=True, stop=True)
                        fw.op("act", "activation", [bof, bnd], [bwin], out=win[:, 0:cn], in_=of[:, 0:cn], func=AF.Exp, scale=nd_[:, ct:ct + 1])
                        fw.op("dve", "tensor_tensor", [bp3, bwin], [bfr], out=fr[:, n - c0 - cn:n - c0][:, ::-1], in0=p3[:, 0:cn], in1=win[:, 0:cn], op=ALU.mult)
                for ct in range(4):
                    fr, bfr = frev[ct]
                    rs = slice(ct * 128, (ct + 1) * 128)
                    fw.dma([bfr], [bGFt[ct]], out=GFt[rs, 128:128 + n], in_=fr[:, 0:n], stream="d2")
                    fw.dma([bzpad], [bGFt[ct]], out=GFt[rs, 0:128], in_=zpad[:], stream="d2")
                    fw.dma([bzpad], [bGFt[ct]], out=GFt[rs, 128 + n:LEN], in_=zpad[:, 0:LEN - 128 - n], stream="d2")
            fw.flush()
        with ExitStack() as es:
            wx, bwx = self.sb(es, "hywx", [128, 8, 128], BF16)
            zraw, bzraw = self.sb(es, "hyzraw", [128, T], F32)
            A, bA = self.sb(es, "hyA", [128, T], F32)
            B, bB = self.sb(es, "hyB", [128, T], F32)
            yvb, byvb = self.sb(es, "hyyvb", [128, T], BF16)
            yout_ = [self.sb(es, f"hyyout{i}", [128, 512], BF16) for i in range(2)]
            Upad, bUpad = self.sb(es, "hyUpad", [128, 32, 128], BF16)
            Upc, bUpc = self.sb(es, "hyUpc", [128, 2, 128], BF16)
            TT_ = [self.sb(es, f"hyTT{i}", [128, 33 * 128], BF16) for i in range(2)]
            yrev, byrev = self.sb(es, "hyyrev", [128, 32, 128], BF16)
            tmp, btmp = self.sb(es, "hytmp", [128, 512], F32)
            swT, bswT = self.sb(es, "hyswT", [128, 12, 3], F32)
            sbT, bsbT = self.sb(es, "hysbT", [128, 12], F32)
            biasT, bbiasT = self.sb(es, "hybiasT", [128, 4], F32)
            fw.dma([], [bswT], out=swT[:], in_=I["hy_swT"][l])
            fw.dma([], [bsbT], out=sbT[:], in_=I["hy_sbT"][l])
            fw.dma([], [bbiasT], out=biasT[:], in_=I["hy_biasT"][l])
            segs = [(0, CTX), (CTX, T)]
            tti = 0
            for ct in range(4):
                for j, dst, bdst in ((1, A, bA), (2, B, bB), (0, B, bB)):
                    if j == 0:
                        fw.op("dve", "tensor_tensor", [bA, bB], [bA], out=A[:], in0=A[:], in1=B[:], op=ALU.mult)
                        fw.op("act", "copy", [bA], [byvb], out=yvb[:], in_=A[:])
                    cidx = j * 4 + ct
                    self.load_w_cols(wx[:], bwx, l, 2560 + j * 512 + ct * 128, 128)
                    for (t0, n) in TG:
                        hb = [bhT[t] for t in self.tiles_of(t0, n)]
                        pz, bpz = self.pf()
                        for k in range(8):
                            fw.op("pe", "matmul", hb + [bwx], [bpz], out=pz[:, 0:n], lhsT=wx[:, k, :], rhs=hT[:, k, t0:t0 + n], start=(k == 0), stop=(k == 7))
                        fw.op("act", "copy", [bpz], [bzraw], out=zraw[:, t0:t0 + n], in_=pz[:, 0:n])
                    fw.op("act", "activation", [bzraw, bswT, bsbT], [bdst], out=dst[:], in_=zraw[:], func=AF.Identity, scale=swT[:, cidx, 1:2], bias=sbT[:, cidx:cidx + 1])
                    for (s, e) in segs:
                        fw.op("dve", "scalar_tensor_tensor", [bzraw, bswT, bdst], [bdst], out=dst[:, s + 1:e], in0=zraw[:, s:e - 1], scalar=swT[:, cidx, 0:1], in1=dst[:, s + 1:e], op0=ALU.mult, op1=ALU.add)
                        fw.op("dve", "scalar_tensor_tensor", [bzraw, bswT, bdst], [bdst], out=dst[:, s:e - 1], in0=zraw[:, s + 1:e], scalar=swT[:, cidx, 2:3], in1=dst[:, s:e - 1], op0=ALU.mult, op1=ALU.add)
                convs = [(CTX, 32, 16, self.GF, self.bGF, 4352, Upad, bUpad)]
                if do_ctx:
                    convs.append((0, 2, 1, self.GFc, self.bGFc, 512, Upc, bUpc))
                for (tok0, nblk, nd, GFt, bGFt, LEN, Up, bUp) in convs:
                    nm = 2 * nd + 1
                    for g0 in range(0, nblk, 8):
                        gn = min(8, nblk - g0)
                        pb_, bpb = self.pb()
                        for b in range(gn):
                            c0 = tok0 + (g0 + b) * 128
                            fw.op("pe", "transpose", [byvb, self.b_identb], [bpb], out=pb_[:, b * 128:(b + 1) * 128], in_=yvb[:, c0:c0 + 128], identity=self.identb[:])
                        fw.op("act", "copy", [bpb], [bUp], out=Up[:, g0:g0 + gn, :], in_=pb_[:, 0:gn * 128].rearrange("p (b c) -> p b c", c=128))
                    for cg in range(8):
                        pc, bpc = self.pf()
                        for c16 in range(16):
                            cc = cg * 16 + c16
                            TT, bTT = TT_[tti % 2]
                            tti += 1
                            row = ct * 128 + cc
                            fw.dma([bGFt[ct]], [bTT], out=TT[:, 0:nm * 128], in_=bass.AP(GFt, row * LEN, [[1, 128], [1, nm * 128]]))
                            morder = [nd] + [m for m in range(nm) if m != nd]
                            for mi, m in enumerate(morder):
                                bo0 = max(0, nd - m)
                                bo1 = min(nblk, nblk + nd - m)
                                fw.op("pe", "matmul", [bTT, bUp], [bpc], out=pc[:, c16 * nblk + bo0:c16 * nblk + bo1], lhsT=TT[:, m * 128:(m + 1) * 128],
                                      rhs=Up[:, m + bo0 - nd:m + bo1 - nd, cc], start=(mi == 0), stop=(mi == nm - 1))
                        fw.op("act", "copy", [bpc], [byrev], out=yrev[:, 0:nblk, cg * 16:(cg + 1) * 16].rearrange("p b c -> p c b"),
                              in_=pc[:, 0:16 * nblk].rearrange("p (c b) -> p c b", b=nblk))
                    for g0 in range(0, nblk, 4):
                        gn = min(4, nblk - g0)
                        pt_, bpt = self.pf()
                        for b in range(gn):
                            fw.op("pe", "matmul", [byrev, self.b_antib], [bpt], out=pt_[:, b * 128:(b + 1) * 128], lhsT=yrev[:, g0 + b, :], rhs=self.antib[:], start=True, stop=True)
                        cs = slice(tok0 + g0 * 128, tok0 + (g0 + gn) * 128)
                        W_ = gn * 128
                        fw.op("dve", "scalar_tensor_tensor", [bA, bbiasT, bpt], [btmp], out=tmp[:, 0:W_], in0=A[:, cs], scalar=biasT[:, ct:ct + 1], in1=pt_[:, 0:W_], op0=ALU.mult, op1=ALU.add)
                        yout, byout = yout_[(g0 // 4) % 2]
                        fw.op("pool", "tensor_tensor", [btmp, bB], [byout], out=yout[:, 0:W_], in0=tmp[:, 0:W_], in1=B[:, cs], op=ALU.mult)
                        fw.dma([byout], [self.bYT[8 + ct]], out=self.YT[1024 + ct * 128:1024 + (ct + 1) * 128, cs], in_=yout[:, 0:W_], stream="d2")
                if not do_ctx:
                    yout, byout = yout_[0]
                    fw.op("pool", "memset", [], [byout], ap=yout[:, 0:CTX], constant=0.0)
                    fw.dma([byout], [self.bYT[8 + ct]], out=self.YT[1024 + ct * 128:1024 + (ct + 1) * 128, 0:CTX], in_=yout[:, 0:CTX], stream="d2")
            if self.debug:
                self.dump_yt("yhy%d" % l, 8, 4)
            fw.flush()

    def phase_merge(self, l, hT, bhT):
        fw, I = self.fw, self.I
        last = (l == DEPTH - 1)
        self.MTs = getattr(self, "MTs", None) or self.scratch("MTs", [D, T], BF16)
        bMTs = [Buf() for _ in TG]
        groups = [(gi, t0, n) for gi, (t0, n) in enumerate(TG) if not (last and gi == 0)]
        groups1 = [(gi, t0 + h_ * 256, 256) for (gi, t0, n) in groups for h_ in range(n // 256)]
        with ExitStack() as es:
            wbr, bwbr = self.sb(es, "mgwbr", [128, 12, D], BF16)
            wg, bwg = self.sb(es, "mgwg", [128, 8, 3072], BF16)
            fw.dma([], [bwbr], q="pool", stream="d1", out=wbr[:], in_=I["w_branch"][l].rearrange("(k p) d -> p k d", p=128))
            for nb in range(3):
                self.load_w_cols(wg[:, :, nb * 1024:(nb + 1) * 1024], bwg, l, 4096 + nb * 1024, 1024)
            yt_ = [self.sb(es, f"mgyt{i}", [128, 12, 256], BF16) for i in range(1)]
            macc, bmacc = self.sb(es, "mgmacc", [128, 8, 256], F32)
            mT, bmT = self.sb(es, "mgmT", [128, 8, 256], BF16)
            sg_ = [self.sb(es, f"mgsg{i}", [128, 256], F32) for i in range(2)]
            tp_ = [self.sb(es, f"mgtp{i}", [128, 256], F32) for i in range(2)]
            it = 0
            for (gi, t0, n) in groups1:
                hb = [bhT[t] for t in self.tiles_of(t0, n)]
                yt, byt = yt_[0]
                fw.dma(self.bYT, [byt], out=yt[:, :, 0:n], in_=self.YT[:, t0:t0 + n].rearrange("(k p) t -> p k t", p=128))
                for nb in range(3):
                    for dc in range(8):
                        sg, bsg = sg_[it % 2]; tp, btp = tp_[it % 2]
                        it += 1
                        pg, bpg = self.pf()
                        for k in range(8):
                            fw.op("pe", "matmul", hb + [bwg], [bpg], out=pg[:, 0:n], lhsT=wg[:, k, nb * 1024 + dc * 128:nb * 1024 + (dc + 1) * 128], rhs=hT[:, k, t0:t0 + n], start=(k == 0), stop=(k == 7))
                        pbn, bpbn = self.pf()
                        for k in range(4):
                            fw.op("pe", "matmul", [byt, bwbr], [bpbn], out=pbn[:, 0:n], lhsT=wbr[:, nb * 4 + k, dc * 128:(dc + 1) * 128], rhs=yt[:, nb * 4 + k, 0:n], start=(k == 0), stop=(k == 3))
                        fw.op("act", "activation", [bpg], [bsg], out=sg[:, 0:n], in_=pg[:, 0:n], func=AF.Sigmoid)
                        if nb == 0:
                            fw.op("dve", "tensor_tensor", [bpbn, bsg], [bmacc], out=macc[:, dc, 0:n], in0=pbn[:, 0:n], in1=sg[:, 0:n], op=ALU.mult)
                        else:
                            fw.op("dve", "tensor_tensor", [bpbn, bsg], [btp], out=tp[:, 0:n], in0=pbn[:, 0:n], in1=sg[:, 0:n], op=ALU.mult)
                            fw.op("pool", "tensor_tensor", [btp, bmacc], [bmacc], out=macc[:, dc, 0:n], in0=tp[:, 0:n], in1=macc[:, dc, 0:n], op=ALU.add)
                fw.op("act", "copy", [bmacc], [bmT], out=mT[:, :, 0:n], in_=macc[:, :, 0:n])
                fw.dma([bmT], [bMTs[gi]], out=self.MTs[:, t0:t0 + n].rearrange("(k p) t -> p k t", p=128), in_=mT[:, :, 0:n], stream="d2")
            fw.flush()
        with ExitStack() as es:
            wout, bwout = self.sb(es, "mgwout", [128, 8, D], BF16)
            fw.dma([], [bwout], q="pool", stream="d1", out=wout[:], in_=I["w_out"][l].rearrange("(k p) d -> p k d", p=128))
            wr, bwr = self.sb(es, "mgwr", [128, 8, 36], F32)
            fw.dma([], [bwr], out=wr[:], in_=I["moe_wr"][l].rearrange("(k p) c -> p k c", p=128))
            brB, bbrB = self.sb(es, "mgbrB", [128, 36], F32)
            fw.dma([], [bbrB], out=brB[:], in_=I["moe_br"][l:l + 1, :].to_broadcast([128, 36]))
            n2g, bn2g = self.sb(es, "mgn2g", [128, D], F32)
            G1t, bG1t = self.sb(es, "mgG1t", [128, 2, D], F32)
            fw.dma([self.b_GBd], [bG1t], out=G1t[:], in_=self.GBd[:, 0:2, :])
            fw.dma([], [bn2g], out=n2g[:], in_=I["norm2_g"][l:l + 1, :].to_broadcast([128, D]))
            mt_ = [self.sb(es, f"mgmt{i}", [128, 8, 512], BF16) for i in range(2)]
            x_ = [self.sb(es, f"mgx{i}", [128, D], F32) for i in range(2)]
            xn_ = [self.sb(es, f"mgxn{i}", [128, D], F32) for i in range(2)]
            xs2, bxs2 = self.sb(es, "mgxs2", [128, D], F32)
            tmpx, btmpx = self.sb(es, "mgtmpx", [128, D], F32)
            st_ = [self.sb(es, f"mgst{i}", [128, 2, 6], F32) for i in range(2)]
            mv_ = [self.sb(es, f"mgmv{i}", [128, 4], F32) for i in range(2)]
            rs_ = [self.sb(es, f"mgrs{i}", [128, 1], F32) for i in range(2)]
            h2f, bh2f = self.sb(es, "mgh2f", [128, 8, 128], F32)
            h2tok_ = [self.sb(es, f"mgh2t{i}", [128, D], BF16) for i in range(2)]
            for ii, (gi, t0, n) in enumerate(groups):
                w = 1 if gi == 0 else 0
                mt, bmt = mt_[ii % 2]
                fw.dma([bMTs[gi]], [bmt], out=mt[:, :, 0:n], in_=self.MTs[:, t0:t0 + n].rearrange("(k p) t -> p k t", p=128))
                for t in self.tiles_of(t0, n):
                    tt = t - t0 // 128
                    x, bx = x_[t % 2]; xn, bxn = xn_[t % 2]; st, bst = st_[t % 2]; mv, bmv = mv_[t % 2]; rs, brs = rs_[t % 2]
                    h2tok, bh2tok = h2tok_[t % 2]
                    fw.dma([self.bXR[t]], [bx], out=x[:], in_=self.XR[t * 128:(t + 1) * 128, :])
                    for half in range(2):
                        hs = slice(half * 512, (half + 1) * 512)
                        pm, bpm = self.pf()
                        for k in range(8):
                            fw.op("pe", "matmul", [bmt, bwout], [bpm], out=pm[:], lhsT=mt[:, k, tt * 128:(tt + 1) * 128], rhs=wout[:, k, hs], start=(k == 0), stop=(k == 7))
                        fw.op("dve", "tensor_tensor", [bpm, bG1t], [btmpx], out=tmpx[:, hs], in0=pm[:], in1=G1t[:, w, hs], op=ALU.mult)
                        fw.op("pool", "tensor_tensor", [btmpx, bx], [bxn], out=xn[:, hs], in0=tmpx[:, hs], in1=x[:, hs], op=ALU.add)
                    fw.dma([bxn], [self.bXR[t]], out=self.XR[t * 128:(t + 1) * 128, :], in_=xn[:], stream="d2")
                    self.rms_stats(xn, bxn, st, bst, mv, bmv, rs, brs)
                    fw.op("dve", "scalar_tensor_tensor", [bxn, brs, bn2g], [bxs2], out=xs2[:], in0=xn[:], scalar=rs[:, 0:1], in1=n2g[:], op0=ALU.mult, op1=ALU.mult)
                    for hf in range(2):
                        pt_, bpt = self.pf()
                        for kk in range(4):
                            k = hf * 4 + kk
                            fw.op("pe", "transpose", [bxs2, self.b_identf], [bpt], out=pt_[:, kk * 128:(kk + 1) * 128], in_=xs2[:, k * 128:(k + 1) * 128], identity=self.identf[:])
                        for kk in range(4):
                            k = hf * 4 + kk
                            fw.op("act", "activation", [bpt, self.b_modT, self.b_modP], [bh2f], out=h2f[:, k, :], in_=pt_[:, kk * 128:(kk + 1) * 128],
                                  func=AF.Identity, scale=self.modP[:, 32 + k, w:w + 1], bias=self.modT[:, 24 + k, w:w + 1])
                    fw.op("dve", "tensor_copy", [bh2f], [bhT[t]], out=hT[:, :, t * 128:(t + 1) * 128], in_=h2f[:])
                    pl, bpl = self.pf()
                    for k in range(8):
                        fw.op("pe", "matmul", [bh2f, bwr], [bpl], out=pl[:, 0:36], lhsT=h2f[:, k, :], rhs=wr[:, k, :], start=(k == 0), stop=(k == 7))
                    fw.op("dve", "tensor_tensor", [bpl, bbrB], [self.b_LG[t]], out=self.LG[:, t, :], in0=pl[:, 0:36], in1=brB[:], op=ALU.add)
                    pb_, bpb = self.pb()
                    for k in range(8):
                        fw.op("pe", "transpose", [bhT[t], self.b_identb], [bpb], out=pb_[:, k * 128:(k + 1) * 128], in_=hT[:, k, t * 128:(t + 1) * 128], identity=self.identb[:])
                    fw.op("act", "copy", [bpb], [bh2tok], out=h2tok[:], in_=pb_[:])
                    fw.dma([bh2tok], [self.bH2[t]], out=self.H2[t * 128:(t + 1) * 128, :], in_=h2tok[:], stream="d2")
            self.dump(f"LG{l}", self.LG[:], [128, NT, 36], self.b_LG)
            fw.flush()

    def phase_moe(self, l):
        fw, I = self.fw, self.I
        last = (l == DEPTH - 1)
        tiles = list(range(2, NT)) if last else list(range(NT))
        NB = 2 * len(tiles) + 32
        IOA = bass.IndirectOffsetOnAxis
        with ExitStack() as es:
            M1, bM1 = self.sb(es, "moM1", [128, NT, 32], F32)
            M2, bM2 = self.sb(es, "moM2", [128, NT, 32], F32)
            MS, bMS = self.sb(es, "moMS", [128, NT, 32], BF16)
            W12, bW12 = self.sb(es, "moW12", [128, NT, 2], F32)
            RK, bRK = self.sb(es, "moRK", [128, NT, 32], F32)
            DST, bDST = self.sb(es, "moDST", [128, NT, 2], F32)
            DSTi, bDSTi = self.sb(es, "moDSTi", [128, NT, 2], I32)
            sc, bsc = self.sb(es, "mosc", [128, 64], F32)
            ig, big = self.sb(es, "moig", [128, 8], F32)
            mx8, bmx8 = self.sb(es, "momx8", [128, 8], F32)
            E12, bE12 = self.sb(es, "moE12", [128, 2, 8], F32)
            trisb, btrisb = self.sb(es, "motris", [128, 128], BF16)
            onesb, bonesb = self.sb(es, "moones", [128, 128], BF16)
            trf, btrf = self.sb(es, "motrf", [128, 128], F32)
            fw.dma([], [btrf], out=trf[:], in_=I["tris"].ap())
            fw.op("dve", "tensor_copy", [btrf], [btrisb], out=trisb[:], in_=trf[:])
            fw.op("pool", "memset", [], [bonesb], ap=onesb[:], constant=1.0)
            run, brun = self.sb(es, "morun", [128, 32], F32)
            fw.op("pool", "memset", [], [brun], ap=run[:], constant=0.0)
            G = sc[:, 0:4]; EG = sc[:, 4:8]
            gmax = sc[:, 8:9]; ngmax = sc[:, 9:10]; sgm = sc[:, 10:11]; ptop = sc[:, 11:12]
            nm1 = sc[:, 12:13]; e21 = sc[:, 13:14]; w1c = sc[:, 14:15]; w2c = sc[:, 15:16]
            S = [bsc]
            for t in tiles:
                Lg = self.LG[:, t, 0:4]
                Le = self.LG[:, t, 4:36]
                bl = self.b_LG[t]
                fw.op("dve", "tensor_reduce", [bl], S, out=gmax, in_=Lg, axis=AX.X, op=ALU.max)
                fw.op("dve", "tensor_scalar", [bl] + S, S, out=G, in0=Lg, scalar1=gmax, scalar2=None, op0=ALU.is_equal)
                fw.op("dve", "tensor_scalar", S, S, out=ngmax, in0=gmax, scalar1=-1.0, scalar2=None, op0=ALU.mult)
                fw.op("act", "activation", [bl] + S, S, out=EG, in_=Lg, func=AF.Exp, bias=ngmax, scale=1.0)
                fw.op("dve", "tensor_reduce", S, S, out=sgm, in_=EG, axis=AX.X, op=ALU.add)
                fw.op("dve", "reciprocal", S, S, out=ptop, in_=sgm)
                fw.op("dve", "tensor_scalar", [bl] + S, [big], out=ig[:], in0=Le[:, 0:8], scalar1=sc[:, 0:1], scalar2=None, op0=ALU.mult)
                for g in range(1, 4):
                    fw.op("dve", "scalar_tensor_tensor", [bl, big] + S, [big], out=ig[:], in0=Le[:, 8 * g:8 * g + 8], scalar=sc[:, g:g + 1], in1=ig[:], op0=ALU.mult, op1=ALU.add)
                fw.op("dve", "max", [big], [bmx8], out=mx8[:], in_=ig[:])
                fw.op("dve", "tensor_scalar", [big, bmx8], [bE12], out=E12[:, 0, :], in0=ig[:], scalar1=mx8[:, 0:1], scalar2=None, op0=ALU.is_equal)
                fw.op("dve", "tensor_scalar", [big, bmx8], [bE12], out=E12[:, 1, :], in0=ig[:], scalar1=mx8[:, 1:2], scalar2=None, op0=ALU.is_equal)
                fw.op("dve", "tensor_scalar", [bmx8] + S, S, out=nm1, in0=mx8[:, 0:1], scalar1=-1.0, scalar2=None, op0=ALU.mult)
                fw.op("act", "activation", [bmx8] + S, S, out=e21, in_=mx8[:, 1:2], func=AF.Exp, bias=nm1, scale=1.0)
                fw.op("dve", "tensor_scalar", S, S, out=w1c, in0=e21, scalar1=1.0, scalar2=None, op0=ALU.add)
                fw.op("dve", "reciprocal", S, S, out=w1c, in_=w1c)
                fw.op("dve", "tensor_tensor", S, S, out=w2c, in0=e21, in1=w1c, op=ALU.mult)
                fw.op("dve", "tensor_scalar", S, [bW12], out=W12[:, t, :], in0=sc[:, 14:16], scalar1=ptop, scalar2=None, op0=ALU.mult)
                for g in range(4):
                    fw.op("dve", "tensor_scalar", [bE12] + S, [bM1], out=M1[:, t, 8 * g:8 * g + 8], in0=E12[:, 0, :], scalar1=sc[:, g:g + 1], scalar2=None, op0=ALU.mult)
                    fw.op("dve", "tensor_scalar", [bE12] + S, [bM2], out=M2[:, t, 8 * g:8 * g + 8], in0=E12[:, 1, :], scalar1=sc[:, g:g + 1], scalar2=None, op0=ALU.mult)
                fw.op("dve", "tensor_tensor", [bM1, bM2], [bMS], out=MS[:, t, :], in0=M1[:, t, :], in1=M2[:, t, :], op=ALU.add)
                pr, bpr = self.pf()
                fw.op("pe", "matmul", [btrisb, bMS], [bpr], out=pr[:, 0:32], lhsT=trisb[:], rhs=MS[:, t, :], start=True, stop=True)
                fw.op("pe", "matmul", [bonesb, bMS], [bpr], out=pr[:, 32:64], lhsT=onesb[:], rhs=MS[:, t, :], start=True, stop=True)
                fw.op("dve", "tensor_tensor", [bpr, brun], [bRK], out=RK[:, t, :], in0=pr[:, 0:32], in1=run[:], op=ALU.add)
                fw.op("dve", "tensor_tensor", [bpr, brun], [brun], out=run[:], in0=pr[:, 32:64], in1=run[:], op=ALU.add)
            nbi, bnbi = self.sb(es, "monbi", [128, 32], I32)
            nbf, bnbf = self.sb(es, "monbf", [128, 32], F32)
            one32, bone32 = self.sb(es, "moone32", [128, 32], F32)
            pend, bpend = self.sb(es, "mopend", [128, 32], F32)
            pst, bpst = self.sb(es, "mopst", [128, 32], F32)
            fw.op("pool", "memset", [], [bone32], ap=one32[:], constant=1.0)
            fw.op("dve", "tensor_scalar", [brun], [bnbi], out=nbi[:], in0=run[:], scalar1=1.0 / 128.0, scalar2=0.49609375, op0=ALU.mult, op1=ALU.add)
            fw.op("dve", "tensor_copy", [bnbi], [bnbf], out=nbf[:], in_=nbi[:])
            fw.op("dve", "tensor_tensor_scan", [bone32, bnbf], [bpend], out=pend[:], data0=one32[:], data1=nbf[:], initial=0.0, op0=ALU.mult, op1=ALU.add)
            fw.op("dve", "tensor_tensor", [bpend, bnbf], [bpst], out=pst[:], in0=pend[:], in1=nbf[:], op=ALU.subtract)
            fw.op("dve", "tensor_scalar", [bpst], [bpst], out=pst[:], in0=pst[:], scalar1=128.0, scalar2=None, op0=ALU.mult)
            tmp32, btmp32 = self.sb(es, "motmp32", [128, 32], F32)
            for t in tiles:
                fw.op("dve", "tensor_tensor", [bRK, bpst], [btmp32], out=tmp32[:], in0=RK[:, t, :], in1=pst[:], op=ALU.add)
                fw.op("dve", "tensor_tensor", [btmp32, bM1], [bRK], out=RK[:, t, :], in0=tmp32[:], in1=M1[:, t, :], op=ALU.mult)
                fw.op("dve", "tensor_reduce", [bRK], [bDST], out=DST[:, t, 0:1], in_=RK[:, t, :], axis=AX.X, op=ALU.add)
                fw.op("dve", "tensor_tensor", [btmp32, bM2], [bRK], out=RK[:, t, :], in0=tmp32[:], in1=M2[:, t, :], op=ALU.mult)
                fw.op("dve", "tensor_reduce", [bRK], [bDST], out=DST[:, t, 1:2], in_=RK[:, t, :], axis=AX.X, op=ALU.add)
            fw.op("dve", "tensor_copy", [bDST], [bDSTi], out=DSTi[:, tiles[0]:NT, :], in_=DST[:, tiles[0]:NT, :])
            iob, biob = self.sb(es, "moiob", [128, NBLK], F32)
            fw.dma([], [biob], out=iob[:], in_=I["iotab"].ap())
            cmp_, bcmp = self.sb(es, "mocmp", [128, NBLK, 32], F32)
            be, bbe = self.sb(es, "mobe", [128, NBLK], F32)
            fw.op("dve", "tensor_tensor", [biob, bpend], [bcmp], out=cmp_[:, 0:NB, :], in0=iob[:, 0:NB].unsqueeze(2).to_broadcast([128, NB, 32]),
                  in1=pend[:].unsqueeze(1).to_broadcast([128, NB, 32]), op=ALU.is_ge)
            fw.op("dve", "tensor_reduce", [bcmp], [bbe], out=be[:, 0:NB], in_=cmp_[:, 0:NB, :], axis=AX.X, op=ALU.add)
            fw.op("dve", "tensor_scalar", [bbe], [bbe], out=be[:, 0:NB], in0=be[:, 0:NB], scalar1=31.0, scalar2=None, op0=ALU.min)
            b1f, bb1f = self.sb(es, "mob1f", [128, NBLK], F32)
            b2f, bb2f = self.sb(es, "mob2f", [128, NBLK], F32)
            wi1, bwi1 = self.sb(es, "mowi1", [128, NBLK, 8], I32)
            wi2, bwi2 = self.sb(es, "mowi2", [128, NBLK, 4], I32)
            fw.op("dve", "tensor_scalar", [bbe], [bb1f], out=b1f[:, 0:NB], in0=be[:, 0:NB], scalar1=1024.0, scalar2=None, op0=ALU.mult)
            fw.op("dve", "tensor_scalar", [bb1f, self.b_rcol], [bb1f], out=b1f[:, 0:NB], in0=b1f[:, 0:NB], scalar1=self.rcol[:, 3:4], scalar2=None, op0=ALU.add)
            fw.op("dve", "tensor_scalar", [bbe], [bb2f], out=b2f[:, 0:NB], in0=be[:, 0:NB], scalar1=512.0, scalar2=None, op0=ALU.mult)
            fw.op("dve", "tensor_scalar", [bb2f, self.b_rcol], [bb2f], out=b2f[:, 0:NB], in0=b2f[:, 0:NB], scalar1=self.rcol[:, 3:4], scalar2=None, op0=ALU.add)
            for k in range(8):
                fw.op("dve", "tensor_scalar", [bb1f], [bwi1], out=wi1[:, 0:NB, k], in0=b1f[:, 0:NB], scalar1=128.0 * k + l * 32768.0, scalar2=None, op0=ALU.add)
            for k in range(4):
                fw.op("dve", "tensor_scalar", [bb2f], [bwi2], out=wi2[:, 0:NB, k], in0=b2f[:, 0:NB], scalar1=128.0 * k + l * 16384.0, scalar2=None, op0=ALU.add)
            self.dump(f"DST{l}", DST[:], [128, NT, 2], [bDST])
            self.dump(f"W12{l}", W12[:], [128, NT, 2], [bW12])
            self.dump(f"BE{l}", be[:], [128, NBLK], [bbe])
            hx_ = [self.sb(es, f"mohx{i}", [128, D], BF16) for i in range(2)]
            zt_, bzt_ = self.sb(es, "mozero", [128, 4, D], BF16)
            fw.op("pool", "memset", [], [bzt_], ap=zt_[:], constant=0.0)
            for b4 in range(0, NB, 4):
                nb4 = min(4, NB - b4)
                fw.dma([bzt_], [self.bMB], out=self.MB[b4 * 128:(b4 + nb4) * 128, :].rearrange("(b p) d -> p b d", p=128), in_=zt_[:, 0:nb4, :], stream="d2")
            for t in tiles:
                hx, bhx = hx_[t % 2]
                fw.dma([self.bH2[t]], [bhx], out=hx[:], in_=self.H2[t * 128:(t + 1) * 128, :])
                for k in range(2):
                    fw.dmaf(lambda e, hx=hx, t=t, k=k: e.indirect_dma_start(out=self.MB[:, :], out_offset=IOA(ap=DSTi[:, t, k:k + 1], axis=0), in_=hx[:, :], in_offset=None),
                            [bhx, bDSTi], [self.bMB])
            X_ = [self.sb(es, f"moX{i}", [128, D], BF16) for i in range(2)]
            XT_ = [self.sb(es, f"moXT{i}", [128, 8, 128], BF16) for i in range(2)]
            w1g_ = [self.sb(es, f"mow1g{i}", [128, 8, 512], BF16) for i in range(2)]
            w3g_ = [self.sb(es, f"mow3g{i}", [128, 8, 512], BF16) for i in range(2)]
            w2g_ = [self.sb(es, f"mow2g{i}", [128, 4, D], BF16) for i in range(2)]
            sl_ = [self.sb(es, f"mosl{i}", [128, 512], F32) for i in range(2)]
            gT_ = [self.sb(es, f"mogT{i}", [128, 512], BF16) for i in range(2)]
            yb_ = [self.sb(es, f"moyb{i}", [128, D], F32) for i in range(2)]
            w1d, w3d, w2d = I["moe_w1"], I["moe_w3"], I["moe_w2"]
            for blk in range(NB):
                X, bX = X_[blk % 2]; XT, bXT = XT_[blk % 2]
                w1g, bw1g = w1g_[blk % 2]; w3g, bw3g = w3g_[blk % 2]; w2g, bw2g = w2g_[blk % 2]
                sl, bsl = sl_[blk % 2]; gT, bgT = gT_[blk % 2]; yb, byb = yb_[blk % 2]
                fw.dma([self.bMB], [bX], out=X[:], in_=self.MB[blk * 128:(blk + 1) * 128, :])
                for k in range(8):
                    fw.dmaf(lambda e, w1g=w1g, k=k, blk=blk: e.indirect_dma_start(out=w1g[:, k, :], out_offset=None, in_=w1d[:, :], in_offset=IOA(ap=wi1[:, blk, k:k + 1], axis=0)),
                            [bwi1], [bw1g])
                    fw.dmaf(lambda e, w3g=w3g, k=k, blk=blk: e.indirect_dma_start(out=w3g[:, k, :], out_offset=None, in_=w3d[:, :], in_offset=IOA(ap=wi1[:, blk, k:k + 1], axis=0)),
                            [bwi1], [bw3g])
                for k in range(4):
                    fw.dmaf(lambda e, w2g=w2g, k=k, blk=blk: e.indirect_dma_start(out=w2g[:, k, :], out_offset=None, in_=w2d[:, :], in_offset=IOA(ap=wi2[:, blk, k:k + 1], axis=0)),
                            [bwi2], [bw2g])
                pb_, bpb = self.pb()
                for k in range(8):
                    fw.op("pe", "transpose", [bX, self.b_identb], [bpb], out=pb_[:, k * 128:(k + 1) * 128], in_=X[:, k * 128:(k + 1) * 128], identity=self.identb[:])
                fw.op("act", "copy", [bpb], [bXT], out=XT[:].rearrange("p k t -> p (k t)"), in_=pb_[:])
                p1, bp1 = self.pf()
                p3, bp3 = self.pf()
                for hc in range(4):
                    for k in range(8):
                        fw.op("pe", "matmul", [bw1g, bXT], [bp1], out=p1[:, hc * 128:(hc + 1) * 128], lhsT=w1g[:, k, hc * 128:(hc + 1) * 128], rhs=XT[:, k, :], start=(k == 0), stop=(k == 7))
                for hc in range(4):
                    for k in range(8):
                        fw.op("pe", "matmul", [bw3g, bXT], [bp3], out=p3[:, hc * 128:(hc + 1) * 128], lhsT=w3g[:, k, hc * 128:(hc + 1) * 128], rhs=XT[:, k, :], start=(k == 0), stop=(k == 7))
                fw.op("act", "activation", [bp1], [bsl], out=sl[:], in_=p1[:], func=AF.Silu)
                fw.op("dve", "tensor_tensor", [bp3, bsl], [bgT], out=gT[:], in0=p3[:], in1=sl[:], op=ALU.mult)
                for half in range(2):
                    py, bpy = self.pf()
                    for hc in range(4):
                        fw.op("pe", "matmul", [bgT, bw2g], [bpy], out=py[:], lhsT=gT[:, hc * 128:(hc + 1) * 128], rhs=w2g[:, hc, half * 512:(half + 1) * 512], start=(hc == 0), stop=(hc == 3))
                    fw.op("act", "copy", [bpy], [byb], out=yb[:, half * 512:(half + 1) * 512], in_=py[:])
                fw.dma([byb], [self.bMY], out=self.MY[blk * 128:(blk + 1) * 128, :], in_=yb[:], stream="d2")
            y1_ = [self.sb(es, f"moy1{i}", [128, D], F32) for i in range(2)]
            G2t, bG2t = self.sb(es, "moG2t", [128, 2, D], F32)
            fw.dma([self.b_GBd], [bG2t], out=G2t[:], in_=self.GBd[:, 2:4, :])
            y2_ = [self.sb(es, f"moy2{i}", [128, D], F32) for i in range(2)]
            xx_ = [self.sb(es, f"moxx{i}", [128, D], F32) for i in range(2)]
            st_ = [self.sb(es, f"most{i}", [128, 2, 6], F32) for i in range(2)]
            mv_ = [self.sb(es, f"momv{i}", [128, 4], F32) for i in range(2)]
            rs_ = [self.sb(es, f"mors{i}", [128, 1], F32) for i in range(2)]
            if last:
                fng, bfng = self.sb(es, "mofng", [128, D], F32)
                fw.dma([], [bfng], out=fng[:], in_=I["final_norm_g"][0:1, :].to_broadcast([128, D]))
            for t in tiles:
                w = 1 if t < 2 else 0
                y1, by1 = y1_[t % 2]; y2, by2 = y2_[t % 2]; xx, bxx = xx_[t % 2]
                fw.dmaf(lambda e, y1=y1, t=t: e.indirect_dma_start(out=y1[:, :], out_offset=None, in_=self.MY[:, :], in_offset=IOA(ap=DSTi[:, t, 0:1], axis=0)), [self.bMY, bDSTi], [by1])
                fw.dmaf(lambda e, y2=y2, t=t: e.indirect_dma_start(out=y2[:, :], out_offset=None, in_=self.MY[:, :], in_offset=IOA(ap=DSTi[:, t, 1:2], axis=0)), [self.bMY, bDSTi], [by2])
                fw.dma([self.bXR[t]], [bxx], out=xx[:], in_=self.XR[t * 128:(t + 1) * 128, :])
                fw.op("dve", "tensor_scalar", [by1, bW12], [by1], out=y1[:], in0=y1[:], scalar1=W12[:, t, 0:1], scalar2=None, op0=ALU.mult)
                fw.op("dve", "scalar_tensor_tensor", [by2, bW12, by1], [by1], out=y1[:], in0=y2[:], scalar=W12[:, t, 1:2], in1=y1[:], op0=ALU.mult, op1=ALU.add)
                fw.op("pool", "tensor_tensor", [by1, bG2t], [by1], out=y1[:], in0=y1[:], in1=G2t[:, w, :], op=ALU.mult)
                fw.op("pool", "tensor_tensor", [by1, bxx], [bxx], out=xx[:], in0=y1[:], in1=xx[:], op=ALU.add)
                if not last:
                    fw.dma([bxx], [self.bXR[t]], out=self.XR[t * 128:(t + 1) * 128, :], in_=xx[:], stream="d2")
                else:
                    st, bst = st_[t % 2]; mv, bmv = mv_[t % 2]; rs, brs = rs_[t % 2]
                    self.rms_stats(xx, bxx, st, bst, mv, bmv, rs, brs)
                    fw.op("dve", "scalar_tensor_tensor", [bxx, brs, bfng], [by2], out=y2[:], in0=xx[:], scalar=rs[:, 0:1], in1=fng[:], op0=ALU.mult, op1=ALU.mult)
                    fw.dma([by2], [self.bout], out=self.out[(t - 2) * 128:(t - 1) * 128, :], in_=y2[:], stream="d2")
            fw.flush()

    def phase_final(self):
        pass


def prep_inputs(inp, b, keys=None):
    f = np.float32
    d = {}
    d.update(_consts())
    d["x"] = np.ascontiguousarray(inp["x"][b])
    d["ctx"] = np.ascontiguousarray(inp["ctx"][b])
    cc = np.stack([inp["c"][b], inp["c_ctx"]], -1)
    d["cT"] = np.ascontiguousarray(cc.reshape(8, 128, 2).transpose(1, 0, 2))
    d["w_mod"] = inp["w_mod"]
    d["b_mod"] = inp["b_mod"]
    d["bmodT"] = np.ascontiguousarray(inp["b_mod"].reshape(DEPTH, 48, 128).transpose(0, 2, 1))
    selw = np.zeros((2, 2, 128), f)
    selw[0, 0] = 1.0
    selw[1, 1] = 1.0
    d["selw"] = selw
    d["norm1_g"] = inp["norm1_g"]
    d["norm2_g"] = inp["norm2_g"]
    d["final_norm_g"] = inp["final_norm_g"].reshape(1, D)
    d["w_in"] = inp["w_in"]
    d["ret_log_decay"] = inp["ret_log_decay"].reshape(DEPTH, 8)
    d["ret_gn_g"] = inp["ret_gn_g"]
    def pairT(a):
        return np.ascontiguousarray(a.reshape(DEPTH, 2, 16, 128).transpose(0, 1, 3, 2))
    d["s5_lre"] = pairT(inp["s5_lambda_re"])
    d["s5_lim"] = pairT(inp["s5_lambda_im"])
    d["s5_ldt"] = pairT(np.repeat(inp["s5_log_dt"][..., None], 64, -1))
    sB = np.zeros((DEPTH, 2, 2, 128, 4, 2, 128), f)
    sC = np.zeros((DEPTH, 2, 2, 128, 16, 64), f)
    for ri, (bk, ck) in enumerate((("s5_b_re", "s5_c_re"), ("s5_b_im", "s5_c_im"))):
        Bm = inp[bk]
        Cm = inp[ck]
        for pt in range(16):
            o, q4 = pt // 4, pt % 4
            for g2 in range(2):
                g = 2 * pt + g2
                sB[:, :, ri, q4 * 32 + g2 * 16:q4 * 32 + g2 * 16 + 16, o, q4 % 2, g2 * 64:(g2 + 1) * 64] = Bm[:, :, g].transpose(0, 1, 3, 2)
                sC[:, :, ri, g2 * 64:(g2 + 1) * 64, pt, (q4 % 2) * 32 + g2 * 16:(q4 % 2) * 32 + g2 * 16 + 16] = Cm[:, :, g].transpose(0, 1, 3, 2)
    d["s5_B"] = sB
    d["s5_C"] = sC
    d["s5_dT"] = np.ascontiguousarray(inp["s5_d"].reshape(DEPTH, 4, 128).transpose(0, 2, 1))
    d["s5_glu_w1"] = inp["s5_glu_w1"]
    d["s5_glu_w2"] = inp["s5_glu_w2"]
    d["hy_swT"] = np.ascontiguousarray(inp["hy_short_w"].transpose(0, 2, 1).reshape(DEPTH, 12, 128, 3).transpose(0, 2, 1, 3))
    d["hy_sbT"] = np.ascontiguousarray(inp["hy_short_b"].reshape(DEPTH, 12, 128).transpose(0, 2, 1))
    d["hy_biasT"] = np.ascontiguousarray(inp["hy_bias"].reshape(DEPTH, 4, 128).transpose(0, 2, 1))
    d["hy_f_w1"] = inp["hy_f_w1"]
    d["hy_f_w2"] = inp["hy_f_w2"]
    d["hy_f_w3"] = inp["hy_f_w3"]
    d["hy_f_b1"] = inp["hy_f_b1"].reshape(DEPTH, 64, 1)
    d["hy_f_b2"] = inp["hy_f_b2"].reshape(DEPTH, 64, 1)
    d["hy_f_fr"] = np.ascontiguousarray(inp["hy_f_freq"].transpose(0, 2, 1))
    d["w_branch"] = inp["w_branch"].reshape(DEPTH, 1536, D)
    d["w_out"] = inp["w_out"]
    d["moe_wr"] = np.ascontiguousarray(np.concatenate([inp["moe_w_group"], inp["moe_w_expert"]], -1))
    d["moe_br"] = np.ascontiguousarray(np.concatenate([inp["moe_b_group"], inp["moe_b_expert"]], -1))
    d["moe_w1"] = inp["moe_w1"].reshape(DEPTH * 32 * 1024, 512)
    d["moe_w3"] = inp["moe_w3"].reshape(DEPTH * 32 * 1024, 512)
    d["moe_w2"] = inp["moe_w2"].reshape(DEPTH * 32 * 512, 1024)
    if keys is not None:
        d = {k: v for k, v in d.items() if k in keys}
    return d


_NC_CACHE = {}


def run_cores(inputs, n_cores=8, debug=None, stop=None, keys=None):
    per_core = [prep_inputs(inputs, c % 4, keys) for c in range(n_cores)]
    bd = Builder(debug=debug)
    bd.stop = stop
    import os
    bd.s5_cut = int(os.environ.get("S5_CUT", "0"))
    if os.environ.get("S5_LIM"):
        bd.s5_lim = tuple(int(v) for v in os.environ["S5_LIM"].split(","))
    nc = bd.build(per_core[0])
    res = run_bass_kernel_spmd(nc, per_core, core_ids=list(range(n_cores)))
    return res, bd


def kernel(**inputs):
    inputs = {k: np.asarray(v) for k, v in inputs.items()}
    res, bd = run_cores(inputs, n_cores=4)
    out = np.stack([res.results[b]["out"] for b in range(4)], 0)
    return out.astype(np.float32)
```

```python
import math
from contextlib import ExitStack
import numpy as np
import ml_dtypes
import concourse.bass as bass
import concourse.mybir as mybir
from concourse.bass_utils import run_bass_kernel_spmd

F32 = mybir.dt.float32
BF16 = mybir.dt.bfloat16
I32 = mybir.dt.int32
ALU = mybir.AluOpType
AF = mybir.ActivationFunctionType
AX = mybir.AxisListType

D = 1024
NT = 34
T = NT * 128
CTX = 256
L = 4096
DEPTH = 2
TG = [(0, 256)] + [(256 + 512 * i, 512) for i in range(8)]
TWO_PI = 2.0 * math.pi
NBLK = 100


class Buf:
    __slots__ = ("name", "lw", "rd")

    def __init__(self, name=""):
        self.name = name
        self.lw = None
        self.rd = {}


class FW:
    ENG = ("pe", "dve", "act", "pool", "sp")
    STREAMS = {"d0": 12, "d1": 16, "d2": 8, "d3": 2}
    ENGNAME = {"pe": "tensor", "dve": "vector", "act": "scalar", "pool": "gpsimd", "sp": "sync"}

    def __init__(self, nc, sems):
        self.nc = nc
        self.sems = sems
        self.ops = {e: [] for e in self.ENG}
        self.cnt = {s: 0 for s in self.sem_names()}
        self.seen = {e: {} for e in self.ENG}
        self.n_ops = 0
        self.ndma = {s: 0 for s in self.STREAMS}

    @classmethod
    def sem_names(cls):
        r = [e for e in cls.ENG if e != "sp"]
        for s, k in cls.STREAMS.items():
            r += [f"{s}_{i}" for i in range(k)]
        return r

    def _emit(self, eng, sem, inc, fn, reads, writes, nosync_same=False, extra_wait=None):
        waits = {}

        def need(sv):
            if sv is None:
                return
            s, v = sv
            if nosync_same and s == eng:
                return
            if waits.get(s, 0) < v:
                waits[s] = v
        if extra_wait is not None:
            need(extra_wait)
        for b in reads:
            need(b.lw)
        for b in writes:
            need(b.lw)
            for s, v in b.rd.items():
                need((s, v))
        wl = []
        for s, v in waits.items():
            if self.seen[eng].get(s, 0) < v:
                self.seen[eng][s] = v
                wl.append((s, v))
        self.cnt[sem] += inc
        v = self.cnt[sem]
        for b in reads:
            if b.rd.get(sem, 0) < v:
                b.rd[sem] = v
        for b in writes:
            b.lw = (sem, v)
            b.rd = {}
        self.ops[eng].append((wl, fn, sem, inc))
        self.n_ops += 1

    def op(self, eng, name, R, W, **kw):
        self._emit(eng, eng, 1, lambda e: getattr(e, name)(**kw), R, W, nosync_same=(eng == "pe"))

    def opf(self, eng, fn, R, W):
        self._emit(eng, eng, 1, fn, R, W, nosync_same=(eng == "pe"))

    def _dma_sem(self, stream):
        i = self.ndma[stream]
        self.ndma[stream] += 1
        sem = f"{stream}_{i % self.STREAMS[stream]}"
        return sem, (sem, self.cnt[sem])

    def dma(self, R, W, q="sp", stream="d0", **kw):
        sem, ew = self._dma_sem(stream)
        self._emit(q, sem, 16, lambda e: e.dma_start(**kw), R, W, extra_wait=ew)

    def dmaf(self, fn, R, W, q="pool", stream="d1"):
        sem, ew = self._dma_sem(stream)
        self._emit(q, sem, 16, fn, R, W, extra_wait=ew)

    def flush(self):
        nc = self.nc
        final = dict(self.cnt)
        sems = self.sems
        comp = ["pe"]
        if not hasattr(self, "newcnt"):
            self.newcnt = {s: 0 for s in comp}
            self.oldbase = {s: 0 for s in comp}
            self.lastmap = {s: {0: 0} for s in comp}
        needed = {s: set() for s in comp}
        for e in self.ENG:
            for wl, fn, sem, inc in self.ops[e]:
                for s_, v in wl:
                    if s_ in needed:
                        needed[s_].add(v)
        vmap = {s: dict(self.lastmap[s]) for s in comp}
        plan = {}
        for e in comp:
            old = self.oldbase[e]
            ops = self.ops[e]
            marks = []
            for idx, (wl, fn, sem, inc) in enumerate(ops):
                if sem != e:
                    marks.append(False)
                    continue
                old += 1
                ck = (old in needed[e]) or (old == final[e])
                if ck:
                    self.newcnt[e] += 1
                    vmap[e][old] = self.newcnt[e]
                marks.append(ck)
            plan[e] = marks
            vmap[e][final[e]] = self.newcnt[e]
            self.oldbase[e] = final[e]
            self.lastmap[e] = {final[e]: self.newcnt[e]}

        def tr(s_, v):
            if s_ in vmap:
                return vmap[s_][v]
            return v
        with nc.Block() as block:
            for e in self.ENG:
                ops = self.ops[e]
                marks = plan.get(e)

                def body(engine, ops=ops, e=e, marks=marks):
                    for idx, (wl, fn, sem, inc) in enumerate(ops):
                        for s_, v in wl:
                            engine.wait_ge(sems[s_], tr(s_, v))
                        ins = fn(engine)
                        if sem in vmap:
                            if marks[idx]:
                                ins.then_inc(sems[sem], 1)
                        else:
                            ins.then_inc(sems[sem], inc)
                    for s_, v in final.items():
                        if v > 0:
                            engine.wait_ge(sems[s_], tr(s_, v))
                getattr(block, self.ENGNAME[e])(body)
        for e in self.ENG:
            self.ops[e] = []
            self.seen[e] = dict(final)


def _consts():
    c = {}
    c["ident_f"] = np.eye(128, dtype=np.float32)
    c["antiid_f"] = np.ascontiguousarray(np.eye(128, dtype=np.float32)[::-1])
    rows = L // 64
    quarter = D // 4
    omega = (1.0 / (np.float32(10000.0) ** (np.arange(quarter, dtype=np.float32) / np.float32(quarter)))).astype(np.float32)
    ang_r = np.arange(rows, dtype=np.float32)[:, None] * omega
    ang_c = np.arange(64, dtype=np.float32)[:, None] * omega
    emb_r = np.concatenate([np.sin(ang_r), np.cos(ang_r)], -1)
    emb_c = np.concatenate([np.sin(ang_c), np.cos(ang_c)], -1)
    pe = np.concatenate([np.broadcast_to(emb_r[:, None, :], (rows, 64, D // 2)),
                         np.broadcast_to(emb_c[None, :, :], (rows, 64, D // 2))], -1)
    c["pe"] = np.ascontiguousarray(pe.reshape(L, D).astype(np.float32))
    j = np.arange(128, dtype=np.float32)[:, None]
    i = np.arange(128, dtype=np.float32)[None, :]
    rt = np.zeros((128, 6, 128), np.float32)
    rt[:, 0] = np.maximum(i - j, 0)
    rt[:, 1] = np.maximum(j - i, 0)
    rt[:, 2] = (i >= j)
    rt[:, 3] = (j > i)
    rt[:, 4] = i + 1.0
    rt[:, 5] = 128.0 - i
    c["rtab"] = rt
    rc = np.zeros((128, 6), np.float32)
    rc[:, 0] = 127.0 - np.arange(128)
    rc[:, 1] = np.arange(128)
    rc[:, 2] = 128.0
    rc[:, 3] = np.arange(128)
    rc[:, 4] = np.arange(128) + 1.0
    rc[:, 5] = 128.0 - np.arange(128)
    c["rcol"] = rc
    for nm, n in (("zT", L), ("zTc", CTX)):
        t = np.linspace(0.0, 1.0, n, dtype=np.float32)[:, None]
        w = (2.0 * math.pi * np.arange(n, dtype=np.float32)[:, None] / n).astype(np.float32)
        f = np.linspace(1e-4, 15, 16, dtype=np.float32)[None]
        z = np.concatenate([t, np.cos(f * w), -np.sin(f * w)], -1).astype(np.float32)
        c[nm] = np.ascontiguousarray(z.T)
        off = (np.abs(np.arange(n) - n // 2).astype(np.float32) / n).astype(np.float32)
        c["off" + nm[2:]] = np.ascontiguousarray(np.broadcast_to(off[None], (128, n)))
    deltas = np.abs(np.linspace(math.log(1e-2) / 1.5, math.log(1e-2) / 0.3, 512, dtype=np.float32))
    c["ndelta"] = np.ascontiguousarray((-deltas).reshape(4, 128).T.astype(np.float32))
    c["iota512"] = np.ascontiguousarray(np.broadcast_to(np.arange(1, 513, dtype=np.float32)[None], (128, 512)))
    tri = (np.arange(128)[:, None] < np.arange(128)[None, :]).astype(np.float32)
    c["tris"] = tri
    c["iotab"] = np.ascontiguousarray(np.broadcast_to(np.arange(NBLK, dtype=np.float32)[None], (128, NBLK)))
    return c


class Builder:
    def __init__(self, debug=None):
        self.debug = debug or []
        self.nc = bass.Bass("TRN2", target_bir_lowering=False)
        self.din = {}
        self.dbg_out = []

    def inp(self, name, shape, dt=F32):
        t = self.nc.dram_tensor(name, list(shape), dt, kind="ExternalInput")
        self.din[name] = t
        return t

    def scratch(self, name, shape, dt):
        return self.nc.dram_tensor(name, list(shape), dt, kind="Internal")

    def build(self, in_shapes):
        nc = self.nc
        I = {k: self.inp(k, v.shape, {np.dtype("float32"): F32, np.dtype("int32"): I32}[v.dtype]) for k, v in in_shapes.items()}
        self.I = I
        self.out = nc.dram_tensor("out", [L, D], F32, kind="ExternalOutput")
        self.XR = self.scratch("XR", [T, D], F32)
        self.YT = self.scratch("YT", [1536, T], BF16)
        self.H2 = self.scratch("H2", [T, D], BF16)
        self.MB = self.scratch("MB", [NBLK * 128, D], BF16)
        self.MY = self.scratch("MY", [NBLK * 128, D], F32)
        self.GF = self.scratch("GF", [512, 4352], BF16)
        self.GFc = self.scratch("GFc", [512, 512], BF16)
        self.bXR = [Buf() for _ in range(NT)]
        self.bYT = [Buf() for _ in range(12)]
        self.bH2 = [Buf() for _ in range(NT)]
        self.bMB = Buf()
        self.bMY = Buf()
        self.bGF = [Buf() for _ in range(4)]
        self.bGFc = [Buf() for _ in range(4)]
        self.bout = Buf()
        with ExitStack() as es:
            self.sems = {s: es.enter_context(nc.semaphore(s)) for s in FW.sem_names()}
            self.fw = FW(nc, self.sems)
            with ExitStack() as gs:
                self.gs = gs
                self.alloc_globals()
                self.phase_init()
                for l in range(DEPTH):
                    self.layer(l)
                    if getattr(self, "stop", None):
                        break
                self.phase_final()
        return nc

    def sb(self, es, name, shape, dt):
        self._uid = getattr(self, "_uid", 0) + 1
        t = es.enter_context(self.nc.sbuf_tensor(f"s{self._uid}_{name}", list(shape), dt))
        assert self.nc.sbuf_bytes_remaining >= 32768 + 128, (name, self.nc.sbuf_bytes_remaining)
        return t, Buf(name)

    def alloc_globals(self):
        nc, gs, fw, I = self.nc, self.gs, self.fw, self.I
        self.PF = []
        for i in range(6):
            t = gs.enter_context(nc.psum_tensor(f"pf{i}", [128, 512], F32))
            self.PF.append((t, Buf(f"pf{i}")))
        self.PB = []
        for i in range(2):
            t = gs.enter_context(nc.psum_tensor(f"pb{i}", [128, 1024], BF16))
            self.PB.append((t, Buf(f"pb{i}")))
        self.pfi = 0
        self.pbi = 0
        self.identf, self.b_identf = self.sb(gs, "identf", [128, 128], F32)
        self.identb, self.b_identb = self.sb(gs, "identb", [128, 128], BF16)
        self.antib, self.b_antib = self.sb(gs, "antib", [128, 128], BF16)
        self.rcol, self.b_rcol = self.sb(gs, "rcol", [128, 6], F32)
        self.epsc, self.b_epsc = self.sb(gs, "epsc", [128, 4], F32)
        self.modT, self.b_modT = self.sb(gs, "modT", [128, 48, 2], F32)
        self.modP, self.b_modP = self.sb(gs, "modP", [128, 48, 2], F32)
        self.GBd = self.scratch("GBd", [128, 4, 1024], F32)
        self.b_GBd = Buf()
        self.scT, self.b_scT = self.sb(gs, "scT", [128, 8, 2], F32)
        self.LG, _ = self.sb(gs, "LG", [128, NT, 36], F32)
        self.b_LG = [Buf() for _ in range(NT)]
        fw.dma([], [self.b_identf], out=self.identf[:], in_=I["ident_f"].ap())
        fw.op("dve", "tensor_copy", [self.b_identf], [self.b_identb], out=self.identb[:], in_=self.identf[:])
        af, baf = self.sb(gs, "antif", [128, 128], F32)
        fw.dma([], [baf], out=af[:], in_=I["antiid_f"].ap())
        fw.op("dve", "tensor_copy", [baf], [self.b_antib], out=self.antib[:], in_=af[:])
        fw.dma([], [self.b_rcol], out=self.rcol[:], in_=I["rcol"].ap())
        fw.op("pool", "memset", [], [self.b_epsc], ap=self.epsc[:, 0:1], constant=1e-6)
        fw.op("pool", "memset", [self.b_epsc], [self.b_epsc], ap=self.epsc[:, 1:2], constant=1e-5)
        fw.op("pool", "memset", [self.b_epsc], [self.b_epsc], ap=self.epsc[:, 2:3], constant=math.pi / 2)
        fw.op("pool", "memset", [self.b_epsc], [self.b_epsc], ap=self.epsc[:, 3:4], constant=0.0)
        ct, bct = self.sb(gs, "cT", [128, 8, 2], F32)
        fw.dma([], [bct], out=ct[:], in_=I["cT"].ap())
        fw.op("act", "activation", [bct], [self.b_scT], out=self.scT[:], in_=ct[:], func=AF.Silu)

    def pf(self):
        r = self.PF[self.pfi % len(self.PF)]
        self.pfi += 1
        return r

    def pb(self):
        r = self.PB[self.pbi % len(self.PB)]
        self.pbi += 1
        return r

    def dump(self, name, ap, shape, R, dt=F32):
        if name not in self.debug:
            return
        t = self.nc.dram_tensor("dbg_" + name, list(shape), dt, kind="ExternalOutput")
        self.dbg_out.append("dbg_" + name)
        self.fw.dma(R, [Buf()], out=t.ap(), in_=ap, stream="d2")

    def phase_init(self):
        fw, I = self.fw, self.I
        with ExitStack() as es:
            xt = [self.sb(es, f"xi{i}", [128, D], F32) for i in range(3)]
            pt = [self.sb(es, f"pi{i}", [128, D], F32) for i in range(3)]
            for t in range(NT):
                x, bx = xt[t % 3]
                if t < 2:
                    fw.dma([], [bx], out=x[:], in_=I["ctx"][t * 128:(t + 1) * 128, :])
                else:
                    p, bp = pt[t % 3]
                    r0 = (t - 2) * 128
                    fw.dma([], [bx], out=x[:], in_=I["x"][r0:r0 + 128, :])
                    fw.dma([], [bp], out=p[:], in_=I["pe"][r0:r0 + 128, :])
                    fw.op("pool", "tensor_tensor", [bx, bp], [bx], out=x[:], in0=x[:], in1=p[:], op=ALU.add)
                fw.dma([bx], [self.bXR[t]], out=self.XR[t * 128:(t + 1) * 128, :], in_=x[:], stream="d2")
            fw.flush()

    def phase_mod(self, l):
        fw, I = self.fw, self.I
        with ExitStack() as es:
            wm = [self.sb(es, f"wm{i}", [128, 8, 512], F32) for i in range(2)]
            bT, bbT = self.sb(es, "bmodT", [128, 48], F32)
            brow, bbrow = self.sb(es, "bmrow", [2, 4, 512], F32)
            rows, brows = self.sb(es, "mrows", [2, 512], F32)
            sel, bsel = self.sb(es, "selw", [2, 2, 128], F32)
            gst_ = [self.sb(es, f"gst{i}", [128, 512], F32) for i in range(2)]
            fw.dma([], [bbT], out=bT[:], in_=I["bmodT"][l])
            for w in range(2):
                for gi, c0 in enumerate((2048, 2560, 5120, 5632)):
                    fw.dma([], [bbrow], out=brow[w:w + 1, gi, :], in_=I["b_mod"][l:l + 1, c0:c0 + 512])
            fw.dma([], [bsel], out=sel[:], in_=I["selw"].ap())
            for cb in range(12):
                w_, bw = wm[cb % 2]
                fw.dma([], [bw], out=w_[:], in_=I["w_mod"][l][:, cb * 512:(cb + 1) * 512].rearrange("(k p) c -> p k c", p=128))
                pt_, bp = self.pf()
                for s in range(4):
                    j = cb * 4 + s
                    for k in range(8):
                        fw.op("pe", "matmul", [bw, self.b_scT], [bp], out=pt_[:, s * 2:(s + 1) * 2], lhsT=w_[:, k, s * 128:(s + 1) * 128],
                              rhs=self.scT[:, k, :], start=(k == 0), stop=(k == 7))
                fw.op("dve", "tensor_tensor", [bp, bbT], [self.b_modT], out=self.modT[:, cb * 4:(cb + 1) * 4, :],
                      in0=pt_[:, 0:8].rearrange("p (s w) -> p s w", w=2),
                      in1=bT[:, cb * 4:(cb + 1) * 4].unsqueeze(2).to_broadcast([128, 4, 2]), op=ALU.add)
                if cb in (4, 5, 10, 11):
                    gi = {4: 0, 5: 1, 10: 2, 11: 3}[cb]
                    pr, bpr = self.pf()
                    for k in range(8):
                        fw.op("pe", "matmul", [bw, self.b_scT], [bpr], out=pr[0:2, :], lhsT=self.scT[:, k, :], rhs=w_[:, k, :],
                              start=(k == 0), stop=(k == 7))
                    fw.op("dve", "tensor_tensor", [bpr, bbrow], [brows], out=rows[:], in0=pr[0:2, :], in1=brow[:, gi, :], op=ALU.add)
                    for w in range(2):
                        pbc, bpbc = self.pf()
                        fw.op("pe", "matmul", [brows, bsel], [bpbc], out=pbc[:], lhsT=sel[:, w, :], rhs=rows[:], start=True, stop=True)
                        slot = (0 if gi < 2 else 2) + w
                        half = gi % 2
                        gst, bgst = gst_[(gi * 2 + w) % 2]
                        fw.op("act", "copy", [bpbc], [bgst], out=gst[:], in_=pbc[:])
                        fw.dma([bgst], [self.b_GBd], out=self.GBd[:, slot, half * 512:(half + 1) * 512], in_=gst[:], stream="d2")
            fw.op("dve", "tensor_scalar", [self.b_modT], [self.b_modP], out=self.modP[:], in0=self.modT[:], scalar1=1.0, scalar2=None, op0=ALU.add)
            self.dump(f"modT{l}", self.modT[:], [128, 48, 2], [self.b_modT])
            fw.flush()

    def rms_stats(self, x, bx, st, bst, mv, bmv, rstd, brstd, eps_col=0):
        fw = self.fw
        fw.op("dve", "bn_stats", [bx], [bst], out=st[:, 0, :], in_=x[:, 0:512])
        fw.op("dve", "bn_stats", [bx, bst], [bst], out=st[:, 1, :], in_=x[:, 512:1024])
        fw.op("dve", "bn_aggr", [bst], [bmv], out=mv[:, 0:2], in_=st[:])
        fw.op("dve", "scalar_tensor_tensor", [bmv], [bmv], out=mv[:, 2:3], in0=mv[:, 0:1], scalar=mv[:, 0:1], in1=mv[:, 1:2], op0=ALU.mult, op1=ALU.add)
        fw.op("act", "activation", [bmv, self.b_epsc], [bmv], out=mv[:, 3:4], in_=mv[:, 2:3], func=AF.Sqrt, bias=self.epsc[:, eps_col:eps_col + 1], scale=1.0)
        fw.op("dve", "reciprocal", [bmv], [brstd], out=rstd[:], in_=mv[:, 3:4])

    def phase_norm1(self, l, hT, bhT):
        fw, I = self.fw, self.I
        with ExitStack() as es:
            gB, bgB = self.sb(es, "n1g", [128, D], F32)
            fw.dma([], [bgB], out=gB[:], in_=I["norm1_g"][l:l + 1, :].to_broadcast([128, D]))
            xs_ = [self.sb(es, f"n1x{i}", [128, D], F32) for i in range(2)]
            xb_ = [self.sb(es, f"n1xb{i}", [128, D], BF16) for i in range(2)]
            st_ = [self.sb(es, f"n1st{i}", [128, 2, 6], F32) for i in range(2)]
            mv_ = [self.sb(es, f"n1mv{i}", [128, 4], F32) for i in range(2)]
            rs_ = [self.sb(es, f"n1rs{i}", [128, 1], F32) for i in range(2)]
            for t in range(NT):
                w = 1 if t < 2 else 0
                x, bx = xs_[t % 2]; xb, bxb = xb_[t % 2]; st, bst = st_[t % 2]; mv, bmv = mv_[t % 2]; rs, brs = rs_[t % 2]
                fw.dma([self.bXR[t]], [bx], out=x[:], in_=self.XR[t * 128:(t + 1) * 128, :])
                self.rms_stats(x, bx, st, bst, mv, bmv, rs, brs)
                fw.op("dve", "scalar_tensor_tensor", [bx, brs, bgB], [bxb], out=xb[:], in0=x[:], scalar=rs[:, 0:1], in1=gB[:], op0=ALU.mult, op1=ALU.mult)
                pb_, bpb = self.pb()
                for k in range(8):
                    fw.op("pe", "transpose", [bxb, self.b_identb], [bpb], out=pb_[:, k * 128:(k + 1) * 128], in_=xb[:, k * 128:(k + 1) * 128], identity=self.identb[:])
                for k in range(8):
                    fw.op("act", "activation", [bpb, self.b_modT, self.b_modP], [bhT[t]], out=hT[:, k, t * 128:(t + 1) * 128], in_=pb_[:, k * 128:(k + 1) * 128],
                          func=AF.Identity, scale=self.modP[:, 8 + k, w:w + 1], bias=self.modT[:, k, w:w + 1])
            fw.flush()

    def layer(self, l):
        fw = self.fw
        self.phase_mod(l)
        with ExitStack() as es:
            hT, _ = self.sb(es, "hT", [128, 8, T], BF16)
            bhT = [Buf(f"hT{t}") for t in range(NT)]
            self.phase_norm1(l, hT, bhT)
            self.dump(f"hT{l}", hT[:], [128, 8, T], bhT, dt=BF16)
            if self.debug:
                fw.flush()
            if self.stop == "norm1":
                return
            self.phase_ret(l, hT, bhT)
            if self.stop == "ret":
                return
            self.phase_s5(l, hT, bhT)
            if self.stop == "s5":
                return
            self.phase_hy(l, hT, bhT)
            if self.stop == "hy":
                return
            self.phase_merge(l, hT, bhT)
        if self.stop == "merge":
            return
        self.phase_moe(l)

    def load_w_cols(self, dst_ap, bdst, l, c0, ncols):
        self.fw.dma([], [bdst], q="pool", stream="d1", out=dst_ap,
                    in_=self.I["w_in"][l][:, c0:c0 + ncols].rearrange("(k p) c -> p k c", p=128))

    def tiles_of(self, t0, n):
        return range(t0 // 128, (t0 + n) // 128)

    def phase_ret(self, l, hT, bhT):
        fw, I = self.fw, self.I
        QS = 128.0 ** -0.5
        with ExitStack() as es:
            rtab, brtab = self.sb(es, "rtab", [128, 6, 128], F32)
            lgB, blgB = self.sb(es, "lgB", [128, 8], F32)
            gnB, bgnB = self.sb(es, "gnB", [128, 512], F32)
            fw.dma([], [brtab], out=rtab[:], in_=I["rtab"].ap())
            fw.dma([], [blgB], out=lgB[:], in_=I["ret_log_decay"][l:l + 1, :].to_broadcast([128, 8]))
            fw.dma([], [bgnB], out=gnB[:], in_=I["ret_gn_g"][l:l + 1, :].to_broadcast([128, 512]))
            MT, bMT = self.sb(es, "MT", [128, 128], F32)
            tm1, btm1 = self.sb(es, "tm1", [128, 128], F32)
            tm2, btm2 = self.sb(es, "tm2", [128, 128], F32)
            dcol, bdcol = self.sb(es, "dcol", [128, 6], F32)
            wqk, bwqk = self.sb(es, "wqk", [128, 8, 256], BF16)
            wkvg, bwkvg = self.sb(es, "wkvg", [128, 8, 384], BF16)
            qT, bqT = self.sb(es, "qT", [128, T], BF16)
            kT, bkT = self.sb(es, "kT", [128, T], BF16)
            ktok, bktok = self.sb(es, "ktok", [128, NT, 128], BF16)
            vtok, bvtok = self.sb(es, "vtok", [128, NT, 128], BF16)
            vdf, bvdf = self.sb(es, "vdf", [128, NT, 128], BF16)
            vdb, bvdb = self.sb(es, "vdb", [128, NT, 128], BF16)
            gs, bgs = self.sb(es, "gs", [128, NT, 128], BF16)
            SfB, bSfB = self.sb(es, "SfB", [128, NT, 128], BF16)
            SbB, bSbB = self.sb(es, "SbB", [128, NT, 128], BF16)
            Sf, bSf = self.sb(es, "Sf", [128, 128], F32)
            Sb, bSb = self.sb(es, "Sb", [128, 128], F32)
            yTt_ = [self.sb(es, f"yTt{i}", [128, 128], BF16) for i in range(2)]
            attm_ = [self.sb(es, f"attm{i}", [128, 128], BF16) for i in range(2)]
            on_ = [self.sb(es, f"on{i}", [128, 128], F32) for i in range(2)]
            yk_ = [self.sb(es, f"yk{i}", [128, 128], BF16) for i in range(2)]
            st_ = [self.sb(es, f"rst{i}", [128, 6], F32) for i in range(2)]
            mv_ = [self.sb(es, f"rmv{i}", [128, 4], F32) for i in range(2)]
            bq_t = [Buf() for _ in range(NT)]
            bk_t = [Buf() for _ in range(NT)]
            btok = [Buf() for _ in range(NT)]
            bSf_t = [Buf() for _ in range(NT)]
            bSb_t = [Buf() for _ in range(NT)]
            for hd in range(4):
                lgf = lgB[:, hd:hd + 1]
                lgb = lgB[:, 4 + hd:5 + hd]
                fw.op("act", "activation", [brtab, blgB], [btm1], out=tm1[:], in_=rtab[:, 0, :], func=AF.Exp, scale=lgf)
                fw.op("act", "activation", [brtab, blgB], [btm2], out=tm2[:], in_=rtab[:, 1, :], func=AF.Exp, scale=lgb)
                fw.op("dve", "tensor_tensor", [btm1, brtab], [btm1], out=tm1[:], in0=tm1[:], in1=rtab[:, 2, :], op=ALU.mult)
                fw.op("dve", "tensor_tensor", [btm2, brtab], [btm2], out=tm2[:], in0=tm2[:], in1=rtab[:, 3, :], op=ALU.mult)
                fw.op("dve", "tensor_tensor", [btm1, btm2], [bMT], out=MT[:], in0=tm1[:], in1=tm2[:], op=ALU.add)
                fw.op("act", "activation", [self.b_rcol, blgB], [bdcol], out=dcol[:, 0:1], in_=self.rcol[:, 0:1], func=AF.Exp, scale=lgf)
                fw.op("act", "activation", [self.b_rcol, blgB, bdcol], [bdcol], out=dcol[:, 1:2], in_=self.rcol[:, 1:2], func=AF.Exp, scale=lgb)
                fw.op("act", "activation", [self.b_rcol, blgB, bdcol], [bdcol], out=dcol[:, 2:3], in_=self.rcol[:, 2:3], func=AF.Exp, scale=lgf)
                fw.op("act", "activation", [self.b_rcol, blgB, bdcol], [bdcol], out=dcol[:, 3:4], in_=self.rcol[:, 2:3], func=AF.Exp, scale=lgb)
                fw.op("act", "activation", [self.b_rcol, blgB, bdcol], [bdcol], out=dcol[:, 4:5], in_=self.rcol[:, 4:5], func=AF.Exp, scale=lgf)
                fw.op("act", "activation", [self.b_rcol, blgB, bdcol], [bdcol], out=dcol[:, 5:6], in_=self.rcol[:, 5:6], func=AF.Exp, scale=lgb)
                self.load_w_cols(wqk[:, :, 0:128], bwqk, l, hd * 128, 128)
                self.load_w_cols(wqk[:, :, 128:256], bwqk, l, 512 + hd * 128, 128)
                self.load_w_cols(wkvg[:, :, 0:128], bwkvg, l, 512 + hd * 128, 128)
                self.load_w_cols(wkvg[:, :, 128:256], bwkvg, l, 1024 + hd * 128, 128)
                self.load_w_cols(wkvg[:, :, 256:384], bwkvg, l, 1536 + hd * 128, 128)
                for (t0, n) in TG:
                    tl = list(self.tiles_of(t0, n))
                    hb = [bhT[t] for t in tl]
                    nb = n // 128
                    pq, bpq = self.pf()
                    for k in range(8):
                        fw.op("pe", "matmul", hb + [bwqk], [bpq], out=pq[:, 0:n], lhsT=wqk[:, k, 0:128], rhs=hT[:, k, t0:t0 + n], start=(k == 0), stop=(k == 7))
                    wq_ = [bq_t[t] for t in tl]
                    fw.op("act", "mul", [bpq], wq_, out=qT[:, t0:t0 + n], in_=pq[:, 0:n], mul=QS)
                    pk, bpk = self.pf()
                    for k in range(8):
                        fw.op("pe", "matmul", hb + [bwqk], [bpk], out=pk[:, 0:n], lhsT=wqk[:, k, 128:256], rhs=hT[:, k, t0:t0 + n], start=(k == 0), stop=(k == 7))
                    fw.op("act", "copy", [bpk], [bk_t[t] for t in tl], out=kT[:, t0:t0 + n], in_=pk[:, 0:n])
                for t in range(NT):
                    pt_, bp = self.pf()
                    for k in range(8):
                        fw.op("pe", "matmul", [bhT[t], bwkvg], [bp], out=pt_[:, 0:384], lhsT=hT[:, k, t * 128:(t + 1) * 128], rhs=wkvg[:, k, :], start=(k == 0), stop=(k == 7))
                    fw.op("act", "copy", [bp], [btok[t]], out=ktok[:, t, :], in_=pt_[:, 0:128])
                    fw.op("act", "copy", [bp], [btok[t]], out=vtok[:, t, :], in_=pt_[:, 128:256])
                    fw.op("dve", "tensor_scalar", [bp, bdcol], [btok[t]], out=vdf[:, t, :], in0=pt_[:, 128:256], scalar1=dcol[:, 0:1], scalar2=None, op0=ALU.mult)
                    fw.op("dve", "tensor_scalar", [bp, bdcol], [btok[t]], out=vdb[:, t, :], in0=pt_[:, 128:256], scalar1=dcol[:, 1:2], scalar2=None, op0=ALU.mult)
                    fw.op("act", "activation", [bp], [btok[t]], out=gs[:, t, :], in_=pt_[:, 256:384], func=AF.Silu)
                for (S, bS, SB, bSB_t, vd, ci, order) in ((Sf, bSf, SfB, bSf_t, vdf, 2, list(range(NT))),
                                                          (Sb, bSb, SbB, bSb_t, vdb, 3, [1, 0] + list(range(NT - 1, 1, -1)))):
                    fw.op("pool", "memset", [], [bS], ap=S[:], constant=0.0)
                    for t in order:
                        fw.op("act", "copy", [bS], [bSB_t[t]], out=SB[:, t, :], in_=S[:])
                        pkv, bpkv = self.pf()
                        fw.op("pe", "matmul", [btok[t]], [bpkv], out=pkv[:, 0:128], lhsT=ktok[:, t, :], rhs=vd[:, t, :], start=True, stop=True)
                        fw.op("dve", "scalar_tensor_tensor", [bS, bpkv, bdcol], [bS], out=S[:], in0=S[:], scalar=dcol[:, ci:ci + 1], in1=pkv[:, 0:128], op0=ALU.mult, op1=ALU.add)
                for t in range(NT):
                    cs = slice(t * 128, (t + 1) * 128)
                    attm, battm = attm_[t % 2]; on, bon = on_[t % 2]; yk, byk = yk_[t % 2]; st, bst = st_[t % 2]; mv, bmv = mv_[t % 2]
                    pa, bpa = self.pf()
                    fw.op("pe", "matmul", [bk_t[t], bq_t[t]], [bpa], out=pa[:, 0:128], lhsT=kT[:, cs], rhs=qT[:, cs], start=True, stop=True)
                    fw.op("dve", "tensor_tensor", [bpa, bMT], [battm], out=attm[:], in0=pa[:, 0:128], in1=MT[:], op=ALU.mult)
                    po, bpo = self.pf()
                    fw.op("pe", "matmul", [battm, btok[t]], [bpo], out=po[:, 0:128], lhsT=attm[:], rhs=vtok[:, t, :], start=True, stop=True)
                    fw.op("pe", "matmul", [bq_t[t], bSf_t[t]], [bpo], out=po[:, 128:256], lhsT=qT[:, cs], rhs=SfB[:, t, :], start=True, stop=True)
                    fw.op("pe", "matmul", [bq_t[t], bSb_t[t]], [bpo], out=po[:, 256:384], lhsT=qT[:, cs], rhs=SbB[:, t, :], start=True, stop=True)
                    fw.op("act", "copy", [bpo], [bon], out=on[:], in_=po[:, 0:128])
                    fw.op("dve", "scalar_tensor_tensor", [bpo, bdcol, bon], [bon], out=on[:], in0=po[:, 128:256], scalar=dcol[:, 4:5], in1=on[:], op0=ALU.mult, op1=ALU.add)
                    fw.op("dve", "scalar_tensor_tensor", [bpo, bdcol, bon], [bon], out=on[:], in0=po[:, 256:384], scalar=dcol[:, 5:6], in1=on[:], op0=ALU.mult, op1=ALU.add)
                    fw.op("dve", "bn_stats", [bon], [bst], out=st[:], in_=on[:])
                    fw.op("dve", "bn_aggr", [bst], [bmv], out=mv[:, 0:2], in_=st[:])
                    fw.op("act", "activation", [bmv, self.b_epsc], [bmv], out=mv[:, 2:3], in_=mv[:, 1:2], func=AF.Sqrt, bias=self.epsc[:, 1:2], scale=1.0)
                    fw.op("dve", "reciprocal", [bmv], [bmv], out=mv[:, 3:4], in_=mv[:, 2:3])
                    fw.op("dve", "tensor_scalar", [bon, bmv], [bon], out=on[:], in0=on[:], scalar1=mv[:, 0:1], scalar2=mv[:, 3:4], op0=ALU.subtract, op1=ALU.mult)
                    fw.op("dve", "tensor_tensor", [bon, bgnB], [bon], out=on[:], in0=on[:], in1=gnB[:, hd * 128:(hd + 1) * 128], op=ALU.mult)
                    fw.op("dve", "tensor_tensor", [bon, btok[t]], [byk], out=yk[:], in0=on[:], in1=gs[:, t, :], op=ALU.mult)
                    pb_, bpb = self.pb()
                    fw.op("pe", "transpose", [byk, self.b_identb], [bpb], out=pb_[:, 0:128], in_=yk[:], identity=self.identb[:])
                    yTt, byTt = yTt_[t % 2]
                    fw.op("act", "copy", [bpb], [byTt], out=yTt[:], in_=pb_[:, 0:128])
                    fw.dma([byTt], [self.bYT[hd]], out=self.YT[hd * 128:(hd + 1) * 128, cs], in_=yTt[:], stream="d2")
            if self.debug:
                self.dump_yt("yret%d" % l, 0, 4)
            fw.flush()

    def dump_yt(self, name, c0, nchunks):
        if name not in self.debug:
            return
        t = self.nc.dram_tensor("dbg_" + name, [nchunks * 128, T], BF16, kind="ExternalOutput")
        self.dbg_out.append("dbg_" + name)
        self.fw.dma([self.bYT[c0 + i] for i in range(nchunks)], [Buf()], out=t.ap(), in_=self.YT[c0 * 128:(c0 + nchunks) * 128, :], stream="d2")

    def sincos(self, src, bsrc, shape, sin_out, cos_out, bout, ki, bki, kf, bkf, eng="dve"):
        fw = self.fw
        PI_ = 3.1415925
        fw.op("dve", "tensor_scalar", [bsrc], [bki], out=ki, in0=src, scalar1=1.0 / TWO_PI, scalar2=None, op0=ALU.mult)
        fw.op("dve", "tensor_copy", [bki], [bkf], out=kf, in_=ki)
        fw.op("dve", "scalar_tensor_tensor", [bkf, bsrc], [bkf], out=kf, in0=kf, scalar=-TWO_PI, in1=src, op0=ALU.mult, op1=ALU.add)
        fw.op("dve", "tensor_scalar", [bkf], [bkf], out=kf, in0=kf, scalar1=-PI_, scalar2=PI_, op0=ALU.max, op1=ALU.min)
        if sin_out is not None:
            fw.op("act", "activation", [bkf], [bout], out=sin_out, in_=kf, func=AF.Sin)
        if cos_out is not None:
            fw.op("dve", "scalar_tensor_tensor", [bkf], [bkf], out=kf, in0=kf, scalar=-1.0, in1=kf, op0=ALU.mult, op1=ALU.max)
            fw.op("act", "activation", [bkf, self.b_epsc], [bout], out=cos_out, in_=kf, func=AF.Sin, scale=-1.0, bias=self.epsc[:, 2:3])

    def phase_s5(self, l, hT, bhT):
        fw, I = self.fw, self.I
        CH = [(0, 256)] + [(256 + 512 * i, 512) for i in range(8)]
        with ExitStack() as es:
            prm, bprm = self.sb(es, "s5prm", [128, 3, 32], F32)
            for dr in range(2):
                fw.dma([], [bprm], out=prm[:, 0, dr * 16:(dr + 1) * 16], in_=I["s5_lre"][l, dr])
                fw.dma([], [bprm], out=prm[:, 1, dr * 16:(dr + 1) * 16], in_=I["s5_lim"][l, dr])
                fw.dma([], [bprm], out=prm[:, 2, dr * 16:(dr + 1) * 16], in_=I["s5_ldt"][l, dr])
            dsc, bdsc = self.sb(es, "s5dsc", [128, 12, 32], F32)
            ki32, bki32 = self.sb(es, "s5ki", [128, 32], I32)
            kf32, bkf32 = self.sb(es, "s5kf", [128, 32], F32)
            DT, LR, TH, R_, SN, CS, AR, AI, DEN, CR, CI, NCR = [dsc[:, i, :] for i in range(12)]
            lre, lim = prm[:, 0, :], prm[:, 1, :]
            B2 = [bprm, bdsc]
            fw.op("act", "activation", [bprm], [bdsc], out=DT, in_=prm[:, 2, :], func=AF.Exp)
            fw.op("dve", "tensor_tensor", B2, [bdsc], out=LR, in0=lre, in1=DT, op=ALU.mult)
            fw.op("dve", "tensor_tensor", B2, [bdsc], out=TH, in0=lim, in1=DT, op=ALU.mult)
            fw.op("act", "activation", [bdsc], [bdsc], out=R_, in_=LR, func=AF.Exp)
            self.sincos(TH, bdsc, None, SN, CS, bdsc, ki32[:], bki32, kf32[:], bkf32)
            fw.op("dve", "tensor_tensor", B2, [bdsc], out=AR, in0=R_, in1=CS, op=ALU.mult)
            fw.op("dve", "tensor_tensor", B2, [bdsc], out=AI, in0=R_, in1=SN, op=ALU.mult)
            fw.op("dve", "tensor_scalar", B2, [bdsc], out=AR, in0=AR, scalar1=-1.0, scalar2=None, op0=ALU.add)
            fw.op("dve", "tensor_tensor", B2, [bdsc], out=DEN, in0=lre, in1=lre, op=ALU.mult)
            fw.op("dve", "tensor_tensor", B2, [bdsc], out=LR, in0=lim, in1=lim, op=ALU.mult)
            fw.op("dve", "tensor_tensor", B2, [bdsc], out=DEN, in0=DEN, in1=LR, op=ALU.add)
            fw.op("dve", "reciprocal", B2, [bdsc], out=DEN, in_=DEN)
            fw.op("dve", "tensor_tensor", B2, [bdsc], out=CR, in0=AR, in1=lre, op=ALU.mult)
            fw.op("dve", "tensor_tensor", B2, [bdsc], out=LR, in0=AI, in1=lim, op=ALU.mult)
            fw.op("dve", "tensor_tensor", B2, [bdsc], out=CR, in0=CR, in1=LR, op=ALU.add)
            fw.op("dve", "tensor_tensor", B2, [bdsc], out=CR, in0=CR, in1=DEN, op=ALU.mult)
            fw.op("dve", "tensor_tensor", B2, [bdsc], out=CI, in0=AI, in1=lre, op=ALU.mult)
            fw.op("dve", "tensor_tensor", B2, [bdsc], out=LR, in0=AR, in1=lim, op=ALU.mult)
            fw.op("dve", "tensor_tensor", B2, [bdsc], out=CI, in0=CI, in1=LR, op=ALU.subtract)
            fw.op("dve", "tensor_tensor", B2, [bdsc], out=CI, in0=CI, in1=DEN, op=ALU.mult)
            fw.op("dve", "tensor_scalar", B2, [bdsc], out=NCR, in0=CR, scalar1=-1.0, scalar2=None, op0=ALU.mult)
            cut = getattr(self, 's5_cut', 0)
            if cut == 1:
                self.dump('dsc', dsc[:], [128, 12, 32], [bdsc])
                fw.flush()
                return
            Bb, bBb = self.sb(es, "s5Bb", [128, 2, 2, 4, 2, 128], BF16)
            Cb, bCb = self.sb(es, "s5Cb", [128, 2, 2, 4, 64], BF16)
            for dr in range(2):
                for ri in range(2):
                    fw.dma([], [bBb], q="pool", stream="d1", out=Bb[:, dr, ri], in_=I["s5_B"][l, dr, ri])
            dT_, bdT = self.sb(es, "s5dT", [128, 4], F32)
            fw.dma([], [bdT], out=dT_[:], in_=I["s5_dT"][l])
            iota, biota = self.sb(es, "s5iota", [128, 512], F32)
            fw.dma([], [biota], out=iota[:], in_=I["iota512"].ap())
            wu, bwu = self.sb(es, "s5wu", [128, 8, 128], BF16)
            uf, buf_ = self.sb(es, "s5uf", [128, T], F32)
            ub, bub = self.sb(es, "s5ub", [128, T], BF16)
            yacc, byacc = self.sb(es, "s5yacc", [128, T], F32)
            gb_ = [self.sb(es, f"s5gb{i}", [128, 512], BF16) for i in range(2)]
            tb = {}
            for nm in ("cos", "sin", "dre", "dim", "rf"):
                tb[nm] = self.sb(es, "s5t_" + nm, [128, 512], F32)
            wk = {}
            for nm in ("pre", "pim", "t1", "t2", "t3", "t4", "gre", "gim", "hre", "nhim"):
                wk[nm] = self.sb(es, "s5w_" + nm, [128, 512], F32)
            hreb, bhreb = self.sb(es, "s5hreb", [128, 512], BF16)
            nhimb, bnhimb = self.sb(es, "s5nhimb", [128, 512], BF16)
            carry, bcarry = self.sb(es, "s5carry", [128, 2], F32)
            self.GS5 = getattr(self, "GS5", None) or self.scratch("GS5", [512, T], BF16)
            bGS5 = [Buf() for _ in range(4)]
            ychunk = [Buf() for _ in CH]
            if cut == 2:
                fw.flush()
                return
            for o in range(4):
                for dr in range(2):
                    for ri in range(2):
                        fw.dma([], [bCb], q="pool", stream="d1", out=Cb[:, dr, ri], in_=I["s5_C"][l, dr, ri][:, o * 4:(o + 1) * 4, :])
                    fw.op("act", "mul", [bCb], [bCb], out=Cb[:, dr, 1], in_=Cb[:, dr, 1], mul=-1.0)
                self.load_w_cols(wu[:], bwu, l, 2048 + o * 128, 128)
                for (t0, n) in TG:
                    hb = [bhT[t] for t in self.tiles_of(t0, n)]
                    pu, bpu = self.pf()
                    for k in range(8):
                        fw.op("pe", "matmul", hb + [bwu], [bpu], out=pu[:, 0:n], lhsT=wu[:, k, :], rhs=hT[:, k, t0:t0 + n], start=(k == 0), stop=(k == 7))
                    fw.op("act", "copy", [bpu], [buf_], out=uf[:, t0:t0 + n], in_=pu[:, 0:n])
                    fw.op("dve", "tensor_copy", [buf_], [bub], out=ub[:, t0:t0 + n], in_=uf[:, t0:t0 + n])
                if cut == 5:
                    fw.flush()
                    return
                fw.op("pool", "memset", [], ychunk, ap=yacc[:], constant=0.0)
                if cut == 3:
                    fw.flush()
                    return
                lim = getattr(self, "s5_lim", None)
                for dr in range(2):
                    if lim is not None and dr >= lim[0]:
                        break
                    for q4 in range(4):
                        pt = o * 4 + q4
                        c = dr * 16 + pt
                        ps = slice(64 * (q4 // 2), 64 * (q4 // 2) + 64)
                        jj = q4 % 2
                        (cosT, bcos), (sinT, bsin) = tb["cos"], tb["sin"]
                        kiT, bkiT = wk["t4"][0][:].bitcast(I32), wk["t4"][1]
                        (dreT, bdre), (dimT, bdim), (rfT, brf) = tb["dre"], tb["dim"], tb["rf"]
                        t1, bt1 = wk["t1"]
                        fw.op("dve", "tensor_scalar", [biota, bdsc], [bt1], out=t1[:], in0=iota[:], scalar1=TH[:, c:c + 1], scalar2=None, op0=ALU.mult)
                        fw.op("dve", "tensor_scalar", [bt1], [bkiT], out=kiT, in0=t1[:], scalar1=1.0 / TWO_PI, scalar2=None, op0=ALU.mult)
                        t2, bt2 = wk["t2"]
                        fw.op("dve", "tensor_copy", [bkiT], [bt2], out=t2[:], in_=kiT)
                        fw.op("dve", "scalar_tensor_tensor", [bt2, bt1], [bt2], out=t2[:], in0=t2[:], scalar=-TWO_PI, in1=t1[:], op0=ALU.mult, op1=ALU.add)
                        fw.op("dve", "tensor_scalar", [bt2], [bt2], out=t2[:], in0=t2[:], scalar1=-3.1415925, scalar2=3.1415925, op0=ALU.max, op1=ALU.min)
                        fw.op("act", "activation", [bt2], [bsin], out=sinT[:], in_=t2[:], func=AF.Sin)
                        fw.op("dve", "scalar_tensor_tensor", [bt2], [bt2], out=t2[:], in0=t2[:], scalar=-1.0, in1=t2[:], op0=ALU.mult, op1=ALU.max)
                        fw.op("act", "activation", [bt2, self.b_epsc], [bcos], out=cosT[:], in_=t2[:], func=AF.Sin, scale=-1.0, bias=self.epsc[:, 2:3])
                        fw.op("dve", "tensor_scalar", [bcos, bdsc], [bdre], out=dreT[:], in0=cosT[:], scalar1=CR[:, c:c + 1], scalar2=None, op0=ALU.mult)
                        fw.op("dve", "scalar_tensor_tensor", [bsin, bdsc, bdre], [bdre], out=dreT[:], in0=sinT[:], scalar=CI[:, c:c + 1], in1=dreT[:], op0=ALU.mult, op1=ALU.add)
                        fw.op("dve", "tensor_scalar", [bcos, bdsc], [bdim], out=dimT[:], in0=cosT[:], scalar1=CI[:, c:c + 1], scalar2=None, op0=ALU.mult)
                        fw.op("dve", "scalar_tensor_tensor", [bsin, bdsc, bdim], [bdim], out=dimT[:], in0=sinT[:], scalar=NCR[:, c:c + 1], in1=dimT[:], op0=ALU.mult, op1=ALU.add)
                        fw.op("dve", "tensor_scalar", [biota], [brf], out=rfT[:], in0=iota[:], scalar1=0.0, scalar2=None, op0=ALU.mult)
                        fw.op("dve", "tensor_scalar", [brf, bdsc], [brf], out=rfT[:], in0=rfT[:], scalar1=R_[:, c:c + 1], scalar2=None, op0=ALU.add)
                        fw.op("pool", "memset", [], [bcarry], ap=carry[:], constant=0.0)
                        order = list(range(9)) if dr == 0 else [0] + list(range(8, 0, -1))
                        if lim is not None:
                            order = order[:lim[1]]
                        for ci_ in order:
                            t0, n = CH[ci_]
                            rv = (lambda ap: ap) if dr == 0 else (lambda ap: ap[:, ::-1])
                            pP, bpP = self.pf()
                            pQ, bpQ = self.pf()
                            fw.op("pe", "matmul", [bBb, bub], [bpP], out=pP[:, 0:n], lhsT=Bb[ps, dr, 0, o, jj, :], rhs=ub[ps, t0:t0 + n], start=True, stop=True)
                            fw.op("pe", "matmul", [bBb, bub], [bpQ], out=pQ[:, 0:n], lhsT=Bb[ps, dr, 1, o, jj, :], rhs=ub[ps, t0:t0 + n], start=True, stop=True)
                            pre, bpre = wk["pre"]; pim, bpim = wk["pim"]
                            fw.op("act", "copy", [bpP], [bpre], out=rv(pre[:, 0:n]), in_=pP[:, 0:n])
                            fw.op("act", "copy", [bpQ], [bpim], out=rv(pim[:, 0:n]), in_=pQ[:, 0:n])
                            t2, bt2 = wk["t2"]; t3, bt3 = wk["t3"]; t4, bt4 = wk["t4"]
                            gre, bgre = wk["gre"]; gim, bgim = wk["gim"]; hre, bhre = wk["hre"]; nhim, bnhim = wk["nhim"]
                            N = slice(0, n)
                            fw.op("dve", "tensor_tensor", [bpre, bdre], [bt1], out=t1[:, N], in0=pre[:, N], in1=dreT[:, N], op=ALU.mult)
                            fw.op("dve", "tensor_tensor", [bpim, bdim], [bt2], out=t2[:, N], in0=pim[:, N], in1=dimT[:, N], op=ALU.mult)
                            fw.op("dve", "tensor_tensor", [bt1, bt2], [bt1], out=t1[:, N], in0=t1[:, N], in1=t2[:, N], op=ALU.subtract)
                            fw.op("pool", "tensor_tensor", [bpre, bdim], [bt3], out=t3[:, N], in0=pre[:, N], in1=dimT[:, N], op=ALU.mult)
                            fw.op("pool", "tensor_tensor", [bpim, bdre], [bt4], out=t4[:, N], in0=pim[:, N], in1=dreT[:, N], op=ALU.mult)
                            fw.op("pool", "tensor_tensor", [bt3, bt4], [bt3], out=t3[:, N], in0=t3[:, N], in1=t4[:, N], op=ALU.add)
                            fw.op("dve", "tensor_tensor_scan", [brf, bt1, bcarry], [bgre], out=gre[:, N], data0=rfT[:, N], data1=t1[:, N], initial=carry[:, 0:1], op0=ALU.mult, op1=ALU.add)
                            fw.op("dve", "tensor_tensor_scan", [brf, bt3, bcarry], [bgim], out=gim[:, N], data0=rfT[:, N], data1=t3[:, N], initial=carry[:, 1:2], op0=ALU.mult, op1=ALU.add)
                            fw.op("dve", "tensor_tensor", [bcos, bgre], [bhre], out=hre[:, N], in0=cosT[:, N], in1=gre[:, N], op=ALU.mult)
                            fw.op("pool", "tensor_tensor", [bsin, bgim], [bt4], out=t4[:, N], in0=sinT[:, N], in1=gim[:, N], op=ALU.mult)
                            fw.op("dve", "tensor_tensor", [bhre, bt4], [bhre], out=hre[:, N], in0=hre[:, N], in1=t4[:, N], op=ALU.subtract)
                            fw.op("pool", "tensor_tensor", [bsin, bgre], [bnhim], out=nhim[:, N], in0=sinT[:, N], in1=gre[:, N], op=ALU.mult)
                            fw.op("pool", "tensor_tensor", [bcos, bgim], [bt2], out=t2[:, N], in0=cosT[:, N], in1=gim[:, N], op=ALU.mult)
                            fw.op("pool", "tensor_tensor", [bnhim, bt2], [bnhim], out=nhim[:, N], in0=nhim[:, N], in1=t2[:, N], op=ALU.add)
                            fw.op("act", "copy", [bhre], [bhreb], out=rv(hreb[:, N]), in_=hre[:, N])
                            fw.op("act", "copy", [bnhim], [bnhimb], out=rv(nhimb[:, N]), in_=nhim[:, N])
                            fw.op("act", "copy", [bhre], [bcarry], out=carry[:, 0:1], in_=hre[:, n - 1:n])
                            fw.op("act", "copy", [bnhim, bcarry], [bcarry], out=carry[:, 1:2], in_=nhim[:, n - 1:n])
                            py, bpy = self.pf()
                            fw.op("pe", "matmul", [bCb, bhreb], [bpy], out=py[ps, 0:n], lhsT=Cb[:, dr, 0, q4, :], rhs=hreb[:, N], start=True, stop=False)
                            fw.op("pe", "matmul", [bCb, bnhimb], [bpy], out=py[ps, 0:n], lhsT=Cb[:, dr, 1, q4, :], rhs=nhimb[:, N], start=False, stop=True)
                            fw.op("dve", "tensor_tensor", [bpy, ychunk[ci_]], [ychunk[ci_]], out=yacc[ps, t0:t0 + n], in0=py[ps, 0:n], in1=yacc[ps, t0:t0 + n], op=ALU.add)
                if cut == 4:
                    fw.flush()
                    return
                fw.op("dve", "scalar_tensor_tensor", [buf_, bdT] + ychunk, ychunk, out=yacc[:], in0=uf[:], scalar=dT_[:, o:o + 1], in1=yacc[:], op0=ALU.mult, op1=ALU.add)
                for gi_, (t0, n) in enumerate(TG):
                    gb, bgb = gb_[gi_ % 2]
                    fw.op("act", "activation", ychunk, [bgb], out=gb[:, 0:n], in_=yacc[:, t0:t0 + n], func=AF.Gelu_apprx_tanh)
                    fw.dma([bgb], [bGS5[o]], out=self.GS5[o * 128:(o + 1) * 128, t0:t0 + n], in_=gb[:, 0:n], stream="d2")
            fw.flush()
        with ExitStack() as es:
            W1b, bW1b = self.sb(es, "s5W1b", [128, 4, 512], BF16)
            W2b, bW2b = self.sb(es, "s5W2b", [128, 4, 512], BF16)
            fw.dma([], [bW1b], q="pool", stream="d1", out=W1b[:], in_=I["s5_glu_w1"][l].rearrange("(k p) c -> p k c", p=128))
            fw.dma([], [bW2b], q="pool", stream="d1", out=W2b[:], in_=I["s5_glu_w2"][l].rearrange("(k p) c -> p k c", p=128))
            g_ = [self.sb(es, f"s5g{i}", [128, 4, 512], BF16) for i in range(2)]
            sg_ = [self.sb(es, f"s5sg{i}", [128, 512], F32) for i in range(2)]
            yo_ = [self.sb(es, f"s5yo{i}", [128, 512], BF16) for i in range(2)]
            for gi, (t0, n) in enumerate(TG):
                g, bg = g_[gi % 2]
                fw.dma(bGS5, [bg], out=g[:, :, 0:n], in_=self.GS5[:, t0:t0 + n].rearrange("(k p) t -> p k t", p=128))
                for c_ in range(4):
                    sg, bsg = sg_[c_ % 2]; yo, byo = yo_[c_ % 2]
                    pA, bpA = self.pf()
                    pB, bpB = self.pf()
                    for k in range(4):
                        fw.op("pe", "matmul", [bW1b, bg], [bpA], out=pA[:, 0:n], lhsT=W1b[:, k, c_ * 128:(c_ + 1) * 128], rhs=g[:, k, 0:n], start=(k == 0), stop=(k == 3))
                    for k in range(4):
                        fw.op("pe", "matmul", [bW2b, bg], [bpB], out=pB[:, 0:n], lhsT=W2b[:, k, c_ * 128:(c_ + 1) * 128], rhs=g[:, k, 0:n], start=(k == 0), stop=(k == 3))
                    fw.op("act", "activation", [bpB], [bsg], out=sg[:, 0:n], in_=pB[:, 0:n], func=AF.Sigmoid)
                    fw.op("dve", "tensor_tensor", [bpA, bsg], [byo], out=yo[:, 0:n], in0=pA[:, 0:n], in1=sg[:, 0:n], op=ALU.mult)
                    fw.dma([byo], [self.bYT[4 + c_]], out=self.YT[512 + c_ * 128:512 + (c_ + 1) * 128, t0:t0 + n], in_=yo[:, 0:n], stream="d2")
            if self.debug:
                self.dump_yt("ys5%d" % l, 4, 4)
            fw.flush()

    def phase_hy(self, l, hT, bhT):
        fw, I = self.fw, self.I
        do_ctx = (l == 0)
        with ExitStack() as es:
            w1, bw1 = self.sb(es, "hfw1", [33, 64], F32)
            w2, bw2 = self.sb(es, "hfw2", [64, 64], F32)
            w3, bw3 = self.sb(es, "hfw3", [64, 512], F32)
            fb, bfb = self.sb(es, "hffb", [64, 6], F32)
            nd_, bnd = self.sb(es, "hfnd", [128, 4], F32)
            fw.dma([], [bw1], out=w1[:], in_=I["hy_f_w1"][l])
            fw.dma([], [bw2], out=w2[:], in_=I["hy_f_w2"][l])
            fw.dma([], [bw3], out=w3[:], in_=I["hy_f_w3"][l])
            fw.dma([], [bfb], out=fb[:, 0:1], in_=I["hy_f_b1"][l])
            fw.dma([], [bfb], out=fb[:, 1:2], in_=I["hy_f_b2"][l])
            fw.dma([], [bfb], out=fb[:, 2:4], in_=I["hy_f_fr"][l])
            fw.dma([], [bnd], out=nd_[:], in_=I["ndelta"].ap())
            fw.op("dve", "tensor_tensor", [bfb], [bfb], out=fb[:, 4:6], in0=fb[:, 0:2], in1=fb[:, 2:4], op=ALU.mult)
            zt_ = [self.sb(es, f"hfz{i}", [33, 512], F32) for i in range(2)]
            of_ = [self.sb(es, f"hfo{i}", [128, 512], F32) for i in range(2)]
            a_, ba_ = self.sb(es, "hfa", [64, 512], F32)
            hh_, bhh = self.sb(es, "hfh", [64, 512], F32)
            ki, bki = self.sb(es, "hfki", [64, 512], I32)
            kf, bkf = self.sb(es, "hfkf", [64, 512], F32)
            win, bwin = self.sb(es, "hfwin", [128, 512], F32)
            frev = [self.sb(es, f"hffr{i}", [128, L], BF16) for i in range(4)]
            zpad, bzpad = self.sb(es, "hfzp", [128, 128], BF16)
            fw.op("pool", "memset", [], [bzpad], ap=zpad[:], constant=0.0)
            jobs = [("zT", "off", L, self.GF, self.bGF, 4352)]
            if do_ctx:
                jobs.append(("zTc", "offc", CTX, self.GFc, self.bGFc, 512))
            for (zn, on, n, GFt, bGFt, LEN) in jobs:
                cn = min(512, n)
                for ch in range(n // cn):
                    c0 = ch * cn
                    zt, bzt = zt_[ch % 2]
                    of, bof = of_[ch % 2]
                    fw.dma([], [bzt], out=zt[:, 0:cn], in_=I[zn][:, c0:c0 + cn])
                    fw.dma([], [bof], out=of[:, 0:cn], in_=I[on][:, c0:c0 + cn])
                    p1, bp1 = self.pf()
                    fw.op("pe", "matmul", [bw1, bzt], [bp1], out=p1[0:64, 0:cn], lhsT=w1[:], rhs=zt[:, 0:cn], start=True, stop=True)
                    fw.op("act", "activation", [bp1, bfb], [ba_], out=a_[:, 0:cn], in_=p1[0:64, 0:cn], func=AF.Identity, scale=fb[:, 2:3], bias=fb[:, 4:5])
                    self.sincos(a_[:, 0:cn], ba_, None, hh_[:, 0:cn], None, bhh, ki[:, 0:cn], bki, kf[:, 0:cn], bkf)
                    p2, bp2 = self.pf()
                    fw.op("pe", "matmul", [bw2, bhh], [bp2], out=p2[0:64, 0:cn], lhsT=w2[:], rhs=hh_[:, 0:cn], start=True, stop=True)
                    fw.op("act", "activation", [bp2, bfb], [ba_], out=a_[:, 0:cn], in_=p2[0:64, 0:cn], func=AF.Identity, scale=fb[:, 3:4], bias=fb[:, 5:6])
                    self.sincos(a_[:, 0:cn], ba_, None, hh_[:, 0:cn], None, bhh, ki[:, 0:cn], bki, kf[:, 0:cn], bkf)
                    for ct in range(4):
                        fr, bfr = frev[ct]
                        p3, bp3 = self.pf()
                        fw.op("pe", "matmul", [bw3, bhh], [bp3], out=p3[:, 0:cn], lhsT=w3[:, ct * 128:(ct + 1) * 128], rhs=hh_[:, 0:cn], start=True, stop=True)
                        fw.op("act", "activation", [bof, bnd], [bwin], out=win[:, 0:cn], in_=of[:, 0:cn], func=AF.Exp, scale=nd_[:, ct:ct + 1])
                        fw.op("dve", "tensor_tensor", [bp3, bwin], [bfr], out=fr[:, n - c0 - cn:n - c0][:, ::-1], in0=p3[:, 0:cn], in1=win[:, 0:cn], op=ALU.mult)
                for ct in range(4):
                    fr, bfr = frev[ct]
                    rs = slice(ct * 128, (ct + 1) * 128)
                    fw.dma([bfr], [bGFt[ct]], out=GFt[rs, 128:128 + n], in_=fr[:, 0:n], stream="d2")
                    fw.dma([bzpad], [bGFt[ct]], out=GFt[rs, 0:128], in_=zpad[:], stream="d2")
                    fw.dma([bzpad], [bGFt[ct]], out=GFt[rs, 128 + n:LEN], in_=zpad[:, 0:LEN - 128 - n], stream="d2")
            fw.flush()
        with ExitStack() as es:
            wx, bwx = self.sb(es, "hywx", [128, 8, 128], BF16)
            zraw, bzraw = self.sb(es, "hyzraw", [128, T], F32)
            A, bA = self.sb(es, "hyA", [128, T], F32)
            B, bB = self.sb(es, "hyB", [128, T], F32)
            yvb, byvb = self.sb(es, "hyyvb", [128, T], BF16)
            yout_ = [self.sb(es, f"hyyout{i}", [128, 512], BF16) for i in range(2)]
            Upad, bUpad = self.sb(es, "hyUpad", [128, 32, 128], BF16)
            Upc, bUpc = self.sb(es, "hyUpc", [128, 2, 128], BF16)
            TT_ = [self.sb(es, f"hyTT{i}", [128, 33 * 128], BF16) for i in range(2)]
            yrev, byrev = self.sb(es, "hyyrev", [128, 32, 128], BF16)
            tmp, btmp = self.sb(es, "hytmp", [128, 512], F32)
            swT, bswT = self.sb(es, "hyswT", [128, 12, 3], F32)
            sbT, bsbT = self.sb(es, "hysbT", [128, 12], F32)
            biasT, bbiasT = self.sb(es, "hybiasT", [128, 4], F32)
            fw.dma([], [bswT], out=swT[:], in_=I["hy_swT"][l])
            fw.dma([], [bsbT], out=sbT[:], in_=I["hy_sbT"][l])
            fw.dma([], [bbiasT], out=biasT[:], in_=I["hy_biasT"][l])
            segs = [(0, CTX), (CTX, T)]
            tti = 0
            for ct in range(4):
                for j, dst, bdst in ((1, A, bA), (2, B, bB), (0, B, bB)):
                    if j == 0:
                        fw.op("dve", "tensor_tensor", [bA, bB], [bA], out=A[:], in0=A[:], in1=B[:], op=ALU.mult)
                        fw.op("act", "copy", [bA], [byvb], out=yvb[:], in_=A[:])
                    cidx = j * 4 + ct
                    self.load_w_cols(wx[:], bwx, l, 2560 + j * 512 + ct * 128, 128)
                    for (t0, n) in TG:
                        hb = [bhT[t] for t in self.tiles_of(t0, n)]
                        pz, bpz = self.pf()
                        for k in range(8):
                            fw.op("pe", "matmul", hb + [bwx], [bpz], out=pz[:, 0:n], lhsT=wx[:, k, :], rhs=hT[:, k, t0:t0 + n], start=(k == 0), stop=(k == 7))
                        fw.op("act", "copy", [bpz], [bzraw], out=zraw[:, t0:t0 + n], in_=pz[:, 0:n])
                    fw.op("act", "activation", [bzraw, bswT, bsbT], [bdst], out=dst[:], in_=zraw[:], func=AF.Identity, scale=swT[:, cidx, 1:2], bias=sbT[:, cidx:cidx + 1])
                    for (s, e) in segs:
                        fw.op("dve", "scalar_tensor_tensor", [bzraw, bswT, bdst], [bdst], out=dst[:, s + 1:e], in0=zraw[:, s:e - 1], scalar=swT[:, cidx, 0:1], in1=dst[:, s + 1:e], op0=ALU.mult, op1=ALU.add)
                        fw.op("dve", "scalar_tensor_tensor", [bzraw, bswT, bdst], [bdst], out=dst[:, s:e - 1], in0=zraw[:, s + 1:e], scalar=swT[:, cidx, 2:3], in1=dst[:, s:e - 1], op0=ALU.mult, op1=ALU.add)
                convs = [(CTX, 32, 16, self.GF, self.bGF, 4352, Upad, bUpad)]
                if do_ctx:
                    convs.append((0, 2, 1, self.GFc, self.bGFc, 512, Upc, bUpc))
                for (tok0, nblk, nd, GFt, bGFt, LEN, Up, bUp) in convs:
                    nm = 2 * nd + 1
                    for g0 in range(0, nblk, 8):
                        gn = min(8, nblk - g0)
                        pb_, bpb = self.pb()
                        for b in range(gn):
                            c0 = tok0 + (g0 + b) * 128
                            fw.op("pe", "transpose", [byvb, self.b_identb], [bpb], out=pb_[:, b * 128:(b + 1) * 128], in_=yvb[:, c0:c0 + 128], identity=self.identb[:])
                        fw.op("act", "copy", [bpb], [bUp], out=Up[:, g0:g0 + gn, :], in_=pb_[:, 0:gn * 128].rearrange("p (b c) -> p b c", c=128))
                    for cg in range(8):
                        pc, bpc = self.pf()
                        for c16 in range(16):
                            cc = cg * 16 + c16
                            TT, bTT = TT_[tti % 2]
                            tti += 1
                            row = ct * 128 + cc
                            fw.dma([bGFt[ct]], [bTT], out=TT[:, 0:nm * 128], in_=bass.AP(GFt, row * LEN, [[1, 128], [1, nm * 128]]))
                            morder = [nd] + [m for m in range(nm) if m != nd]
                            for mi, m in enumerate(morder):
                                bo0 = max(0, nd - m)
                                bo1 = min(nblk, nblk + nd - m)
                                fw.op("pe", "matmul", [bTT, bUp], [bpc], out=pc[:, c16 * nblk + bo0:c16 * nblk + bo1], lhsT=TT[:, m * 128:(m + 1) * 128],
                                      rhs=Up[:, m + bo0 - nd:m + bo1 - nd, cc], start=(mi == 0), stop=(mi == nm - 1))
                        fw.op("act", "copy", [bpc], [byrev], out=yrev[:, 0:nblk, cg * 16:(cg + 1) * 16].rearrange("p b c -> p c b"),
                              in_=pc[:, 0:16 * nblk].rearrange("p (c b) -> p c b", b=nblk))
                    for g0 in range(0, nblk, 4):
                        gn = min(4, nblk - g0)
                        pt_, bpt = self.pf()
                        for b in range(gn):
                            fw.op("pe", "matmul", [byrev, self.b_antib], [bpt], out=pt_[:, b * 128:(b + 1) * 128], lhsT=yrev[:, g0 + b, :], rhs=self.antib[:], start=True, stop=True)
                        cs = slice(tok0 + g0 * 128, tok0 + (g0 + gn) * 128)
                        W_ = gn * 128
                        fw.op("dve", "scalar_tensor_tensor", [bA, bbiasT, bpt], [btmp], out=tmp[:, 0:W_], in0=A[:, cs], scalar=biasT[:, ct:ct + 1], in1=pt_[:, 0:W_], op0=ALU.mult, op1=ALU.add)
                        yout, byout = yout_[(g0 // 4) % 2]
                        fw.op("pool", "tensor_tensor", [btmp, bB], [byout], out=yout[:, 0:W_], in0=tmp[:, 0:W_], in1=B[:, cs], op=ALU.mult)
                        fw.dma([byout], [self.bYT[8 + ct]], out=self.YT[1024 + ct * 128:1024 + (ct + 1) * 128, cs], in_=yout[:, 0:W_], stream="d2")
                if not do_ctx:
                    yout, byout = yout_[0]
                    fw.op("pool", "memset", [], [byout], ap=yout[:, 0:CTX], constant=0.0)
                    fw.dma([byout], [self.bYT[8 + ct]], out=self.YT[1024 + ct * 128:1024 + (ct + 1) * 128, 0:CTX], in_=yout[:, 0:CTX], stream="d2")
            if self.debug:
                self.dump_yt("yhy%d" % l, 8, 4)
            fw.flush()

    def phase_merge(self, l, hT, bhT):
        fw, I = self.fw, self.I
        last = (l == DEPTH - 1)
        self.MTs = getattr(self, "MTs", None) or self.scratch("MTs", [D, T], BF16)
        bMTs = [Buf() for _ in TG]
        groups = [(gi, t0, n) for gi, (t0, n) in enumerate(TG) if not (last and gi == 0)]
        groups1 = [(gi, t0 + h_ * 256, 256) for (gi, t0, n) in groups for h_ in range(n // 256)]
        with ExitStack() as es:
            wbr, bwbr = self.sb(es, "mgwbr", [128, 12, D], BF16)
            wg, bwg = self.sb(es, "mgwg", [128, 8, 3072], BF16)
            fw.dma([], [bwbr], q="pool", stream="d1", out=wbr[:], in_=I["w_branch"][l].rearrange("(k p) d -> p k d", p=128))
            for nb in range(3):
                self.load_w_cols(wg[:, :, nb * 1024:(nb + 1) * 1024], bwg, l, 4096 + nb * 1024, 1024)
            yt_ = [self.sb(es, f"mgyt{i}", [128, 12, 256], BF16) for i in range(1)]
            macc, bmacc = self.sb(es, "mgmacc", [128, 8, 256], F32)
            mT, bmT = self.sb(es, "mgmT", [128, 8, 256], BF16)
            sg_ = [self.sb(es, f"mgsg{i}", [128, 256], F32) for i in range(2)]
            tp_ = [self.sb(es, f"mgtp{i}", [128, 256], F32) for i in range(2)]
            it = 0
            for (gi, t0, n) in groups1:
                hb = [bhT[t] for t in self.tiles_of(t0, n)]
                yt, byt = yt_[0]
                fw.dma(self.bYT, [byt], out=yt[:, :, 0:n], in_=self.YT[:, t0:t0 + n].rearrange("(k p) t -> p k t", p=128))
                for nb in range(3):
                    for dc in range(8):
                        sg, bsg = sg_[it % 2]; tp, btp = tp_[it % 2]
                        it += 1
                        pg, bpg = self.pf()
                        for k in range(8):
                            fw.op("pe", "matmul", hb + [bwg], [bpg], out=pg[:, 0:n], lhsT=wg[:, k, nb * 1024 + dc * 128:nb * 1024 + (dc + 1) * 128], rhs=hT[:, k, t0:t0 + n], start=(k == 0), stop=(k == 7))
                        pbn, bpbn = self.pf()
                        for k in range(4):
                            fw.op("pe", "matmul", [byt, bwbr], [bpbn], out=pbn[:, 0:n], lhsT=wbr[:, nb * 4 + k, dc * 128:(dc + 1) * 128], rhs=yt[:, nb * 4 + k, 0:n], start=(k == 0), stop=(k == 3))
                        fw.op("act", "activation", [bpg], [bsg], out=sg[:, 0:n], in_=pg[:, 0:n], func=AF.Sigmoid)
                        if nb == 0:
                            fw.op("dve", "tensor_tensor", [bpbn, bsg], [bmacc], out=macc[:, dc, 0:n], in0=pbn[:, 0:n], in1=sg[:, 0:n], op=ALU.mult)
                        else:
                            fw.op("dve", "tensor_tensor", [bpbn, bsg], [btp], out=tp[:, 0:n], in0=pbn[:, 0:n], in1=sg[:, 0:n], op=ALU.mult)
                            fw.op("pool", "tensor_tensor", [btp, bmacc], [bmacc], out=macc[:, dc, 0:n], in0=tp[:, 0:n], in1=macc[:, dc, 0:n], op=ALU.add)
                fw.op("act", "copy", [bmacc], [bmT], out=mT[:, :, 0:n], in_=macc[:, :, 0:n])
                fw.dma([bmT], [bMTs[gi]], out=self.MTs[:, t0:t0 + n].rearrange("(k p) t -> p k t", p=128), in_=mT[:, :, 0:n], stream="d2")
            fw.flush()
        with ExitStack() as es:
            wout, bwout = self.sb(es, "mgwout", [128, 8, D], BF16)
            fw.dma([], [bwout], q="pool", stream="d1", out=wout[:], in_=I["w_out"][l].rearrange("(k p) d -> p k d", p=128))
            wr, bwr = self.sb(es, "mgwr", [128, 8, 36], F32)
            fw.dma([], [bwr], out=wr[:], in_=I["moe_wr"][l].rearrange("(k p) c -> p k c", p=128))
            brB, bbrB = self.sb(es, "mgbrB", [128, 36], F32)
            fw.dma([], [bbrB], out=brB[:], in_=I["moe_br"][l:l + 1, :].to_broadcast([128, 36]))
            n2g, bn2g = self.sb(es, "mgn2g", [128, D], F32)
            G1t, bG1t = self.sb(es, "mgG1t", [128, 2, D], F32)
            fw.dma([self.b_GBd], [bG1t], out=G1t[:], in_=self.GBd[:, 0:2, :])
            fw.dma([], [bn2g], out=n2g[:], in_=I["norm2_g"][l:l + 1, :].to_broadcast([128, D]))
            mt_ = [self.sb(es, f"mgmt{i}", [128, 8, 512], BF16) for i in range(2)]
            x_ = [self.sb(es, f"mgx{i}", [128, D], F32) for i in range(2)]
            xn_ = [self.sb(es, f"mgxn{i}", [128, D], F32) for i in range(2)]
            xs2, bxs2 = self.sb(es, "mgxs2", [128, D], F32)
            tmpx, btmpx = self.sb(es, "mgtmpx", [128, D], F32)
            st_ = [self.sb(es, f"mgst{i}", [128, 2, 6], F32) for i in range(2)]
            mv_ = [self.sb(es, f"mgmv{i}", [128, 4], F32) for i in range(2)]
            rs_ = [self.sb(es, f"mgrs{i}", [128, 1], F32) for i in range(2)]
            h2f, bh2f = self.sb(es, "mgh2f", [128, 8, 128], F32)
            h2tok_ = [self.sb(es, f"mgh2t{i}", [128, D], BF16) for i in range(2)]
            for ii, (gi, t0, n) in enumerate(groups):
                w = 1 if gi == 0 else 0
                mt, bmt = mt_[ii % 2]
                fw.dma([bMTs[gi]], [bmt], out=mt[:, :, 0:n], in_=self.MTs[:, t0:t0 + n].rearrange("(k p) t -> p k t", p=128))
                for t in self.tiles_of(t0, n):
                    tt = t - t0 // 128
                    x, bx = x_[t % 2]; xn, bxn = xn_[t % 2]; st, bst = st_[t % 2]; mv, bmv = mv_[t % 2]; rs, brs = rs_[t % 2]
                    h2tok, bh2tok = h2tok_[t % 2]
                    fw.dma([self.bXR[t]], [bx], out=x[:], in_=self.XR[t * 128:(t + 1) * 128, :])
                    for half in range(2):
                        hs = slice(half * 512, (half + 1) * 512)
                        pm, bpm = self.pf()
                        for k in range(8):
                            fw.op("pe", "matmul", [bmt, bwout], [bpm], out=pm[:], lhsT=mt[:, k, tt * 128:(tt + 1) * 128], rhs=wout[:, k, hs], start=(k == 0), stop=(k == 7))
                        fw.op("dve", "tensor_tensor", [bpm, bG1t], [btmpx], out=tmpx[:, hs], in0=pm[:], in1=G1t[:, w, hs], op=ALU.mult)
                        fw.op("pool", "tensor_tensor", [btmpx, bx], [bxn], out=xn[:, hs], in0=tmpx[:, hs], in1=x[:, hs], op=ALU.add)
                    fw.dma([bxn], [self.bXR[t]], out=self.XR[t * 128:(t + 1) * 128, :], in_=xn[:], stream="d2")
                    self.rms_stats(xn, bxn, st, bst, mv, bmv, rs, brs)
                    fw.op("dve", "scalar_tensor_tensor", [bxn, brs, bn2g], [bxs2], out=xs2[:], in0=xn[:], scalar=rs[:, 0:1], in1=n2g[:], op0=ALU.mult, op1=ALU.mult)
                    for hf in range(2):
                        pt_, bpt = self.pf()
                        for kk in range(4):
                            k = hf * 4 + kk
                            fw.op("pe", "transpose", [bxs2, self.b_identf], [bpt], out=pt_[:, kk * 128:(kk + 1) * 128], in_=xs2[:, k * 128:(k + 1) * 128], identity=self.identf[:])
                        for kk in range(4):
                            k = hf * 4 + kk
                            fw.op("act", "activation", [bpt, self.b_modT, self.b_modP], [bh2f], out=h2f[:, k, :], in_=pt_[:, kk * 128:(kk + 1) * 128],
                                  func=AF.Identity, scale=self.modP[:, 32 + k, w:w + 1], bias=self.modT[:, 24 + k, w:w + 1])
                    fw.op("dve", "tensor_copy", [bh2f], [bhT[t]], out=hT[:, :, t * 128:(t + 1) * 128], in_=h2f[:])
                    pl, bpl = self.pf()
                    for k in range(8):
                        fw.op("pe", "matmul", [bh2f, bwr], [bpl], out=pl[:, 0:36], lhsT=h2f[:, k, :], rhs=wr[:, k, :], start=(k == 0), stop=(k == 7))
                    fw.op("dve", "tensor_tensor", [bpl, bbrB], [self.b_LG[t]], out=self.LG[:, t, :], in0=pl[:, 0:36], in1=brB[:], op=ALU.add)
                    pb_, bpb = self.pb()
                    for k in range(8):
                        fw.op("pe", "transpose", [bhT[t], self.b_identb], [bpb], out=pb_[:, k * 128:(k + 1) * 128], in_=hT[:, k, t * 128:(t + 1) * 128], identity=self.identb[:])
                    fw.op("act", "copy", [bpb], [bh2tok], out=h2tok[:], in_=pb_[:])
                    fw.dma([bh2tok], [self.bH2[t]], out=self.H2[t * 128:(t + 1) * 128, :], in_=h2tok[:], stream="d2")
            self.dump(f"LG{l}", self.LG[:], [128, NT, 36], self.b_LG)
            fw.flush()

    def phase_moe(self, l):
        fw, I = self.fw, self.I
        last = (l == DEPTH - 1)
        tiles = list(range(2, NT)) if last else list(range(NT))
        NB = 2 * len(tiles) + 32
        IOA = bass.IndirectOffsetOnAxis
        with ExitStack() as es:
            M1, bM1 = self.sb(es, "moM1", [128, NT, 32], F32)
            M2, bM2 = self.sb(es, "moM2", [128, NT, 32], F32)
            MS, bMS = self.sb(es, "moMS", [128, NT, 32], BF16)
            W12, bW12 = self.sb(es, "moW12", [128, NT, 2], F32)
            RK, bRK = self.sb(es, "moRK", [128, NT, 32], F32)
            DST, bDST = self.sb(es, "moDST", [128, NT, 2], F32)
            DSTi, bDSTi = self.sb(es, "moDSTi", [128, NT, 2], I32)
            sc, bsc = self.sb(es, "mosc", [128, 64], F32)
            ig, big = self.sb(es, "moig", [128, 8], F32)
            mx8, bmx8 = self.sb(es, "momx8", [128, 8], F32)
            E12, bE12 = self.sb(es, "moE12", [128, 2, 8], F32)
            trisb, btrisb = self.sb(es, "motris", [128, 128], BF16)
            onesb, bonesb = self.sb(es, "moones", [128, 128], BF16)
            trf, btrf = self.sb(es, "motrf", [128, 128], F32)
            fw.dma([], [btrf], out=trf[:], in_=I["tris"].ap())
            fw.op("dve", "tensor_copy", [btrf], [btrisb], out=trisb[:], in_=trf[:])
            fw.op("pool", "memset", [], [bonesb], ap=onesb[:], constant=1.0)
            run, brun = self.sb(es, "morun", [128, 32], F32)
            fw.op("pool", "memset", [], [brun], ap=run[:], constant=0.0)
            G = sc[:, 0:4]; EG = sc[:, 4:8]
            gmax = sc[:, 8:9]; ngmax = sc[:, 9:10]; sgm = sc[:, 10:11]; ptop = sc[:, 11:12]
            nm1 = sc[:, 12:13]; e21 = sc[:, 13:14]; w1c = sc[:, 14:15]; w2c = sc[:, 15:16]
            S = [bsc]
            for t in tiles:
                Lg = self.LG[:, t, 0:4]
                Le = self.LG[:, t, 4:36]
                bl = self.b_LG[t]
                fw.op("dve", "tensor_reduce", [bl], S, out=gmax, in_=Lg, axis=AX.X, op=ALU.max)
                fw.op("dve", "tensor_scalar", [bl] + S, S, out=G, in0=Lg, scalar1=gmax, scalar2=None, op0=ALU.is_equal)
                fw.op("dve", "tensor_scalar", S, S, out=ngmax, in0=gmax, scalar1=-1.0, scalar2=None, op0=ALU.mult)
                fw.op("act", "activation", [bl] + S, S, out=EG, in_=Lg, func=AF.Exp, bias=ngmax, scale=1.0)
                fw.op("dve", "tensor_reduce", S, S, out=sgm, in_=EG, axis=AX.X, op=ALU.add)
                fw.op("dve", "reciprocal", S, S, out=ptop, in_=sgm)
                fw.op("dve", "tensor_scalar", [bl] + S, [big], out=ig[:], in0=Le[:, 0:8], scalar1=sc[:, 0:1], scalar2=None, op0=ALU.mult)
                for g in range(1, 4):
                    fw.op("dve", "scalar_tensor_tensor", [bl, big] + S, [big], out=ig[:], in0=Le[:, 8 * g:8 * g + 8], scalar=sc[:, g:g + 1], in1=ig[:], op0=ALU.mult, op1=ALU.add)
                fw.op("dve", "max", [big], [bmx8], out=mx8[:], in_=ig[:])
                fw.op("dve", "tensor_scalar", [big, bmx8], [bE12], out=E12[:, 0, :], in0=ig[:], scalar1=mx8[:, 0:1], scalar2=None, op0=ALU.is_equal)
                fw.op("dve", "tensor_scalar", [big, bmx8], [bE12], out=E12[:, 1, :], in0=ig[:], scalar1=mx8[:, 1:2], scalar2=None, op0=ALU.is_equal)
                fw.op("dve", "tensor_scalar", [bmx8] + S, S, out=nm1, in0=mx8[:, 0:1], scalar1=-1.0, scalar2=None, op0=ALU.mult)
                fw.op("act", "activation", [bmx8] + S, S, out=e21, in_=mx8[:, 1:2], func=AF.Exp, bias=nm1, scale=1.0)
                fw.op("dve", "tensor_scalar", S, S, out=w1c, in0=e21, scalar1=1.0, scalar2=None, op0=ALU.add)
                fw.op("dve", "reciprocal", S, S, out=w1c, in_=w1c)
                fw.op("dve", "tensor_tensor", S, S, out=w2c, in0=e21, in1=w1c, op=ALU.mult)
                fw.op("dve", "tensor_scalar", S, [bW12], out=W12[:, t, :], in0=sc[:, 14:16], scalar1=ptop, scalar2=None, op0=ALU.mult)
                for g in range(4):
                    fw.op("dve", "tensor_scalar", [bE12] + S, [bM1], out=M1[:, t, 8 * g:8 * g + 8], in0=E12[:, 0, :], scalar1=sc[:, g:g + 1], scalar2=None, op0=ALU.mult)
                    fw.op("dve", "tensor_scalar", [bE12] + S, [bM2], out=M2[:, t, 8 * g:8 * g + 8], in0=E12[:, 1, :], scalar1=sc[:, g:g + 1], scalar2=None, op0=ALU.mult)
                fw.op("dve", "tensor_tensor", [bM1, bM2], [bMS], out=MS[:, t, :], in0=M1[:, t, :], in1=M2[:, t, :], op=ALU.add)
                pr, bpr = self.pf()
                fw.op("pe", "matmul", [btrisb, bMS], [bpr], out=pr[:, 0:32], lhsT=trisb[:], rhs=MS[:, t, :], start=True, stop=True)
                fw.op("pe", "matmul", [bonesb, bMS], [bpr], out=pr[:, 32:64], lhsT=onesb[:], rhs=MS[:, t, :], start=True, stop=True)
                fw.op("dve", "tensor_tensor", [bpr, brun], [bRK], out=RK[:, t, :], in0=pr[:, 0:32], in1=run[:], op=ALU.add)
                fw.op("dve", "tensor_tensor", [bpr, brun], [brun], out=run[:], in0=pr[:, 32:64], in1=run[:], op=ALU.add)
            nbi, bnbi = self.sb(es, "monbi", [128, 32], I32)
            nbf, bnbf = self.sb(es, "monbf", [128, 32], F32)
            one32, bone32 = self.sb(es, "moone32", [128, 32], F32)
            pend, bpend = self.sb(es, "mopend", [128, 32], F32)
            pst, bpst = self.sb(es, "mopst", [128, 32], F32)
            fw.op("pool", "memset", [], [bone32], ap=one32[:], constant=1.0)
            fw.op("dve", "tensor_scalar", [brun], [bnbi], out=nbi[:], in0=run[:], scalar1=1.0 / 128.0, scalar2=0.49609375, op0=ALU.mult, op1=ALU.add)
            fw.op("dve", "tensor_copy", [bnbi], [bnbf], out=nbf[:], in_=nbi[:])
            fw.op("dve", "tensor_tensor_scan", [bone32, bnbf], [bpend], out=pend[:], data0=one32[:], data1=nbf[:], initial=0.0, op0=ALU.mult, op1=ALU.add)
            fw.op("dve", "tensor_tensor", [bpend, bnbf], [bpst], out=pst[:], in0=pend[:], in1=nbf[:], op=ALU.subtract)
            fw.op("dve", "tensor_scalar", [bpst], [bpst], out=pst[:], in0=pst[:], scalar1=128.0, scalar2=None, op0=ALU.mult)
            tmp32, btmp32 = self.sb(es, "motmp32", [128, 32], F32)
            for t in tiles:
                fw.op("dve", "tensor_tensor", [bRK, bpst], [btmp32], out=tmp32[:], in0=RK[:, t, :], in1=pst[:], op=ALU.add)
                fw.op("dve", "tensor_tensor", [btmp32, bM1], [bRK], out=RK[:, t, :], in0=tmp32[:], in1=M1[:, t, :], op=ALU.mult)
                fw.op("dve", "tensor_reduce", [bRK], [bDST], out=DST[:, t, 0:1], in_=RK[:, t, :], axis=AX.X, op=ALU.add)
                fw.op("dve", "tensor_tensor", [btmp32, bM2], [bRK], out=RK[:, t, :], in0=tmp32[:], in1=M2[:, t, :], op=ALU.mult)
                fw.op("dve", "tensor_reduce", [bRK], [bDST], out=DST[:, t, 1:2], in_=RK[:, t, :], axis=AX.X, op=ALU.add)
            fw.op("dve", "tensor_copy", [bDST], [bDSTi], out=DSTi[:, tiles[0]:NT, :], in_=DST[:, tiles[0]:NT, :])
            iob, biob = self.sb(es, "moiob", [128, NBLK], F32)
            fw.dma([], [biob], out=iob[:], in_=I["iotab"].ap())
            cmp_, bcmp = self.sb(es, "mocmp", [128, NBLK, 32], F32)
            be, bbe = self.sb(es, "mobe", [128, NBLK], F32)
            fw.op("dve", "tensor_tensor", [biob, bpend], [bcmp], out=cmp_[:, 0:NB, :], in0=iob[:, 0:NB].unsqueeze(2).to_broadcast([128, NB, 32]),
                  in1=pend[:].unsqueeze(1).to_broadcast([128, NB, 32]), op=ALU.is_ge)
            fw.op("dve", "tensor_reduce", [bcmp], [bbe], out=be[:, 0:NB], in_=cmp_[:, 0:NB, :], axis=AX.X, op=ALU.add)
            fw.op("dve", "tensor_scalar", [bbe], [bbe], out=be[:, 0:NB], in0=be[:, 0:NB], scalar1=31.0, scalar2=None, op0=ALU.min)
            b1f, bb1f = self.sb(es, "mob1f", [128, NBLK], F32)
            b2f, bb2f = self.sb(es, "mob2f", [128, NBLK], F32)
            wi1, bwi1 = self.sb(es, "mowi1", [128, NBLK, 8], I32)
            wi2, bwi2 = self.sb(es, "mowi2", [128, NBLK, 4], I32)
            fw.op("dve", "tensor_scalar", [bbe], [bb1f], out=b1f[:, 0:NB], in0=be[:, 0:NB], scalar1=1024.0, scalar2=None, op0=ALU.mult)
            fw.op("dve", "tensor_scalar", [bb1f, self.b_rcol], [bb1f], out=b1f[:, 0:NB], in0=b1f[:, 0:NB], scalar1=self.rcol[:, 3:4], scalar2=None, op0=ALU.add)
            fw.op("dve", "tensor_scalar", [bbe], [bb2f], out=b2f[:, 0:NB], in0=be[:, 0:NB], scalar1=512.0, scalar2=None, op0=ALU.mult)
            fw.op("dve", "tensor_scalar", [bb2f, self.b_rcol], [bb2f], out=b2f[:, 0:NB], in0=b2f[:, 0:NB], scalar1=self.rcol[:, 3:4], scalar2=None, op0=ALU.add)
            for k in range(8):
                fw.op("dve", "tensor_scalar", [bb1f], [bwi1], out=wi1[:, 0:NB, k], in0=b1f[:, 0:NB], scalar1=128.0 * k + l * 32768.0, scalar2=None, op0=ALU.add)
            for k in range(4):
                fw.op("dve", "tensor_scalar", [bb2f], [bwi2], out=wi2[:, 0:NB, k], in0=b2f[:, 0:NB], scalar1=128.0 * k + l * 16384.0, scalar2=None, op0=ALU.add)
            self.dump(f"DST{l}", DST[:], [128, NT, 2], [bDST])
            self.dump(f"W12{l}", W12[:], [128, NT, 2], [bW12])
            self.dump(f"BE{l}", be[:], [128, NBLK], [bbe])
            hx_ = [self.sb(es, f"mohx{i}", [128, D], BF16) for i in range(2)]
            zt_, bzt_ = self.sb(es, "mozero", [128, 4, D], BF16)
            fw.op("pool", "memset", [], [bzt_], ap=zt_[:], constant=0.0)
            for b4 in range(0, NB, 4):
                nb4 = min(4, NB - b4)
                fw.dma([bzt_], [self.bMB], out=self.MB[b4 * 128:(b4 + nb4) * 128, :].rearrange("(b p) d -> p b d", p=128), in_=zt_[:, 0:nb4, :], stream="d2")
            for t in tiles:
                hx, bhx = hx_[t % 2]
                fw.dma([self.bH2[t]], [bhx], out=hx[:], in_=self.H2[t * 128:(t + 1) * 128, :])
                for k in range(2):
                    fw.dmaf(lambda e, hx=hx, t=t, k=k: e.indirect_dma_start(out=self.MB[:, :], out_offset=IOA(ap=DSTi[:, t, k:k + 1], axis=0), in_=hx[:, :], in_offset=None),
                            [bhx, bDSTi], [self.bMB])
            X_ = [self.sb(es, f"moX{i}", [128, D], BF16) for i in range(2)]
            XT_ = [self.sb(es, f"moXT{i}", [128, 8, 128], BF16) for i in range(2)]
            w1g_ = [self.sb(es, f"mow1g{i}", [128, 8, 512], BF16) for i in range(2)]
            w3g_ = [self.sb(es, f"mow3g{i}", [128, 8, 512], BF16) for i in range(2)]
            w2g_ = [self.sb(es, f"mow2g{i}", [128, 4, D], BF16) for i in range(2)]
            sl_ = [self.sb(es, f"mosl{i}", [128, 512], F32) for i in range(2)]
            gT_ = [self.sb(es, f"mogT{i}", [128, 512], BF16) for i in range(2)]
            gk_ = [self.sb(es, f"mogk{i}", [128, 512], BF16) for i in range(2)]
            yb_ = [self.sb(es, f"moyb{i}", [128, D], F32) for i in range(2)]
            w1d, w3d, w2d = I["moe_w1"], I["moe_w3"], I["moe_w2"]
            for blk in range(NB):
                X, bX = X_[blk % 2]; XT, bXT = XT_[blk % 2]
                w1g, bw1g = w1g_[blk % 2]; w3g, bw3g = w3g_[blk % 2]; w2g, bw2g = w2g_[blk % 2]
                sl, bsl = sl_[blk % 2]; gT, bgT = gT_[blk % 2]; yb, byb = yb_[blk % 2]
                fw.dma([self.bMB], [bX], out=X[:], in_=self.MB[blk * 128:(blk + 1) * 128, :])
                for k in range(8):
                    fw.dmaf(lambda e, w1g=w1g, k=k, blk=blk: e.indirect_dma_start(out=w1g[:, k, :], out_offset=None, in_=w1d[:, :], in_offset=IOA(ap=wi1[:, blk, k:k + 1], axis=0)),
                            [bwi1], [bw1g])
                    fw.dmaf(lambda e, w3g=w3g, k=k, blk=blk: e.indirect_dma_start(out=w3g[:, k, :], out_offset=None, in_=w3d[:, :], in_offset=IOA(ap=wi1[:, blk, k:k + 1], axis=0)),
                            [bwi1], [bw3g])
                for k in range(4):
                    fw.dmaf(lambda e, w2g=w2g, k=k, blk=blk: e.indirect_dma_start(out=w2g[:, k, :], out_offset=None, in_=w2d[:, :], in_offset=IOA(ap=wi2[:, blk, k:k + 1], axis=0)),
                            [bwi2], [bw2g])
                pb_, bpb = self.pb()
                for k in range(8):
                    fw.op("pe", "transpose", [bX, self.b_identb], [bpb], out=pb_[:, k * 128:(k + 1) * 128], in_=X[:, k * 128:(k + 1) * 128], identity=self.identb[:])
                fw.op("act", "copy", [bpb], [bXT], out=XT[:].rearrange("p k t -> p (k t)"), in_=pb_[:])
                p1, bp1 = self.pf()
                p3, bp3 = self.pf()
                for k in range(8):
                    fw.op("pe", "matmul", [bw1g, bXT], [bp1], out=p1[:], lhsT=XT[:, k, :], rhs=w1g[:, k, :], start=(k == 0), stop=(k == 7))
                for k in range(8):
                    fw.op("pe", "matmul", [bw3g, bXT], [bp3], out=p3[:], lhsT=XT[:, k, :], rhs=w3g[:, k, :], start=(k == 0), stop=(k == 7))
                gk, bgk = gk_[blk % 2]
                fw.op("act", "activation", [bp1], [bsl], out=sl[:], in_=p1[:], func=AF.Silu)
                fw.op("dve", "tensor_tensor", [bp3, bsl], [bgk], out=gk[:], in0=p3[:], in1=sl[:], op=ALU.mult)
                pg_, bpg_ = self.pb()
                for hc in range(4):
                    fw.op("pe", "transpose", [bgk, self.b_identb], [bpg_], out=pg_[:, hc * 128:(hc + 1) * 128], in_=gk[:, hc * 128:(hc + 1) * 128], identity=self.identb[:])
                fw.op("act", "copy", [bpg_], [bgT], out=gT[:], in_=pg_[:, 0:512])
                for half in range(2):
                    py, bpy = self.pf()
                    for hc in range(4):
                        fw.op("pe", "matmul", [bgT, bw2g], [bpy], out=py[:], lhsT=gT[:, hc * 128:(hc + 1) * 128], rhs=w2g[:, hc, half * 512:(half + 1) * 512], start=(hc == 0), stop=(hc == 3))
                    fw.op("act", "copy", [bpy], [byb], out=yb[:, half * 512:(half + 1) * 512], in_=py[:])
                fw.dma([byb], [self.bMY], out=self.MY[blk * 128:(blk + 1) * 128, :], in_=yb[:], stream="d2")
            y1_ = [self.sb(es, f"moy1{i}", [128, D], F32) for i in range(2)]
            G2t, bG2t = self.sb(es, "moG2t", [128, 2, D], F32)
            fw.dma([self.b_GBd], [bG2t], out=G2t[:], in_=self.GBd[:, 2:4, :])
            y2_ = [self.sb(es, f"moy2{i}", [128, D], F32) for i in range(2)]
            xx_ = [self.sb(es, f"moxx{i}", [128, D], F32) for i in range(2)]
            st_ = [self.sb(es, f"most{i}", [128, 2, 6], F32) for i in range(2)]
            mv_ = [self.sb(es, f"momv{i}", [128, 4], F32) for i in range(2)]
            rs_ = [self.sb(es, f"mors{i}", [128, 1], F32) for i in range(2)]
            if last:
                fng, bfng = self.sb(es, "mofng", [128, D], F32)
                fw.dma([], [bfng], out=fng[:], in_=I["final_norm_g"][0:1, :].to_broadcast([128, D]))
            for t in tiles:
                w = 1 if t < 2 else 0
                y1, by1 = y1_[t % 2]; y2, by2 = y2_[t % 2]; xx, bxx = xx_[t % 2]
                fw.dmaf(lambda e, y1=y1, t=t: e.indirect_dma_start(out=y1[:, :], out_offset=None, in_=self.MY[:, :], in_offset=IOA(ap=DSTi[:, t, 0:1], axis=0)), [self.bMY, bDSTi], [by1])
                fw.dmaf(lambda e, y2=y2, t=t: e.indirect_dma_start(out=y2[:, :], out_offset=None, in_=self.MY[:, :], in_offset=IOA(ap=DSTi[:, t, 1:2], axis=0)), [self.bMY, bDSTi], [by2])
                fw.dma([self.bXR[t]], [bxx], out=xx[:], in_=self.XR[t * 128:(t + 1) * 128, :])
                fw.op("dve", "tensor_scalar", [by1, bW12], [by1], out=y1[:], in0=y1[:], scalar1=W12[:, t, 0:1], scalar2=None, op0=ALU.mult)
                fw.op("dve", "scalar_tensor_tensor", [by2, bW12, by1], [by1], out=y1[:], in0=y2[:], scalar=W12[:, t, 1:2], in1=y1[:], op0=ALU.mult, op1=ALU.add)
                fw.op("pool", "tensor_tensor", [by1, bG2t], [by1], out=y1[:], in0=y1[:], in1=G2t[:, w, :], op=ALU.mult)
                fw.op("pool", "tensor_tensor", [by1, bxx], [bxx], out=xx[:], in0=y1[:], in1=xx[:], op=ALU.add)
                if not last:
                    fw.dma([bxx], [self.bXR[t]], out=self.XR[t * 128:(t + 1) * 128, :], in_=xx[:], stream="d2")
                else:
                    st, bst = st_[t % 2]; mv, bmv = mv_[t % 2]; rs, brs = rs_[t % 2]
                    self.rms_stats(xx, bxx, st, bst, mv, bmv, rs, brs)
                    fw.op("dve", "scalar_tensor_tensor", [bxx, brs, bfng], [by2], out=y2[:], in0=xx[:], scalar=rs[:, 0:1], in1=fng[:], op0=ALU.mult, op1=ALU.mult)
                    fw.dma([by2], [self.bout], out=self.out[(t - 2) * 128:(t - 1) * 128, :], in_=y2[:], stream="d2")
            fw.flush()

    def phase_final(self):
        pass


def prep_inputs(inp, b, keys=None):
    f = np.float32
    d = {}
    d.update(_consts())
    d["x"] = np.ascontiguousarray(inp["x"][b])
    d["ctx"] = np.ascontiguousarray(inp["ctx"][b])
    cc = np.stack([inp["c"][b], inp["c_ctx"]], -1)
    d["cT"] = np.ascontiguousarray(cc.reshape(8, 128, 2).transpose(1, 0, 2))
    d["w_mod"] = inp["w_mod"]
    d["b_mod"] = inp["b_mod"]
    d["bmodT"] = np.ascontiguousarray(inp["b_mod"].reshape(DEPTH, 48, 128).transpose(0, 2, 1))
    selw = np.zeros((2, 2, 128), f)
    selw[0, 0] = 1.0
    selw[1, 1] = 1.0
    d["selw"] = selw
    d["norm1_g"] = inp["norm1_g"]
    d["norm2_g"] = inp["norm2_g"]
    d["final_norm_g"] = inp["final_norm_g"].reshape(1, D)
    d["w_in"] = inp["w_in"]
    d["ret_log_decay"] = inp["ret_log_decay"].reshape(DEPTH, 8)
    d["ret_gn_g"] = inp["ret_gn_g"]
    def pairT(a):
        return np.ascontiguousarray(a.reshape(DEPTH, 2, 16, 128).transpose(0, 1, 3, 2))
    d["s5_lre"] = pairT(inp["s5_lambda_re"])
    d["s5_lim"] = pairT(inp["s5_lambda_im"])
    d["s5_ldt"] = pairT(np.repeat(inp["s5_log_dt"][..., None], 64, -1))
    sB = np.zeros((DEPTH, 2, 2, 128, 4, 2, 128), f)
    sC = np.zeros((DEPTH, 2, 2, 128, 16, 64), f)
    for ri, (bk, ck) in enumerate((("s5_b_re", "s5_c_re"), ("s5_b_im", "s5_c_im"))):
        Bm = inp[bk]
        Cm = inp[ck]
        for pt in range(16):
            o, q4 = pt // 4, pt % 4
            for g2 in range(2):
                g = 2 * pt + g2
                sB[:, :, ri, q4 * 32 + g2 * 16:q4 * 32 + g2 * 16 + 16, o, q4 % 2, g2 * 64:(g2 + 1) * 64] = Bm[:, :, g].transpose(0, 1, 3, 2)
                sC[:, :, ri, g2 * 64:(g2 + 1) * 64, pt, (q4 % 2) * 32 + g2 * 16:(q4 % 2) * 32 + g2 * 16 + 16] = Cm[:, :, g].transpose(0, 1, 3, 2)
    d["s5_B"] = sB
    d["s5_C"] = sC
    d["s5_dT"] = np.ascontiguousarray(inp["s5_d"].reshape(DEPTH, 4, 128).transpose(0, 2, 1))
    d["s5_glu_w1"] = inp["s5_glu_w1"]
    d["s5_glu_w2"] = inp["s5_glu_w2"]
    d["hy_swT"] = np.ascontiguousarray(inp["hy_short_w"].transpose(0, 2, 1).reshape(DEPTH, 12, 128, 3).transpose(0, 2, 1, 3))
    d["hy_sbT"] = np.ascontiguousarray(inp["hy_short_b"].reshape(DEPTH, 12, 128).transpose(0, 2, 1))
    d["hy_biasT"] = np.ascontiguousarray(inp["hy_bias"].reshape(DEPTH, 4, 128).transpose(0, 2, 1))
    d["hy_f_w1"] = inp["hy_f_w1"]
    d["hy_f_w2"] = inp["hy_f_w2"]
    d["hy_f_w3"] = inp["hy_f_w3"]
    d["hy_f_b1"] = inp["hy_f_b1"].reshape(DEPTH, 64, 1)
    d["hy_f_b2"] = inp["hy_f_b2"].reshape(DEPTH, 64, 1)
    d["hy_f_fr"] = np.ascontiguousarray(inp["hy_f_freq"].transpose(0, 2, 1))
    d["w_branch"] = inp["w_branch"].reshape(DEPTH, 1536, D)
    d["w_out"] = inp["w_out"]
    d["moe_wr"] = np.ascontiguousarray(np.concatenate([inp["moe_w_group"], inp["moe_w_expert"]], -1))
    d["moe_br"] = np.ascontiguousarray(np.concatenate([inp["moe_b_group"], inp["moe_b_expert"]], -1))
    d["moe_w1"] = inp["moe_w1"].reshape(DEPTH * 32 * 1024, 512)
    d["moe_w3"] = inp["moe_w3"].reshape(DEPTH * 32 * 1024, 512)
    d["moe_w2"] = inp["moe_w2"].reshape(DEPTH * 32 * 512, 1024)
    if keys is not None:
        d = {k: v for k, v in d.items() if k in keys}
    return d


_NC_CACHE = {}


def run_cores(inputs, n_cores=8, debug=None, stop=None, keys=None):
    per_core = [prep_inputs(inputs, c % 4, keys) for c in range(n_cores)]
    bd = Builder(debug=debug)
    bd.stop = stop
    import os
    bd.s5_cut = int(os.environ.get("S5_CUT", "0"))
    if os.environ.get("S5_LIM"):
        bd.s5_lim = tuple(int(v) for v in os.environ["S5_LIM"].split(","))
    nc = bd.build(per_core[0])
    res = run_bass_kernel_spmd(nc, per_core, core_ids=list(range(n_cores)))
    return res, bd


def kernel(**inputs):
    inputs = {k: np.asarray(v) for k, v in inputs.items()}
    res, bd = run_cores(inputs, n_cores=4)
    out = np.stack([res.results[b]["out"] for b in range(4)], 0)
    return out.astype(np.float32)
```
